# Optimizing a Trainium2 kernel written in Bass

```python
import math
import jax, jax.numpy as jnp
from jax import lax
import numpy as np

D_MODEL = 1024
BATCH = 4
SEQ = 8192
DEPTH = 1

D_SSM = D_MODEL // 2
SSM_GROUP = 16
SSM_GROUPS = D_SSM // SSM_GROUP
SSM_STATE = 64
SSM_DT_MIN = 0.001
SSM_DT_MAX = 0.1
N_HEADS = 8
HEAD_DIM = 64
D_ATT = N_HEADS * HEAD_DIM
ROT_DIM = HEAD_DIM // 4
ROPE_THETA = 500000.0
MOBA_BLOCK = 256
MOBA_TOPK = 3
Q_CHUNK = 128
D_IN = D_SSM + 3 * D_ATT + 2 * D_MODEL
PEER_HEADS = 8
PEER_KEYS = 128
PEER_EXPERTS = PEER_KEYS * PEER_KEYS
PEER_QDIM = 256
PEER_HALF = PEER_QDIM // 2
PEER_TOPK = 16
PEER_CHUNK = 128
D_PLE = 256
EPS = 1e-6
NEG = -1e30

kernel_name = "hybrid_s5_moba_peer_block"


def rmsnorm(x, g):
    x32 = x.astype(jnp.float32)
    y = x32 * lax.rsqrt(jnp.mean(x32 * x32, axis=-1, keepdims=True) + EPS)
    return (y * g.astype(jnp.float32)).astype(x.dtype)


def rotary_tables(positions):
    inv_freq = ROPE_THETA ** (-jnp.arange(0, ROT_DIM, 2, dtype=jnp.float32) / ROT_DIM)
    ang = positions.astype(jnp.float32)[..., None] * inv_freq
    return jnp.cos(ang)[:, None], jnp.sin(ang)[:, None]


def apply_partial_rope(t, cos, sin):
    half = ROT_DIM // 2
    t32 = t[..., :ROT_DIM].astype(jnp.float32)
    t1, t2 = t32[..., :half], t32[..., half:]
    rot = jnp.concatenate([t1 * cos - t2 * sin, t2 * cos + t1 * sin], axis=-1).astype(t.dtype)
    return jnp.concatenate([rot, t[..., ROT_DIM:]], axis=-1)


def s5_mixer(u, log_dt, a_re, a_im, b_re, b_im, c_re, c_im, d_skip, w_glu):
    f32 = jnp.float32
    Bsz, S, _ = u.shape
    u32 = u.astype(f32)
    ut = u32.reshape(Bsz, S, SSM_GROUPS, SSM_GROUP).transpose(1, 0, 2, 3)
    dt = jnp.exp(log_dt.astype(f32))[:, None]
    ar, ai = a_re.astype(f32), a_im.astype(f32)
    mag = jnp.exp(dt * ar)
    abar_re, abar_im = mag * jnp.cos(dt * ai), mag * jnp.sin(dt * ai)
    den = ar * ar + ai * ai
    nr, ni = abar_re - 1.0, abar_im
    f_re = (nr * ar + ni * ai) / den
    f_im = (ni * ar - nr * ai) / den
    br, bi = b_re.astype(f32), b_im.astype(f32)
    bb_re = f_re[..., None] * br - f_im[..., None] * bi
    bb_im = f_re[..., None] * bi + f_im[..., None] * br
    xin_re = jnp.einsum('sbgc,gnc->sbgn', ut, bb_re)
    xin_im = jnp.einsum('sbgc,gnc->sbgn', ut, bb_im)
    a_s_re = jnp.broadcast_to(abar_re[None, None], (S, 1, SSM_GROUPS, SSM_STATE))
    a_s_im = jnp.broadcast_to(abar_im[None, None], (S, 1, SSM_GROUPS, SSM_STATE))

    def combine(e1, e2):
        a1r, a1i, b1r, b1i = e1
        a2r, a2i, b2r, b2i = e2
        return (a2r * a1r - a2i * a1i,
                a2r * a1i + a2i * a1r,
                a2r * b1r - a2i * b1i + b2r,
                a2r * b1i + a2i * b1r + b2i)

    _, _, h_re, h_im = lax.associative_scan(combine, (a_s_re, a_s_im, xin_re, xin_im), axis=0)
    y = (jnp.einsum('sbgn,gcn->bsgc', h_re, c_re.astype(f32))
         - jnp.einsum('sbgn,gcn->bsgc', h_im, c_im.astype(f32)))
    y = y.reshape(Bsz, S, D_SSM) + d_skip.astype(f32) * u32
    y = jax.nn.gelu(y)
    y = y * jax.nn.sigmoid(y @ w_glu.astype(f32))
    return y.astype(u.dtype)


def moba_attention(q, k, v):
    f32 = jnp.float32
    Bsz, H, S, hd = q.shape
    nb = -(-S // MOBA_BLOCK)
    pad = nb * MOBA_BLOCK - S
    kp = jnp.pad(k, ((0, 0), (0, 0), (0, pad), (0, 0)))
    vp = jnp.pad(v, ((0, 0), (0, 0), (0, pad), (0, 0)))
    kb = kp.reshape(Bsz, H, nb, MOBA_BLOCK, hd)
    vb = vp.reshape(Bsz, H, nb, MOBA_BLOCK, hd)
    kmean = jnp.mean(kb.astype(f32), axis=3)
    qblk = jnp.arange(S) // MOBA_BLOCK
    gate = jnp.einsum('bhsd,bhnd->bhsn', q.astype(f32), kmean)
    past = jnp.arange(nb)[None, :] < qblk[:, None]
    gate = jnp.where(past, gate, NEG)
    k_sel = min(MOBA_TOPK, nb)
    _, sel = lax.top_k(gate, k_sel)
    sel_ok = sel < qblk[:, None]
    nc = S // Q_CHUNK
    scale = hd ** -0.5
    h_idx = jnp.arange(H)[:, None, None]

    def chunk(i):
        b = i // nc
        start = (i % nc) * Q_CHUNK
        qc = lax.dynamic_slice(q, (b, 0, start, 0), (1, H, Q_CHUNK, hd))[0]
        selc = lax.dynamic_slice(sel, (b, 0, start, 0), (1, H, Q_CHUNK, k_sel))[0]
        okc = lax.dynamic_slice(sel_ok, (b, 0, start, 0), (1, H, Q_CHUNK, k_sel))[0]
        kb_b = lax.dynamic_index_in_dim(kb, b, 0, keepdims=False)
        vb_b = lax.dynamic_index_in_dim(vb, b, 0, keepdims=False)
        kg = kb_b[h_idx, selc]
        vg = vb_b[h_idx, selc]
        own = (start // MOBA_BLOCK) * MOBA_BLOCK
        k_own = lax.dynamic_slice(kp, (b, 0, own, 0), (1, H, MOBA_BLOCK, hd))[0]
        v_own = lax.dynamic_slice(vp, (b, 0, own, 0), (1, H, MOBA_BLOCK, hd))[0]
        qpos = start + jnp.arange(Q_CHUNK)
        kpos = own + jnp.arange(MOBA_BLOCK)
        s_sel = jnp.einsum('hqd,hqjkd->hqjk', qc, kg).astype(f32) * scale
        s_sel = jnp.where(okc[..., None], s_sel, NEG).reshape(H, Q_CHUNK, k_sel * MOBA_BLOCK)
        s_own = jnp.einsum('hqd,hkd->hqk', qc, k_own).astype(f32) * scale
        s_own = jnp.where(kpos[None, :] <= qpos[:, None], s_own, NEG)
        probs = jax.nn.softmax(jnp.concatenate([s_sel, s_own], axis=-1), axis=-1).astype(v.dtype)
        p_sel = probs[..., :k_sel * MOBA_BLOCK].reshape(H, Q_CHUNK, k_sel, MOBA_BLOCK)
        p_own = probs[..., k_sel * MOBA_BLOCK:]
        return (jnp.einsum('hqjk,hqjkd->hqd', p_sel, vg)
                + jnp.einsum('hqk,hkd->hqd', p_own, v_own))

    out = lax.map(chunk, jnp.arange(Bsz * nc))
    return out.reshape(Bsz, nc, H, Q_CHUNK, hd).transpose(0, 2, 1, 3, 4).reshape(Bsz, H, S, hd)


def peer_ffn(h, w_q, keys1, keys2, u_tab, v_tab):
    f32 = jnp.float32
    Bsz, S, D = h.shape
    q = (h @ w_q).astype(f32).reshape(Bsz, S, PEER_HEADS, 2, PEER_HALF)
    s1 = jnp.einsum('bshd,hnd->bshn', q[..., 0, :], keys1.astype(f32))
    s2 = jnp.einsum('bshd,hnd->bshn', q[..., 1, :], keys2.astype(f32))
    v1, i1 = lax.top_k(s1, PEER_TOPK)
    v2, i2 = lax.top_k(s2, PEER_TOPK)
    cand = (v1[..., :, None] + v2[..., None, :]).reshape(Bsz, S, PEER_HEADS, PEER_TOPK * PEER_TOPK)
    top, flat = lax.top_k(cand, PEER_TOPK)
    e1 = jnp.take_along_axis(i1, flat // PEER_TOPK, axis=-1)
    e2 = jnp.take_along_axis(i2, flat % PEER_TOPK, axis=-1)
    experts = e1 * PEER_KEYS + e2
    gates = jax.nn.softmax(top, axis=-1).astype(h.dtype)
    T = Bsz * S
    nct = T // PEER_CHUNK
    hf = h.reshape(nct, PEER_CHUNK, D)
    ef = experts.reshape(nct, PEER_CHUNK, PEER_HEADS, PEER_TOPK)
    gf = gates.reshape(nct, PEER_CHUNK, PEER_HEADS, PEER_TOPK)

    def chunk(args):
        hc, ec, gc = args
        act = jax.nn.gelu(jnp.einsum('td,thkd->thk', hc, u_tab[ec]))
        return jnp.einsum('thk,thkd->td', gc * act, v_tab[ec])

    return lax.map(chunk, (hf, ef, gf)).reshape(Bsz, S, D)


def setup_inputs(seed: int = 0) -> dict:
    key = jax.random.key(seed)
    ks = jax.random.split(key, 32)
    nrm = lambda k, shape, s: jax.random.normal(k, shape, jnp.float32) * s
    L = DEPTH
    n_idx = jnp.arange(SSM_STATE, dtype=jnp.float32)
    return {
        "x": nrm(ks[0], (BATCH, SEQ, D_MODEL), 1.0),
        "p": nrm(ks[1], (DEPTH, BATCH, SEQ, D_PLE), 1.0),
        "positions": jnp.broadcast_to(jnp.arange(SEQ, dtype=jnp.int32), (BATCH, SEQ)),
        "g_mix": 1.0 + nrm(ks[2], (L, D_MODEL), 0.01),
        "w_in": nrm(ks[3], (L, D_MODEL, D_IN), D_MODEL ** -0.5),
        "ssm_log_dt": jax.random.uniform(ks[4], (L, SSM_GROUPS), jnp.float32,
                                         math.log(SSM_DT_MIN), math.log(SSM_DT_MAX)),
        "ssm_a_re": -0.5 + nrm(ks[5], (L, SSM_GROUPS, SSM_STATE), 0.01),
        "ssm_a_im": math.pi * n_idx + nrm(ks[6], (L, SSM_GROUPS, SSM_STATE), 0.01),
        "ssm_b_re": nrm(ks[7], (L, SSM_GROUPS, SSM_STATE, SSM_GROUP), (2 * SSM_GROUP) ** -0.5),
        "ssm_b_im": nrm(ks[8], (L, SSM_GROUPS, SSM_STATE, SSM_GROUP), (2 * SSM_GROUP) ** -0.5),
        "ssm_c_re": nrm(ks[9], (L, SSM_GROUPS, SSM_GROUP, SSM_STATE), SSM_STATE ** -0.5),
        "ssm_c_im": nrm(ks[10], (L, SSM_GROUPS, SSM_GROUP, SSM_STATE), SSM_STATE ** -0.5),
        "ssm_d": nrm(ks[11], (L, D_SSM), 1.0),
        "ssm_w_glu": nrm(ks[12], (L, D_SSM, D_SSM), D_SSM ** -0.5),
        "w_proj_ssm": nrm(ks[13], (L, D_SSM, D_MODEL), D_SSM ** -0.5),
        "w_proj_att": nrm(ks[14], (L, D_ATT, D_MODEL), D_ATT ** -0.5),
        "w_out": nrm(ks[15], (L, D_MODEL, D_MODEL), D_MODEL ** -0.5),
        "g_ffn": 1.0 + nrm(ks[16], (L, D_MODEL), 0.01),
        "peer_w_q": nrm(ks[17], (L, D_MODEL, PEER_HEADS * PEER_QDIM), D_MODEL ** -0.5),
        "peer_keys1": nrm(ks[18], (L, PEER_HEADS, PEER_KEYS, PEER_HALF), PEER_HALF ** -0.5),
        "peer_keys2": nrm(ks[19], (L, PEER_HEADS, PEER_KEYS, PEER_HALF), PEER_HALF ** -0.5),
        "peer_u": nrm(ks[20], (L, PEER_EXPERTS, D_MODEL), D_MODEL ** -0.5),
        "peer_v": nrm(ks[21], (L, PEER_EXPERTS, D_MODEL), PEER_HEADS ** -0.5),
        "g_ple": 1.0 + nrm(ks[22], (L, D_MODEL), 0.01),
        "ple_w_gate": nrm(ks[23], (L, D_MODEL, D_MODEL), D_MODEL ** -0.5),
        "ple_w_proj": nrm(ks[24], (L, D_PLE, D_MODEL), D_PLE ** -0.5),
        "g_final": 1.0 + nrm(ks[25], (D_MODEL,), 0.01),
    }


def reference(x, p, positions, g_mix, w_in, ssm_log_dt, ssm_a_re, ssm_a_im, ssm_b_re, ssm_b_im,
              ssm_c_re, ssm_c_im, ssm_d, ssm_w_glu, w_proj_ssm, w_proj_att, w_out, g_ffn,
              peer_w_q, peer_keys1, peer_keys2, peer_u, peer_v, g_ple, ple_w_gate, ple_w_proj,
              g_final):
    Bsz, S, _ = x.shape
    cos, sin = rotary_tables(positions)
    splits = [D_SSM, D_SSM + D_ATT, D_SSM + 2 * D_ATT, D_SSM + 3 * D_ATT, D_SSM + 3 * D_ATT + D_MODEL]

    def heads(t):
        return t.reshape(Bsz, S, N_HEADS, HEAD_DIM).transpose(0, 2, 1, 3)

    for i in range(DEPTH):
        h = rmsnorm(x, g_mix[i])
        z = h @ w_in[i]
        u_ssm, q, k, v, z_ga, z_gb = jnp.split(z, splits, axis=-1)
        y_a = s5_mixer(u_ssm, ssm_log_dt[i], ssm_a_re[i], ssm_a_im[i], ssm_b_re[i], ssm_b_im[i],
                       ssm_c_re[i], ssm_c_im[i], ssm_d[i], ssm_w_glu[i]) @ w_proj_ssm[i]
        qh = apply_partial_rope(heads(q), cos, sin)
        kh = apply_partial_rope(heads(k), cos, sin)
        att = moba_attention(qh, kh, heads(v))
        y_b = att.transpose(0, 2, 1, 3).reshape(Bsz, S, D_ATT) @ w_proj_att[i]
        merged = jax.nn.sigmoid(z_ga) * y_a + jax.nn.sigmoid(z_gb) * y_b
        x = x + merged @ w_out[i]
        x = x + peer_ffn(rmsnorm(x, g_ffn[i]), peer_w_q[i], peer_keys1[i], peer_keys2[i],
                         peer_u[i], peer_v[i])
        e = p[i] @ ple_w_proj[i]
        x = x + jax.nn.sigmoid(rmsnorm(x, g_ple[i]) @ ple_w_gate[i]) * e
    return rmsnorm(x, g_final)
```

```python
import numpy as np
import concourse.bass as bass
import concourse.mybir as mybir
from concourse.bass_utils import run_bass_kernel_spmd

F32 = mybir.dt.float32
BF16 = mybir.dt.bfloat16
I32 = mybir.dt.int32
ALU = mybir.AluOpType
AF = mybir.ActivationFunctionType
AX = mybir.AxisListType


class Tok:
    __slots__ = ("w", "r", "name")

    def __init__(self, name=""):
        self.w = None
        self.r = {}
        self.name = name


class Sched:
    ENGS = ("pe", "act", "dve", "pool", "sp")

    def __init__(self, nc):
        self.nc = nc
        self.ops = {e: [] for e in self.ENGS}
        self.dma_cnt = {}
        self.dma_keys = []

    @staticmethod
    def _evkey(ev):
        return (ev[0], ev[1])

    def _collect(self, reads, writes):
        deps = {}

        def add(ev):
            if ev is None:
                return
            k = self._evkey(ev)
            if k not in deps or deps[k][2] < ev[2]:
                deps[k] = ev

        for t in reads:
            add(t.w)
        for t in writes:
            add(t.w)
            for ev in t.r.values():
                add(ev)
        return deps

    def _commit(self, ev, reads, writes):
        for t in reads:
            k = self._evkey(ev)
            t.r[k] = ev
        for t in writes:
            t.w = ev
            t.r = {}

    def op(self, eng, fn, reads=(), writes=()):
        deps = self._collect(reads, writes)
        idx = len(self.ops[eng])
        ev = ("e", eng, idx)
        if eng == "pe":
            deps.pop(("e", "pe"), None)
        self.ops[eng].append(dict(fn=fn, deps=list(deps.values()), dma=None, signal=False))
        self._commit(ev, reads, writes)
        return ev

    def dma(self, q, key, out, in_, reads=(), writes=()):
        deps = self._collect(reads, writes)
        if key not in self.dma_cnt:
            self.dma_cnt[key] = 0
            self.dma_keys.append(key)
        n = self.dma_cnt[key]
        if n > 0:
            k = ("d", key)
            deps[k] = ("d", key, n)
        self.dma_cnt[key] = n + 1
        ev = ("d", key, n + 1)
        self.ops[q].append(dict(fn=lambda e, o=out, i=in_: e.dma_start(out=o, in_=i),
                                deps=list(deps.values()), dma=key, signal=False))
        self._commit(ev, reads, writes)
        return ev

    def emit(self, final_keys=()):
        nc = self.nc
        ops = self.ops
        for e in self.ENGS:
            for o in ops[e]:
                for d in o["deps"]:
                    if d[0] == "e":
                        ops[d[1]][d[2]]["signal"] = True
        for e in self.ENGS:
            last = None
            for o in ops[e]:
                if "barrier" in o:
                    if last is not None and e != "sp":
                        last["signal"] = True
                else:
                    last = o
        sigval = {}
        for e in self.ENGS:
            c = 0
            vals = []
            for o in ops[e]:
                if o["signal"]:
                    c += 1
                vals.append(c)
            sigval[e] = vals
        barvals = {}
        for e in self.ENGS:
            for i, o in enumerate(ops[e]):
                if "barrier" in o:
                    barvals[(e, o["barrier"])] = sigval[e][i]
        from contextlib import ExitStack
        with ExitStack() as st:
            esem = {e: st.enter_context(nc.semaphore("s_" + e)) for e in self.ENGS if e != "sp"}
            dsem = {k: st.enter_context(nc.semaphore("d_%d" % i)) for i, k in enumerate(self.dma_keys)}
            bsem = st.enter_context(nc.semaphore("s_bar"))
            block = st.enter_context(nc.Block())

            def run(ename, eng):
                waited = {}
                for o in ops[ename]:
                    if "barrier" in o:
                        k = o["barrier"]
                        if ename == "sp":
                            for key, cnt in o["dcnt"].items():
                                if cnt > 0 and waited.get(("d", key), 0) < 16 * cnt:
                                    eng.wait_ge(dsem[key], 16 * cnt)
                            for e2 in esem:
                                v = barvals[(e2, k)]
                                if v > 0:
                                    eng.wait_ge(esem[e2], v)
                            eng.sem_inc(bsem, 1)
                        else:
                            eng.wait_ge(bsem, k)
                        for key, cnt in o["dcnt"].items():
                            waited[("d", key)] = max(waited.get(("d", key), 0), 16 * cnt)
                        for e2 in esem:
                            waited[("e", e2)] = max(waited.get(("e", e2), 0), barvals[(e2, k)])
                        continue
                    for d in sorted(o["deps"]):
                        if d[0] == "e":
                            sem = esem[d[1]]
                            val = sigval[d[1]][d[2]]
                        else:
                            sem = dsem[d[1]]
                            val = 16 * d[2]
                        wk = (d[0], d[1])
                        if waited.get(wk, 0) >= val:
                            continue
                        waited[wk] = val
                        eng.wait_ge(sem, val)
                    ins = o["fn"](eng)
                    if o["dma"] is not None:
                        ins.then_inc(dsem[o["dma"]], 16)
                    elif o["signal"]:
                        ins.then_inc(esem[ename], 1)
                if ename == "sp":
                    for k in self.dma_keys:
                        eng.wait_ge(dsem[k], 16 * self.dma_cnt[k])

            @block.sync
            def _(e):
                run("sp", e)

            @block.tensor
            def _(e):
                run("pe", e)

            @block.scalar
            def _(e):
                run("act", e)

            @block.vector
            def _(e):
                run("dve", e)

            @block.gpsimd
            def _(e):
                run("pool", e)


NT_OWN = 4096
NT_LOC = 8192
PI = float(np.pi)
BIG = 30000.0


class Buf:
    __slots__ = ("ap", "t")

    def __init__(self, ap, name=""):
        self.ap = ap
        self.t = Tok(name)

    def __getitem__(self, k):
        return self.ap[k]


class KB:
    def __init__(self, nc):
        self.nc = nc
        self.S = Sched(nc)
        self.big = nc.alloc_sbuf_tensor("bigsb", [128, 53000], F32)
        self.off = 0
        self.persist = 0
        self.nbar = 0

    def sb(self, shape, dt, name=""):
        n = int(np.prod(shape[1:]))
        esz = 4 if dt in (F32, I32, mybir.dt.uint32) else 2
        nw = (n * esz + 63) // 64 * 16
        assert self.off + nw <= 53000, ("sbuf overflow", name, self.off, nw)
        ap = self.big[:, self.off:self.off + nw]
        self.off += nw
        if dt != F32:
            ap = ap.bitcast(dt)
        ap = ap[:, 0:n]
        if len(shape) == 3:
            ap = ap.rearrange("p (a b) -> p a b", a=shape[1])
        elif len(shape) == 4:
            ap = ap.rearrange("p (a b c) -> p a b c", a=shape[1], b=shape[2])
        elif len(shape) == 5:
            ap = ap.rearrange("p (a b c d) -> p a b c d", a=shape[1], b=shape[2], c=shape[3])
        if shape[0] != 128:
            ap = ap[0:shape[0]]
        return Buf(ap, name)

    def phase_reset(self):
        self.off = self.persist

    def op(self, eng, fn, r=(), w=()):
        return self.S.op(eng, fn, [b.t for b in r], [b.t for b in w])

    def dma(self, key, out, in_, r=(), w=(), q="sp"):
        return self.S.dma(q, key, out, in_, [b.t for b in r], [b.t for b in w])

    def mm(self, pbuf, out, lhsT, rhs, start, stop, r):
        self.op("pe", lambda e: e.matmul(out, lhsT=lhsT, rhs=rhs, start=start, stop=stop), r, [pbuf])

    def tr(self, pbuf, out, in_, ident, r):
        self.op("pe", lambda e: e.transpose(out=out, in_=in_, identity=ident), r, [pbuf])

    def tt(self, eng, out, a, b, op, r, w):
        self.op(eng, lambda e: e.tensor_tensor(out=out, in0=a, in1=b, op=op), r, w)

    def ts(self, eng, out, a, s1, s2, op0, op1, r, w):
        if s2 is None:
            self.op(eng, lambda e: e.tensor_scalar(out=out, in0=a, scalar1=s1, scalar2=None, op0=op0), r, w)
        else:
            self.op(eng, lambda e: e.tensor_scalar(out=out, in0=a, scalar1=s1, scalar2=s2, op0=op0, op1=op1), r, w)

    def stt(self, eng, out, a, s, b, op0, op1, r, w):
        self.op(eng, lambda e: e.scalar_tensor_tensor(out=out, in0=a, scalar=s, in1=b, op0=op0, op1=op1), r, w)

    def cp(self, eng, out, a, r, w):
        if eng == "act":
            self.op("act", lambda e: e.activation(out=out, in_=a, func=AF.Copy), r, w)
        else:
            self.op(eng, lambda e: e.tensor_copy(out=out, in_=a), r, w)

    def act(self, out, a, func, r, w, bias=None, scale=None, accum=None):
        kw = {}
        if bias is not None:
            kw["bias"] = bias
        if scale is not None:
            kw["scale"] = scale
        if accum is not None:
            kw["accum_out"] = accum
        self.op("act", lambda e: e.activation(out=out, in_=a, func=func, **kw), r, w)

    def memset(self, eng, out, val, w):
        self.op(eng, lambda e: e.memset(out, val), (), w)

    def barrier(self):
        S = self.S
        self.nbar += 1
        k = self.nbar
        for e in S.ENGS:
            S.ops[e].append(dict(barrier=k, fn=None, deps=[], dma=None, signal=False,
                                 dcnt=dict(S.dma_cnt)))


def build_program(stop_after=None, debug=()):
    nc = bass.Bass("TRN2", target_bir_lowering=False)
    kb = KB(nc)
    K = kb

    def din(name, shape, dt=F32):
        return nc.dram_tensor(name, list(shape), dt, kind="ExternalInput").ap()

    def dscr(name, shape, dt):
        kind = "ExternalOutput" if name in debug else "Internal"
        return nc.dram_tensor(name, list(shape), dt, kind=kind).ap()

    xc = din("xc", [NT_LOC, 1024])
    pc = din("pc", [NT_OWN, 256])
    posc = din("posc", [1, NT_LOC], I32)
    validc = din("validc", [1, 512])
    ownc = din("ownc", [1, 512])
    ident_d = din("ident", [128, 128])
    iota_d = din("iota", [128, 128])
    invf_d = din("invf", [128, 2])
    koh_d = din("koh", [32, NT_LOC])
    cm_d = din("cmask", [4, 128, 512])
    g_mix = din("g_mix", [1, 1024])
    w_in = din("w_in", [1024, 4096])
    w_perm = din("w_perm", [1024, 1024])
    s5 = {n: din("s5_" + n, shp) for n, shp in [
        ("ldt", [128, 16]), ("are", [128, 16]), ("aim", [128, 16]),
        ("bre", [128, 256]), ("bim", [128, 256]), ("cre", [128, 256]), ("cim", [128, 256])]}
    ssm_d = din("ssm_d", [1, 512])
    w_glu = din("w_glu", [512, 512])
    w_ps = din("w_ps", [512, 1024])
    w_pa = din("w_pa", [512, 1024])
    w_out = din("w_out", [1024, 1024])
    g_ffn = din("g_ffn", [1, 1024])
    w_q = din("w_q", [1024, 2048])
    keys = din("keys", [16, 128, 128])
    peer_u = din("peer_u", [16384, 1024])
    peer_v = din("peer_v", [16384, 1024])
    g_ple = din("g_ple", [1, 1024])
    w_pg = din("w_pg", [1024, 1024])
    w_pp = din("w_pp", [256, 1024])
    g_fin = din("g_fin", [1, 1024])
    out = nc.dram_tensor("out", [NT_OWN, 1024], F32, kind="ExternalOutput").ap()

    qT_s = dscr("qT_s", [4, 128, NT_OWN], BF16)
    kT_s = dscr("kT_s", [4, 128, NT_LOC], BF16)
    v_s = dscr("v_s", [NT_LOC, 8 * 65], BF16)
    gT_s = dscr("gT_s", [16, 128, NT_OWN], BF16)
    ssmT_s = dscr("ssmT_s", [4, 128, NT_OWN], BF16)
    attT_s = dscr("attT_s", [4, 128, NT_OWN], BF16)
    x1_s = dscr("x1_s", [NT_OWN, 1024], F32)
    h2T_s = dscr("h2T_s", [8, 128, NT_OWN], BF16)
    sc_s = dscr("sc_s", [NT_OWN, 16 * 128], F32)
    G_s = dscr("G_s", [32, 128, 128, 128], BF16)
    x2_s = dscr("x2_s", [NT_OWN, 1024], F32)
    UT5_s = dscr("UT5_s", [8, 128, 32 * 128], BF16)
    UT_s = dscr("UT_s", [128, 128, 1024], BF16)

    from contextlib import ExitStack
    st = ExitStack()
    PS = []
    for i in range(8):
        t = st.enter_context(nc.psum_tensor("ps%d" % i, [128, 512], F32))
        PS.append(Buf(t[:], "ps%d" % i))
    psi = [0]

    def nps():
        b = PS[psi[0] % 8]
        psi[0] += 1
        return b

    idf = K.sb([128, 128], F32, "idf")
    idb = K.sb([128, 128], BF16, "idb")
    K.dma("c0", idf.ap, ident_d, w=[idf])
    K.cp("dve", idb.ap, idf.ap, [idf], [idb])
    ksum = K.sb([128, 4, 32], F32, "ksum")
    K.persist = K.off

    ut5tok = [Buf(None, "ut5_%d" % i) for i in range(8)]

    def rmsnorm_tile(xt, gt, hb, sq, ss, rs):
        K.act(sq.ap, xt.ap, AF.Square, [xt], [sq, ss], accum=ss.ap)
        K.act(rs.ap, ss.ap, AF.Sqrt, [ss], [rs], scale=1.0 / 1024, bias=eps_b.ap)
        K.op("dve", lambda e: e.reciprocal(out=rs.ap, in_=rs.ap), [rs], [rs])
        K.stt("dve", hb.ap, xt.ap, rs.ap[:, 0:1], gt.ap, ALU.mult, ALU.mult, [xt, rs, gt], [hb])

    def load_w_bf(dst, src_ap, rows_kc, ncols, stage, key):
        for kc in range(rows_kc):
            K.dma(key, stage.ap[:, 0:ncols], src_ap[kc * 128:(kc + 1) * 128, :], w=[stage])
            K.cp("dve" if kc % 2 == 0 else "act", dst.ap[:, kc, :], stage.ap[:, 0:ncols], [stage], [dst])

    eps_b = K.sb([128, 1], F32, "eps")
    K.memset("dve", eps_b.ap, 1e-6, [eps_b])
    K.persist = K.off

    def phase_A():
        K.phase_reset()
        win = K.sb([128, 8, 3584], BF16, "win")
        wu = K.sb([128, 8, 512], BF16, "wuA")
        Ustk = K.sb([128, 32, 8, 16], BF16, "UstkA")
        UTo = K.sb([128, 32, 128], BF16, "UToA")
        wpm = K.sb([128, 8, 1024], BF16, "wpm")
        stageA = K.sb([128, 1792], F32, "stageA")
        stageB = K.sb([128, 1792], F32, "stageB")
        stage = stageA
        gt = K.sb([128, 1024], F32, "gmix")
        invf = K.sb([128, 2], F32, "invf")
        xts = [K.sb([128, 1024], F32, "xt%d" % i) for i in range(2)]
        sq = K.sb([128, 1024], BF16, "sq")
        ss = K.sb([128, 1], F32, "ss")
        rs = K.sb([128, 1], F32, "rs")
        hbs = [K.sb([128, 1024], BF16, "hb%d" % i) for i in range(2)]
        hTs = [K.sb([128, 8, 1024], BF16, "hT%d" % i) for i in range(2)]
        posi = K.sb([128, 1024], I32, "posi")
        ang = K.sb([128, 1024], F32, "ang")
        tmpa = K.sb([128, 1024], F32, "tmpa")
        tmpi = K.sb([128, 1024], I32, "tmpi")
        cosTs = [K.sb([128, 1024], F32, "cosT%d" % i) for i in range(2)]
        sinTs = [K.sb([128, 1024], F32, "sinT%d" % i) for i in range(2)]
        t1s = [K.sb([128, 512], F32, "t1_%d" % i) for i in range(2)]
        t2s = [K.sb([128, 512], F32, "t2_%d" % i) for i in range(2)]
        obf = [K.sb([128, 512], BF16, "obf%d" % i) for i in range(2)]
        vts = [K.sb([128, 8, 65], BF16, "vt%d" % i) for i in range(2)]
        for v in vts:
            K.memset("pool", v.ap, 1.0, [v])
        K.dma("c0", gt.ap, g_mix.partition_broadcast(128), w=[gt])
        K.dma("c0", invf.ap, invf_d, w=[invf])
        n_st = [0]

        def stream_w(dst_ap, src_ap, ncols):
            i = n_st[0]
            n_st[0] += 1
            stg_ = (stageA, stageB)[i % 2]
            K.dma("wst%d" % (i % 2), stg_.ap[:, 0:ncols], src_ap, w=[stg_])
            K.cp("dve" if i % 2 == 0 else "act", dst_ap, stg_.ap[:, 0:ncols], [stg_], [win])

        for kc in range(8):
            stream_w(win.ap[:, kc, 0:1792], w_in[kc * 128:(kc + 1) * 128, 512:2304], 1792)
            stream_w(win.ap[:, kc, 1792:3584], w_in[kc * 128:(kc + 1) * 128, 2304:4096], 1792)
        for kc in range(8):
            i = n_st[0]
            n_st[0] += 1
            stg_ = (stageA, stageB)[i % 2]
            K.dma("wst%d" % (i % 2), stg_.ap[:, 0:1024], w_perm[kc * 128:(kc + 1) * 128, :], w=[stg_])
            K.cp("dve" if i % 2 == 0 else "act", wpm.ap[:, kc, :], stg_.ap[:, 0:1024], [stg_], [wpm])
        for kc in range(8):
            i = n_st[0]
            n_st[0] += 1
            stg_ = (stageA, stageB)[i % 2]
            K.dma("wst%d" % (i % 2), stg_.ap[:, 0:512], w_in[kc * 128:(kc + 1) * 128, 0:512], w=[stg_])
            K.cp("dve" if i % 2 == 0 else "act", wu.ap[:, kc, :], stg_.ap[:, 0:512], [stg_], [wu])

        def sincos(dst, phase):
            K.ts("dve", tmpa.ap, ang.ap, phase, 1.0 / (2 * PI), ALU.add, ALU.mult, [ang], [tmpa])
            K.cp("dve", tmpi.ap, tmpa.ap, [tmpa], [tmpi])
            K.cp("dve", tmpa.ap, tmpi.ap, [tmpi], [tmpa])
            K.stt("dve", tmpa.ap, tmpa.ap, -2 * PI, ang.ap, ALU.mult, ALU.add, [tmpa, ang], [tmpa])
            K.ts("dve", tmpa.ap, tmpa.ap, phase, None, ALU.add, None, [tmpa], [tmpa])
            K.ts("dve", dst.ap, tmpa.ap, PI, -2 * PI, ALU.is_gt, ALU.mult, [tmpa], [dst])
            K.tt("dve", tmpa.ap, tmpa.ap, dst.ap, ALU.add, [tmpa, dst], [tmpa])
            K.ts("dve", dst.ap, tmpa.ap, -PI, 2 * PI, ALU.is_lt, ALU.mult, [tmpa], [dst])
            K.tt("dve", tmpa.ap, tmpa.ap, dst.ap, ALU.add, [tmpa, dst], [tmpa])
            K.act(dst.ap, tmpa.ap, AF.Sin, [tmpa], [dst])

        xic = [0]

        def prep(blk):
            tb = blk * 1024
            hT = hTs[blk % 2]
            cosT, sinT = cosTs[blk % 2], sinTs[blk % 2]
            xi = xic[0]
            K.dma("pos", posi.ap, posc[:, tb:tb + 1024].partition_broadcast(128), w=[posi])
            K.cp("dve", ang.ap, posi.ap, [posi], [ang])
            K.ts("dve", ang.ap, ang.ap, invf.ap[:, 0:1], None, ALU.mult, None, [ang, invf], [ang])
            sincos(cosT, PI / 2)
            sincos(sinT, 0.0)
            K.ts("dve", sinT.ap, sinT.ap, invf.ap[:, 1:2], None, ALU.mult, None, [sinT, invf], [sinT])
            for ti in range(8):
                xt = xts[xi % 2]
                hb = hbs[xi % 2]
                xi += 1
                K.dma("x%d" % (xi % 2), xt.ap, xc[tb + ti * 128: tb + (ti + 1) * 128, :], w=[xt])
                rmsnorm_tile(xt, gt, hb, sq, ss, rs)
                p = nps()
                pv = p.ap.bitcast(BF16).rearrange("p (a b) -> p a b", a=8)
                for kc in range(8):
                    K.tr(p, pv[:, kc, :], hb.ap[:, kc * 128:(kc + 1) * 128], idb.ap, [hb, idb])
                K.cp("act", hT.ap[:, :, ti * 128:(ti + 1) * 128], pv, [p], [hT])
            xic[0] = xi

        oic = [0]

        def compute(blk):
            own = blk >= 4
            tb = blk * 1024
            hT = hTs[blk % 2]
            cosT, sinT = cosTs[blk % 2], sinTs[blk % 2]
            oi = oic[0]
            for j in range(8):
                p = nps()
                for kc in range(8):
                    K.mm(p, p.ap, hT.ap[:, kc, j:1024:8], wu.ap[:, kc, :], kc == 0, kc == 7, [hT, wu])
                K.cp("act" if j % 2 else "dve", Ustk.ap[:, :, j, :], p.ap.rearrange("p (g c) -> p g c", g=32), [p], [Ustk])
            for g0 in range(0, 32, 8):
                p = nps()
                pv = p.ap.bitcast(BF16).rearrange("p (a b) -> p a b", a=8)
                for gi in range(8):
                    K.tr(p, pv[:, gi, :], Ustk.ap[:, g0 + gi, :, :].rearrange("p a b -> p (a b)"), idb.ap, [Ustk, idb])
                K.cp("act", UTo.ap[:, g0:g0 + 8, :], pv, [p], [UTo])
            K.dma("utA", UT5_s[blk], UTo.ap.rearrange("p a b -> p (a b)"), r=[UTo], w=[ut5tok[blk]])
            for which in (["q", "k"] if own else ["k"]):
                cbase = 0 if which == "q" else 512
                for c in range(4):
                    for half in range(2):
                        pa = nps()
                        pb = nps()
                        for kc in range(8):
                            K.mm(pa, pa.ap, win.ap[:, kc, cbase + c * 128: cbase + (c + 1) * 128],
                                 hT.ap[:, kc, half * 512:(half + 1) * 512], kc == 0, kc == 7, [win, hT])
                        for kc in range(8):
                            K.mm(pb, pb.ap, wpm.ap[:, kc, cbase + c * 128: cbase + (c + 1) * 128],
                                 hT.ap[:, kc, half * 512:(half + 1) * 512], kc == 0, kc == 7, [wpm, hT])
                        t1, t2 = t1s[oi % 2], t2s[oi % 2]
                        K.tt("dve", t1.ap, pa.ap, cosT.ap[:, half * 512:(half + 1) * 512], ALU.mult, [pa, cosT], [t1])
                        K.tt("dve", t2.ap, pb.ap, sinT.ap[:, half * 512:(half + 1) * 512], ALU.mult, [pb, sinT], [t2])
                        K.tt("dve", t1.ap, t1.ap, t2.ap, ALU.add, [t1, t2], [t1])
                        ob = obf[oi % 2]
                        oi += 1
                        K.cp("act", ob.ap, t1.ap, [t1], [ob])
                        t0 = tb + half * 512
                        if which == "q":
                            K.dma("oq%d" % (oi % 2), qT_s[c, :, t0 - NT_OWN: t0 - NT_OWN + 512], ob.ap, r=[ob])
                        else:
                            K.dma("oq%d" % (oi % 2), kT_s[c, :, t0: t0 + 512], ob.ap, r=[ob])
                            K.op("dve", lambda e, c=c, b0=t0 // 256, t1=t1: e.tensor_reduce(
                                out=ksum.ap[:, c, b0:b0 + 2], in_=t1.ap.rearrange("p (a b) -> p a b", a=2),
                                axis=AX.X, op=ALU.add), [t1], [ksum])
            for ti in range(8):
                p = nps()
                for kc in range(8):
                    K.mm(p, p.ap, hT.ap[:, kc, ti * 128:(ti + 1) * 128], win.ap[:, kc, 1024:1536],
                         kc == 0, kc == 7, [hT, win])
                vt = vts[ti % 2]
                K.cp("act", vt.ap[:, :, 0:64], p.ap.rearrange("p (h d) -> p h d", h=8), [p], [vt])
                K.dma("ov%d" % (ti % 2), v_s[tb + ti * 128: tb + (ti + 1) * 128, :].rearrange("p (h d) -> p h d", h=8),
                      vt.ap, r=[vt])
            if own:
                for c in range(16):
                    for half in range(2):
                        p = nps()
                        for kc in range(8):
                            K.mm(p, p.ap, win.ap[:, kc, 1536 + c * 128: 1536 + (c + 1) * 128],
                                 hT.ap[:, kc, half * 512:(half + 1) * 512], kc == 0, kc == 7, [win, hT])
                        ob = obf[oi % 2]
                        oi += 1
                        K.act(ob.ap, p.ap, AF.Sigmoid, [p], [ob])
                        t0 = tb + half * 512 - NT_OWN
                        K.dma("oq%d" % (oi % 2), gT_s[c, :, t0:t0 + 512], ob.ap, r=[ob])
            oic[0] = oi

        prep(0)
        for blk in range(8):
            if blk + 1 < 8:
                prep(blk + 1)
            compute(blk)
        K.barrier()

    def phase_S():
        K.phase_reset()
        sm = lambda name, n=16: K.sb([128, n], F32, name)
        wglu = K.sb([128, 4, 512], BF16, "wglu")
        WS = K.sb([128, 16, 2, 2, 128], BF16, "WS")
        WY1 = K.sb([128, 16, 2, 2, 128], BF16, "WY1")
        WY2 = K.sb([128, 32, 128], BF16, "WY2")
        Ct = K.sb([128, 16, 128], F32, "Ct")
        St = K.sb([128, 16, 128], F32, "St")
        r8 = sm("r8")
        Dre = sm("Dre")
        Dim = sm("Dim")
        car_re = sm("car_re")
        car_im = sm("car_im")
        ta, tb_, tc_ = sm("ta"), sm("tb"), sm("tc")
        mark = K.off
        stage = K.sb([128, 512], F32, "stageS")
        ldt, are, aim = sm("ldt"), sm("are"), sm("aim")
        bre = K.sb([128, 16, 16], F32, "bre")
        bim = K.sb([128, 16, 16], F32, "bim")
        cre = K.sb([128, 16, 16], F32, "cre")
        cim = K.sb([128, 16, 16], F32, "cim")
        ncim = K.sb([128, 16, 16], F32, "ncim")
        for t_, nm in ((ldt, "ldt"), (are, "are"), (aim, "aim")):
            K.dma("c0", t_.ap, s5[nm], w=[t_])
        for t_, nm in ((bre, "bre"), (bim, "bim"), (cre, "cre"), (cim, "cim")):
            K.dma("c0", t_.ap.rearrange("p a b -> p (a b)"), s5[nm], w=[t_])
        for kc in range(4):
            K.dma("wst", stage.ap, w_glu[kc * 128:(kc + 1) * 128, :], w=[stage])
            K.cp("dve", wglu.ap[:, kc, :], stage.ap, [stage], [wglu])
        dt_, xr, th, mag, cs, sn = sm("dt"), sm("xr"), sm("th"), sm("mag"), sm("cs"), sm("sn")
        abr, abi, den, nr, fre, fim = sm("abr"), sm("abi"), sm("den"), sm("nr"), sm("fre"), sm("fim")
        ti_ = K.sb([128, 16], I32, "ti")
        V_ = "dve"
        K.act(dt_.ap, ldt.ap, AF.Exp, [ldt], [dt_])
        K.tt(V_, xr.ap, dt_.ap, are.ap, ALU.mult, [dt_, are], [xr])
        K.tt(V_, th.ap, dt_.ap, aim.ap, ALU.mult, [dt_, aim], [th])
        K.act(mag.ap, xr.ap, AF.Exp, [xr], [mag])
        K.act(r8.ap, xr.ap, AF.Exp, [xr], [r8], scale=8.0)

        def sin_small(dst, src, phase):
            K.ts(V_, ta.ap, src.ap, phase, 1.0 / (2 * PI), ALU.add, ALU.mult, [src], [ta])
            K.cp(V_, ti_.ap, ta.ap, [ta], [ti_])
            K.cp(V_, ta.ap, ti_.ap, [ti_], [ta])
            K.stt(V_, ta.ap, ta.ap, -2 * PI, src.ap, ALU.mult, ALU.add, [ta, src], [ta])
            K.ts(V_, ta.ap, ta.ap, phase, None, ALU.add, None, [ta], [ta])
            K.ts(V_, tb_.ap, ta.ap, PI, -2 * PI, ALU.is_gt, ALU.mult, [ta], [tb_])
            K.tt(V_, ta.ap, ta.ap, tb_.ap, ALU.add, [ta, tb_], [ta])
            K.ts(V_, tb_.ap, ta.ap, -PI, 2 * PI, ALU.is_lt, ALU.mult, [ta], [tb_])
            K.tt(V_, ta.ap, ta.ap, tb_.ap, ALU.add, [ta, tb_], [ta])
            K.act(dst.ap, ta.ap, AF.Sin, [ta], [dst])

        sin_small(cs, th, PI / 2)
        sin_small(sn, th, 0.0)
        K.tt(V_, abr.ap, mag.ap, cs.ap, ALU.mult, [mag, cs], [abr])
        K.tt(V_, abi.ap, mag.ap, sn.ap, ALU.mult, [mag, sn], [abi])
        K.tt(V_, den.ap, are.ap, are.ap, ALU.mult, [are], [den])
        K.tt(V_, ta.ap, aim.ap, aim.ap, ALU.mult, [aim], [ta])
        K.tt(V_, den.ap, den.ap, ta.ap, ALU.add, [den, ta], [den])
        K.op(V_, lambda e: e.reciprocal(out=den.ap, in_=den.ap), [den], [den])
        K.ts(V_, nr.ap, abr.ap, -1.0, None, ALU.add, None, [abr], [nr])
        K.tt(V_, ta.ap, nr.ap, are.ap, ALU.mult, [nr, are], [ta])
        K.tt(V_, tb_.ap, abi.ap, aim.ap, ALU.mult, [abi, aim], [tb_])
        K.tt(V_, ta.ap, ta.ap, tb_.ap, ALU.add, [ta, tb_], [ta])
        K.tt(V_, fre.ap, ta.ap, den.ap, ALU.mult, [ta, den], [fre])
        K.tt(V_, ta.ap, abi.ap, are.ap, ALU.mult, [abi, are], [ta])
        K.tt(V_, tb_.ap, nr.ap, aim.ap, ALU.mult, [nr, aim], [tb_])
        K.tt(V_, ta.ap, ta.ap, tb_.ap, ALU.subtract, [ta, tb_], [ta])
        K.tt(V_, fim.ap, ta.ap, den.ap, ALU.mult, [ta, den], [fim])
        pwf_re = K.sb([128, 16, 9], F32, "pwf_re")
        pwf_im = K.sb([128, 16, 9], F32, "pwf_im")
        pwr_re = K.sb([128, 16, 8], F32, "pwr_re")
        pwr_im = K.sb([128, 16, 8], F32, "pwr_im")
        K.memset(V_, pwf_re.ap[:, :, 0], 1.0, [pwf_re])
        K.memset(V_, pwf_im.ap[:, :, 0], 0.0, [pwf_im])
        for d in range(8):
            K.tt(V_, ta.ap, pwf_re.ap[:, :, d], abr.ap, ALU.mult, [pwf_re, abr], [ta])
            K.tt(V_, tb_.ap, pwf_im.ap[:, :, d], abi.ap, ALU.mult, [pwf_im, abi], [tb_])
            K.tt(V_, pwf_re.ap[:, :, d + 1], ta.ap, tb_.ap, ALU.subtract, [ta, tb_], [pwf_re])
            K.tt(V_, ta.ap, pwf_re.ap[:, :, d], abi.ap, ALU.mult, [pwf_re, abi], [ta])
            K.tt(V_, tb_.ap, pwf_im.ap[:, :, d], abr.ap, ALU.mult, [pwf_im, abr], [tb_])
            K.tt(V_, pwf_im.ap[:, :, d + 1], ta.ap, tb_.ap, ALU.add, [ta, tb_], [pwf_im])
        for j in range(8):
            K.cp(V_, pwr_re.ap[:, :, j], pwf_re.ap[:, :, 7 - j], [pwf_re], [pwr_re])
            K.cp(V_, pwr_im.ap[:, :, j], pwf_im.ap[:, :, 7 - j], [pwf_im], [pwr_im])
        K.cp(V_, Dre.ap, pwf_re.ap[:, :, 8], [pwf_re], [Dre])
        K.cp(V_, Dim.ap, pwf_im.ap[:, :, 8], [pwf_im], [Dim])
        ur, ui, rr = sm("ur"), sm("ui"), sm("rr")
        K.op(V_, lambda e: e.reciprocal(out=rr.ap, in_=r8.ap), [r8], [rr])
        K.tt(V_, ur.ap, Dre.ap, rr.ap, ALU.mult, [Dre, rr], [ur])
        K.tt(V_, ui.ap, Dim.ap, rr.ap, ALU.mult, [Dim, rr], [ui])
        tm1 = K.sb([128, 16, 64], F32, "tm1")
        tm2 = K.sb([128, 16, 64], F32, "tm2")
        K.memset(V_, Ct.ap[:, :, 0], 1.0, [Ct])
        K.memset(V_, St.ap[:, :, 0], 0.0, [St])
        for k in range(7):
            n = 1 << k
            urb = ur.ap.unsqueeze(2).broadcast_to([128, 16, n])
            uib = ui.ap.unsqueeze(2).broadcast_to([128, 16, n])
            K.tt(V_, tm1.ap[:, :, 0:n], Ct.ap[:, :, 0:n], urb, ALU.mult, [Ct, ur], [tm1])
            K.tt(V_, tm2.ap[:, :, 0:n], St.ap[:, :, 0:n], uib, ALU.mult, [St, ui], [tm2])
            K.tt(V_, Ct.ap[:, :, n:2 * n], tm1.ap[:, :, 0:n], tm2.ap[:, :, 0:n], ALU.subtract, [tm1, tm2], [Ct])
            K.tt(V_, tm1.ap[:, :, 0:n], Ct.ap[:, :, 0:n], uib, ALU.mult, [Ct, ui], [tm1])
            K.tt(V_, tm2.ap[:, :, 0:n], St.ap[:, :, 0:n], urb, ALU.mult, [St, ur], [tm2])
            K.tt(V_, St.ap[:, :, n:2 * n], tm1.ap[:, :, 0:n], tm2.ap[:, :, 0:n], ALU.add, [tm1, tm2], [St])
            K.tt(V_, ta.ap, ur.ap, ur.ap, ALU.mult, [ur], [ta])
            K.tt(V_, tb_.ap, ui.ap, ui.ap, ALU.mult, [ui], [tb_])
            K.tt(V_, tc_.ap, ur.ap, ui.ap, ALU.mult, [ur, ui], [tc_])
            K.tt(V_, ur.ap, ta.ap, tb_.ap, ALU.subtract, [ta, tb_], [ur])
            K.ts(V_, ui.ap, tc_.ap, 2.0, None, ALU.mult, None, [tc_], [ui])
        bbr = K.sb([128, 16, 16], F32, "bbr")
        bbi = K.sb([128, 16, 16], F32, "bbi")
        t3a = K.sb([128, 16, 16], F32, "t3a")
        t3b = K.sb([128, 16, 16], F32, "t3b")
        freb = fre.ap.unsqueeze(2).broadcast_to([128, 16, 16])
        fimb = fim.ap.unsqueeze(2).broadcast_to([128, 16, 16])
        K.tt(V_, t3a.ap, bre.ap, freb, ALU.mult, [bre, fre], [t3a])
        K.tt(V_, t3b.ap, bim.ap, fimb, ALU.mult, [bim, fim], [t3b])
        K.tt(V_, bbr.ap, t3a.ap, t3b.ap, ALU.subtract, [t3a, t3b], [bbr])
        K.tt(V_, t3a.ap, bim.ap, freb, ALU.mult, [bim, fre], [t3a])
        K.tt(V_, t3b.ap, bre.ap, fimb, ALU.mult, [bre, fim], [t3b])
        K.tt(V_, bbi.ap, t3a.ap, t3b.ap, ALU.add, [t3a, t3b], [bbi])
        K.ts(V_, ncim.ap, cim.ap, -1.0, None, ALU.mult, None, [cim], [ncim])
        Fre = K.sb([128, 16, 15, 16], F32, "Fre")
        Fim = K.sb([128, 16, 15, 16], F32, "Fim")
        t4a = K.sb([128, 16, 8, 16], F32, "t4a")
        t4b = K.sb([128, 16, 8, 16], F32, "t4b")
        K.memset("pool", Fre.ap, 0.0, [Fre])
        K.memset("pool", Fim.ap, 0.0, [Fim])
        S4 = [128, 16, 8, 16]
        prb = pwr_re.ap.unsqueeze(3).broadcast_to(S4)
        pib = pwr_im.ap.unsqueeze(3).broadcast_to(S4)
        bbrb = bbr.ap.unsqueeze(2).broadcast_to(S4)
        bbib = bbi.ap.unsqueeze(2).broadcast_to(S4)
        K.tt(V_, t4a.ap, prb, bbrb, ALU.mult, [pwr_re, bbr], [t4a])
        K.tt(V_, t4b.ap, pib, bbib, ALU.mult, [pwr_im, bbi], [t4b])
        K.tt(V_, Fre.ap[:, :, 0:8, :], t4a.ap, t4b.ap, ALU.subtract, [t4a, t4b], [Fre])
        K.tt(V_, t4a.ap, prb, bbib, ALU.mult, [pwr_re, bbi], [t4a])
        K.tt(V_, t4b.ap, pib, bbrb, ALU.mult, [pwr_im, bbr], [t4b])
        K.tt(V_, Fim.ap[:, :, 0:8, :], t4a.ap, t4b.ap, ALU.add, [t4a, t4b], [Fim])
        K.memset("pool", WS.ap, 0.0, [WS])
        K.memset("pool", WY1.ap, 0.0, [WY1])
        for p_ in range(16):
            for ri, Ft in ((0, Fre), (1, Fim)):
                ps = nps()
                K.tr(ps, ps.ap[:, 0:128], Ft.ap[:, p_, 0:8, :].rearrange("p a b -> p (a b)"), idf.ap, [Ft, idf])
                K.cp(V_, WS.ap[:, p_, ri, 0, 0:64], ps.ap[:, 0:64], [ps], [WS])
                K.cp(V_, WS.ap[:, p_, ri, 1, 64:128], ps.ap[:, 64:128], [ps], [WS])
        pfr = pwf_re.ap[:, :, 1:9].unsqueeze(3).broadcast_to(S4)
        pfi = pwf_im.ap[:, :, 1:9].unsqueeze(3).broadcast_to(S4)
        creb = cre.ap.unsqueeze(2).broadcast_to(S4)
        cimb = cim.ap.unsqueeze(2).broadcast_to(S4)
        K.tt(V_, t4a.ap, creb, pfr, ALU.mult, [cre, pwf_re], [t4a])
        K.tt(V_, t4b.ap, cimb, pfi, ALU.mult, [cim, pwf_im], [t4b])
        K.tt(V_, t4a.ap, t4a.ap, t4b.ap, ALU.subtract, [t4a, t4b], [t4a])
        for g2 in range(2):
            K.cp(V_, WY1.ap[g2 * 64:(g2 + 1) * 64, :, 0, g2, :],
                 t4a.ap[g2 * 64:(g2 + 1) * 64].rearrange("p a b c -> p a (b c)"), [t4a], [WY1])
        K.tt(V_, t4a.ap, creb, pfi, ALU.mult, [cre, pwf_im], [t4a])
        K.tt(V_, t4b.ap, cimb, pfr, ALU.mult, [cim, pwf_re], [t4b])
        K.tt(V_, t4a.ap, t4a.ap, t4b.ap, ALU.add, [t4a, t4b], [t4a])
        K.ts(V_, t4a.ap, t4a.ap, -1.0, None, ALU.mult, None, [t4a], [t4a])
        for g2 in range(2):
            K.cp(V_, WY1.ap[g2 * 64:(g2 + 1) * 64, :, 1, g2, :],
                 t4a.ap[g2 * 64:(g2 + 1) * 64].rearrange("p a b c -> p a (b c)"), [t4a], [WY1])
        dB = K.sb([128, 512], F32, "dB")
        dI = K.sb([128, 32, 8, 16], F32, "dI")
        K.dma("c0", dB.ap, ssm_d.partition_broadcast(128), w=[dB])
        K.tt(V_, dI.ap, idf.ap.rearrange("p (a b) -> p a b", a=8).unsqueeze(1).broadcast_to([128, 32, 8, 16]),
             dB.ap.rearrange("p (g c) -> p g c", g=32).unsqueeze(2).broadcast_to([128, 32, 8, 16]), ALU.mult,
             [idf, dB], [dI])
        for g in range(32):
            p_, g2 = g // 2, g % 2
            if g % 4 == 0:
                ps = nps()
            o0 = (g % 4) * 128
            sl = slice(g2 * 64, (g2 + 1) * 64)
            for j in range(8):
                oap = ps.ap[:, o0 + j * 16: o0 + (j + 1) * 16]
                K.mm(ps, oap, Fre.ap[sl, p_, 7 - j:15 - j, :].rearrange("p a b -> p (a b)"), cre.ap[sl, p_, :],
                     True, False, [Fre, cre])
                K.mm(ps, oap, Fim.ap[sl, p_, 7 - j:15 - j, :].rearrange("p a b -> p (a b)"), ncim.ap[sl, p_, :],
                     False, True, [Fim, ncim])
            if g % 4 == 3:
                K.tt(V_, WY2.ap[:, g - 3:g + 1, :], ps.ap.rearrange("p (g k) -> p g k", g=4),
                     dI.ap[:, g - 3:g + 1].rearrange("p g a b -> p g (a b)"), ALU.add, [ps, dI], [WY2])
        K.memset(V_, car_re.ap, 0.0, [car_re])
        K.memset(V_, car_im.ap, 0.0, [car_im])
        K.barrier()
        K.off = mark
        UTs = [K.sb([128, 32, 128], BF16, "UT%d" % i) for i in range(2)]
        Sres = [K.sb([128, 16, 128], F32, "Sre%d" % i) for i in range(2)]
        Sims = [K.sb([128, 16, 128], F32, "Sim%d" % i) for i in range(2)]
        gre = K.sb([128, 16, 128], F32, "gre")
        gim = K.sb([128, 16, 128], F32, "gim")
        u1 = K.sb([128, 16, 128], F32, "u1")
        u2 = K.sb([128, 16, 128], F32, "u2")
        Pre = K.sb([128, 16, 129], BF16, "Pre")
        Pim = K.sb([128, 16, 129], BF16, "Pim")
        ytm = K.sb([128, 8, 512], BF16, "ytm")
        yT = K.sb([128, 4, 1024], BF16, "yT")
        sg = K.sb([128, 512], BF16, "sg")
        sos = [K.sb([128, 4, 512], BF16, "so%d" % i) for i in range(2)]

        def S1(blk):
            UT, Sre, Sim = UTs[blk % 2], Sres[blk % 2], Sims[blk % 2]
            K.dma("ut%d" % (blk % 2), UT.ap.rearrange("p a b -> p (a b)"), UT5_s[blk], r=[ut5tok[blk]], w=[UT])
            for ri, Sx in ((0, Sre), (1, Sim)):
                for p0 in range(0, 16, 4):
                    p = nps()
                    for pi_ in range(4):
                        pp = p0 + pi_
                        K.mm(p, p.ap[:, pi_ * 128:(pi_ + 1) * 128], WS.ap[:, pp, ri, 0, :], UT.ap[:, 2 * pp, :], True, False, [WS, UT])
                        K.mm(p, p.ap[:, pi_ * 128:(pi_ + 1) * 128], WS.ap[:, pp, ri, 1, :], UT.ap[:, 2 * pp + 1, :], False, True, [WS, UT])
                    K.cp("act", Sx.ap[:, p0:p0 + 4, :], p.ap.rearrange("p (a b) -> p a b", a=4), [p], [Sx])

        def SC(blk):
            own = blk >= 4
            Sre, Sim = Sres[blk % 2], Sims[blk % 2]
            if own:
                K.cp(V_, Pre.ap[:, :, 0], car_re.ap, [car_re], [Pre])
                K.cp(V_, Pim.ap[:, :, 0], car_im.ap, [car_im], [Pim])
            K.tt(V_, ta.ap, Dre.ap, car_re.ap, ALU.mult, [Dre, car_re], [ta])
            K.tt(V_, tb_.ap, Dim.ap, car_im.ap, ALU.mult, [Dim, car_im], [tb_])
            K.tt(V_, ta.ap, ta.ap, tb_.ap, ALU.subtract, [ta, tb_], [ta])
            K.tt(V_, Sre.ap[:, :, 0], Sre.ap[:, :, 0], ta.ap, ALU.add, [Sre, ta], [Sre])
            K.tt(V_, ta.ap, Dre.ap, car_im.ap, ALU.mult, [Dre, car_im], [ta])
            K.tt(V_, tb_.ap, Dim.ap, car_re.ap, ALU.mult, [Dim, car_re], [tb_])
            K.tt(V_, ta.ap, ta.ap, tb_.ap, ALU.add, [ta, tb_], [ta])
            K.tt(V_, Sim.ap[:, :, 0], Sim.ap[:, :, 0], ta.ap, ALU.add, [Sim, ta], [Sim])
            K.tt("dve", u1.ap, Ct.ap, Sre.ap, ALU.mult, [Ct, Sre], [u1])
            K.tt("pool", u2.ap, St.ap, Sim.ap, ALU.mult, [St, Sim], [u2])
            K.tt("dve", gre.ap, u1.ap, u2.ap, ALU.add, [u1, u2], [gre])
            K.tt("dve", u1.ap, Ct.ap, Sim.ap, ALU.mult, [Ct, Sim], [u1])
            K.tt("pool", u2.ap, St.ap, Sre.ap, ALU.mult, [St, Sre], [u2])
            K.tt("dve", gim.ap, u1.ap, u2.ap, ALU.subtract, [u1, u2], [gim])
            for pp in range(16):
                rb = r8.ap[:, pp:pp + 1].to_broadcast([128, 128])
                K.op("dve", lambda e, pp=pp, rb=rb, Sre=Sre: e.tensor_tensor_scan(out=Sre.ap[:, pp, :], data0=rb, data1=gre.ap[:, pp, :],
                                                                                 initial=0.0, op0=ALU.mult, op1=ALU.add), [gre, r8], [Sre])
                K.op("dve", lambda e, pp=pp, rb=rb, Sim=Sim: e.tensor_tensor_scan(out=Sim.ap[:, pp, :], data0=rb, data1=gim.ap[:, pp, :],
                                                                                 initial=0.0, op0=ALU.mult, op1=ALU.add), [gim, r8], [Sim])
            K.tt("dve", u1.ap, Ct.ap, Sre.ap, ALU.mult, [Ct, Sre], [u1])
            K.tt("pool", u2.ap, St.ap, Sim.ap, ALU.mult, [St, Sim], [u2])
            K.tt("dve", gre.ap, u1.ap, u2.ap, ALU.subtract, [u1, u2], [gre])
            K.tt("dve", u1.ap, Ct.ap, Sim.ap, ALU.mult, [Ct, Sim], [u1])
            K.tt("pool", u2.ap, St.ap, Sre.ap, ALU.mult, [St, Sre], [u2])
            K.tt("dve", gim.ap, u1.ap, u2.ap, ALU.add, [u1, u2], [gim])
            K.cp(V_, car_re.ap, gre.ap[:, :, 127], [gre], [car_re])
            K.cp(V_, car_im.ap, gim.ap[:, :, 127], [gim], [car_im])
            if own:
                K.cp("act", Pre.ap[:, :, 1:129], gre.ap, [gre], [Pre])
                K.cp("act", Pim.ap[:, :, 1:129], gim.ap, [gim], [Pim])

        def SY(blk):
            tb = blk * 1024
            UT = UTs[blk % 2]
            for g0 in range(0, 32, 4):
                p = nps()
                for gi in range(4):
                    g = g0 + gi
                    pp, g2 = g // 2, g % 2
                    oap = p.ap[:, gi * 128:(gi + 1) * 128]
                    K.mm(p, oap, Pre.ap[:, pp, 0:128], WY1.ap[:, pp, 0, g2, :], True, False, [Pre, WY1])
                    K.mm(p, oap, Pim.ap[:, pp, 0:128], WY1.ap[:, pp, 1, g2, :], False, False, [Pim, WY1])
                    K.mm(p, oap, UT.ap[:, g, :], WY2.ap[:, g, :], False, True, [UT, WY2])
                K.act(ytm.ap.rearrange("p j (g c) -> p g j c", g=32)[:, g0:g0 + 4],
                      p.ap.rearrange("p (g j c) -> p g j c", g=4, j=8), AF.Gelu_apprx_tanh, [p], [ytm])
            for j in range(8):
                if j % 2 == 0:
                    p = nps()
                    pv = p.ap.bitcast(BF16).rearrange("p (a b) -> p a b", a=8)
                for cc in range(4):
                    K.tr(p, pv[:, (j % 2) * 4 + cc, :], ytm.ap[:, j, cc * 128:(cc + 1) * 128], idb.ap, [ytm, idb])
                K.cp("act", yT.ap[:, :, j:1024:8], pv[:, (j % 2) * 4:(j % 2) * 4 + 4, :], [p], [yT])
            for half in range(2):
                for co in range(4):
                    p = nps()
                    for kc in range(4):
                        K.mm(p, p.ap, wglu.ap[:, kc, co * 128:(co + 1) * 128], yT.ap[:, kc, half * 512:(half + 1) * 512],
                             kc == 0, kc == 3, [wglu, yT])
                    K.act(sg.ap, p.ap, AF.Sigmoid, [p], [sg])
                    so = sos[half]
                    K.tt("pool", so.ap[:, co, :], yT.ap[:, co, half * 512:(half + 1) * 512], sg.ap,
                         ALU.mult, [yT, sg], [so])
                t0_ = tb - NT_OWN + half * 512
                K.dma("oS%d" % half, ssmT_s[:, :, t0_: t0_ + 512].rearrange("c p t -> p c t"), sos[half].ap, r=[sos[half]])

        S1(0)
        for blk in range(8):
            if blk + 1 < 8:
                S1(blk + 1)
            SC(blk)
            if blk >= 4:
                SY(blk)
        K.barrier()

    def phase_B():
        K.phase_reset()
        wpsB = K.sb([128, 4, 1024], BF16, "wpsB")
        wpaB = K.sb([128, 4, 1024], BF16, "wpaB")
        woutB = K.sb([128, 8, 1024], BF16, "woutB")
        wqB = K.sb([128, 8, 2048], BF16, "wqB")
        wstg2 = K.sb([128, 2048], F32, "wstg2")
        wchunks = ([(wpsB, w_ps, kc, 1024) for kc in range(4)] + [(wpaB, w_pa, kc, 1024) for kc in range(4)]
                   + [(woutB, w_out, kc, 1024) for kc in range(8)] + [(wqB, w_q, kc, 2048) for kc in range(8)])
        KTs = [K.sb([96, NT_LOC], BF16, "KT%d" % i) for i in range(2)]
        QTs = [K.sb([96, NT_OWN], BF16, "QT%d" % i) for i in range(2)]
        Vs = [K.sb([128, 64, 65], BF16, "V%d" % i) for i in range(2)]
        QA = K.sb([64, NT_OWN], BF16, "QA")
        att = K.sb([128, 32, 512], BF16, "att")
        kmax = K.sb([64, 1], F32, "kmax")
        kmaxb = K.sb([64, 1], BF16, "kmaxb")
        ksb = K.sb([64, 32], BF16, "ksb")
        valid = K.sb([128, 512], F32, "valid")
        ownm = K.sb([128, 512], F32, "ownm")
        negb = K.sb([128, 512], F32, "negb")
        stg = K.sb([128, 2048], F32, "stgB")
        cm = K.sb([128, 4, 512], BF16, "cm")
        NG = 4
        gms = [K.sb([128, 32], F32, "gm%d" % i) for i in range(NG)]
        m8s = [K.sb([128, 8], F32, "m8%d" % i) for i in range(NG)]
        sels = [K.sb([128, 32], F32, "sel%d" % i) for i in range(NG)]
        sexs = [K.sb([128, 128], BF16, "sex%d" % i) for i in range(NG)]
        mraws = [K.sb([128, 4], F32, "mraw%d" % i) for i in range(2)]
        PTs = [K.sb([128, 512], BF16, "PT%d" % i) for i in range(3)]
        rls = [K.sb([128, 1], F32, "rl%d" % i) for i in range(4)]
        ostg = [K.sb([128, 4, 128], BF16, "ostg%d" % i) for i in range(2)]
        K.dma("c0", valid.ap, validc.partition_broadcast(128), w=[valid])
        K.dma("c0", ownm.ap, ownc.partition_broadcast(128), w=[ownm])
        K.ts("dve", negb.ap, valid.ap, -1.0, 1e30, ALU.add, ALU.mult, [valid], [negb])
        for i in range(4):
            K.dma("c0", stg.ap[:, 0:512], cm_d[i], w=[stg])
            K.cp("dve", cm.ap[:, i, :], stg.ap[:, 0:512], [stg], [cm])
        gm4 = K.sb([128, 4, 32], F32, "gm4")
        m84 = K.sb([128, 4, 8], F32, "m84")
        sel4 = K.sb([128, 4, 32], F32, "sel4")
        sex4 = K.sb([128, 4, 128], BF16, "sex4")
        K.memset("pool", sex4.ap, 0.0, [sex4])
        for kb_ in KTs:
            for c4 in range(4):
                K.dma("c0", stg.ap[64:96, :], koh_d[:, c4 * 2048:(c4 + 1) * 2048], w=[stg])
                K.cp("dve", kb_.ap[64:96, c4 * 2048:(c4 + 1) * 2048], stg.ap[64:96, :], [stg], [kb_])
        for h in range(8):
            hp, pb = h // 2, (h % 2) * 64
            KT, QT, V = KTs[h % 2], QTs[h % 2], Vs[h % 2]
            K.dma("bk%d" % (h % 2), KT.ap[0:64, :], kT_s[hp, pb:pb + 64, :], w=[KT])
            K.dma("bq%d" % (h % 2), QT.ap[0:64, :], qT_s[hp, pb:pb + 64, :], w=[QT])
            K.dma("bv%d" % (h % 2), V.ap, v_s.rearrange("(t p) c -> p t c", p=128)[:, :, h * 65:(h + 1) * 65], w=[V])
            K.act(QA.ap, QT.ap[0:64, :], AF.Abs, [QT], [QA])
            K.op("dve", lambda e, KT=KT: e.tensor_reduce(out=kmax.ap, in_=KT.ap[0:64, :], axis=AX.X, op=ALU.max,
                                                         apply_absolute_value=True), [KT], [kmax])
            K.cp("dve", kmaxb.ap, kmax.ap, [kmax], [kmaxb])
            K.cp("dve", ksb.ap, ksum.ap[pb:pb + 64, hp, :], [ksum], [ksb])
            for qg in range(8):
                q0 = qg * 512
                pg = PS[qg % 3]
                c0 = (qg // 3) * 132
                for j in range(4):
                    K.mm(pg, pg.ap[:, c0 + j * 32: c0 + (j + 1) * 32], QT.ap[0:64, q0 + j * 128: q0 + (j + 1) * 128],
                         ksb.ap, True, True, [QT, ksb])
                    K.mm(pg, pg.ap[:, c0 + 128 + j: c0 + 129 + j], QA.ap[:, q0 + j * 128: q0 + (j + 1) * 128],
                         kmaxb.ap, True, True, [QA, kmaxb])
            for qg in range(8):
                q0 = qg * 512
                pg = PS[qg % 3]
                c0 = (qg // 3) * 132
                mraw = mraws[qg % 2]
                K.cp("dve", mraw.ap, pg.ap[:, c0 + 128: c0 + 132], [pg], [mraw])
                S4 = [128, 2, 2, 32]
                qsl = slice(2 * qg * 32, (2 * qg + 2) * 32)
                bq = lambda t_: t_.ap[:, qsl].rearrange("p (a b) -> p a b", a=2).unsqueeze(2).broadcast_to(S4)
                g4 = gm4.ap.rearrange("p (a c) b -> p a c b", a=2)
                s4 = sel4.ap.rearrange("p (a c) b -> p a c b", a=2)
                K.tt("dve", g4, pg.ap[:, c0:c0 + 128].rearrange("p (a c b) -> p a c b", a=2, c=2), bq(negb), ALU.add, [pg, negb], [gm4])
                for j in range(4):
                    K.op("dve", lambda e, j=j: e.max(out=m84.ap[:, j, :], in_=gm4.ap[:, j, :]), [gm4], [m84])
                K.tt("dve", sel4.ap, gm4.ap, m84.ap[:, :, 2:3].broadcast_to([128, 4, 32]), ALU.is_ge, [gm4, m84], [sel4])
                K.tt("dve", s4, s4, bq(valid), ALU.mult, [sel4, valid], [sel4])
                K.tt("dve", s4, s4, bq(ownm), ALU.add, [sel4, ownm], [sel4])
                K.ts("dve", sel4.ap, sel4.ap, -1.0, BIG, ALU.add, ALU.mult, [sel4], [sel4])
                K.tt("dve", sex4.ap[:, :, 64:96], sel4.ap, mraw.ap.unsqueeze(2).broadcast_to([128, 4, 32]), ALU.subtract,
                     [sel4, mraw], [sex4])
                pt = PS[3]
                for j in range(4):
                    K.mm(pt, pt.ap[:, j * 128:(j + 1) * 128], sex4.ap[:, j, :], idb.ap, True, True, [sex4, idb])
                K.cp("act", QT.ap[64:96, q0:q0 + 512], pt.ap[64:96, :], [pt], [QT])
            for qg in range(8):
                q0 = qg * 512
                nkt = 36 + 4 * qg

                def qk(kt):
                    ps = PS[kt % 3]
                    last_own = kt >= nkt - 4
                    K.mm(ps, ps.ap, KT.ap[:, kt * 128:(kt + 1) * 128], QT.ap[:, q0:q0 + 512],
                         True, not last_own, [KT, QT])
                    if last_own:
                        K.mm(ps, ps.ap, idb.ap, cm.ap[:, kt - (nkt - 4), :], False, True, [idb, cm])

                qk(0)
                qk(1)
                for kt in range(nkt):
                    if kt + 2 < nkt:
                        qk(kt + 2)
                    ps = PS[kt % 3]
                    PT = PTs[kt % 3]
                    K.act(PT.ap, ps.ap, AF.Exp, [ps], [PT], scale=0.125)
                    for j in range(4):
                        po = PS[4 + j]
                        K.mm(po, po.ap[:, 0:65], PT.ap[:, j * 128:(j + 1) * 128], V.ap[:, kt, :],
                             kt == 0, kt == nkt - 1, [PT, V])
                for j in range(4):
                    po = PS[4 + j]
                    K.op("dve", lambda e, po=po, j=j: e.reciprocal(out=rls[j].ap, in_=po.ap[:, 64:65]), [po], [rls[j]])
                for j in range(4):
                    po = PS[4 + j]
                    K.ts("dve", att.ap[:, qg * 4 + j, h * 64:(h + 1) * 64], po.ap[:, 0:64], rls[j].ap[:, 0:1], None,
                         ALU.mult, None, [po, rls[j]], [att])
                wi = h * 8 + qg
                wbufs = (stg, wstg2)
                if wi < len(wchunks):
                    dst, src, kc, ncols = wchunks[wi]
                    K.dma("bw%d" % (wi % 2), wbufs[wi % 2].ap[:, 0:ncols], src[kc * 128:(kc + 1) * 128, :], w=[wbufs[wi % 2]])
                if 1 <= wi <= len(wchunks):
                    dst, src, kc, ncols = wchunks[wi - 1]
                    K.cp("dve", dst.ap[:, kc, :], wbufs[(wi - 1) % 2].ap[:, 0:ncols], [wbufs[(wi - 1) % 2]], [dst])
        for qt in range(32):
            p = PS[qt % 2]
            pv = p.ap.bitcast(BF16)[:, 0:512].rearrange("p (a b) -> p a b", a=4)
            for c in range(4):
                K.tr(p, pv[:, c, :], att.ap[:, qt, c * 128:(c + 1) * 128], idb.ap, [att, idb])
            og = ostg[qt % 2]
            K.cp("act", og.ap, pv, [p], [og])
            K.dma("ob%d" % (qt % 2), attT_s[:, :, qt * 128:(qt + 1) * 128].rearrange("c p t -> p c t"), og.ap, r=[og])
        K.barrier()

    def phase_C1():
        K.phase_reset()
        wps = K.sb([128, 4, 1024], BF16, "wps")
        wpa = K.sb([128, 4, 1024], BF16, "wpa")
        wout = K.sb([128, 8, 1024], BF16, "wout")
        wq = K.sb([128, 8, 2048], BF16, "wq")
        keysT = K.sb([128, 16, 128], F32, "keysT")
        gf = K.sb([128, 1024], F32, "gffn")
        stage = K.sb([128, 2048], F32, "stageC")
        ssmT = K.sb([128, 4, 512], BF16, "ssmT")
        attT = K.sb([128, 4, 512], BF16, "attT")
        gaT = K.sb([128, 8, 512], BF16, "gaT")
        gbT = K.sb([128, 8, 512], BF16, "gbT")
        mT = K.sb([128, 8, 512], BF16, "mT")
        t1cs = [K.sb([128, 512], F32, "t1C%d" % i) for i in range(2)]
        t2cs = [K.sb([128, 512], F32, "t2C%d" % i) for i in range(2)]
        xts = [K.sb([128, 1024], F32, "xtC%d" % i) for i in range(2)]
        x1s = [K.sb([128, 1024], F32, "x1C%d" % i) for i in range(2)]
        hbs = [K.sb([128, 1024], BF16, "hbC%d" % i) for i in range(2)]
        sq = K.sb([128, 1024], BF16, "sqC")
        ss = K.sb([128, 1], F32, "ssC")
        rs = K.sb([128, 1], F32, "rsC")
        h2T = K.sb([128, 8, 512], BF16, "h2T")
        qT = K.sb([128, 16, 512], F32, "qT")
        scs = [K.sb([128, 16, 128], F32, "sc%d" % i) for i in range(2)]
        K.dma("c0", gf.ap, g_ffn.partition_broadcast(128), w=[gf])
        for ch in range(16):
            K.dma("wst", stage.ap[:, 0:128], keys[ch], w=[stage])
            p = nps()
            K.tr(p, p.ap[:, 0:128], stage.ap[:, 0:128], idf.ap, [stage, idf])
            K.cp("dve", keysT.ap[:, ch, :], p.ap[:, 0:128], [p], [keysT])
        xi = 0

        def c1_loads(tg_):
            ta_ = tg_ * 512
            K.dma("c1a", ssmT.ap, ssmT_s[:, :, ta_:ta_ + 512].rearrange("c p t -> p c t"), w=[ssmT])
            K.dma("c1b", attT.ap, attT_s[:, :, ta_:ta_ + 512].rearrange("c p t -> p c t"), w=[attT])
            K.dma("c1c", gaT.ap, gT_s[0:8, :, ta_:ta_ + 512].rearrange("c p t -> p c t"), w=[gaT])
            K.dma("c1d", gbT.ap, gT_s[8:16, :, ta_:ta_ + 512].rearrange("c p t -> p c t"), w=[gbT])

        c1_loads(0)
        for tg in range(8):
            t0 = tg * 512
            for c in range(8):
                pa = nps()
                pb = nps()
                for kc in range(4):
                    K.mm(pa, pa.ap, wps.ap[:, kc, c * 128:(c + 1) * 128], ssmT.ap[:, kc, :], kc == 0, kc == 3, [wps, ssmT])
                for kc in range(4):
                    K.mm(pb, pb.ap, wpa.ap[:, kc, c * 128:(c + 1) * 128], attT.ap[:, kc, :], kc == 0, kc == 3, [wpa, attT])
                t1, t2 = t1cs[c % 2], t2cs[c % 2]
                K.tt("dve", t1.ap, pa.ap, gaT.ap[:, c, :], ALU.mult, [pa, gaT], [t1])
                K.tt("dve", t2.ap, pb.ap, gbT.ap[:, c, :], ALU.mult, [pb, gbT], [t2])
                K.tt("dve", mT.ap[:, c, :], t1.ap, t2.ap, ALU.add, [t1, t2], [mT])
            if tg + 1 < 8:
                c1_loads(tg + 1)
            for j in range(4):
                xt = xts[xi % 2]
                x1 = x1s[xi % 2]
                hb = hbs[xi % 2]
                xi += 1
                r0 = t0 + j * 128
                K.dma("x%d" % (xi % 2), xt.ap, xc[NT_OWN + r0: NT_OWN + r0 + 128, :], w=[xt])
                for half in range(2):
                    p = nps()
                    for kc in range(8):
                        K.mm(p, p.ap, mT.ap[:, kc, j * 128:(j + 1) * 128], wout.ap[:, kc, half * 512:(half + 1) * 512],
                             kc == 0, kc == 7, [mT, wout])
                    K.tt("dve", x1.ap[:, half * 512:(half + 1) * 512], p.ap, xt.ap[:, half * 512:(half + 1) * 512], ALU.add,
                         [p, xt], [x1])
                K.dma("x1o%d" % (xi % 2), x1_s[r0:r0 + 128, :], x1.ap, r=[x1])
                rmsnorm_tile(x1, gf, hb, sq, ss, rs)
                p = nps()
                pv = p.ap.bitcast(BF16).rearrange("p (a b) -> p a b", a=8)
                for kc in range(8):
                    K.tr(p, pv[:, kc, :], hb.ap[:, kc * 128:(kc + 1) * 128], idb.ap, [hb, idb])
                K.cp("act", h2T.ap[:, :, j * 128:(j + 1) * 128], pv, [p], [h2T])
            K.dma("h2o", h2T_s[:, :, t0:t0 + 512].rearrange("c p t -> p c t"), h2T.ap, r=[h2T])
            for ch in range(16):
                p = nps()
                for kc in range(8):
                    K.mm(p, p.ap, wq.ap[:, kc, ch * 128:(ch + 1) * 128], h2T.ap[:, kc, :], kc == 0, kc == 7, [wq, h2T])
                K.cp("act" if ch % 2 else "dve", qT.ap[:, ch, :], p.ap, [p], [qT])
            for j in range(4):
                sc = scs[j % 2]
                for c0 in range(0, 16, 4):
                    p = nps()
                    for i in range(4):
                        K.mm(p, p.ap[:, i * 128:(i + 1) * 128], qT.ap[:, c0 + i, j * 128:(j + 1) * 128], keysT.ap[:, c0 + i, :],
                             True, True, [qT, keysT])
                    K.cp("act" if (c0 // 4) % 2 else "dve", sc.ap[:, c0:c0 + 4, :], p.ap.rearrange("p (a b) -> p a b", a=4), [p], [sc])
                r0 = t0 + j * 128
                K.dma("sco%d" % (j % 2), sc_s[r0:r0 + 128, :], sc.ap.rearrange("p a b -> p (a b)"), r=[sc])
        K.barrier()

    U32 = mybir.dt.uint32

    def phase_C2():
        K.phase_reset()
        iot = K.sb([128, 128], F32, "iota")
        K.dma("c0", iot.ap, iota_d, w=[iot])
        ss_ = [K.sb([128, 16, 128], F32, "s%d" % i) for i in range(2)]
        v = K.sb([128, 16, 16], F32, "v")
        idx = K.sb([128, 8, 16], U32, "idx")
        idxf = K.sb([128, 128], F32, "idxf")
        idxT = K.sb([128, 128], F32, "idxT")
        top = K.sb([128, 8, 16], F32, "top")
        e16 = K.sb([128, 8, 16], F32, "e16")
        Z = K.sb([128, 8], F32, "Z")
        bE = K.sb([128, 8], F32, "bE")
        v1m = K.sb([128, 8, 16], F32, "v1m")
        sums = [K.sb([128, 16, 128], F32, "sum%d" % i) for i in range(3)]
        Es = [K.sb([128, 16, 128], BF16, "E%d" % i) for i in range(3)]
        Btm = K.sb([128, 8, 16, 128], BF16, "Btm")
        BT = K.sb([128, 128, 128], BF16, "BT")
        AT = K.sb([128, 128, 128], BF16, "AT")
        Gst = K.sb([128, 128, 128], BF16, "Gst")
        works = [K.sb([128, 128], F32, "wk%d" % i) for i in range(16)]
        work2s = [K.sb([128, 256], F32, "wk2%d" % i) for i in range(8)]
        K.dma("c2s0", ss_[0].ap.rearrange("p a b -> p (a b)"), sc_s[0:128, :], w=[ss_[0]])
        for tl in range(32):
            r0 = tl * 128
            s_ = ss_[tl % 2]
            if tl + 1 < 32:
                sn_ = ss_[(tl + 1) % 2]
                K.dma("c2s%d" % ((tl + 1) % 2), sn_.ap.rearrange("p a b -> p (a b)"), sc_s[r0 + 128:r0 + 256, :], w=[sn_])
            for ch in range(16):
                K.op("dve", lambda e, ch=ch, s_=s_: e.max(out=v.ap[:, ch, 0:8], in_=s_.ap[:, ch, :]), [s_], [v])
            for ch in range(0, 16, 2):
                K.op("dve", lambda e, ch=ch, s_=s_: e.max_index(out=idx.ap[:, ch // 2, 0:8], in_max=v.ap[:, ch, 0:8],
                                                                in_values=s_.ap[:, ch, :]), [v, s_], [idx])
            for ch in range(16):
                K.op("dve", lambda e, ch=ch, s_=s_: e.match_replace(out=works[ch].ap, in_to_replace=v.ap[:, ch, 0:8],
                                                                    in_values=s_.ap[:, ch, :], imm_value=-1e30), [v, s_], [works[ch]])
            for ch in range(16):
                K.op("dve", lambda e, ch=ch: e.max(out=v.ap[:, ch, 8:16], in_=works[ch].ap), [works[ch]], [v])
            for ch in range(0, 16, 2):
                K.op("dve", lambda e, ch=ch: e.max_index(out=idx.ap[:, ch // 2, 8:16], in_max=v.ap[:, ch, 8:16],
                                                         in_values=works[ch].ap), [v, works[ch]], [idx])
            vv = v.ap.rearrange("p (h s) k -> p h s k", s=2)
            v1 = vv[:, :, 0, :]
            v2 = vv[:, :, 1, :]
            cand_ap = sums[0].ap.rearrange("p a b -> p (a b)").rearrange("p (h c) -> p h c", h=8)
            K.tt("dve", cand_ap.rearrange("p h (i j) -> p h i j", i=16), v1.unsqueeze(3).broadcast_to([128, 8, 16, 16]),
                 v2.unsqueeze(2).broadcast_to([128, 8, 16, 16]), ALU.add, [v], [sums[0]])
            for h in range(8):
                K.op("dve", lambda e, h=h: e.max(out=top.ap[:, h, 0:8], in_=cand_ap[:, h, :]), [sums[0]], [top])
            for h in range(8):
                K.op("dve", lambda e, h=h: e.match_replace(out=work2s[h].ap, in_to_replace=top.ap[:, h, 0:8],
                                                           in_values=cand_ap[:, h, :], imm_value=-1e30), [top, sums[0]], [work2s[h]])
            for h in range(8):
                K.op("dve", lambda e, h=h: e.max(out=top.ap[:, h, 8:16], in_=work2s[h].ap), [work2s[h]], [top])
            mxb = top.ap[:, :, 0:1].broadcast_to([128, 8, 16])
            taub = top.ap[:, :, 15:16].broadcast_to([128, 8, 16])
            K.tt("dve", e16.ap, top.ap, mxb, ALU.subtract, [top], [e16])
            K.act(e16.ap, e16.ap, AF.Exp, [e16], [e16])
            K.op("dve", lambda e: e.tensor_reduce(out=Z.ap, in_=e16.ap, axis=AX.X, op=ALU.add), [e16], [Z])
            K.act(Z.ap, Z.ap, AF.Ln, [Z], [Z])
            K.tt("dve", bE.ap, top.ap[:, :, 15], top.ap[:, :, 0], ALU.subtract, [top], [bE])
            K.tt("dve", bE.ap, bE.ap, Z.ap, ALU.subtract, [bE, Z], [bE])
            K.tt("dve", v1m.ap, v1, taub, ALU.subtract, [v, top], [v1m])
            K.cp("dve", idxf.ap, idx.ap.rearrange("p h k -> p (h k)"), [idx], [idxf])
            p = nps()
            K.tr(p, p.ap[:, 0:128], idxf.ap, idf.ap, [idxf, idf])
            K.cp("dve", idxT.ap, p.ap[:, 0:128], [p], [idxT])
            K.tt("dve", AT.ap, iot.ap.unsqueeze(1).broadcast_to([128, 128, 128]),
                 idxT.ap.unsqueeze(2).broadcast_to([128, 128, 128]), ALU.is_equal, [iot, idxT], [AT])
            def bsum(h):
                sm_ = sums[h % 3]
                K.tt("pool" if h % 2 == 0 else "dve", sm_.ap, v1m.ap[:, h, :].unsqueeze(2).broadcast_to([128, 16, 128]),
                     s_.ap[:, 2 * h + 1, :].unsqueeze(1).broadcast_to([128, 16, 128]), ALU.add, [v1m, s_], [sm_])
                K.act(Es[h % 3].ap, sm_.ap, AF.Exp, [sm_, bE], [Es[h % 3]], bias=bE.ap[:, h:h + 1])

            bsum(0)
            bsum(1)
            for h in range(8):
                if h + 2 < 8:
                    bsum(h + 2)
                K.stt("dve", Btm.ap[:, h], sums[h % 3].ap, -1e-5, Es[h % 3].ap, ALU.is_ge, ALU.mult, [sums[h % 3], Es[h % 3]], [Btm])
            for b0 in range(0, 128, 8):
                p = nps()
                pv = p.ap.bitcast(BF16).rearrange("p (a b) -> p a b", a=8)
                for k in range(8):
                    K.tr(p, pv[:, k, :], Btm.ap[:, :, :, b0 + k].rearrange("p h i -> p (h i)"), idb.ap, [Btm, idb])
                K.cp("act", BT.ap[:, :, b0:b0 + 8], pv.rearrange("p b t -> p t b"), [p], [BT])
            for t4 in range(0, 128, 4):
                p = nps()
                for k in range(4):
                    t = t4 + k
                    K.mm(p, p.ap[:, k * 128:(k + 1) * 128], BT.ap[:, t, :], AT.ap[:, t, :], True, True, [BT, AT])
                K.cp("act", Gst.ap[:, :, t4:t4 + 4],
                     p.ap.rearrange("p (t a) -> p a t", t=4), [p], [Gst])
            K.dma("c2g", G_s[tl], Gst.ap, r=[Gst])
        K.barrier()

    def phase_D():
        K.phase_reset()
        TG = 1024
        NG_ = NT_OWN // TG
        h2T = K.sb([128, 8, TG], BF16, "h2TD")
        Ubs = [K.sb([128, 1024], F32, "Ub%d" % i) for i in range(3)]
        Ubb = [K.sb([128, 1024], BF16, "Ubb%d" % i) for i in range(2)]
        UbTs = [K.sb([128, 8, 128], BF16, "UbT%d" % i) for i in range(3)]
        Ghs = [K.sb([128, 8, 8, 128], BF16, "Gh%d" % i) for i in range(2)]
        ges = [K.sb([128, 512], BF16, "ge%d" % i) for i in range(2)]
        W = K.sb([128, 16, TG], BF16, "W")
        Vbs = [K.sb([128, 1024], F32, "Vb%d" % i) for i in range(3)]
        Vbf = K.sb([128, 16, 1024], BF16, "Vbf")
        acc = K.sb([128, 8, 1024], F32, "acc")
        x1t = [K.sb([128, 1024], F32, "x1D%d" % i) for i in range(2)]
        uttok = [Buf(None, "ut%d" % i) for i in range(128)]
        NI = NG_ * 128

        def load(n):
            grp, a = n // 128, n % 128
            t0 = grp * TG
            if grp == 0:
                K.dma("du%d" % (n % 3), Ubs[n % 3].ap, peer_u[a * 128:(a + 1) * 128, :], w=[Ubs[n % 3]])
            else:
                K.dma("du%d" % (n % 3), UbTs[n % 3].ap.rearrange("p a b -> p (a b)"), UT_s[a], r=[uttok[a]], w=[UbTs[n % 3]])
            if a % 8 == 0:
                Gh = Ghs[(n // 8) % 2]
                tl0 = t0 // 128
                K.dma("dg%d" % ((n // 8) % 2), Gh.ap, G_s[tl0:tl0 + 8, :, a:a + 8, :].rearrange("tl b a t -> b tl a t"), w=[Gh])
            K.dma("dv%d" % (n % 3), Vbs[n % 3].ap, peer_v[a * 128:(a + 1) * 128, :], w=[Vbs[n % 3]])

        def trans(n):
            if n // 128 != 0:
                return
            Ub, UbT, ub = Ubs[n % 3], UbTs[n % 3], Ubb[n % 2]
            K.cp("dve", ub.ap, Ub.ap, [Ub], [ub])
            p = nps()
            pv = p.ap.bitcast(BF16).rearrange("p (a b) -> p a b", a=8)
            for kc in range(8):
                K.tr(p, pv[:, kc, :], ub.ap[:, kc * 128:(kc + 1) * 128], idb.ap, [ub, idb])
            K.cp("act", UbT.ap, pv, [p], [UbT])

        load(0)
        load(1)
        trans(0)
        for n in range(NI):
            grp, a = n // 128, n % 128
            t0 = grp * TG
            si = a % 16
            if a == 0:
                K.dma("dh", h2T.ap, h2T_s[:, :, t0:t0 + TG].rearrange("c p t -> p c t"), w=[h2T])
                K.memset("pool", acc.ap, 0.0, [acc])
            if n + 2 < NI:
                load(n + 2)
            if n + 1 < NI:
                trans(n + 1)
            UbT, Vb = UbTs[n % 3], Vbs[n % 3]
            Gh = Ghs[(n // 8) % 2]
            if grp == 0:
                K.dma("dut%d" % (n % 3), UT_s[a], UbT.ap.rearrange("p a b -> p (a b)"), r=[UbT], w=[uttok[a]])
            K.cp("act", Vbf.ap[:, si, :], Vb.ap, [Vb], [Vbf])
            for hf in range(TG // 512):
                p = nps()
                for kc in range(8):
                    K.mm(p, p.ap, UbT.ap[:, kc, :], h2T.ap[:, kc, hf * 512:(hf + 1) * 512], kc == 0, kc == 7, [UbT, h2T])
                ge = ges[hf % 2]
                K.act(ge.ap, p.ap, AF.Gelu_apprx_tanh, [p], [ge])
                K.tt("dve", W.ap[:, si, hf * 512:(hf + 1) * 512].rearrange("p (a b) -> p a b", a=4),
                     ge.ap.rearrange("p (a b) -> p a b", a=4), Gh.ap[:, hf * 4:(hf + 1) * 4, a % 8, :], ALU.mult,
                     [ge, Gh], [W])
            if si == 15:
                for j in range(TG // 128):
                    for hf in range(2):
                        p = nps()
                        for s2 in range(16):
                            K.mm(p, p.ap, W.ap[:, s2, j * 128:(j + 1) * 128], Vbf.ap[:, s2, hf * 512:(hf + 1) * 512],
                                 s2 == 0, s2 == 15, [W, Vbf])
                        K.tt("dve", acc.ap[:, j, hf * 512:(hf + 1) * 512], p.ap, acc.ap[:, j, hf * 512:(hf + 1) * 512], ALU.add,
                             [p, acc], [acc])
            if a == 127:
                for j in range(TG // 128):
                    xt = x1t[j % 2]
                    r0 = t0 + j * 128
                    K.dma("dx%d" % (j % 2), xt.ap, x1_s[r0:r0 + 128, :], w=[xt])
                    K.tt("dve", xt.ap, xt.ap, acc.ap[:, j, :], ALU.add, [xt, acc], [xt])
                    K.dma("dx%d" % (j % 2), x2_s[r0:r0 + 128, :], xt.ap, r=[xt])
        K.barrier()

    def phase_E():
        K.phase_reset()
        wpg = K.sb([128, 8, 1024], BF16, "wpg")
        wpp = K.sb([128, 2, 1024], BF16, "wpp")
        stage = K.sb([128, 1024], F32, "stageE")
        gp = K.sb([128, 1024], F32, "gple")
        gfin = K.sb([128, 1024], F32, "gfin")
        xts = [K.sb([128, 1024], F32, "xtE%d" % i) for i in range(3)]
        pts = [K.sb([128, 256], F32, "ptE%d" % i) for i in range(3)]
        pb_s = [K.sb([128, 256], BF16, "pbE%d" % i) for i in range(2)]
        pTs = [K.sb([128, 2, 128], BF16, "pTE%d" % i) for i in range(2)]
        hb_s = [K.sb([128, 1024], BF16, "hbE%d" % i) for i in range(2)]
        hTs_ = [K.sb([128, 8, 128], BF16, "hTE%d" % i) for i in range(2)]
        sq_s = [K.sb([128, 1024], BF16, "sqE%d" % i) for i in range(2)]
        ss_s = [K.sb([128, 1], F32, "ssE%d" % i) for i in range(4)]
        rs_s = [K.sb([128, 1], F32, "rsE%d" % i) for i in range(4)]
        sgts = [K.sb([128, 1024], F32, "sgE%d" % i) for i in range(2)]
        outs = [K.sb([128, 1024], F32, "oE%d" % i) for i in range(2)]
        K.dma("c0", gp.ap, g_ple.partition_broadcast(128), w=[gp])
        K.dma("c0", gfin.ap, g_fin.partition_broadcast(128), w=[gfin])
        load_w_bf(wpg, w_pg, 8, 1024, stage, "wst")
        load_w_bf(wpp, w_pp, 2, 1024, stage, "wst")
        def e_vars(tl):
            return dict(r0=tl * 128)

        def front(tl):
            r0 = tl * 128
            xt, pt, ot = xts[tl % 3], pts[tl % 3], outs[tl % 2]
            pb_, pT, hb, hT, sq, sgt = pb_s[tl % 2], pTs[tl % 2], hb_s[tl % 2], hTs_[tl % 2], sq_s[tl % 2], sgts[tl % 2]
            ss, rs = ss_s[tl % 2], rs_s[tl % 2]
            ss2, rs2 = ss_s[2 + tl % 2], rs_s[2 + tl % 2]
            K.dma("ex%d" % (tl % 3), xt.ap, x2_s[r0:r0 + 128, :], w=[xt])
            K.dma("ep%d" % (tl % 3), pt.ap, pc[r0:r0 + 128, :], w=[pt])
            rmsnorm_tile(xt, gp, hb, sq, ss, rs)
            p = nps()
            pv = p.ap.bitcast(BF16).rearrange("p (a b) -> p a b", a=8)
            for kc in range(8):
                K.tr(p, pv[:, kc, :], hb.ap[:, kc * 128:(kc + 1) * 128], idb.ap, [hb, idb])
            K.cp("act", hT.ap, pv, [p], [hT])
            K.cp("dve", pb_.ap, pt.ap, [pt], [pb_])
            p = nps()
            pv = p.ap.bitcast(BF16).rearrange("p (a b) -> p a b", a=8)
            for kc in range(2):
                K.tr(p, pv[:, kc, :], pb_.ap[:, kc * 128:(kc + 1) * 128], idb.ap, [pb_, idb])
            K.cp("act", pT.ap, pv[:, 0:2, :], [p], [pT])

        def front2(tl):
            pT, hT, sgt = pTs[tl % 2], hTs_[tl % 2], sgts[tl % 2]
            for hf in range(2):
                pg = nps()
                pe_ = nps()
                for kc in range(8):
                    K.mm(pg, pg.ap, hT.ap[:, kc, :], wpg.ap[:, kc, hf * 512:(hf + 1) * 512], kc == 0, kc == 7, [hT, wpg])
                for kc in range(2):
                    K.mm(pe_, pe_.ap, pT.ap[:, kc, :], wpp.ap[:, kc, hf * 512:(hf + 1) * 512], kc == 0, kc == 1, [pT, wpp])
                K.act(sgt.ap[:, hf * 512:(hf + 1) * 512], pg.ap, AF.Sigmoid, [pg], [sgt])
                K.tt("dve", sgt.ap[:, hf * 512:(hf + 1) * 512], pe_.ap, sgt.ap[:, hf * 512:(hf + 1) * 512], ALU.mult, [pe_, sgt], [sgt])

        def back(tl):
            r0 = tl * 128
            xt, ot = xts[tl % 3], outs[tl % 2]
            sq, sgt = sq_s[tl % 2], sgts[tl % 2]
            ss2, rs2 = ss_s[2 + tl % 2], rs_s[2 + tl % 2]
            K.tt("pool", xt.ap, xt.ap, sgt.ap, ALU.add, [xt, sgt], [xt])
            K.act(sq.ap, xt.ap, AF.Square, [xt], [sq, ss2], accum=ss2.ap)
            K.act(rs2.ap, ss2.ap, AF.Sqrt, [ss2], [rs2], scale=1.0 / 1024, bias=eps_b.ap)
            K.op("dve", lambda e, rs2=rs2: e.reciprocal(out=rs2.ap, in_=rs2.ap), [rs2], [rs2])
            K.stt("dve", ot.ap, xt.ap, rs2.ap[:, 0:1], gfin.ap, ALU.mult, ALU.mult, [xt, rs2, gfin], [ot])
            K.dma("eo%d" % (tl % 2), out[r0:r0 + 128, :], ot.ap, r=[ot])

        front(0)
        front(1)
        front2(0)
        for tl in range(32):
            if tl + 2 < 32:
                front(tl + 2)
            if tl + 1 < 32:
                front2(tl + 1)
            back(tl)
        K.barrier()

    phases = {"A": phase_A, "B": phase_B, "S": phase_S, "C1": phase_C1, "C2": phase_C2, "D": phase_D, "E": phase_E}
    return nc, kb, st, locals()


def _consts():
    ident = np.eye(128, dtype=np.float32)
    invf = np.zeros((128, 2), np.float32)
    for p in range(128):
        hd = p % 64
        if hd < 16:
            invf[p, 0] = np.float32(500000.0) ** np.float32(-(2 * (hd % 8)) / 16.0)
            invf[p, 1] = -1.0 if hd < 8 else 1.0
    eoh = np.zeros((32, NT_LOC), np.float32)
    for n in range(32):
        eoh[n, n * 256:(n + 1) * 256] = 1.0
    cm = np.zeros((4, 128, 512), np.float32)
    for kt in range(4):
        for kp in range(128):
            kpos = kt * 128 + kp
            q = np.arange(512)
            same = (q // 256) == (kpos // 256)
            cm[kt, kp, :] = np.where(same & (kpos > q), -BIG, 0.0)
    return ident, invf, eoh, cm


def make_in_maps(inp, cores=range(8)):
    f = lambda a: np.ascontiguousarray(np.asarray(a))
    x = f(inp["x"])
    p = f(inp["p"])[0]
    pos = f(inp["positions"]).astype(np.int32)
    ident, invf, eoh, cm = _consts()
    w_in = f(inp["w_in"])[0]
    perm = np.arange(1024)
    for c in range(1024):
        hd = c % 64
        if hd < 8:
            perm[c] = c + 8
        elif hd < 16:
            perm[c] = c - 8
    w_perm = f(w_in[:, 512:1536][:, perm])

    def pair(a):
        a = f(a)[0]
        sh = a.shape
        a = a.reshape(16, 2, 64, *sh[2:])
        a = np.moveaxis(a, 0, 2)
        return f(a.reshape(128, 16, -1).reshape(128, -1))

    ldt = f(inp["ssm_log_dt"])[0]
    ldt_l = f(np.broadcast_to(ldt.reshape(16, 2, 1), (16, 2, 64)).transpose(1, 2, 0).reshape(128, 16))
    cre = f(inp["ssm_c_re"])[0].transpose(0, 2, 1)
    cim = f(inp["ssm_c_im"])[0].transpose(0, 2, 1)
    shared = {
        "ident": ident, "iota": np.ascontiguousarray(np.broadcast_to(np.arange(128, dtype=np.float32), (128, 128))), "invf": invf, "koh": eoh, "cmask": cm,
        "g_mix": f(inp["g_mix"]), "w_in": w_in, "w_perm": w_perm,
        "s5_ldt": ldt_l, "s5_are": pair(inp["ssm_a_re"]), "s5_aim": pair(inp["ssm_a_im"]),
        "s5_bre": pair(inp["ssm_b_re"]), "s5_bim": pair(inp["ssm_b_im"]),
        "s5_cre": pair(cre[None]), "s5_cim": pair(cim[None]),
        "ssm_d": f(inp["ssm_d"]), "w_glu": f(inp["ssm_w_glu"])[0],
        "w_ps": f(inp["w_proj_ssm"])[0], "w_pa": f(inp["w_proj_att"])[0], "w_out": f(inp["w_out"])[0],
        "g_ffn": f(inp["g_ffn"]), "w_q": f(inp["peer_w_q"])[0],
        "keys": f(np.stack([f(inp["peer_keys1"])[0], f(inp["peer_keys2"])[0]], axis=1).reshape(16, 128, 128)),
        "peer_u": f(inp["peer_u"])[0], "peer_v": f(inp["peer_v"])[0],
        "g_ple": f(inp["g_ple"]), "w_pg": f(inp["ple_w_gate"])[0], "w_pp": f(inp["ple_w_proj"])[0],
        "g_fin": f(inp["g_final"]).reshape(1, 1024),
    }
    maps = []
    for c in cores:
        b, half = c // 2, c % 2
        xc = np.zeros((NT_LOC, 1024), np.float32)
        posc = np.zeros((1, NT_LOC), np.int32)
        if half == 1:
            xc[:] = x[b]
            posc[0] = pos[b]
        else:
            xc[NT_OWN:] = x[b, :NT_OWN]
            posc[0, NT_OWN:] = pos[b, :NT_OWN]
        valid = np.zeros((16, 32), np.float32)
        own = np.zeros((16, 32), np.float32)
        for qb in range(16, 32):
            for n in range(32):
                if n < qb and (half == 1 or n >= 16):
                    valid[qb - 16, n] = 1.0
            own[qb - 16, qb] = 1.0
        m = dict(shared)
        m.update({"xc": xc, "pc": f(p[b, half * NT_OWN:(half + 1) * NT_OWN]), "posc": posc,
                  "validc": valid.reshape(1, 512), "ownc": own.reshape(1, 512)})
        maps.append(m)
    return maps


_CACHE = {}


def kernel(**inputs):
    if "prog" not in _CACHE:
        nc, kb, st, L = build_program()
        for ph in ("A", "S", "B", "C1", "C2", "D", "E"):
            L["phases"][ph]()
        kb.S.emit()
        st.close()
        _CACHE["prog"] = nc
    nc = _CACHE["prog"]
    maps = make_in_maps(inputs, range(8))
    res = run_bass_kernel_spmd(nc, maps, core_ids=list(range(8)))
    out = np.zeros((4, 8192, 1024), np.float32)
    for c in range(8):
        b, half = c // 2, c % 2
        out[b, half * NT_OWN:(half + 1) * NT_OWN] = np.asarray(res.results[c]["out"])
    return out
```

```python
import numpy as np
import concourse.bass as bass
import concourse.mybir as mybir
from concourse.bass_utils import run_bass_kernel_spmd

F32 = mybir.dt.float32
BF16 = mybir.dt.bfloat16
I32 = mybir.dt.int32
ALU = mybir.AluOpType
AF = mybir.ActivationFunctionType
AX = mybir.AxisListType


class Tok:
    __slots__ = ("w", "r", "name")

    def __init__(self, name=""):
        self.w = None
        self.r = {}
        self.name = name


class Sched:
    ENGS = ("pe", "act", "dve", "pool", "sp")

    def __init__(self, nc):
        self.nc = nc
        self.ops = {e: [] for e in self.ENGS}
        self.dma_cnt = {}
        self.dma_keys = []

    @staticmethod
    def _evkey(ev):
        return (ev[0], ev[1])

    def _collect(self, reads, writes):
        deps = {}

        def add(ev):
            if ev is None:
                return
            k = self._evkey(ev)
            if k not in deps or deps[k][2] < ev[2]:
                deps[k] = ev

        for t in reads:
            add(t.w)
        for t in writes:
            add(t.w)
            for ev in t.r.values():
                add(ev)
        return deps

    def _commit(self, ev, reads, writes):
        for t in reads:
            k = self._evkey(ev)
            t.r[k] = ev
        for t in writes:
            t.w = ev
            t.r = {}

    def op(self, eng, fn, reads=(), writes=()):
        deps = self._collect(reads, writes)
        idx = len(self.ops[eng])
        ev = ("e", eng, idx)
        if eng == "pe":
            deps.pop(("e", "pe"), None)
        self.ops[eng].append(dict(fn=fn, deps=list(deps.values()), dma=None, signal=False))
        self._commit(ev, reads, writes)
        return ev

    def dma(self, q, key, out, in_, reads=(), writes=()):
        deps = self._collect(reads, writes)
        if key not in self.dma_cnt:
            self.dma_cnt[key] = 0
            self.dma_keys.append(key)
        n = self.dma_cnt[key]
        if n > 0:
            k = ("d", key)
            deps[k] = ("d", key, n)
        self.dma_cnt[key] = n + 1
        ev = ("d", key, n + 1)
        self.ops[q].append(dict(fn=lambda e, o=out, i=in_: e.dma_start(out=o, in_=i),
                                deps=list(deps.values()), dma=key, signal=False))
        self._commit(ev, reads, writes)
        return ev

    def emit(self, final_keys=()):
        nc = self.nc
        ops = self.ops
        for e in self.ENGS:
            for o in ops[e]:
                for d in o["deps"]:
                    if d[0] == "e":
                        ops[d[1]][d[2]]["signal"] = True
        for e in self.ENGS:
            last = None
            for o in ops[e]:
                if "barrier" in o:
                    if last is not None and e != "sp":
                        last["signal"] = True
                else:
                    last = o
        sigval = {}
        for e in self.ENGS:
            c = 0
            vals = []
            for o in ops[e]:
                if o["signal"]:
                    c += 1
                vals.append(c)
            sigval[e] = vals
        barvals = {}
        for e in self.ENGS:
            for i, o in enumerate(ops[e]):
                if "barrier" in o:
                    barvals[(e, o["barrier"])] = sigval[e][i]
        from contextlib import ExitStack
        with ExitStack() as st:
            esem = {e: st.enter_context(nc.semaphore("s_" + e)) for e in self.ENGS if e != "sp"}
            dsem = {k: st.enter_context(nc.semaphore("d_%d" % i)) for i, k in enumerate(self.dma_keys)}
            bsem = st.enter_context(nc.semaphore("s_bar"))
            block = st.enter_context(nc.Block())

            def run(ename, eng):
                waited = {}
                for o in ops[ename]:
                    if "barrier" in o:
                        k = o["barrier"]
                        if ename == "sp":
                            for key, cnt in o["dcnt"].items():
                                if cnt > 0 and waited.get(("d", key), 0) < 16 * cnt:
                                    eng.wait_ge(dsem[key], 16 * cnt)
                            for e2 in esem:
                                v = barvals[(e2, k)]
                                if v > 0:
                                    eng.wait_ge(esem[e2], v)
                            eng.sem_inc(bsem, 1)
                        else:
                            eng.wait_ge(bsem, k)
                        for key, cnt in o["dcnt"].items():
                            waited[("d", key)] = max(waited.get(("d", key), 0), 16 * cnt)
                        for e2 in esem:
                            waited[("e", e2)] = max(waited.get(("e", e2), 0), barvals[(e2, k)])
                        continue
                    for d in sorted(o["deps"]):
                        if d[0] == "e":
                            sem = esem[d[1]]
                            val = sigval[d[1]][d[2]]
                        else:
                            sem = dsem[d[1]]
                            val = 16 * d[2]
                        wk = (d[0], d[1])
                        if waited.get(wk, 0) >= val:
                            continue
                        waited[wk] = val
                        eng.wait_ge(sem, val)
                    ins = o["fn"](eng)
                    if o["dma"] is not None:
                        ins.then_inc(dsem[o["dma"]], 16)
                    elif o["signal"]:
                        ins.then_inc(esem[ename], 1)
                if ename == "sp":
                    for k in self.dma_keys:
                        eng.wait_ge(dsem[k], 16 * self.dma_cnt[k])

            @block.sync
            def _(e):
                run("sp", e)

            @block.tensor
            def _(e):
                run("pe", e)

            @block.scalar
            def _(e):
                run("act", e)

            @block.vector
            def _(e):
                run("dve", e)

            @block.gpsimd
            def _(e):
                run("pool", e)


NT_OWN = 4096
NT_LOC = 8192
PI = float(np.pi)
BIG = 30000.0


class Buf:
    __slots__ = ("ap", "t")

    def __init__(self, ap, name=""):
        self.ap = ap
        self.t = Tok(name)

    def __getitem__(self, k):
        return self.ap[k]


class KB:
    def __init__(self, nc):
        self.nc = nc
        self.S = Sched(nc)
        self.big = nc.alloc_sbuf_tensor("bigsb", [128, 53000], F32)
        self.off = 0
        self.persist = 0
        self.nbar = 0

    def sb(self, shape, dt, name=""):
        n = int(np.prod(shape[1:]))
        esz = 4 if dt in (F32, I32, mybir.dt.uint32) else 2
        nw = (n * esz + 63) // 64 * 16
        assert self.off + nw <= 53000, ("sbuf overflow", name, self.off, nw)
        ap = self.big[:, self.off:self.off + nw]
        self.off += nw
        if dt != F32:
            ap = ap.bitcast(dt)
        ap = ap[:, 0:n]
        if len(shape) == 3:
            ap = ap.rearrange("p (a b) -> p a b", a=shape[1])
        elif len(shape) == 4:
            ap = ap.rearrange("p (a b c) -> p a b c", a=shape[1], b=shape[2])
        elif len(shape) == 5:
            ap = ap.rearrange("p (a b c d) -> p a b c d", a=shape[1], b=shape[2], c=shape[3])
        if shape[0] != 128:
            ap = ap[0:shape[0]]
        return Buf(ap, name)

    def phase_reset(self):
        self.off = self.persist

    def op(self, eng, fn, r=(), w=()):
        return self.S.op(eng, fn, [b.t for b in r], [b.t for b in w])

    def dma(self, key, out, in_, r=(), w=(), q="sp"):
        return self.S.dma(q, key, out, in_, [b.t for b in r], [b.t for b in w])

    def mm(self, pbuf, out, lhsT, rhs, start, stop, r):
        self.op("pe", lambda e: e.matmul(out, lhsT=lhsT, rhs=rhs, start=start, stop=stop), r, [pbuf])

    def tr(self, pbuf, out, in_, ident, r):
        self.op("pe", lambda e: e.transpose(out=out, in_=in_, identity=ident), r, [pbuf])

    def tt(self, eng, out, a, b, op, r, w):
        self.op(eng, lambda e: e.tensor_tensor(out=out, in0=a, in1=b, op=op), r, w)

    def ts(self, eng, out, a, s1, s2, op0, op1, r, w):
        if s2 is None:
            self.op(eng, lambda e: e.tensor_scalar(out=out, in0=a, scalar1=s1, scalar2=None, op0=op0), r, w)
        else:
            self.op(eng, lambda e: e.tensor_scalar(out=out, in0=a, scalar1=s1, scalar2=s2, op0=op0, op1=op1), r, w)

    def stt(self, eng, out, a, s, b, op0, op1, r, w):
        self.op(eng, lambda e: e.scalar_tensor_tensor(out=out, in0=a, scalar=s, in1=b, op0=op0, op1=op1), r, w)

    def cp(self, eng, out, a, r, w):
        if eng == "act":
            self.op("act", lambda e: e.activation(out=out, in_=a, func=AF.Copy), r, w)
        else:
            self.op(eng, lambda e: e.tensor_copy(out=out, in_=a), r, w)

    def act(self, out, a, func, r, w, bias=None, scale=None, accum=None):
        kw = {}
        if bias is not None:
            kw["bias"] = bias
        if scale is not None:
            kw["scale"] = scale
        if accum is not None:
            kw["accum_out"] = accum
        self.op("act", lambda e: e.activation(out=out, in_=a, func=func, **kw), r, w)

    def memset(self, eng, out, val, w):
        self.op(eng, lambda e: e.memset(out, val), (), w)

    def barrier(self):
        S = self.S
        self.nbar += 1
        k = self.nbar
        for e in S.ENGS:
            S.ops[e].append(dict(barrier=k, fn=None, deps=[], dma=None, signal=False,
                                 dcnt=dict(S.dma_cnt)))


def build_program(stop_after=None, debug=()):
    nc = bass.Bass("TRN2", target_bir_lowering=False)
    kb = KB(nc)
    K = kb

    def din(name, shape, dt=F32):
        return nc.dram_tensor(name, list(shape), dt, kind="ExternalInput").ap()

    def dscr(name, shape, dt):
        kind = "ExternalOutput" if name in debug else "Internal"
        return nc.dram_tensor(name, list(shape), dt, kind=kind).ap()

    xc = din("xc", [NT_LOC, 1024])
    pc = din("pc", [NT_OWN, 256])
    posc = din("posc", [1, NT_LOC], I32)
    validc = din("validc", [1, 512])
    ownc = din("ownc", [1, 512])
    ident_d = din("ident", [128, 128])
    iota_d = din("iota", [128, 128])
    invf_d = din("invf", [128, 2])
    koh_d = din("koh", [32, NT_LOC])
    cm_d = din("cmask", [4, 128, 512])
    g_mix = din("g_mix", [1, 1024])
    w_in = din("w_in", [1024, 4096])
    w_perm = din("w_perm", [1024, 1024])
    s5 = {n: din("s5_" + n, shp) for n, shp in [
        ("ldt", [128, 16]), ("are", [128, 16]), ("aim", [128, 16]),
        ("bre", [128, 256]), ("bim", [128, 256]), ("cre", [128, 256]), ("cim", [128, 256])]}
    ssm_d = din("ssm_d", [1, 512])
    w_glu = din("w_glu", [512, 512])
    w_ps = din("w_ps", [512, 1024])
    w_pa = din("w_pa", [512, 1024])
    w_out = din("w_out", [1024, 1024])
    g_ffn = din("g_ffn", [1, 1024])
    w_q = din("w_q", [1024, 2048])
    keys = din("keys", [16, 128, 128])
    peer_u = din("peer_u", [16384, 1024])
    peer_v = din("peer_v", [16384, 1024])
    g_ple = din("g_ple", [1, 1024])
    w_pg = din("w_pg", [1024, 1024])
    w_pp = din("w_pp", [256, 1024])
    g_fin = din("g_fin", [1, 1024])
    out = nc.dram_tensor("out", [NT_OWN, 1024], F32, kind="ExternalOutput").ap()

    qT_s = dscr("qT_s", [4, 128, NT_OWN], BF16)
    kT_s = dscr("kT_s", [4, 128, NT_LOC], BF16)
    v_s = dscr("v_s", [NT_LOC, 8 * 65], BF16)
    gT_s = dscr("gT_s", [16, 128, NT_OWN], BF16)
    ssmT_s = dscr("ssmT_s", [4, 128, NT_OWN], BF16)
    attT_s = dscr("attT_s", [4, 128, NT_OWN], BF16)
    x1_s = dscr("x1_s", [NT_OWN, 1024], F32)
    h2T_s = dscr("h2T_s", [8, 128, NT_OWN], BF16)
    sc_s = dscr("sc_s", [NT_OWN, 16 * 128], F32)
    G_s = dscr("G_s", [32, 128, 128, 128], BF16)
    x2_s = dscr("x2_s", [NT_OWN, 1024], F32)
    UT5_s = dscr("UT5_s", [8, 128, 32 * 128], BF16)
    UT_s = dscr("UT_s", [128, 128, 1024], BF16)

    from contextlib import ExitStack
    st = ExitStack()
    PS = []
    for i in range(8):
        t = st.enter_context(nc.psum_tensor("ps%d" % i, [128, 512], F32))
        PS.append(Buf(t[:], "ps%d" % i))
    psi = [0]

    def nps():
        b = PS[psi[0] % 8]
        psi[0] += 1
        return b

    idf = K.sb([128, 128], F32, "idf")
    idb = K.sb([128, 128], BF16, "idb")
    K.dma("c0", idf.ap, ident_d, w=[idf])
    K.cp("dve", idb.ap, idf.ap, [idf], [idb])
    ksum = K.sb([128, 4, 32], F32, "ksum")
    K.persist = K.off

    ut5tok = [Buf(None, "ut5_%d" % i) for i in range(8)]

    def rmsnorm_tile(xt, gt, hb, sq, ss, rs):
        K.act(sq.ap, xt.ap, AF.Square, [xt], [sq, ss], accum=ss.ap)
        K.act(rs.ap, ss.ap, AF.Sqrt, [ss], [rs], scale=1.0 / 1024, bias=eps_b.ap)
        K.op("dve", lambda e: e.reciprocal(out=rs.ap, in_=rs.ap), [rs], [rs])
        K.stt("dve", hb.ap, xt.ap, rs.ap[:, 0:1], gt.ap, ALU.mult, ALU.mult, [xt, rs, gt], [hb])

    def load_w_bf(dst, src_ap, rows_kc, ncols, stage, key):
        for kc in range(rows_kc):
            K.dma(key, stage.ap[:, 0:ncols], src_ap[kc * 128:(kc + 1) * 128, :], w=[stage])
            K.cp("dve" if kc % 2 == 0 else "act", dst.ap[:, kc, :], stage.ap[:, 0:ncols], [stage], [dst])

    eps_b = K.sb([128, 1], F32, "eps")
    K.memset("dve", eps_b.ap, 1e-6, [eps_b])
    K.persist = K.off

    def phase_A():
        K.phase_reset()
        win = K.sb([128, 8, 3584], BF16, "win")
        wu = K.sb([128, 8, 512], BF16, "wuA")
        Ustk = K.sb([128, 32, 8, 16], BF16, "UstkA")
        UTo = K.sb([128, 32, 128], BF16, "UToA")
        wpm = K.sb([128, 8, 1024], BF16, "wpm")
        stageA = K.sb([128, 1792], F32, "stageA")
        stageB = K.sb([128, 1792], F32, "stageB")
        stage = stageA
        gt = K.sb([128, 1024], F32, "gmix")
        invf = K.sb([128, 2], F32, "invf")
        xts = [K.sb([128, 1024], F32, "xt%d" % i) for i in range(2)]
        sq = K.sb([128, 1024], BF16, "sq")
        ss = K.sb([128, 1], F32, "ss")
        rs = K.sb([128, 1], F32, "rs")
        hbs = [K.sb([128, 1024], BF16, "hb%d" % i) for i in range(2)]
        hTs = [K.sb([128, 8, 1024], BF16, "hT%d" % i) for i in range(2)]
        posi = K.sb([128, 1024], I32, "posi")
        ang = K.sb([128, 1024], F32, "ang")
        tmpa = K.sb([128, 1024], F32, "tmpa")
        tmpi = K.sb([128, 1024], I32, "tmpi")
        cosTs = [K.sb([128, 1024], F32, "cosT%d" % i) for i in range(2)]
        sinTs = [K.sb([128, 1024], F32, "sinT%d" % i) for i in range(2)]
        t1s = [K.sb([128, 512], F32, "t1_%d" % i) for i in range(2)]
        t2s = [K.sb([128, 512], F32, "t2_%d" % i) for i in range(2)]
        obf = [K.sb([128, 512], BF16, "obf%d" % i) for i in range(2)]
        vts = [K.sb([128, 8, 65], BF16, "vt%d" % i) for i in range(2)]
        for v in vts:
            K.memset("pool", v.ap, 1.0, [v])
        K.dma("c0", gt.ap, g_mix.partition_broadcast(128), w=[gt])
        K.dma("c0", invf.ap, invf_d, w=[invf])
        n_st = [0]

        def stream_w(dst_ap, src_ap, ncols):
            i = n_st[0]
            n_st[0] += 1
            stg_ = (stageA, stageB)[i % 2]
            K.dma("wst%d" % (i % 2), stg_.ap[:, 0:ncols], src_ap, w=[stg_])
            K.cp("dve" if i % 2 == 0 else "act", dst_ap, stg_.ap[:, 0:ncols], [stg_], [win])

        for kc in range(8):
            stream_w(win.ap[:, kc, 0:1792], w_in[kc * 128:(kc + 1) * 128, 512:2304], 1792)
            stream_w(win.ap[:, kc, 1792:3584], w_in[kc * 128:(kc + 1) * 128, 2304:4096], 1792)
        for kc in range(8):
            i = n_st[0]
            n_st[0] += 1
            stg_ = (stageA, stageB)[i % 2]
            K.dma("wst%d" % (i % 2), stg_.ap[:, 0:1024], w_perm[kc * 128:(kc + 1) * 128, :], w=[stg_])
            K.cp("dve" if i % 2 == 0 else "act", wpm.ap[:, kc, :], stg_.ap[:, 0:1024], [stg_], [wpm])
        for kc in range(8):
            i = n_st[0]
            n_st[0] += 1
            stg_ = (stageA, stageB)[i % 2]
            K.dma("wst%d" % (i % 2), stg_.ap[:, 0:512], w_in[kc * 128:(kc + 1) * 128, 0:512], w=[stg_])
            K.cp("dve" if i % 2 == 0 else "act", wu.ap[:, kc, :], stg_.ap[:, 0:512], [stg_], [wu])

        def sincos(dst, phase):
            K.ts("dve", tmpa.ap, ang.ap, phase, 1.0 / (2 * PI), ALU.add, ALU.mult, [ang], [tmpa])
            K.cp("dve", tmpi.ap, tmpa.ap, [tmpa], [tmpi])
            K.cp("dve", tmpa.ap, tmpi.ap, [tmpi], [tmpa])
            K.stt("dve", tmpa.ap, tmpa.ap, -2 * PI, ang.ap, ALU.mult, ALU.add, [tmpa, ang], [tmpa])
            K.ts("dve", tmpa.ap, tmpa.ap, phase, None, ALU.add, None, [tmpa], [tmpa])
            K.ts("dve", dst.ap, tmpa.ap, PI, -2 * PI, ALU.is_gt, ALU.mult, [tmpa], [dst])
            K.tt("dve", tmpa.ap, tmpa.ap, dst.ap, ALU.add, [tmpa, dst], [tmpa])
            K.ts("dve", dst.ap, tmpa.ap, -PI, 2 * PI, ALU.is_lt, ALU.mult, [tmpa], [dst])
            K.tt("dve", tmpa.ap, tmpa.ap, dst.ap, ALU.add, [tmpa, dst], [tmpa])
            K.act(dst.ap, tmpa.ap, AF.Sin, [tmpa], [dst])

        xic = [0]

        def prep(blk):
            tb = blk * 1024
            hT = hTs[blk % 2]
            cosT, sinT = cosTs[blk % 2], sinTs[blk % 2]
            xi = xic[0]
            K.dma("pos", posi.ap, posc[:, tb:tb + 1024].partition_broadcast(128), w=[posi])
            K.cp("dve", ang.ap, posi.ap, [posi], [ang])
            K.ts("dve", ang.ap, ang.ap, invf.ap[:, 0:1], None, ALU.mult, None, [ang, invf], [ang])
            sincos(cosT, PI / 2)
            sincos(sinT, 0.0)
            K.ts("dve", sinT.ap, sinT.ap, invf.ap[:, 1:2], None, ALU.mult, None, [sinT, invf], [sinT])
            for ti in range(8):
                xt = xts[xi % 2]
                hb = hbs[xi % 2]
                xi += 1
                K.dma("x%d" % (xi % 2), xt.ap, xc[tb + ti * 128: tb + (ti + 1) * 128, :], w=[xt])
                rmsnorm_tile(xt, gt, hb, sq, ss, rs)
                p = nps()
                pv = p.ap.bitcast(BF16).rearrange("p (a b) -> p a b", a=8)
                for kc in range(8):
                    K.tr(p, pv[:, kc, :], hb.ap[:, kc * 128:(kc + 1) * 128], idb.ap, [hb, idb])
                K.cp("act", hT.ap[:, :, ti * 128:(ti + 1) * 128], pv, [p], [hT])
            xic[0] = xi

        oic = [0]

        def compute(blk):
            own = blk >= 4
            tb = blk * 1024
            hT = hTs[blk % 2]
            cosT, sinT = cosTs[blk % 2], sinTs[blk % 2]
            oi = oic[0]
            for j in range(8):
                p = nps()
                for kc in range(8):
                    K.mm(p, p.ap, hT.ap[:, kc, j:1024:8], wu.ap[:, kc, :], kc == 0, kc == 7, [hT, wu])
                K.cp("act" if j % 2 else "dve", Ustk.ap[:, :, j, :], p.ap.rearrange("p (g c) -> p g c", g=32), [p], [Ustk])
            for g0 in range(0, 32, 8):
                p = nps()
                pv = p.ap.bitcast(BF16).rearrange("p (a b) -> p a b", a=8)
                for gi in range(8):
                    K.tr(p, pv[:, gi, :], Ustk.ap[:, g0 + gi, :, :].rearrange("p a b -> p (a b)"), idb.ap, [Ustk, idb])
                K.cp("act", UTo.ap[:, g0:g0 + 8, :], pv, [p], [UTo])
            K.dma("utA", UT5_s[blk], UTo.ap.rearrange("p a b -> p (a b)"), r=[UTo], w=[ut5tok[blk]])
            for which in (["q", "k"] if own else ["k"]):
                cbase = 0 if which == "q" else 512
                for c in range(4):
                    for half in range(2):
                        pa = nps()
                        pb = nps()
                        for kc in range(8):
                            K.mm(pa, pa.ap, win.ap[:, kc, cbase + c * 128: cbase + (c + 1) * 128],
                                 hT.ap[:, kc, half * 512:(half + 1) * 512], kc == 0, kc == 7, [win, hT])
                        for kc in range(8):
                            K.mm(pb, pb.ap, wpm.ap[:, kc, cbase + c * 128: cbase + (c + 1) * 128],
                                 hT.ap[:, kc, half * 512:(half + 1) * 512], kc == 0, kc == 7, [wpm, hT])
                        t1, t2 = t1s[oi % 2], t2s[oi % 2]
                        K.tt("dve", t1.ap, pa.ap, cosT.ap[:, half * 512:(half + 1) * 512], ALU.mult, [pa, cosT], [t1])
                        K.tt("dve", t2.ap, pb.ap, sinT.ap[:, half * 512:(half + 1) * 512], ALU.mult, [pb, sinT], [t2])
                        K.tt("dve", t1.ap, t1.ap, t2.ap, ALU.add, [t1, t2], [t1])
                        ob = obf[oi % 2]
                        oi += 1
                        K.cp("act", ob.ap, t1.ap, [t1], [ob])
                        t0 = tb + half * 512
                        if which == "q":
                            K.dma("oq%d" % (oi % 2), qT_s[c, :, t0 - NT_OWN: t0 - NT_OWN + 512], ob.ap, r=[ob])
                        else:
                            K.dma("oq%d" % (oi % 2), kT_s[c, :, t0: t0 + 512], ob.ap, r=[ob])
                            K.op("dve", lambda e, c=c, b0=t0 // 256, t1=t1: e.tensor_reduce(
                                out=ksum.ap[:, c, b0:b0 + 2], in_=t1.ap.rearrange("p (a b) -> p a b", a=2),
                                axis=AX.X, op=ALU.add), [t1], [ksum])
            for ti in range(8):
                p = nps()
                for kc in range(8):
                    K.mm(p, p.ap, hT.ap[:, kc, ti * 128:(ti + 1) * 128], win.ap[:, kc, 1024:1536],
                         kc == 0, kc == 7, [hT, win])
                vt = vts[ti % 2]
                K.cp("act", vt.ap[:, :, 0:64], p.ap.rearrange("p (h d) -> p h d", h=8), [p], [vt])
                K.dma("ov%d" % (ti % 2), v_s[tb + ti * 128: tb + (ti + 1) * 128, :].rearrange("p (h d) -> p h d", h=8),
                      vt.ap, r=[vt])
            if own:
                for c in range(16):
                    for half in range(2):
                        p = nps()
                        for kc in range(8):
                            K.mm(p, p.ap, win.ap[:, kc, 1536 + c * 128: 1536 + (c + 1) * 128],
                                 hT.ap[:, kc, half * 512:(half + 1) * 512], kc == 0, kc == 7, [win, hT])
                        ob = obf[oi % 2]
                        oi += 1
                        K.act(ob.ap, p.ap, AF.Sigmoid, [p], [ob])
                        t0 = tb + half * 512 - NT_OWN
                        K.dma("oq%d" % (oi % 2), gT_s[c, :, t0:t0 + 512], ob.ap, r=[ob])
            oic[0] = oi

        prep(0)
        for blk in range(8):
            if blk + 1 < 8:
                prep(blk + 1)
            compute(blk)
        K.barrier()

    def phase_S():
        K.phase_reset()
        sm = lambda name, n=16: K.sb([128, n], F32, name)
        wglu = K.sb([128, 4, 512], BF16, "wglu")
        WS = K.sb([128, 16, 2, 2, 128], BF16, "WS")
        WY1 = K.sb([128, 16, 2, 2, 128], BF16, "WY1")
        WY2 = K.sb([128, 32, 128], BF16, "WY2")
        Ct = K.sb([128, 16, 128], F32, "Ct")
        St = K.sb([128, 16, 128], F32, "St")
        r8 = sm("r8")
        Dre = sm("Dre")
        Dim = sm("Dim")
        car_re = sm("car_re")
        car_im = sm("car_im")
        ta, tb_, tc_ = sm("ta"), sm("tb"), sm("tc")
        mark = K.off
        stage = K.sb([128, 512], F32, "stageS")
        ldt, are, aim = sm("ldt"), sm("are"), sm("aim")
        bre = K.sb([128, 16, 16], F32, "bre")
        bim = K.sb([128, 16, 16], F32, "bim")
        cre = K.sb([128, 16, 16], F32, "cre")
        cim = K.sb([128, 16, 16], F32, "cim")
        ncim = K.sb([128, 16, 16], F32, "ncim")
        for t_, nm in ((ldt, "ldt"), (are, "are"), (aim, "aim")):
            K.dma("c0", t_.ap, s5[nm], w=[t_])
        for t_, nm in ((bre, "bre"), (bim, "bim"), (cre, "cre"), (cim, "cim")):
            K.dma("c0", t_.ap.rearrange("p a b -> p (a b)"), s5[nm], w=[t_])
        for kc in range(4):
            K.dma("wst", stage.ap, w_glu[kc * 128:(kc + 1) * 128, :], w=[stage])
            K.cp("dve", wglu.ap[:, kc, :], stage.ap, [stage], [wglu])
        dt_, xr, th, mag, cs, sn = sm("dt"), sm("xr"), sm("th"), sm("mag"), sm("cs"), sm("sn")
        abr, abi, den, nr, fre, fim = sm("abr"), sm("abi"), sm("den"), sm("nr"), sm("fre"), sm("fim")
        ti_ = K.sb([128, 16], I32, "ti")
        V_ = "dve"
        K.act(dt_.ap, ldt.ap, AF.Exp, [ldt], [dt_])
        K.tt(V_, xr.ap, dt_.ap, are.ap, ALU.mult, [dt_, are], [xr])
        K.tt(V_, th.ap, dt_.ap, aim.ap, ALU.mult, [dt_, aim], [th])
        K.act(mag.ap, xr.ap, AF.Exp, [xr], [mag])
        K.act(r8.ap, xr.ap, AF.Exp, [xr], [r8], scale=8.0)

        def sin_small(dst, src, phase):
            K.ts(V_, ta.ap, src.ap, phase, 1.0 / (2 * PI), ALU.add, ALU.mult, [src], [ta])
            K.cp(V_, ti_.ap, ta.ap, [ta], [ti_])
            K.cp(V_, ta.ap, ti_.ap, [ti_], [ta])
            K.stt(V_, ta.ap, ta.ap, -2 * PI, src.ap, ALU.mult, ALU.add, [ta, src], [ta])
            K.ts(V_, ta.ap, ta.ap, phase, None, ALU.add, None, [ta], [ta])
            K.ts(V_, tb_.ap, ta.ap, PI, -2 * PI, ALU.is_gt, ALU.mult, [ta], [tb_])
            K.tt(V_, ta.ap, ta.ap, tb_.ap, ALU.add, [ta, tb_], [ta])
            K.ts(V_, tb_.ap, ta.ap, -PI, 2 * PI, ALU.is_lt, ALU.mult, [ta], [tb_])
            K.tt(V_, ta.ap, ta.ap, tb_.ap, ALU.add, [ta, tb_], [ta])
            K.act(dst.ap, ta.ap, AF.Sin, [ta], [dst])

        sin_small(cs, th, PI / 2)
        sin_small(sn, th, 0.0)
        K.tt(V_, abr.ap, mag.ap, cs.ap, ALU.mult, [mag, cs], [abr])
        K.tt(V_, abi.ap, mag.ap, sn.ap, ALU.mult, [mag, sn], [abi])
        K.tt(V_, den.ap, are.ap, are.ap, ALU.mult, [are], [den])
        K.tt(V_, ta.ap, aim.ap, aim.ap, ALU.mult, [aim], [ta])
        K.tt(V_, den.ap, den.ap, ta.ap, ALU.add, [den, ta], [den])
        K.op(V_, lambda e: e.reciprocal(out=den.ap, in_=den.ap), [den], [den])
        K.ts(V_, nr.ap, abr.ap, -1.0, None, ALU.add, None, [abr], [nr])
        K.tt(V_, ta.ap, nr.ap, are.ap, ALU.mult, [nr, are], [ta])
        K.tt(V_, tb_.ap, abi.ap, aim.ap, ALU.mult, [abi, aim], [tb_])
        K.tt(V_, ta.ap, ta.ap, tb_.ap, ALU.add, [ta, tb_], [ta])
        K.tt(V_, fre.ap, ta.ap, den.ap, ALU.mult, [ta, den], [fre])
        K.tt(V_, ta.ap, abi.ap, are.ap, ALU.mult, [abi, are], [ta])
        K.tt(V_, tb_.ap, nr.ap, aim.ap, ALU.mult, [nr, aim], [tb_])
        K.tt(V_, ta.ap, ta.ap, tb_.ap, ALU.subtract, [ta, tb_], [ta])
        K.tt(V_, fim.ap, ta.ap, den.ap, ALU.mult, [ta, den], [fim])
        pwf_re = K.sb([128, 16, 9], F32, "pwf_re")
        pwf_im = K.sb([128, 16, 9], F32, "pwf_im")
        pwr_re = K.sb([128, 16, 8], F32, "pwr_re")
        pwr_im = K.sb([128, 16, 8], F32, "pwr_im")
        K.memset(V_, pwf_re.ap[:, :, 0], 1.0, [pwf_re])
        K.memset(V_, pwf_im.ap[:, :, 0], 0.0, [pwf_im])
        for d in range(8):
            K.tt(V_, ta.ap, pwf_re.ap[:, :, d], abr.ap, ALU.mult, [pwf_re, abr], [ta])
            K.tt(V_, tb_.ap, pwf_im.ap[:, :, d], abi.ap, ALU.mult, [pwf_im, abi], [tb_])
            K.tt(V_, pwf_re.ap[:, :, d + 1], ta.ap, tb_.ap, ALU.subtract, [ta, tb_], [pwf_re])
            K.tt(V_, ta.ap, pwf_re.ap[:, :, d], abi.ap, ALU.mult, [pwf_re, abi], [ta])
            K.tt(V_, tb_.ap, pwf_im.ap[:, :, d], abr.ap, ALU.mult, [pwf_im, abr], [tb_])
            K.tt(V_, pwf_im.ap[:, :, d + 1], ta.ap, tb_.ap, ALU.add, [ta, tb_], [pwf_im])
        for j in range(8):
            K.cp(V_, pwr_re.ap[:, :, j], pwf_re.ap[:, :, 7 - j], [pwf_re], [pwr_re])
            K.cp(V_, pwr_im.ap[:, :, j], pwf_im.ap[:, :, 7 - j], [pwf_im], [pwr_im])
        K.cp(V_, Dre.ap, pwf_re.ap[:, :, 8], [pwf_re], [Dre])
        K.cp(V_, Dim.ap, pwf_im.ap[:, :, 8], [pwf_im], [Dim])
        ur, ui, rr = sm("ur"), sm("ui"), sm("rr")
        K.op(V_, lambda e: e.reciprocal(out=rr.ap, in_=r8.ap), [r8], [rr])
        K.tt(V_, ur.ap, Dre.ap, rr.ap, ALU.mult, [Dre, rr], [ur])
        K.tt(V_, ui.ap, Dim.ap, rr.ap, ALU.mult, [Dim, rr], [ui])
        tm1 = K.sb([128, 16, 64], F32, "tm1")
        tm2 = K.sb([128, 16, 64], F32, "tm2")
        K.memset(V_, Ct.ap[:, :, 0], 1.0, [Ct])
        K.memset(V_, St.ap[:, :, 0], 0.0, [St])
        for k in range(7):
            n = 1 << k
            urb = ur.ap.unsqueeze(2).broadcast_to([128, 16, n])
            uib = ui.ap.unsqueeze(2).broadcast_to([128, 16, n])
            K.tt(V_, tm1.ap[:, :, 0:n], Ct.ap[:, :, 0:n], urb, ALU.mult, [Ct, ur], [tm1])
            K.tt(V_, tm2.ap[:, :, 0:n], St.ap[:, :, 0:n], uib, ALU.mult, [St, ui], [tm2])
            K.tt(V_, Ct.ap[:, :, n:2 * n], tm1.ap[:, :, 0:n], tm2.ap[:, :, 0:n], ALU.subtract, [tm1, tm2], [Ct])
            K.tt(V_, tm1.ap[:, :, 0:n], Ct.ap[:, :, 0:n], uib, ALU.mult, [Ct, ui], [tm1])
            K.tt(V_, tm2.ap[:, :, 0:n], St.ap[:, :, 0:n], urb, ALU.mult, [St, ur], [tm2])
            K.tt(V_, St.ap[:, :, n:2 * n], tm1.ap[:, :, 0:n], tm2.ap[:, :, 0:n], ALU.add, [tm1, tm2], [St])
            K.tt(V_, ta.ap, ur.ap, ur.ap, ALU.mult, [ur], [ta])
            K.tt(V_, tb_.ap, ui.ap, ui.ap, ALU.mult, [ui], [tb_])
            K.tt(V_, tc_.ap, ur.ap, ui.ap, ALU.mult, [ur, ui], [tc_])
            K.tt(V_, ur.ap, ta.ap, tb_.ap, ALU.subtract, [ta, tb_], [ur])
            K.ts(V_, ui.ap, tc_.ap, 2.0, None, ALU.mult, None, [tc_], [ui])
        bbr = K.sb([128, 16, 16], F32, "bbr")
        bbi = K.sb([128, 16, 16], F32, "bbi")
        t3a = K.sb([128, 16, 16], F32, "t3a")
        t3b = K.sb([128, 16, 16], F32, "t3b")
        freb = fre.ap.unsqueeze(2).broadcast_to([128, 16, 16])
        fimb = fim.ap.unsqueeze(2).broadcast_to([128, 16, 16])
        K.tt(V_, t3a.ap, bre.ap, freb, ALU.mult, [bre, fre], [t3a])
        K.tt(V_, t3b.ap, bim.ap, fimb, ALU.mult, [bim, fim], [t3b])
        K.tt(V_, bbr.ap, t3a.ap, t3b.ap, ALU.subtract, [t3a, t3b], [bbr])
        K.tt(V_, t3a.ap, bim.ap, freb, ALU.mult, [bim, fre], [t3a])
        K.tt(V_, t3b.ap, bre.ap, fimb, ALU.mult, [bre, fim], [t3b])
        K.tt(V_, bbi.ap, t3a.ap, t3b.ap, ALU.add, [t3a, t3b], [bbi])
        K.ts(V_, ncim.ap, cim.ap, -1.0, None, ALU.mult, None, [cim], [ncim])
        Fre = K.sb([128, 16, 15, 16], F32, "Fre")
        Fim = K.sb([128, 16, 15, 16], F32, "Fim")
        t4a = K.sb([128, 16, 8, 16], F32, "t4a")
        t4b = K.sb([128, 16, 8, 16], F32, "t4b")
        K.memset("pool", Fre.ap, 0.0, [Fre])
        K.memset("pool", Fim.ap, 0.0, [Fim])
        S4 = [128, 16, 8, 16]
        prb = pwr_re.ap.unsqueeze(3).broadcast_to(S4)
        pib = pwr_im.ap.unsqueeze(3).broadcast_to(S4)
        bbrb = bbr.ap.unsqueeze(2).broadcast_to(S4)
        bbib = bbi.ap.unsqueeze(2).broadcast_to(S4)
        K.tt(V_, t4a.ap, prb, bbrb, ALU.mult, [pwr_re, bbr], [t4a])
        K.tt(V_, t4b.ap, pib, bbib, ALU.mult, [pwr_im, bbi], [t4b])
        K.tt(V_, Fre.ap[:, :, 0:8, :], t4a.ap, t4b.ap, ALU.subtract, [t4a, t4b], [Fre])
        K.tt(V_, t4a.ap, prb, bbib, ALU.mult, [pwr_re, bbi], [t4a])
        K.tt(V_, t4b.ap, pib, bbrb, ALU.mult, [pwr_im, bbr], [t4b])
        K.tt(V_, Fim.ap[:, :, 0:8, :], t4a.ap, t4b.ap, ALU.add, [t4a, t4b], [Fim])
        K.memset("pool", WS.ap, 0.0, [WS])
        K.memset("pool", WY1.ap, 0.0, [WY1])
        for p_ in range(16):
            for ri, Ft in ((0, Fre), (1, Fim)):
                ps = nps()
                K.tr(ps, ps.ap[:, 0:128], Ft.ap[:, p_, 0:8, :].rearrange("p a b -> p (a b)"), idf.ap, [Ft, idf])
                K.cp(V_, WS.ap[:, p_, ri, 0, 0:64], ps.ap[:, 0:64], [ps], [WS])
                K.cp(V_, WS.ap[:, p_, ri, 1, 64:128], ps.ap[:, 64:128], [ps], [WS])
        pfr = pwf_re.ap[:, :, 1:9].unsqueeze(3).broadcast_to(S4)
        pfi = pwf_im.ap[:, :, 1:9].unsqueeze(3).broadcast_to(S4)
        creb = cre.ap.unsqueeze(2).broadcast_to(S4)
        cimb = cim.ap.unsqueeze(2).broadcast_to(S4)
        K.tt(V_, t4a.ap, creb, pfr, ALU.mult, [cre, pwf_re], [t4a])
        K.tt(V_, t4b.ap, cimb, pfi, ALU.mult, [cim, pwf_im], [t4b])
        K.tt(V_, t4a.ap, t4a.ap, t4b.ap, ALU.subtract, [t4a, t4b], [t4a])
        for g2 in range(2):
            K.cp(V_, WY1.ap[g2 * 64:(g2 + 1) * 64, :, 0, g2, :],
                 t4a.ap[g2 * 64:(g2 + 1) * 64].rearrange("p a b c -> p a (b c)"), [t4a], [WY1])
        K.tt(V_, t4a.ap, creb, pfi, ALU.mult, [cre, pwf_im], [t4a])
        K.tt(V_, t4b.ap, cimb, pfr, ALU.mult, [cim, pwf_re], [t4b])
        K.tt(V_, t4a.ap, t4a.ap, t4b.ap, ALU.add, [t4a, t4b], [t4a])
        K.ts(V_, t4a.ap, t4a.ap, -1.0, None, ALU.mult, None, [t4a], [t4a])
        for g2 in range(2):
            K.cp(V_, WY1.ap[g2 * 64:(g2 + 1) * 64, :, 1, g2, :],
                 t4a.ap[g2 * 64:(g2 + 1) * 64].rearrange("p a b c -> p a (b c)"), [t4a], [WY1])
        dB = K.sb([128, 512], F32, "dB")
        dI = K.sb([128, 32, 8, 16], F32, "dI")
        K.dma("c0", dB.ap, ssm_d.partition_broadcast(128), w=[dB])
        K.tt(V_, dI.ap, idf.ap.rearrange("p (a b) -> p a b", a=8).unsqueeze(1).broadcast_to([128, 32, 8, 16]),
             dB.ap.rearrange("p (g c) -> p g c", g=32).unsqueeze(2).broadcast_to([128, 32, 8, 16]), ALU.mult,
             [idf, dB], [dI])
        for g in range(32):
            p_, g2 = g // 2, g % 2
            if g % 4 == 0:
                ps = nps()
            o0 = (g % 4) * 128
            sl = slice(g2 * 64, (g2 + 1) * 64)
            for j in range(8):
                oap = ps.ap[:, o0 + j * 16: o0 + (j + 1) * 16]
                K.mm(ps, oap, Fre.ap[sl, p_, 7 - j:15 - j, :].rearrange("p a b -> p (a b)"), cre.ap[sl, p_, :],
                     True, False, [Fre, cre])
                K.mm(ps, oap, Fim.ap[sl, p_, 7 - j:15 - j, :].rearrange("p a b -> p (a b)"), ncim.ap[sl, p_, :],
                     False, True, [Fim, ncim])
            if g % 4 == 3:
                K.tt(V_, WY2.ap[:, g - 3:g + 1, :], ps.ap.rearrange("p (g k) -> p g k", g=4),
                     dI.ap[:, g - 3:g + 1].rearrange("p g a b -> p g (a b)"), ALU.add, [ps, dI], [WY2])
        K.memset(V_, car_re.ap, 0.0, [car_re])
        K.memset(V_, car_im.ap, 0.0, [car_im])
        K.barrier()
        K.off = mark
        UTs = [K.sb([128, 32, 128], BF16, "UT%d" % i) for i in range(2)]
        Sres = [K.sb([128, 16, 128], F32, "Sre%d" % i) for i in range(2)]
        Sims = [K.sb([128, 16, 128], F32, "Sim%d" % i) for i in range(2)]
        gre = K.sb([128, 16, 128], F32, "gre")
        gim = K.sb([128, 16, 128], F32, "gim")
        u1 = K.sb([128, 16, 128], F32, "u1")
        u2 = K.sb([128, 16, 128], F32, "u2")
        Pre = K.sb([128, 16, 129], BF16, "Pre")
        Pim = K.sb([128, 16, 129], BF16, "Pim")
        ytm = K.sb([128, 8, 512], BF16, "ytm")
        yT = K.sb([128, 4, 1024], BF16, "yT")
        sg = K.sb([128, 512], BF16, "sg")
        sos = [K.sb([128, 4, 512], BF16, "so%d" % i) for i in range(2)]

        def S1(blk):
            UT, Sre, Sim = UTs[blk % 2], Sres[blk % 2], Sims[blk % 2]
            K.dma("ut%d" % (blk % 2), UT.ap.rearrange("p a b -> p (a b)"), UT5_s[blk], r=[ut5tok[blk]], w=[UT])
            for ri, Sx in ((0, Sre), (1, Sim)):
                for p0 in range(0, 16, 4):
                    p = nps()
                    for pi_ in range(4):
                        pp = p0 + pi_
                        K.mm(p, p.ap[:, pi_ * 128:(pi_ + 1) * 128], WS.ap[:, pp, ri, 0, :], UT.ap[:, 2 * pp, :], True, False, [WS, UT])
                        K.mm(p, p.ap[:, pi_ * 128:(pi_ + 1) * 128], WS.ap[:, pp, ri, 1, :], UT.ap[:, 2 * pp + 1, :], False, True, [WS, UT])
                    K.cp("act", Sx.ap[:, p0:p0 + 4, :], p.ap.rearrange("p (a b) -> p a b", a=4), [p], [Sx])

        def SC(blk):
            own = blk >= 4
            Sre, Sim = Sres[blk % 2], Sims[blk % 2]
            if own:
                K.cp(V_, Pre.ap[:, :, 0], car_re.ap, [car_re], [Pre])
                K.cp(V_, Pim.ap[:, :, 0], car_im.ap, [car_im], [Pim])
            K.tt(V_, ta.ap, Dre.ap, car_re.ap, ALU.mult, [Dre, car_re], [ta])
            K.tt(V_, tb_.ap, Dim.ap, car_im.ap, ALU.mult, [Dim, car_im], [tb_])
            K.tt(V_, ta.ap, ta.ap, tb_.ap, ALU.subtract, [ta, tb_], [ta])
            K.tt(V_, Sre.ap[:, :, 0], Sre.ap[:, :, 0], ta.ap, ALU.add, [Sre, ta], [Sre])
            K.tt(V_, ta.ap, Dre.ap, car_im.ap, ALU.mult, [Dre, car_im], [ta])
            K.tt(V_, tb_.ap, Dim.ap, car_re.ap, ALU.mult, [Dim, car_re], [tb_])
            K.tt(V_, ta.ap, ta.ap, tb_.ap, ALU.add, [ta, tb_], [ta])
            K.tt(V_, Sim.ap[:, :, 0], Sim.ap[:, :, 0], ta.ap, ALU.add, [Sim, ta], [Sim])
            K.tt("dve", u1.ap, Ct.ap, Sre.ap, ALU.mult, [Ct, Sre], [u1])
            K.tt("pool", u2.ap, St.ap, Sim.ap, ALU.mult, [St, Sim], [u2])
            K.tt("dve", gre.ap, u1.ap, u2.ap, ALU.add, [u1, u2], [gre])
            K.tt("dve", u1.ap, Ct.ap, Sim.ap, ALU.mult, [Ct, Sim], [u1])
            K.tt("pool", u2.ap, St.ap, Sre.ap, ALU.mult, [St, Sre], [u2])
            K.tt("dve", gim.ap, u1.ap, u2.ap, ALU.subtract, [u1, u2], [gim])
            for pp in range(16):
                rb = r8.ap[:, pp:pp + 1].to_broadcast([128, 128])
                K.op("dve", lambda e, pp=pp, rb=rb, Sre=Sre: e.tensor_tensor_scan(out=Sre.ap[:, pp, :], data0=rb, data1=gre.ap[:, pp, :],
                                                                                 initial=0.0, op0=ALU.mult, op1=ALU.add), [gre, r8], [Sre])
                K.op("dve", lambda e, pp=pp, rb=rb, Sim=Sim: e.tensor_tensor_scan(out=Sim.ap[:, pp, :], data0=rb, data1=gim.ap[:, pp, :],
                                                                                 initial=0.0, op0=ALU.mult, op1=ALU.add), [gim, r8], [Sim])
            K.tt("dve", u1.ap, Ct.ap, Sre.ap, ALU.mult, [Ct, Sre], [u1])
            K.tt("pool", u2.ap, St.ap, Sim.ap, ALU.mult, [St, Sim], [u2])
            K.tt("dve", gre.ap, u1.ap, u2.ap, ALU.subtract, [u1, u2], [gre])
            K.tt("dve", u1.ap, Ct.ap, Sim.ap, ALU.mult, [Ct, Sim], [u1])
            K.tt("pool", u2.ap, St.ap, Sre.ap, ALU.mult, [St, Sre], [u2])
            K.tt("dve", gim.ap, u1.ap, u2.ap, ALU.add, [u1, u2], [gim])
            K.cp(V_, car_re.ap, gre.ap[:, :, 127], [gre], [car_re])
            K.cp(V_, car_im.ap, gim.ap[:, :, 127], [gim], [car_im])
            if own:
                K.cp("act", Pre.ap[:, :, 1:129], gre.ap, [gre], [Pre])
                K.cp("act", Pim.ap[:, :, 1:129], gim.ap, [gim], [Pim])

        def SY(blk):
            tb = blk * 1024
            UT = UTs[blk % 2]
            for g0 in range(0, 32, 4):
                p = nps()
                for gi in range(4):
                    g = g0 + gi
                    pp, g2 = g // 2, g % 2
                    oap = p.ap[:, gi * 128:(gi + 1) * 128]
                    K.mm(p, oap, Pre.ap[:, pp, 0:128], WY1.ap[:, pp, 0, g2, :], True, False, [Pre, WY1])
                    K.mm(p, oap, Pim.ap[:, pp, 0:128], WY1.ap[:, pp, 1, g2, :], False, False, [Pim, WY1])
                    K.mm(p, oap, UT.ap[:, g, :], WY2.ap[:, g, :], False, True, [UT, WY2])
                K.act(ytm.ap.rearrange("p j (g c) -> p g j c", g=32)[:, g0:g0 + 4],
                      p.ap.rearrange("p (g j c) -> p g j c", g=4, j=8), AF.Gelu_apprx_tanh, [p], [ytm])
            for j in range(8):
                if j % 2 == 0:
                    p = nps()
                    pv = p.ap.bitcast(BF16).rearrange("p (a b) -> p a b", a=8)
                for cc in range(4):
                    K.tr(p, pv[:, (j % 2) * 4 + cc, :], ytm.ap[:, j, cc * 128:(cc + 1) * 128], idb.ap, [ytm, idb])
                K.cp("act", yT.ap[:, :, j:1024:8], pv[:, (j % 2) * 4:(j % 2) * 4 + 4, :], [p], [yT])
            for half in range(2):
                for co in range(4):
                    p = nps()
                    for kc in range(4):
                        K.mm(p, p.ap, wglu.ap[:, kc, co * 128:(co + 1) * 128], yT.ap[:, kc, half * 512:(half + 1) * 512],
                             kc == 0, kc == 3, [wglu, yT])
                    K.act(sg.ap, p.ap, AF.Sigmoid, [p], [sg])
                    so = sos[half]
                    K.tt("pool", so.ap[:, co, :], yT.ap[:, co, half * 512:(half + 1) * 512], sg.ap,
                         ALU.mult, [yT, sg], [so])
                t0_ = tb - NT_OWN + half * 512
                K.dma("oS%d" % half, ssmT_s[:, :, t0_: t0_ + 512].rearrange("c p t -> p c t"), sos[half].ap, r=[sos[half]])

        S1(0)
        for blk in range(8):
            if blk + 1 < 8:
                S1(blk + 1)
            SC(blk)
            if blk >= 4:
                SY(blk)
        K.barrier()

    def phase_B():
        K.phase_reset()
        wpsB = K.sb([128, 4, 1024], BF16, "wpsB")
        wpaB = K.sb([128, 4, 1024], BF16, "wpaB")
        woutB = K.sb([128, 8, 1024], BF16, "woutB")
        wqB = K.sb([128, 8, 2048], BF16, "wqB")
        wstg2 = K.sb([128, 2048], F32, "wstg2")
        wchunks = ([(wpsB, w_ps, kc, 1024) for kc in range(4)] + [(wpaB, w_pa, kc, 1024) for kc in range(4)]
                   + [(woutB, w_out, kc, 1024) for kc in range(8)] + [(wqB, w_q, kc, 2048) for kc in range(8)])
        KTs = [K.sb([96, NT_LOC], BF16, "KT%d" % i) for i in range(2)]
        QTs = [K.sb([96, NT_OWN], BF16, "QT%d" % i) for i in range(2)]
        Vs = [K.sb([128, 64, 65], BF16, "V%d" % i) for i in range(2)]
        QA = K.sb([64, NT_OWN], BF16, "QA")
        att = K.sb([128, 32, 512], BF16, "att")
        kmax = K.sb([64, 1], F32, "kmax")
        kmaxb = K.sb([64, 1], BF16, "kmaxb")
        ksb = K.sb([64, 32], BF16, "ksb")
        valid = K.sb([128, 512], F32, "valid")
        ownm = K.sb([128, 512], F32, "ownm")
        negb = K.sb([128, 512], F32, "negb")
        stg = K.sb([128, 2048], F32, "stgB")
        cm = K.sb([128, 4, 512], BF16, "cm")
        NG = 4
        gms = [K.sb([128, 32], F32, "gm%d" % i) for i in range(NG)]
        m8s = [K.sb([128, 8], F32, "m8%d" % i) for i in range(NG)]
        sels = [K.sb([128, 32], F32, "sel%d" % i) for i in range(NG)]
        sexs = [K.sb([128, 128], BF16, "sex%d" % i) for i in range(NG)]
        mraws = [K.sb([128, 4], F32, "mraw%d" % i) for i in range(2)]
        PTs = [K.sb([128, 512], BF16, "PT%d" % i) for i in range(3)]
        rls = [K.sb([128, 1], F32, "rl%d" % i) for i in range(4)]
        ostg = [K.sb([128, 4, 128], BF16, "ostg%d" % i) for i in range(2)]
        K.dma("c0", valid.ap, validc.partition_broadcast(128), w=[valid])
        K.dma("c0", ownm.ap, ownc.partition_broadcast(128), w=[ownm])
        K.ts("dve", negb.ap, valid.ap, -1.0, 1e30, ALU.add, ALU.mult, [valid], [negb])
        for i in range(4):
            K.dma("c0", stg.ap[:, 0:512], cm_d[i], w=[stg])
            K.cp("dve", cm.ap[:, i, :], stg.ap[:, 0:512], [stg], [cm])
        gm4 = K.sb([128, 4, 32], F32, "gm4")
        m84 = K.sb([128, 4, 8], F32, "m84")
        sel4 = K.sb([128, 4, 32], F32, "sel4")
        sex4 = K.sb([128, 4, 128], BF16, "sex4")
        K.memset("pool", sex4.ap, 0.0, [sex4])
        for kb_ in KTs:
            for c4 in range(4):
                K.dma("c0", stg.ap[64:96, :], koh_d[:, c4 * 2048:(c4 + 1) * 2048], w=[stg])
                K.cp("dve", kb_.ap[64:96, c4 * 2048:(c4 + 1) * 2048], stg.ap[64:96, :], [stg], [kb_])
        for h in range(8):
            hp, pb = h // 2, (h % 2) * 64
            KT, QT, V = KTs[h % 2], QTs[h % 2], Vs[h % 2]
            K.dma("bk%d" % (h % 2), KT.ap[0:64, :], kT_s[hp, pb:pb + 64, :], w=[KT])
            K.dma("bq%d" % (h % 2), QT.ap[0:64, :], qT_s[hp, pb:pb + 64, :], w=[QT])
            K.dma("bv%d" % (h % 2), V.ap, v_s.rearrange("(t p) c -> p t c", p=128)[:, :, h * 65:(h + 1) * 65], w=[V])
            K.act(QA.ap, QT.ap[0:64, :], AF.Abs, [QT], [QA])
            K.op("dve", lambda e, KT=KT: e.tensor_reduce(out=kmax.ap, in_=KT.ap[0:64, :], axis=AX.X, op=ALU.max,
                                                         apply_absolute_value=True), [KT], [kmax])
            K.cp("dve", kmaxb.ap, kmax.ap, [kmax], [kmaxb])
            K.cp("dve", ksb.ap, ksum.ap[pb:pb + 64, hp, :], [ksum], [ksb])
            for qg in range(8):
                q0 = qg * 512
                pg = PS[qg % 3]
                c0 = (qg // 3) * 132
                for j in range(4):
                    K.mm(pg, pg.ap[:, c0 + j * 32: c0 + (j + 1) * 32], QT.ap[0:64, q0 + j * 128: q0 + (j + 1) * 128],
                         ksb.ap, True, True, [QT, ksb])
                    K.mm(pg, pg.ap[:, c0 + 128 + j: c0 + 129 + j], QA.ap[:, q0 + j * 128: q0 + (j + 1) * 128],
                         kmaxb.ap, True, True, [QA, kmaxb])
            for qg in range(8):
                q0 = qg * 512
                pg = PS[qg % 3]
                c0 = (qg // 3) * 132
                mraw = mraws[qg % 2]
                K.cp("dve", mraw.ap, pg.ap[:, c0 + 128: c0 + 132], [pg], [mraw])
                S4 = [128, 2, 2, 32]
                qsl = slice(2 * qg * 32, (2 * qg + 2) * 32)
                bq = lambda t_: t_.ap[:, qsl].rearrange("p (a b) -> p a b", a=2).unsqueeze(2).broadcast_to(S4)
                g4 = gm4.ap.rearrange("p (a c) b -> p a c b", a=2)
                s4 = sel4.ap.rearrange("p (a c) b -> p a c b", a=2)
                K.tt("dve", g4, pg.ap[:, c0:c0 + 128].rearrange("p (a c b) -> p a c b", a=2, c=2), bq(negb), ALU.add, [pg, negb], [gm4])
                for j in range(4):
                    K.op("dve", lambda e, j=j: e.max(out=m84.ap[:, j, :], in_=gm4.ap[:, j, :]), [gm4], [m84])
                K.tt("dve", sel4.ap, gm4.ap, m84.ap[:, :, 2:3].broadcast_to([128, 4, 32]), ALU.is_ge, [gm4, m84], [sel4])
                K.tt("dve", s4, s4, bq(valid), ALU.mult, [sel4, valid], [sel4])
                K.tt("dve", s4, s4, bq(ownm), ALU.add, [sel4, ownm], [sel4])
                K.ts("dve", sel4.ap, sel4.ap, -1.0, BIG, ALU.add, ALU.mult, [sel4], [sel4])
                K.tt("dve", sex4.ap[:, :, 64:96], sel4.ap, mraw.ap.unsqueeze(2).broadcast_to([128, 4, 32]), ALU.subtract,
                     [sel4, mraw], [sex4])
                pt = PS[3]
                for j in range(4):
                    K.mm(pt, pt.ap[:, j * 128:(j + 1) * 128], sex4.ap[:, j, :], idb.ap, True, True, [sex4, idb])
                K.cp("act", QT.ap[64:96, q0:q0 + 512], pt.ap[64:96, :], [pt], [QT])
            for qg in range(8):
                q0 = qg * 512
                nkt = 36 + 4 * qg

                def qk(kt):
                    ps = PS[kt % 3]
                    last_own = kt >= nkt - 4
                    K.mm(ps, ps.ap, KT.ap[:, kt * 128:(kt + 1) * 128], QT.ap[:, q0:q0 + 512],
                         True, not last_own, [KT, QT])
                    if last_own:
                        K.mm(ps, ps.ap, idb.ap, cm.ap[:, kt - (nkt - 4), :], False, True, [idb, cm])

                qk(0)
                qk(1)
                for kt in range(nkt):
                    if kt + 2 < nkt:
                        qk(kt + 2)
                    ps = PS[kt % 3]
                    PT = PTs[kt % 3]
                    K.act(PT.ap, ps.ap, AF.Exp, [ps], [PT], scale=0.125)
                    for j in range(4):
                        po = PS[4 + j]
                        K.mm(po, po.ap[:, 0:65], PT.ap[:, j * 128:(j + 1) * 128], V.ap[:, kt, :],
                             kt == 0, kt == nkt - 1, [PT, V])
                for j in range(4):
                    po = PS[4 + j]
                    K.op("dve", lambda e, po=po, j=j: e.reciprocal(out=rls[j].ap, in_=po.ap[:, 64:65]), [po], [rls[j]])
                for j in range(4):
                    po = PS[4 + j]
                    K.ts("dve", att.ap[:, qg * 4 + j, h * 64:(h + 1) * 64], po.ap[:, 0:64], rls[j].ap[:, 0:1], None,
                         ALU.mult, None, [po, rls[j]], [att])
                wi = h * 8 + qg
                wbufs = (stg, wstg2)
                if wi < len(wchunks):
                    dst, src, kc, ncols = wchunks[wi]
                    K.dma("bw%d" % (wi % 2), wbufs[wi % 2].ap[:, 0:ncols], src[kc * 128:(kc + 1) * 128, :], w=[wbufs[wi % 2]])
                if 1 <= wi <= len(wchunks):
                    dst, src, kc, ncols = wchunks[wi - 1]
                    K.cp("dve", dst.ap[:, kc, :], wbufs[(wi - 1) % 2].ap[:, 0:ncols], [wbufs[(wi - 1) % 2]], [dst])
        for qt in range(32):
            p = PS[qt % 2]
            pv = p.ap.bitcast(BF16)[:, 0:512].rearrange("p (a b) -> p a b", a=4)
            for c in range(4):
                K.tr(p, pv[:, c, :], att.ap[:, qt, c * 128:(c + 1) * 128], idb.ap, [att, idb])
            og = ostg[qt % 2]
            K.cp("act", og.ap, pv, [p], [og])
            K.dma("ob%d" % (qt % 2), attT_s[:, :, qt * 128:(qt + 1) * 128].rearrange("c p t -> p c t"), og.ap, r=[og])
        K.barrier()

    def phase_C1():
        K.phase_reset()
        wps = K.sb([128, 4, 1024], BF16, "wps")
        wpa = K.sb([128, 4, 1024], BF16, "wpa")
        wout = K.sb([128, 8, 1024], BF16, "wout")
        wq = K.sb([128, 8, 2048], BF16, "wq")
        keysT = K.sb([128, 16, 128], F32, "keysT")
        gf = K.sb([128, 1024], F32, "gffn")
        stage = K.sb([128, 2048], F32, "stageC")
        ssmT = K.sb([128, 4, 512], BF16, "ssmT")
        attT = K.sb([128, 4, 512], BF16, "attT")
        gaT = K.sb([128, 8, 512], BF16, "gaT")
        gbT = K.sb([128, 8, 512], BF16, "gbT")
        mT = K.sb([128, 8, 512], BF16, "mT")
        t1cs = [K.sb([128, 512], F32, "t1C%d" % i) for i in range(2)]
        t2cs = [K.sb([128, 512], F32, "t2C%d" % i) for i in range(2)]
        xts = [K.sb([128, 1024], F32, "xtC%d" % i) for i in range(2)]
        x1s = [K.sb([128, 1024], F32, "x1C%d" % i) for i in range(2)]
        hbs = [K.sb([128, 1024], BF16, "hbC%d" % i) for i in range(2)]
        sq = K.sb([128, 1024], BF16, "sqC")
        ss = K.sb([128, 1], F32, "ssC")
        rs = K.sb([128, 1], F32, "rsC")
        h2T = K.sb([128, 8, 512], BF16, "h2T")
        qT = K.sb([128, 16, 512], F32, "qT")
        scs = [K.sb([128, 16, 128], F32, "sc%d" % i) for i in range(2)]
        K.dma("c0", gf.ap, g_ffn.partition_broadcast(128), w=[gf])
        for ch in range(16):
            K.dma("wst", stage.ap[:, 0:128], keys[ch], w=[stage])
            p = nps()
            K.tr(p, p.ap[:, 0:128], stage.ap[:, 0:128], idf.ap, [stage, idf])
            K.cp("dve", keysT.ap[:, ch, :], p.ap[:, 0:128], [p], [keysT])
        xi = 0

        def c1_loads(tg_):
            ta_ = tg_ * 512
            K.dma("c1a", ssmT.ap, ssmT_s[:, :, ta_:ta_ + 512].rearrange("c p t -> p c t"), w=[ssmT])
            K.dma("c1b", attT.ap, attT_s[:, :, ta_:ta_ + 512].rearrange("c p t -> p c t"), w=[attT])
            K.dma("c1c", gaT.ap, gT_s[0:8, :, ta_:ta_ + 512].rearrange("c p t -> p c t"), w=[gaT])
            K.dma("c1d", gbT.ap, gT_s[8:16, :, ta_:ta_ + 512].rearrange("c p t -> p c t"), w=[gbT])

        c1_loads(0)
        for tg in range(8):
            t0 = tg * 512
            for c in range(8):
                pa = nps()
                pb = nps()
                for kc in range(4):
                    K.mm(pa, pa.ap, wps.ap[:, kc, c * 128:(c + 1) * 128], ssmT.ap[:, kc, :], kc == 0, kc == 3, [wps, ssmT])
                for kc in range(4):
                    K.mm(pb, pb.ap, wpa.ap[:, kc, c * 128:(c + 1) * 128], attT.ap[:, kc, :], kc == 0, kc == 3, [wpa, attT])
                t1, t2 = t1cs[c % 2], t2cs[c % 2]
                K.tt("dve", t1.ap, pa.ap, gaT.ap[:, c, :], ALU.mult, [pa, gaT], [t1])
                K.tt("dve", t2.ap, pb.ap, gbT.ap[:, c, :], ALU.mult, [pb, gbT], [t2])
                K.tt("dve", mT.ap[:, c, :], t1.ap, t2.ap, ALU.add, [t1, t2], [mT])
            if tg + 1 < 8:
                c1_loads(tg + 1)
            for j in range(4):
                xt = xts[xi % 2]
                x1 = x1s[xi % 2]
                hb = hbs[xi % 2]
                xi += 1
                r0 = t0 + j * 128
                K.dma("x%d" % (xi % 2), xt.ap, xc[NT_OWN + r0: NT_OWN + r0 + 128, :], w=[xt])
                for half in range(2):
                    p = nps()
                    for kc in range(8):
                        K.mm(p, p.ap, mT.ap[:, kc, j * 128:(j + 1) * 128], wout.ap[:, kc, half * 512:(half + 1) * 512],
                             kc == 0, kc == 7, [mT, wout])
                    K.tt("dve", x1.ap[:, half * 512:(half + 1) * 512], p.ap, xt.ap[:, half * 512:(half + 1) * 512], ALU.add,
                         [p, xt], [x1])
                K.dma("x1o%d" % (xi % 2), x1_s[r0:r0 + 128, :], x1.ap, r=[x1])
                rmsnorm_tile(x1, gf, hb, sq, ss, rs)
                p = nps()
                pv = p.ap.bitcast(BF16).rearrange("p (a b) -> p a b", a=8)
                for kc in range(8):
                    K.tr(p, pv[:, kc, :], hb.ap[:, kc * 128:(kc + 1) * 128], idb.ap, [hb, idb])
                K.cp("act", h2T.ap[:, :, j * 128:(j + 1) * 128], pv, [p], [h2T])
            K.dma("h2o", h2T_s[:, :, t0:t0 + 512].rearrange("c p t -> p c t"), h2T.ap, r=[h2T])
            for ch in range(16):
                p = nps()
                for kc in range(8):
                    K.mm(p, p.ap, wq.ap[:, kc, ch * 128:(ch + 1) * 128], h2T.ap[:, kc, :], kc == 0, kc == 7, [wq, h2T])
                K.cp("act" if ch % 2 else "dve", qT.ap[:, ch, :], p.ap, [p], [qT])
            for j in range(4):
                sc = scs[j % 2]
                for c0 in range(0, 16, 4):
                    p = nps()
                    for i in range(4):
                        K.mm(p, p.ap[:, i * 128:(i + 1) * 128], qT.ap[:, c0 + i, j * 128:(j + 1) * 128], keysT.ap[:, c0 + i, :],
                             True, True, [qT, keysT])
                    K.cp("act" if (c0 // 4) % 2 else "dve", sc.ap[:, c0:c0 + 4, :], p.ap.rearrange("p (a b) -> p a b", a=4), [p], [sc])
                r0 = t0 + j * 128
                K.dma("sco%d" % (j % 2), sc_s[r0:r0 + 128, :], sc.ap.rearrange("p a b -> p (a b)"), r=[sc])
        K.barrier()

    U32 = mybir.dt.uint32

    def phase_C2():
        K.phase_reset()
        iot = K.sb([128, 128], F32, "iota")
        K.dma("c0", iot.ap, iota_d, w=[iot])
        ss_ = [K.sb([128, 16, 128], F32, "s%d" % i) for i in range(2)]
        v = K.sb([128, 16, 16], F32, "v")
        idx = K.sb([128, 8, 16], U32, "idx")
        idxf = K.sb([128, 128], F32, "idxf")
        idxT = K.sb([128, 128], F32, "idxT")
        top = K.sb([128, 8, 16], F32, "top")
        e16 = K.sb([128, 8, 16], F32, "e16")
        Z = K.sb([128, 8], F32, "Z")
        bE = K.sb([128, 8], F32, "bE")
        v1m = K.sb([128, 8, 16], F32, "v1m")
        sums = [K.sb([128, 16, 128], F32, "sum%d" % i) for i in range(3)]
        Es = [K.sb([128, 16, 128], BF16, "E%d" % i) for i in range(3)]
        Btm = K.sb([128, 8, 16, 128], BF16, "Btm")
        BT = K.sb([128, 128, 128], BF16, "BT")
        AT = K.sb([128, 128, 128], BF16, "AT")
        Gst = K.sb([128, 128, 128], BF16, "Gst")
        works = [K.sb([128, 128], F32, "wk%d" % i) for i in range(16)]
        work2s = [K.sb([128, 256], F32, "wk2%d" % i) for i in range(8)]
        K.dma("c2s0", ss_[0].ap.rearrange("p a b -> p (a b)"), sc_s[0:128, :], w=[ss_[0]])
        for tl in range(32):
            r0 = tl * 128
            s_ = ss_[tl % 2]
            if tl + 1 < 32:
                sn_ = ss_[(tl + 1) % 2]
                K.dma("c2s%d" % ((tl + 1) % 2), sn_.ap.rearrange("p a b -> p (a b)"), sc_s[r0 + 128:r0 + 256, :], w=[sn_])
            for ch in range(16):
                K.op("dve", lambda e, ch=ch, s_=s_: e.max(out=v.ap[:, ch, 0:8], in_=s_.ap[:, ch, :]), [s_], [v])
            for ch in range(0, 16, 2):
                K.op("dve", lambda e, ch=ch, s_=s_: e.max_index(out=idx.ap[:, ch // 2, 0:8], in_max=v.ap[:, ch, 0:8],
                                                                in_values=s_.ap[:, ch, :]), [v, s_], [idx])
            for ch in range(16):
                K.op("dve", lambda e, ch=ch, s_=s_: e.match_replace(out=works[ch].ap, in_to_replace=v.ap[:, ch, 0:8],
                                                                    in_values=s_.ap[:, ch, :], imm_value=-1e30), [v, s_], [works[ch]])
            for ch in range(16):
                K.op("dve", lambda e, ch=ch: e.max(out=v.ap[:, ch, 8:16], in_=works[ch].ap), [works[ch]], [v])
            for ch in range(0, 16, 2):
                K.op("dve", lambda e, ch=ch: e.max_index(out=idx.ap[:, ch // 2, 8:16], in_max=v.ap[:, ch, 8:16],
                                                         in_values=works[ch].ap), [v, works[ch]], [idx])
            vv = v.ap.rearrange("p (h s) k -> p h s k", s=2)
            v1 = vv[:, :, 0, :]
            v2 = vv[:, :, 1, :]
            cand_ap = sums[0].ap.rearrange("p a b -> p (a b)").rearrange("p (h c) -> p h c", h=8)
            K.tt("dve", cand_ap.rearrange("p h (i j) -> p h i j", i=16), v1.unsqueeze(3).broadcast_to([128, 8, 16, 16]),
                 v2.unsqueeze(2).broadcast_to([128, 8, 16, 16]), ALU.add, [v], [sums[0]])
            for h in range(8):
                K.op("dve", lambda e, h=h: e.max(out=top.ap[:, h, 0:8], in_=cand_ap[:, h, :]), [sums[0]], [top])
            for h in range(8):
                K.op("dve", lambda e, h=h: e.match_replace(out=work2s[h].ap, in_to_replace=top.ap[:, h, 0:8],
                                                           in_values=cand_ap[:, h, :], imm_value=-1e30), [top, sums[0]], [work2s[h]])
            for h in range(8):
                K.op("dve", lambda e, h=h: e.max(out=top.ap[:, h, 8:16], in_=work2s[h].ap), [work2s[h]], [top])
            mxb = top.ap[:, :, 0:1].broadcast_to([128, 8, 16])
            taub = top.ap[:, :, 15:16].broadcast_to([128, 8, 16])
            K.tt("dve", e16.ap, top.ap, mxb, ALU.subtract, [top], [e16])
            K.act(e16.ap, e16.ap, AF.Exp, [e16], [e16])
            K.op("dve", lambda e: e.tensor_reduce(out=Z.ap, in_=e16.ap, axis=AX.X, op=ALU.add), [e16], [Z])
            K.act(Z.ap, Z.ap, AF.Ln, [Z], [Z])
            K.tt("dve", bE.ap, top.ap[:, :, 15], top.ap[:, :, 0], ALU.subtract, [top], [bE])
            K.tt("dve", bE.ap, bE.ap, Z.ap, ALU.subtract, [bE, Z], [bE])
            K.tt("dve", v1m.ap, v1, taub, ALU.subtract, [v, top], [v1m])
            K.cp("dve", idxf.ap, idx.ap.rearrange("p h k -> p (h k)"), [idx], [idxf])
            p = nps()
            K.tr(p, p.ap[:, 0:128], idxf.ap, idf.ap, [idxf, idf])
            K.cp("dve", idxT.ap, p.ap[:, 0:128], [p], [idxT])
            K.tt("dve", AT.ap, iot.ap.unsqueeze(1).broadcast_to([128, 128, 128]),
                 idxT.ap.unsqueeze(2).broadcast_to([128, 128, 128]), ALU.is_equal, [iot, idxT], [AT])
            def bsum(h):
                sm_ = sums[h % 3]
                K.tt("pool" if h % 2 == 0 else "dve", sm_.ap, v1m.ap[:, h, :].unsqueeze(2).broadcast_to([128, 16, 128]),
                     s_.ap[:, 2 * h + 1, :].unsqueeze(1).broadcast_to([128, 16, 128]), ALU.add, [v1m, s_], [sm_])
                K.act(Es[h % 3].ap, sm_.ap, AF.Exp, [sm_, bE], [Es[h % 3]], bias=bE.ap[:, h:h + 1])

            bsum(0)
            bsum(1)
            for h in range(8):
                if h + 2 < 8:
                    bsum(h + 2)
                K.stt("dve", Btm.ap[:, h], sums[h % 3].ap, -1e-5, Es[h % 3].ap, ALU.is_ge, ALU.mult, [sums[h % 3], Es[h % 3]], [Btm])
            for b0 in range(0, 128, 8):
                p = nps()
                pv = p.ap.bitcast(BF16).rearrange("p (a b) -> p a b", a=8)
                for k in range(8):
                    K.tr(p, pv[:, k, :], Btm.ap[:, :, :, b0 + k].rearrange("p h i -> p (h i)"), idb.ap, [Btm, idb])
                K.cp("act", BT.ap[:, :, b0:b0 + 8], pv.rearrange("p b t -> p t b"), [p], [BT])
            for t4 in range(0, 128, 4):
                p = nps()
                for k in range(4):
                    t = t4 + k
                    K.mm(p, p.ap[:, k * 128:(k + 1) * 128], BT.ap[:, t, :], AT.ap[:, t, :], True, True, [BT, AT])
                K.cp("act", Gst.ap[:, :, t4:t4 + 4],
                     p.ap.rearrange("p (t a) -> p a t", t=4), [p], [Gst])
            K.dma("c2g", G_s[tl], Gst.ap, r=[Gst])
        K.barrier()

    def phase_D():
        K.phase_reset()
        TG = 1024
        NG_ = NT_OWN // TG
        h2T = K.sb([128, 8, TG], BF16, "h2TD")
        Ubs = [K.sb([128, 1024], F32, "Ub%d" % i) for i in range(3)]
        Ubb = [K.sb([128, 1024], BF16, "Ubb%d" % i) for i in range(2)]
        UbTs = [K.sb([128, 8, 128], BF16, "UbT%d" % i) for i in range(3)]
        Ghs = [K.sb([128, 8, 8, 128], BF16, "Gh%d" % i) for i in range(2)]
        ges = [K.sb([128, 512], BF16, "ge%d" % i) for i in range(2)]
        W = K.sb([128, 16, TG], BF16, "W")
        Vbs = [K.sb([128, 1024], F32, "Vb%d" % i) for i in range(3)]
        Vbf = K.sb([128, 16, 1024], BF16, "Vbf")
        acc = K.sb([128, 8, 1024], F32, "acc")
        x1t = [K.sb([128, 1024], F32, "x1D%d" % i) for i in range(2)]
        uttok = [Buf(None, "ut%d" % i) for i in range(128)]
        NI = NG_ * 128

        def load(n):
            grp, a = n // 128, n % 128
            t0 = grp * TG
            if grp == 0:
                K.dma("du%d" % (n % 3), Ubs[n % 3].ap, peer_u[a * 128:(a + 1) * 128, :], w=[Ubs[n % 3]])
            else:
                K.dma("du%d" % (n % 3), UbTs[n % 3].ap.rearrange("p a b -> p (a b)"), UT_s[a], r=[uttok[a]], w=[UbTs[n % 3]])
            if a % 8 == 0:
                Gh = Ghs[(n // 8) % 2]
                tl0 = t0 // 128
                K.dma("dg%d" % ((n // 8) % 2), Gh.ap, G_s[tl0:tl0 + 8, :, a:a + 8, :].rearrange("tl b a t -> b tl a t"), w=[Gh])
            K.dma("dv%d" % (n % 3), Vbs[n % 3].ap, peer_v[a * 128:(a + 1) * 128, :], w=[Vbs[n % 3]])

        def trans(n):
            if n // 128 != 0:
                return
            Ub, UbT, ub = Ubs[n % 3], UbTs[n % 3], Ubb[n % 2]
            K.cp("dve", ub.ap, Ub.ap, [Ub], [ub])
            p = nps()
            pv = p.ap.bitcast(BF16).rearrange("p (a b) -> p a b", a=8)
            for kc in range(8):
                K.tr(p, pv[:, kc, :], ub.ap[:, kc * 128:(kc + 1) * 128], idb.ap, [ub, idb])
            K.cp("act", UbT.ap, pv, [p], [UbT])

        load(0)
        load(1)
        trans(0)
        for n in range(NI):
            grp, a = n // 128, n % 128
            t0 = grp * TG
            si = a % 16
            if a == 0:
                K.dma("dh", h2T.ap, h2T_s[:, :, t0:t0 + TG].rearrange("c p t -> p c t"), w=[h2T])
                K.memset("pool", acc.ap, 0.0, [acc])
            if n + 2 < NI:
                load(n + 2)
            if n + 1 < NI:
                trans(n + 1)
            UbT, Vb = UbTs[n % 3], Vbs[n % 3]
            Gh = Ghs[(n // 8) % 2]
            if grp == 0:
                K.dma("dut%d" % (n % 3), UT_s[a], UbT.ap.rearrange("p a b -> p (a b)"), r=[UbT], w=[uttok[a]])
            K.cp("act", Vbf.ap[:, si, :], Vb.ap, [Vb], [Vbf])
            for hf in range(TG // 512):
                p = nps()
                for kc in range(8):
                    K.mm(p, p.ap, UbT.ap[:, kc, :], h2T.ap[:, kc, hf * 512:(hf + 1) * 512], kc == 0, kc == 7, [UbT, h2T])
                ge = ges[hf % 2]
                K.act(ge.ap, p.ap, AF.Gelu_apprx_tanh, [p], [ge])
                K.tt("dve", W.ap[:, si, hf * 512:(hf + 1) * 512].rearrange("p (a b) -> p a b", a=4),
                     ge.ap.rearrange("p (a b) -> p a b", a=4), Gh.ap[:, hf * 4:(hf + 1) * 4, a % 8, :], ALU.mult,
                     [ge, Gh], [W])
            if si == 15:
                for j in range(TG // 128):
                    for hf in range(2):
                        p = nps()
                        for s2 in range(16):
                            K.mm(p, p.ap, W.ap[:, s2, j * 128:(j + 1) * 128], Vbf.ap[:, s2, hf * 512:(hf + 1) * 512],
                                 s2 == 0, s2 == 15, [W, Vbf])
                        K.tt("dve", acc.ap[:, j, hf * 512:(hf + 1) * 512], p.ap, acc.ap[:, j, hf * 512:(hf + 1) * 512], ALU.add,
                             [p, acc], [acc])
            if a == 127:
                for j in range(TG // 128):
                    xt = x1t[j % 2]
                    r0 = t0 + j * 128
                    K.dma("dx%d" % (j % 2), xt.ap, x1_s[r0:r0 + 128, :], w=[xt])
                    K.tt("dve", xt.ap, xt.ap, acc.ap[:, j, :], ALU.add, [xt, acc], [xt])
                    K.dma("dx%d" % (j % 2), x2_s[r0:r0 + 128, :], xt.ap, r=[xt])
        K.barrier()

    def phase_E():
        K.phase_reset()
        wpg = K.sb([128, 8, 1024], BF16, "wpg")
        wpp = K.sb([128, 2, 1024], BF16, "wpp")
        stage = K.sb([128, 1024], F32, "stageE")
        gp = K.sb([128, 1024], F32, "gple")
        gfin = K.sb([128, 1024], F32, "gfin")
        xts = [K.sb([128, 1024], F32, "xtE%d" % i) for i in range(4)]
        pts = [K.sb([128, 256], F32, "ptE%d" % i) for i in range(2)]
        pb_s = [K.sb([128, 256], BF16, "pbE%d" % i) for i in range(2)]
        pTs = [K.sb([128, 2, 128], BF16, "pTE%d" % i) for i in range(2)]
        hb_s = [K.sb([128, 1024], BF16, "hbE%d" % i) for i in range(2)]
        hTs_ = [K.sb([128, 8, 128], BF16, "hTE%d" % i) for i in range(2)]
        sq_s = [K.sb([128, 1024], BF16, "sqE%d" % i) for i in range(2)]
        ss_s = [K.sb([128, 1], F32, "ssE%d" % i) for i in range(4)]
        rs_s = [K.sb([128, 1], F32, "rsE%d" % i) for i in range(4)]
        sgts = [K.sb([128, 1024], F32, "sgE%d" % i) for i in range(2)]
        outs = [K.sb([128, 1024], F32, "oE%d" % i) for i in range(2)]
        K.dma("c0", gp.ap, g_ple.partition_broadcast(128), w=[gp])
        K.dma("c0", gfin.ap, g_fin.partition_broadcast(128), w=[gfin])
        load_w_bf(wpg, w_pg, 8, 1024, stage, "wst")
        load_w_bf(wpp, w_pp, 2, 1024, stage, "wst")
        def e_vars(tl):
            return dict(r0=tl * 128)

        def front(tl):
            r0 = tl * 128
            xt, pt, ot = xts[tl % 4], pts[tl % 2], outs[tl % 2]
            pb_, pT, hb, hT, sq, sgt = pb_s[tl % 2], pTs[tl % 2], hb_s[tl % 2], hTs_[tl % 2], sq_s[tl % 2], sgts[tl % 2]
            ss, rs = ss_s[tl % 2], rs_s[tl % 2]
            ss2, rs2 = ss_s[2 + tl % 2], rs_s[2 + tl % 2]
            K.dma("ex%d" % (tl % 4), xt.ap, x2_s[r0:r0 + 128, :], w=[xt])
            K.dma("ep%d" % (tl % 2), pt.ap, pc[r0:r0 + 128, :], w=[pt])
            rmsnorm_tile(xt, gp, hb, sq, ss, rs)
            K.cp("dve", pb_.ap, pt.ap, [pt], [pb_])

        def front1b(tl):
            pb_, pT, hb, hT = pb_s[tl % 2], pTs[tl % 2], hb_s[tl % 2], hTs_[tl % 2]
            p = nps()
            pv = p.ap.bitcast(BF16).rearrange("p (a b) -> p a b", a=8)
            for kc in range(8):
                K.tr(p, pv[:, kc, :], hb.ap[:, kc * 128:(kc + 1) * 128], idb.ap, [hb, idb])
            K.cp("act", hT.ap, pv, [p], [hT])
            p = nps()
            pv = p.ap.bitcast(BF16).rearrange("p (a b) -> p a b", a=8)
            for kc in range(2):
                K.tr(p, pv[:, kc, :], pb_.ap[:, kc * 128:(kc + 1) * 128], idb.ap, [pb_, idb])
            K.cp("act", pT.ap, pv[:, 0:2, :], [p], [pT])

        def front2(tl):
            pT, hT, sgt = pTs[tl % 2], hTs_[tl % 2], sgts[tl % 2]
            for hf in range(2):
                pg = nps()
                pe_ = nps()
                for kc in range(8):
                    K.mm(pg, pg.ap, hT.ap[:, kc, :], wpg.ap[:, kc, hf * 512:(hf + 1) * 512], kc == 0, kc == 7, [hT, wpg])
                for kc in range(2):
                    K.mm(pe_, pe_.ap, pT.ap[:, kc, :], wpp.ap[:, kc, hf * 512:(hf + 1) * 512], kc == 0, kc == 1, [pT, wpp])
                K.act(sgt.ap[:, hf * 512:(hf + 1) * 512], pg.ap, AF.Sigmoid, [pg], [sgt])
                K.tt("dve", sgt.ap[:, hf * 512:(hf + 1) * 512], pe_.ap, sgt.ap[:, hf * 512:(hf + 1) * 512], ALU.mult, [pe_, sgt], [sgt])

        def back(tl):
            r0 = tl * 128
            xt, ot = xts[tl % 4], outs[tl % 2]
            sq, sgt = sq_s[tl % 2], sgts[tl % 2]
            ss2, rs2 = ss_s[2 + tl % 2], rs_s[2 + tl % 2]
            K.tt("pool", xt.ap, xt.ap, sgt.ap, ALU.add, [xt, sgt], [xt])
            K.act(sq.ap, xt.ap, AF.Square, [xt], [sq, ss2], accum=ss2.ap)
            K.act(rs2.ap, ss2.ap, AF.Sqrt, [ss2], [rs2], scale=1.0 / 1024, bias=eps_b.ap)
            K.op("dve", lambda e, rs2=rs2: e.reciprocal(out=rs2.ap, in_=rs2.ap), [rs2], [rs2])
            K.stt("dve", ot.ap, xt.ap, rs2.ap[:, 0:1], gfin.ap, ALU.mult, ALU.mult, [xt, rs2, gfin], [ot])
            K.dma("eo%d" % (tl % 2), out[r0:r0 + 128, :], ot.ap, r=[ot])

        for r_ in range(-3, 32):
            for stage_fn, off_ in ((back, 0), (front2, 1), (front1b, 2), (front, 3)):
                tl_ = r_ + off_
                if 0 <= tl_ < 32:
                    stage_fn(tl_)
        K.barrier()

    phases = {"A": phase_A, "B": phase_B, "S": phase_S, "C1": phase_C1, "C2": phase_C2, "D": phase_D, "E": phase_E}
    return nc, kb, st, locals()


def _consts():
    ident = np.eye(128, dtype=np.float32)
    invf = np.zeros((128, 2), np.float32)
    for p in range(128):
        hd = p % 64
        if hd < 16:
            invf[p, 0] = np.float32(500000.0) ** np.float32(-(2 * (hd % 8)) / 16.0)
            invf[p, 1] = -1.0 if hd < 8 else 1.0
    eoh = np.zeros((32, NT_LOC), np.float32)
    for n in range(32):
        eoh[n, n * 256:(n + 1) * 256] = 1.0
    cm = np.zeros((4, 128, 512), np.float32)
    for kt in range(4):
        for kp in range(128):
            kpos = kt * 128 + kp
            q = np.arange(512)
            same = (q // 256) == (kpos // 256)
            cm[kt, kp, :] = np.where(same & (kpos > q), -BIG, 0.0)
    return ident, invf, eoh, cm


def make_in_maps(inp, cores=range(8)):
    f = lambda a: np.ascontiguousarray(np.asarray(a))
    x = f(inp["x"])
    p = f(inp["p"])[0]
    pos = f(inp["positions"]).astype(np.int32)
    ident, invf, eoh, cm = _consts()
    w_in = f(inp["w_in"])[0]
    perm = np.arange(1024)
    for c in range(1024):
        hd = c % 64
        if hd < 8:
            perm[c] = c + 8
        elif hd < 16:
            perm[c] = c - 8
    w_perm = f(w_in[:, 512:1536][:, perm])

    def pair(a):
        a = f(a)[0]
        sh = a.shape
        a = a.reshape(16, 2, 64, *sh[2:])
        a = np.moveaxis(a, 0, 2)
        return f(a.reshape(128, 16, -1).reshape(128, -1))

    ldt = f(inp["ssm_log_dt"])[0]
    ldt_l = f(np.broadcast_to(ldt.reshape(16, 2, 1), (16, 2, 64)).transpose(1, 2, 0).reshape(128, 16))
    cre = f(inp["ssm_c_re"])[0].transpose(0, 2, 1)
    cim = f(inp["ssm_c_im"])[0].transpose(0, 2, 1)
    shared = {
        "ident": ident, "iota": np.ascontiguousarray(np.broadcast_to(np.arange(128, dtype=np.float32), (128, 128))), "invf": invf, "koh": eoh, "cmask": cm,
        "g_mix": f(inp["g_mix"]), "w_in": w_in, "w_perm": w_perm,
        "s5_ldt": ldt_l, "s5_are": pair(inp["ssm_a_re"]), "s5_aim": pair(inp["ssm_a_im"]),
        "s5_bre": pair(inp["ssm_b_re"]), "s5_bim": pair(inp["ssm_b_im"]),
        "s5_cre": pair(cre[None]), "s5_cim": pair(cim[None]),
        "ssm_d": f(inp["ssm_d"]), "w_glu": f(inp["ssm_w_glu"])[0],
        "w_ps": f(inp["w_proj_ssm"])[0], "w_pa": f(inp["w_proj_att"])[0], "w_out": f(inp["w_out"])[0],
        "g_ffn": f(inp["g_ffn"]), "w_q": f(inp["peer_w_q"])[0],
        "keys": f(np.stack([f(inp["peer_keys1"])[0], f(inp["peer_keys2"])[0]], axis=1).reshape(16, 128, 128)),
        "peer_u": f(inp["peer_u"])[0], "peer_v": f(inp["peer_v"])[0],
        "g_ple": f(inp["g_ple"]), "w_pg": f(inp["ple_w_gate"])[0], "w_pp": f(inp["ple_w_proj"])[0],
        "g_fin": f(inp["g_final"]).reshape(1, 1024),
    }
    maps = []
    for c in cores:
        b, half = c // 2, c % 2
        xc = np.zeros((NT_LOC, 1024), np.float32)
        posc = np.zeros((1, NT_LOC), np.int32)
        if half == 1:
            xc[:] = x[b]
            posc[0] = pos[b]
        else:
            xc[NT_OWN:] = x[b, :NT_OWN]
            posc[0, NT_OWN:] = pos[b, :NT_OWN]
        valid = np.zeros((16, 32), np.float32)
        own = np.zeros((16, 32), np.float32)
        for qb in range(16, 32):
            for n in range(32):
                if n < qb and (half == 1 or n >= 16):
                    valid[qb - 16, n] = 1.0
            own[qb - 16, qb] = 1.0
        m = dict(shared)
        m.update({"xc": xc, "pc": f(p[b, half * NT_OWN:(half + 1) * NT_OWN]), "posc": posc,
                  "validc": valid.reshape(1, 512), "ownc": own.reshape(1, 512)})
        maps.append(m)
    return maps


_CACHE = {}


def kernel(**inputs):
    if "prog" not in _CACHE:
        nc, kb, st, L = build_program()
        for ph in ("A", "S", "B", "C1", "C2", "D", "E"):
            L["phases"][ph]()
        kb.S.emit()
        st.close()
        _CACHE["prog"] = nc
    nc = _CACHE["prog"]
    maps = make_in_maps(inputs, range(8))
    res = run_bass_kernel_spmd(nc, maps, core_ids=list(range(8)))
    out = np.zeros((4, 8192, 1024), np.float32)
    for c in range(8):
        b, half = c // 2, c % 2
        out[b, half * NT_OWN:(half + 1) * NT_OWN] = np.asarray(res.results[c]["out"])
    return out
```

```python
import numpy as np
import concourse.bass as bass
import concourse.mybir as mybir
from concourse.bass_utils import run_bass_kernel_spmd

F32 = mybir.dt.float32
BF16 = mybir.dt.bfloat16
I32 = mybir.dt.int32
ALU = mybir.AluOpType
AF = mybir.ActivationFunctionType
AX = mybir.AxisListType


class Tok:
    __slots__ = ("w", "r", "name")

    def __init__(self, name=""):
        self.w = None
        self.r = {}
        self.name = name


class Sched:
    ENGS = ("pe", "act", "dve", "pool", "sp")

    def __init__(self, nc):
        self.nc = nc
        self.ops = {e: [] for e in self.ENGS}
        self.dma_cnt = {}
        self.dma_keys = []

    @staticmethod
    def _evkey(ev):
        return (ev[0], ev[1])

    def _collect(self, reads, writes):
        deps = {}

        def add(ev):
            if ev is None:
                return
            k = self._evkey(ev)
            if k not in deps or deps[k][2] < ev[2]:
                deps[k] = ev

        for t in reads:
            add(t.w)
        for t in writes:
            add(t.w)
            for ev in t.r.values():
                add(ev)
        return deps

    def _commit(self, ev, reads, writes):
        for t in reads:
            k = self._evkey(ev)
            t.r[k] = ev
        for t in writes:
            t.w = ev
            t.r = {}

    def op(self, eng, fn, reads=(), writes=()):
        deps = self._collect(reads, writes)
        idx = len(self.ops[eng])
        ev = ("e", eng, idx)
        if eng == "pe":
            deps.pop(("e", "pe"), None)
        self.ops[eng].append(dict(fn=fn, deps=list(deps.values()), dma=None, signal=False))
        self._commit(ev, reads, writes)
        return ev

    def dma(self, q, key, out, in_, reads=(), writes=()):
        deps = self._collect(reads, writes)
        if key not in self.dma_cnt:
            self.dma_cnt[key] = 0
            self.dma_keys.append(key)
        n = self.dma_cnt[key]
        if n > 0:
            k = ("d", key)
            deps[k] = ("d", key, n)
        self.dma_cnt[key] = n + 1
        ev = ("d", key, n + 1)
        self.ops[q].append(dict(fn=lambda e, o=out, i=in_: e.dma_start(out=o, in_=i),
                                deps=list(deps.values()), dma=key, signal=False))
        self._commit(ev, reads, writes)
        return ev

    def emit(self, final_keys=()):
        nc = self.nc
        ops = self.ops
        for e in self.ENGS:
            for o in ops[e]:
                for d in o["deps"]:
                    if d[0] == "e":
                        ops[d[1]][d[2]]["signal"] = True
        for e in self.ENGS:
            last = None
            for o in ops[e]:
                if "barrier" in o:
                    if last is not None and e != "sp":
                        last["signal"] = True
                else:
                    last = o
        sigval = {}
        for e in self.ENGS:
            c = 0
            vals = []
            for o in ops[e]:
                if o["signal"]:
                    c += 1
                vals.append(c)
            sigval[e] = vals
        barvals = {}
        for e in self.ENGS:
            for i, o in enumerate(ops[e]):
                if "barrier" in o:
                    barvals[(e, o["barrier"])] = sigval[e][i]
        from contextlib import ExitStack
        with ExitStack() as st:
            esem = {e: st.enter_context(nc.semaphore("s_" + e)) for e in self.ENGS if e != "sp"}
            dsem = {k: st.enter_context(nc.semaphore("d_%d" % i)) for i, k in enumerate(self.dma_keys)}
            bsem = st.enter_context(nc.semaphore("s_bar"))
            block = st.enter_context(nc.Block())

            def run(ename, eng):
                waited = {}
                for o in ops[ename]:
                    if "barrier" in o:
                        k = o["barrier"]
                        if ename == "sp":
                            for key, cnt in o["dcnt"].items():
                                if cnt > 0 and waited.get(("d", key), 0) < 16 * cnt:
                                    eng.wait_ge(dsem[key], 16 * cnt)
                            for e2 in esem:
                                v = barvals[(e2, k)]
                                if v > 0:
                                    eng.wait_ge(esem[e2], v)
                            eng.sem_inc(bsem, 1)
                        else:
                            eng.wait_ge(bsem, k)
                        for key, cnt in o["dcnt"].items():
                            waited[("d", key)] = max(waited.get(("d", key), 0), 16 * cnt)
                        for e2 in esem:
                            waited[("e", e2)] = max(waited.get(("e", e2), 0), barvals[(e2, k)])
                        continue
                    for d in sorted(o["deps"]):
                        if d[0] == "e":
                            sem = esem[d[1]]
                            val = sigval[d[1]][d[2]]
                        else:
                            sem = dsem[d[1]]
                            val = 16 * d[2]
                        wk = (d[0], d[1])
                        if waited.get(wk, 0) >= val:
                            continue
                        waited[wk] = val
                        eng.wait_ge(sem, val)
                    ins = o["fn"](eng)
                    if o["dma"] is not None:
                        ins.then_inc(dsem[o["dma"]], 16)
                    elif o["signal"]:
                        ins.then_inc(esem[ename], 1)
                if ename == "sp":
                    for k in self.dma_keys:
                        eng.wait_ge(dsem[k], 16 * self.dma_cnt[k])

            @block.sync
            def _(e):
                run("sp", e)

            @block.tensor
            def _(e):
                run("pe", e)

            @block.scalar
            def _(e):
                run("act", e)

            @block.vector
            def _(e):
                run("dve", e)

            @block.gpsimd
            def _(e):
                run("pool", e)


NT_OWN = 4096
NT_LOC = 8192
PI = float(np.pi)
BIG = 30000.0


class Buf:
    __slots__ = ("ap", "t")

    def __init__(self, ap, name=""):
        self.ap = ap
        self.t = Tok(name)

    def __getitem__(self, k):
        return self.ap[k]


class KB:
    def __init__(self, nc):
        self.nc = nc
        self.S = Sched(nc)
        self.big = nc.alloc_sbuf_tensor("bigsb", [128, 53000], F32)
        self.off = 0
        self.persist = 0
        self.nbar = 0

    def sb(self, shape, dt, name=""):
        n = int(np.prod(shape[1:]))
        esz = 4 if dt in (F32, I32, mybir.dt.uint32) else 2
        nw = (n * esz + 63) // 64 * 16
        assert self.off + nw <= 53000, ("sbuf overflow", name, self.off, nw)
        ap = self.big[:, self.off:self.off + nw]
        self.off += nw
        if dt != F32:
            ap = ap.bitcast(dt)
        ap = ap[:, 0:n]
        if len(shape) == 3:
            ap = ap.rearrange("p (a b) -> p a b", a=shape[1])
        elif len(shape) == 4:
            ap = ap.rearrange("p (a b c) -> p a b c", a=shape[1], b=shape[2])
        elif len(shape) == 5:
            ap = ap.rearrange("p (a b c d) -> p a b c d", a=shape[1], b=shape[2], c=shape[3])
        if shape[0] != 128:
            ap = ap[0:shape[0]]
        return Buf(ap, name)

    def phase_reset(self):
        self.off = self.persist

    def op(self, eng, fn, r=(), w=()):
        return self.S.op(eng, fn, [b.t for b in r], [b.t for b in w])

    def dma(self, key, out, in_, r=(), w=(), q="sp"):
        return self.S.dma(q, key, out, in_, [b.t for b in r], [b.t for b in w])

    def mm(self, pbuf, out, lhsT, rhs, start, stop, r):
        self.op("pe", lambda e: e.matmul(out, lhsT=lhsT, rhs=rhs, start=start, stop=stop), r, [pbuf])

    def tr(self, pbuf, out, in_, ident, r):
        self.op("pe", lambda e: e.transpose(out=out, in_=in_, identity=ident), r, [pbuf])

    def tt(self, eng, out, a, b, op, r, w):
        self.op(eng, lambda e: e.tensor_tensor(out=out, in0=a, in1=b, op=op), r, w)

    def ts(self, eng, out, a, s1, s2, op0, op1, r, w):
        if s2 is None:
            self.op(eng, lambda e: e.tensor_scalar(out=out, in0=a, scalar1=s1, scalar2=None, op0=op0), r, w)
        else:
            self.op(eng, lambda e: e.tensor_scalar(out=out, in0=a, scalar1=s1, scalar2=s2, op0=op0, op1=op1), r, w)

    def stt(self, eng, out, a, s, b, op0, op1, r, w):
        self.op(eng, lambda e: e.scalar_tensor_tensor(out=out, in0=a, scalar=s, in1=b, op0=op0, op1=op1), r, w)

    def cp(self, eng, out, a, r, w):
        if eng == "act":
            self.op("act", lambda e: e.activation(out=out, in_=a, func=AF.Copy), r, w)
        else:
            self.op(eng, lambda e: e.tensor_copy(out=out, in_=a), r, w)

    def act(self, out, a, func, r, w, bias=None, scale=None, accum=None):
        kw = {}
        if bias is not None:
            kw["bias"] = bias
        if scale is not None:
            kw["scale"] = scale
        if accum is not None:
            kw["accum_out"] = accum
        self.op("act", lambda e: e.activation(out=out, in_=a, func=func, **kw), r, w)

    def memset(self, eng, out, val, w):
        self.op(eng, lambda e: e.memset(out, val), (), w)

    def barrier(self):
        S = self.S
        self.nbar += 1
        k = self.nbar
        for e in S.ENGS:
            S.ops[e].append(dict(barrier=k, fn=None, deps=[], dma=None, signal=False,
                                 dcnt=dict(S.dma_cnt)))


def build_program(stop_after=None, debug=()):
    nc = bass.Bass("TRN2", target_bir_lowering=False)
    kb = KB(nc)
    K = kb

    def din(name, shape, dt=F32):
        return nc.dram_tensor(name, list(shape), dt, kind="ExternalInput").ap()

    def dscr(name, shape, dt):
        kind = "ExternalOutput" if name in debug else "Internal"
        return nc.dram_tensor(name, list(shape), dt, kind=kind).ap()

    xc = din("xc", [NT_LOC, 1024])
    pc = din("pc", [NT_OWN, 256])
    posc = din("posc", [1, NT_LOC], I32)
    validc = din("validc", [1, 512])
    ownc = din("ownc", [1, 512])
    ident_d = din("ident", [128, 128])
    iota_d = din("iota", [128, 128])
    invf_d = din("invf", [128, 2])
    koh_d = din("koh", [32, NT_LOC])
    cm_d = din("cmask", [4, 128, 512])
    g_mix = din("g_mix", [1, 1024])
    w_in = din("w_in", [1024, 4096])
    w_perm = din("w_perm", [1024, 1024])
    s5 = {n: din("s5_" + n, shp) for n, shp in [
        ("ldt", [128, 16]), ("are", [128, 16]), ("aim", [128, 16]),
        ("bre", [128, 256]), ("bim", [128, 256]), ("cre", [128, 256]), ("cim", [128, 256])]}
    ssm_d = din("ssm_d", [1, 512])
    w_glu = din("w_glu", [512, 512])
    w_ps = din("w_ps", [512, 1024])
    w_pa = din("w_pa", [512, 1024])
    w_out = din("w_out", [1024, 1024])
    g_ffn = din("g_ffn", [1, 1024])
    w_q = din("w_q", [1024, 2048])
    keys = din("keys", [16, 128, 128])
    peer_u = din("peer_u", [16384, 1024])
    peer_v = din("peer_v", [16384, 1024])
    g_ple = din("g_ple", [1, 1024])
    w_pg = din("w_pg", [1024, 1024])
    w_pp = din("w_pp", [256, 1024])
    g_fin = din("g_fin", [1, 1024])
    out = nc.dram_tensor("out", [NT_OWN, 1024], F32, kind="ExternalOutput").ap()

    qT_s = dscr("qT_s", [4, 128, NT_OWN], BF16)
    kT_s = dscr("kT_s", [4, 128, NT_LOC], BF16)
    v_s = dscr("v_s", [NT_LOC, 8 * 65], BF16)
    gT_s = dscr("gT_s", [16, 128, NT_OWN], BF16)
    ssmT_s = dscr("ssmT_s", [4, 128, NT_OWN], BF16)
    attT_s = dscr("attT_s", [4, 128, NT_OWN], BF16)
    x1_s = dscr("x1_s", [NT_OWN, 1024], F32)
    h2T_s = dscr("h2T_s", [8, 128, NT_OWN], BF16)
    sc_s = dscr("sc_s", [NT_OWN, 16 * 128], F32)
    G_s = dscr("G_s", [32, 128, 128, 128], BF16)
    x2_s = dscr("x2_s", [NT_OWN, 1024], F32)
    UT5_s = dscr("UT5_s", [8, 128, 32 * 128], BF16)
    UT_s = dscr("UT_s", [128, 128, 1024], BF16)

    from contextlib import ExitStack
    st = ExitStack()
    PS = []
    for i in range(8):
        t = st.enter_context(nc.psum_tensor("ps%d" % i, [128, 512], F32))
        PS.append(Buf(t[:], "ps%d" % i))
    psi = [0]

    def nps():
        b = PS[psi[0] % 8]
        psi[0] += 1
        return b

    idf = K.sb([128, 128], F32, "idf")
    idb = K.sb([128, 128], BF16, "idb")
    K.dma("c0", idf.ap, ident_d, w=[idf])
    K.cp("dve", idb.ap, idf.ap, [idf], [idb])
    ksum = K.sb([128, 4, 32], F32, "ksum")
    K.persist = K.off

    ut5tok = [Buf(None, "ut5_%d" % i) for i in range(8)]

    def rmsnorm_tile(xt, gt, hb, sq, ss, rs):
        K.act(sq.ap, xt.ap, AF.Square, [xt], [sq, ss], accum=ss.ap)
        K.act(rs.ap, ss.ap, AF.Sqrt, [ss], [rs], scale=1.0 / 1024, bias=eps_b.ap)
        K.op("dve", lambda e: e.reciprocal(out=rs.ap, in_=rs.ap), [rs], [rs])
        K.stt("dve", hb.ap, xt.ap, rs.ap[:, 0:1], gt.ap, ALU.mult, ALU.mult, [xt, rs, gt], [hb])

    def load_w_bf(dst, src_ap, rows_kc, ncols, stage, key):
        for kc in range(rows_kc):
            K.dma(key, stage.ap[:, 0:ncols], src_ap[kc * 128:(kc + 1) * 128, :], w=[stage])
            K.cp("dve" if kc % 2 == 0 else "act", dst.ap[:, kc, :], stage.ap[:, 0:ncols], [stage], [dst])

    eps_b = K.sb([128, 1], F32, "eps")
    K.memset("dve", eps_b.ap, 1e-6, [eps_b])
    K.persist = K.off

    def phase_A():
        K.phase_reset()
        win = K.sb([128, 8, 3584], BF16, "win")
        wu = K.sb([128, 8, 512], BF16, "wuA")
        Ustk = K.sb([128, 32, 8, 16], BF16, "UstkA")
        UTo = K.sb([128, 32, 128], BF16, "UToA")
        wpm = K.sb([128, 8, 1024], BF16, "wpm")
        stageA = K.sb([128, 1792], F32, "stageA")
        stageB = K.sb([128, 1792], F32, "stageB")
        stage = stageA
        gt = K.sb([128, 1024], F32, "gmix")
        invf = K.sb([128, 2], F32, "invf")
        xts = [K.sb([128, 1024], F32, "xt%d" % i) for i in range(2)]
        sq = K.sb([128, 1024], BF16, "sq")
        ss = K.sb([128, 1], F32, "ss")
        rs = K.sb([128, 1], F32, "rs")
        hbs = [K.sb([128, 1024], BF16, "hb%d" % i) for i in range(2)]
        hTs = [K.sb([128, 8, 1024], BF16, "hT%d" % i) for i in range(2)]
        posi = K.sb([128, 1024], I32, "posi")
        ang = K.sb([128, 1024], F32, "ang")
        tmpa = K.sb([128, 1024], F32, "tmpa")
        tmpi = K.sb([128, 1024], I32, "tmpi")
        cosTs = [K.sb([128, 1024], F32, "cosT%d" % i) for i in range(2)]
        sinTs = [K.sb([128, 1024], F32, "sinT%d" % i) for i in range(2)]
        t1s = [K.sb([128, 512], F32, "t1_%d" % i) for i in range(2)]
        t2s = [K.sb([128, 512], F32, "t2_%d" % i) for i in range(2)]
        obf = [K.sb([128, 512], BF16, "obf%d" % i) for i in range(2)]
        vts = [K.sb([128, 8, 65], BF16, "vt%d" % i) for i in range(2)]
        for v in vts:
            K.memset("pool", v.ap, 1.0, [v])
        K.dma("c0", gt.ap, g_mix.partition_broadcast(128), w=[gt])
        K.dma("c0", invf.ap, invf_d, w=[invf])
        n_st = [0]

        def stream_w(dst_ap, src_ap, ncols):
            i = n_st[0]
            n_st[0] += 1
            stg_ = (stageA, stageB)[i % 2]
            K.dma("wst%d" % (i % 2), stg_.ap[:, 0:ncols], src_ap, w=[stg_])
            K.cp("dve" if i % 2 == 0 else "act", dst_ap, stg_.ap[:, 0:ncols], [stg_], [win])

        for kc in range(8):
            stream_w(win.ap[:, kc, 0:1792], w_in[kc * 128:(kc + 1) * 128, 512:2304], 1792)
            stream_w(win.ap[:, kc, 1792:3584], w_in[kc * 128:(kc + 1) * 128, 2304:4096], 1792)
        for kc in range(8):
            i = n_st[0]
            n_st[0] += 1
            stg_ = (stageA, stageB)[i % 2]
            K.dma("wst%d" % (i % 2), stg_.ap[:, 0:1024], w_perm[kc * 128:(kc + 1) * 128, :], w=[stg_])
            K.cp("dve" if i % 2 == 0 else "act", wpm.ap[:, kc, :], stg_.ap[:, 0:1024], [stg_], [wpm])
        for kc in range(8):
            i = n_st[0]
            n_st[0] += 1
            stg_ = (stageA, stageB)[i % 2]
            K.dma("wst%d" % (i % 2), stg_.ap[:, 0:512], w_in[kc * 128:(kc + 1) * 128, 0:512], w=[stg_])
            K.cp("dve" if i % 2 == 0 else "act", wu.ap[:, kc, :], stg_.ap[:, 0:512], [stg_], [wu])

        def sincos(dst, phase):
            K.ts("dve", tmpa.ap, ang.ap, phase, 1.0 / (2 * PI), ALU.add, ALU.mult, [ang], [tmpa])
            K.cp("dve", tmpi.ap, tmpa.ap, [tmpa], [tmpi])
            K.cp("dve", tmpa.ap, tmpi.ap, [tmpi], [tmpa])
            K.stt("dve", tmpa.ap, tmpa.ap, -2 * PI, ang.ap, ALU.mult, ALU.add, [tmpa, ang], [tmpa])
            K.ts("dve", tmpa.ap, tmpa.ap, phase, None, ALU.add, None, [tmpa], [tmpa])
            K.ts("dve", dst.ap, tmpa.ap, PI, -2 * PI, ALU.is_gt, ALU.mult, [tmpa], [dst])
            K.tt("dve", tmpa.ap, tmpa.ap, dst.ap, ALU.add, [tmpa, dst], [tmpa])
            K.ts("dve", dst.ap, tmpa.ap, -PI, 2 * PI, ALU.is_lt, ALU.mult, [tmpa], [dst])
            K.tt("dve", tmpa.ap, tmpa.ap, dst.ap, ALU.add, [tmpa, dst], [tmpa])
            K.act(dst.ap, tmpa.ap, AF.Sin, [tmpa], [dst])

        xic = [0]

        def prep(blk):
            tb = blk * 1024
            hT = hTs[blk % 2]
            cosT, sinT = cosTs[blk % 2], sinTs[blk % 2]
            xi = xic[0]
            K.dma("pos", posi.ap, posc[:, tb:tb + 1024].partition_broadcast(128), w=[posi])
            K.cp("dve", ang.ap, posi.ap, [posi], [ang])
            K.ts("dve", ang.ap, ang.ap, invf.ap[:, 0:1], None, ALU.mult, None, [ang, invf], [ang])
            sincos(cosT, PI / 2)
            sincos(sinT, 0.0)
            K.ts("dve", sinT.ap, sinT.ap, invf.ap[:, 1:2], None, ALU.mult, None, [sinT, invf], [sinT])
            for ti in range(8):
                xt = xts[xi % 2]
                hb = hbs[xi % 2]
                xi += 1
                K.dma("x%d" % (xi % 2), xt.ap, xc[tb + ti * 128: tb + (ti + 1) * 128, :], w=[xt])
                rmsnorm_tile(xt, gt, hb, sq, ss, rs)

                def a_tr(ti=ti, hb=hb, hT=hT):
                    p = nps()
                    pv = p.ap.bitcast(BF16).rearrange("p (a b) -> p a b", a=8)
                    for kc in range(8):
                        K.tr(p, pv[:, kc, :], hb.ap[:, kc * 128:(kc + 1) * 128], idb.ap, [hb, idb])
                    K.cp("act", hT.ap[:, :, ti * 128:(ti + 1) * 128], pv, [p], [hT])

                if ti > 0:
                    pend_a()
                pend_a = a_tr
            pend_a()
            xic[0] = xi

        oic = [0]

        def compute(blk):
            own = blk >= 4
            tb = blk * 1024
            hT = hTs[blk % 2]
            cosT, sinT = cosTs[blk % 2], sinTs[blk % 2]
            oi = oic[0]
            for j in range(8):
                p = nps()
                for kc in range(8):
                    K.mm(p, p.ap, hT.ap[:, kc, j:1024:8], wu.ap[:, kc, :], kc == 0, kc == 7, [hT, wu])
                K.cp("act" if j % 2 else "dve", Ustk.ap[:, :, j, :], p.ap.rearrange("p (g c) -> p g c", g=32), [p], [Ustk])
            for g0 in range(0, 32, 8):
                p = nps()
                pv = p.ap.bitcast(BF16).rearrange("p (a b) -> p a b", a=8)
                for gi in range(8):
                    K.tr(p, pv[:, gi, :], Ustk.ap[:, g0 + gi, :, :].rearrange("p a b -> p (a b)"), idb.ap, [Ustk, idb])
                K.cp("act", UTo.ap[:, g0:g0 + 8, :], pv, [p], [UTo])
            K.dma("utA", UT5_s[blk], UTo.ap.rearrange("p a b -> p (a b)"), r=[UTo], w=[ut5tok[blk]])
            for which in (["q", "k"] if own else ["k"]):
                cbase = 0 if which == "q" else 512
                for c in range(4):
                    for half in range(2):
                        pa = nps()
                        pb = nps()
                        for kc in range(8):
                            K.mm(pa, pa.ap, win.ap[:, kc, cbase + c * 128: cbase + (c + 1) * 128],
                                 hT.ap[:, kc, half * 512:(half + 1) * 512], kc == 0, kc == 7, [win, hT])
                        for kc in range(8):
                            K.mm(pb, pb.ap, wpm.ap[:, kc, cbase + c * 128: cbase + (c + 1) * 128],
                                 hT.ap[:, kc, half * 512:(half + 1) * 512], kc == 0, kc == 7, [wpm, hT])
                        t1, t2 = t1s[oi % 2], t2s[oi % 2]
                        K.tt("dve", t1.ap, pa.ap, cosT.ap[:, half * 512:(half + 1) * 512], ALU.mult, [pa, cosT], [t1])
                        K.tt("dve", t2.ap, pb.ap, sinT.ap[:, half * 512:(half + 1) * 512], ALU.mult, [pb, sinT], [t2])
                        K.tt("dve", t1.ap, t1.ap, t2.ap, ALU.add, [t1, t2], [t1])
                        ob = obf[oi % 2]
                        oi += 1
                        K.cp("act", ob.ap, t1.ap, [t1], [ob])
                        t0 = tb + half * 512
                        if which == "q":
                            K.dma("oq%d" % (oi % 2), qT_s[c, :, t0 - NT_OWN: t0 - NT_OWN + 512], ob.ap, r=[ob])
                        else:
                            K.dma("oq%d" % (oi % 2), kT_s[c, :, t0: t0 + 512], ob.ap, r=[ob])
                            K.op("dve", lambda e, c=c, b0=t0 // 256, t1=t1: e.tensor_reduce(
                                out=ksum.ap[:, c, b0:b0 + 2], in_=t1.ap.rearrange("p (a b) -> p a b", a=2),
                                axis=AX.X, op=ALU.add), [t1], [ksum])
            for ti in range(8):
                p = nps()
                for kc in range(8):
                    K.mm(p, p.ap, hT.ap[:, kc, ti * 128:(ti + 1) * 128], win.ap[:, kc, 1024:1536],
                         kc == 0, kc == 7, [hT, win])
                vt = vts[ti % 2]
                K.cp("act", vt.ap[:, :, 0:64], p.ap.rearrange("p (h d) -> p h d", h=8), [p], [vt])
                K.dma("ov%d" % (ti % 2), v_s[tb + ti * 128: tb + (ti + 1) * 128, :].rearrange("p (h d) -> p h d", h=8),
                      vt.ap, r=[vt])
            if own:
                for c in range(16):
                    for half in range(2):
                        p = nps()
                        for kc in range(8):
                            K.mm(p, p.ap, win.ap[:, kc, 1536 + c * 128: 1536 + (c + 1) * 128],
                                 hT.ap[:, kc, half * 512:(half + 1) * 512], kc == 0, kc == 7, [win, hT])
                        ob = obf[oi % 2]
                        oi += 1
                        K.act(ob.ap, p.ap, AF.Sigmoid, [p], [ob])
                        t0 = tb + half * 512 - NT_OWN
                        K.dma("oq%d" % (oi % 2), gT_s[c, :, t0:t0 + 512], ob.ap, r=[ob])
            oic[0] = oi

        prep(0)
        for blk in range(8):
            if blk + 1 < 8:
                prep(blk + 1)
            compute(blk)
        K.barrier()

    def phase_S():
        K.phase_reset()
        sm = lambda name, n=16: K.sb([128, n], F32, name)
        wglu = K.sb([128, 4, 512], BF16, "wglu")
        WS = K.sb([128, 16, 2, 2, 128], BF16, "WS")
        WY1 = K.sb([128, 16, 2, 2, 128], BF16, "WY1")
        WY2 = K.sb([128, 32, 128], BF16, "WY2")
        Ct = K.sb([128, 16, 128], F32, "Ct")
        St = K.sb([128, 16, 128], F32, "St")
        r8 = sm("r8")
        Dre = sm("Dre")
        Dim = sm("Dim")
        car_re = sm("car_re")
        car_im = sm("car_im")
        ta, tb_, tc_ = sm("ta"), sm("tb"), sm("tc")
        mark = K.off
        stage = K.sb([128, 512], F32, "stageS")
        ldt, are, aim = sm("ldt"), sm("are"), sm("aim")
        bre = K.sb([128, 16, 16], F32, "bre")
        bim = K.sb([128, 16, 16], F32, "bim")
        cre = K.sb([128, 16, 16], F32, "cre")
        cim = K.sb([128, 16, 16], F32, "cim")
        ncim = K.sb([128, 16, 16], F32, "ncim")
        for t_, nm in ((ldt, "ldt"), (are, "are"), (aim, "aim")):
            K.dma("c0", t_.ap, s5[nm], w=[t_])
        for t_, nm in ((bre, "bre"), (bim, "bim"), (cre, "cre"), (cim, "cim")):
            K.dma("c0", t_.ap.rearrange("p a b -> p (a b)"), s5[nm], w=[t_])
        for kc in range(4):
            K.dma("wst", stage.ap, w_glu[kc * 128:(kc + 1) * 128, :], w=[stage])
            K.cp("dve", wglu.ap[:, kc, :], stage.ap, [stage], [wglu])
        dt_, xr, th, mag, cs, sn = sm("dt"), sm("xr"), sm("th"), sm("mag"), sm("cs"), sm("sn")
        abr, abi, den, nr, fre, fim = sm("abr"), sm("abi"), sm("den"), sm("nr"), sm("fre"), sm("fim")
        ti_ = K.sb([128, 16], I32, "ti")
        V_ = "dve"
        K.act(dt_.ap, ldt.ap, AF.Exp, [ldt], [dt_])
        K.tt(V_, xr.ap, dt_.ap, are.ap, ALU.mult, [dt_, are], [xr])
        K.tt(V_, th.ap, dt_.ap, aim.ap, ALU.mult, [dt_, aim], [th])
        K.act(mag.ap, xr.ap, AF.Exp, [xr], [mag])
        K.act(r8.ap, xr.ap, AF.Exp, [xr], [r8], scale=8.0)

        def sin_small(dst, src, phase):
            K.ts(V_, ta.ap, src.ap, phase, 1.0 / (2 * PI), ALU.add, ALU.mult, [src], [ta])
            K.cp(V_, ti_.ap, ta.ap, [ta], [ti_])
            K.cp(V_, ta.ap, ti_.ap, [ti_], [ta])
            K.stt(V_, ta.ap, ta.ap, -2 * PI, src.ap, ALU.mult, ALU.add, [ta, src], [ta])
            K.ts(V_, ta.ap, ta.ap, phase, None, ALU.add, None, [ta], [ta])
            K.ts(V_, tb_.ap, ta.ap, PI, -2 * PI, ALU.is_gt, ALU.mult, [ta], [tb_])
            K.tt(V_, ta.ap, ta.ap, tb_.ap, ALU.add, [ta, tb_], [ta])
            K.ts(V_, tb_.ap, ta.ap, -PI, 2 * PI, ALU.is_lt, ALU.mult, [ta], [tb_])
            K.tt(V_, ta.ap, ta.ap, tb_.ap, ALU.add, [ta, tb_], [ta])
            K.act(dst.ap, ta.ap, AF.Sin, [ta], [dst])

        sin_small(cs, th, PI / 2)
        sin_small(sn, th, 0.0)
        K.tt(V_, abr.ap, mag.ap, cs.ap, ALU.mult, [mag, cs], [abr])
        K.tt(V_, abi.ap, mag.ap, sn.ap, ALU.mult, [mag, sn], [abi])
        K.tt(V_, den.ap, are.ap, are.ap, ALU.mult, [are], [den])
        K.tt(V_, ta.ap, aim.ap, aim.ap, ALU.mult, [aim], [ta])
        K.tt(V_, den.ap, den.ap, ta.ap, ALU.add, [den, ta], [den])
        K.op(V_, lambda e: e.reciprocal(out=den.ap, in_=den.ap), [den], [den])
        K.ts(V_, nr.ap, abr.ap, -1.0, None, ALU.add, None, [abr], [nr])
        K.tt(V_, ta.ap, nr.ap, are.ap, ALU.mult, [nr, are], [ta])
        K.tt(V_, tb_.ap, abi.ap, aim.ap, ALU.mult, [abi, aim], [tb_])
        K.tt(V_, ta.ap, ta.ap, tb_.ap, ALU.add, [ta, tb_], [ta])
        K.tt(V_, fre.ap, ta.ap, den.ap, ALU.mult, [ta, den], [fre])
        K.tt(V_, ta.ap, abi.ap, are.ap, ALU.mult, [abi, are], [ta])
        K.tt(V_, tb_.ap, nr.ap, aim.ap, ALU.mult, [nr, aim], [tb_])
        K.tt(V_, ta.ap, ta.ap, tb_.ap, ALU.subtract, [ta, tb_], [ta])
        K.tt(V_, fim.ap, ta.ap, den.ap, ALU.mult, [ta, den], [fim])
        pwf_re = K.sb([128, 16, 9], F32, "pwf_re")
        pwf_im = K.sb([128, 16, 9], F32, "pwf_im")
        pwr_re = K.sb([128, 16, 8], F32, "pwr_re")
        pwr_im = K.sb([128, 16, 8], F32, "pwr_im")
        K.memset(V_, pwf_re.ap[:, :, 0], 1.0, [pwf_re])
        K.memset(V_, pwf_im.ap[:, :, 0], 0.0, [pwf_im])
        for d in range(8):
            K.tt(V_, ta.ap, pwf_re.ap[:, :, d], abr.ap, ALU.mult, [pwf_re, abr], [ta])
            K.tt(V_, tb_.ap, pwf_im.ap[:, :, d], abi.ap, ALU.mult, [pwf_im, abi], [tb_])
            K.tt(V_, pwf_re.ap[:, :, d + 1], ta.ap, tb_.ap, ALU.subtract, [ta, tb_], [pwf_re])
            K.tt(V_, ta.ap, pwf_re.ap[:, :, d], abi.ap, ALU.mult, [pwf_re, abi], [ta])
            K.tt(V_, tb_.ap, pwf_im.ap[:, :, d], abr.ap, ALU.mult, [pwf_im, abr], [tb_])
            K.tt(V_, pwf_im.ap[:, :, d + 1], ta.ap, tb_.ap, ALU.add, [ta, tb_], [pwf_im])
        for j in range(8):
            K.cp(V_, pwr_re.ap[:, :, j], pwf_re.ap[:, :, 7 - j], [pwf_re], [pwr_re])
            K.cp(V_, pwr_im.ap[:, :, j], pwf_im.ap[:, :, 7 - j], [pwf_im], [pwr_im])
        K.cp(V_, Dre.ap, pwf_re.ap[:, :, 8], [pwf_re], [Dre])
        K.cp(V_, Dim.ap, pwf_im.ap[:, :, 8], [pwf_im], [Dim])
        ur, ui, rr = sm("ur"), sm("ui"), sm("rr")
        K.op(V_, lambda e: e.reciprocal(out=rr.ap, in_=r8.ap), [r8], [rr])
        K.tt(V_, ur.ap, Dre.ap, rr.ap, ALU.mult, [Dre, rr], [ur])
        K.tt(V_, ui.ap, Dim.ap, rr.ap, ALU.mult, [Dim, rr], [ui])
        tm1 = K.sb([128, 16, 64], F32, "tm1")
        tm2 = K.sb([128, 16, 64], F32, "tm2")
        K.memset(V_, Ct.ap[:, :, 0], 1.0, [Ct])
        K.memset(V_, St.ap[:, :, 0], 0.0, [St])
        for k in range(7):
            n = 1 << k
            urb = ur.ap.unsqueeze(2).broadcast_to([128, 16, n])
            uib = ui.ap.unsqueeze(2).broadcast_to([128, 16, n])
            K.tt(V_, tm1.ap[:, :, 0:n], Ct.ap[:, :, 0:n], urb, ALU.mult, [Ct, ur], [tm1])
            K.tt(V_, tm2.ap[:, :, 0:n], St.ap[:, :, 0:n], uib, ALU.mult, [St, ui], [tm2])
            K.tt(V_, Ct.ap[:, :, n:2 * n], tm1.ap[:, :, 0:n], tm2.ap[:, :, 0:n], ALU.subtract, [tm1, tm2], [Ct])
            K.tt(V_, tm1.ap[:, :, 0:n], Ct.ap[:, :, 0:n], uib, ALU.mult, [Ct, ui], [tm1])
            K.tt(V_, tm2.ap[:, :, 0:n], St.ap[:, :, 0:n], urb, ALU.mult, [St, ur], [tm2])
            K.tt(V_, St.ap[:, :, n:2 * n], tm1.ap[:, :, 0:n], tm2.ap[:, :, 0:n], ALU.add, [tm1, tm2], [St])
            K.tt(V_, ta.ap, ur.ap, ur.ap, ALU.mult, [ur], [ta])
            K.tt(V_, tb_.ap, ui.ap, ui.ap, ALU.mult, [ui], [tb_])
            K.tt(V_, tc_.ap, ur.ap, ui.ap, ALU.mult, [ur, ui], [tc_])
            K.tt(V_, ur.ap, ta.ap, tb_.ap, ALU.subtract, [ta, tb_], [ur])
            K.ts(V_, ui.ap, tc_.ap, 2.0, None, ALU.mult, None, [tc_], [ui])
        bbr = K.sb([128, 16, 16], F32, "bbr")
        bbi = K.sb([128, 16, 16], F32, "bbi")
        t3a = K.sb([128, 16, 16], F32, "t3a")
        t3b = K.sb([128, 16, 16], F32, "t3b")
        freb = fre.ap.unsqueeze(2).broadcast_to([128, 16, 16])
        fimb = fim.ap.unsqueeze(2).broadcast_to([128, 16, 16])
        K.tt(V_, t3a.ap, bre.ap, freb, ALU.mult, [bre, fre], [t3a])
        K.tt(V_, t3b.ap, bim.ap, fimb, ALU.mult, [bim, fim], [t3b])
        K.tt(V_, bbr.ap, t3a.ap, t3b.ap, ALU.subtract, [t3a, t3b], [bbr])
        K.tt(V_, t3a.ap, bim.ap, freb, ALU.mult, [bim, fre], [t3a])
        K.tt(V_, t3b.ap, bre.ap, fimb, ALU.mult, [bre, fim], [t3b])
        K.tt(V_, bbi.ap, t3a.ap, t3b.ap, ALU.add, [t3a, t3b], [bbi])
        K.ts(V_, ncim.ap, cim.ap, -1.0, None, ALU.mult, None, [cim], [ncim])
        Fre = K.sb([128, 16, 15, 16], F32, "Fre")
        Fim = K.sb([128, 16, 15, 16], F32, "Fim")
        t4a = K.sb([128, 16, 8, 16], F32, "t4a")
        t4b = K.sb([128, 16, 8, 16], F32, "t4b")
        K.memset("pool", Fre.ap, 0.0, [Fre])
        K.memset("pool", Fim.ap, 0.0, [Fim])
        S4 = [128, 16, 8, 16]
        prb = pwr_re.ap.unsqueeze(3).broadcast_to(S4)
        pib = pwr_im.ap.unsqueeze(3).broadcast_to(S4)
        bbrb = bbr.ap.unsqueeze(2).broadcast_to(S4)
        bbib = bbi.ap.unsqueeze(2).broadcast_to(S4)
        K.tt(V_, t4a.ap, prb, bbrb, ALU.mult, [pwr_re, bbr], [t4a])
        K.tt(V_, t4b.ap, pib, bbib, ALU.mult, [pwr_im, bbi], [t4b])
        K.tt(V_, Fre.ap[:, :, 0:8, :], t4a.ap, t4b.ap, ALU.subtract, [t4a, t4b], [Fre])
        K.tt(V_, t4a.ap, prb, bbib, ALU.mult, [pwr_re, bbi], [t4a])
        K.tt(V_, t4b.ap, pib, bbrb, ALU.mult, [pwr_im, bbr], [t4b])
        K.tt(V_, Fim.ap[:, :, 0:8, :], t4a.ap, t4b.ap, ALU.add, [t4a, t4b], [Fim])
        K.memset("pool", WS.ap, 0.0, [WS])
        K.memset("pool", WY1.ap, 0.0, [WY1])
        for p_ in range(16):
            for ri, Ft in ((0, Fre), (1, Fim)):
                ps = nps()
                K.tr(ps, ps.ap[:, 0:128], Ft.ap[:, p_, 0:8, :].rearrange("p a b -> p (a b)"), idf.ap, [Ft, idf])
                K.cp(V_, WS.ap[:, p_, ri, 0, 0:64], ps.ap[:, 0:64], [ps], [WS])
                K.cp(V_, WS.ap[:, p_, ri, 1, 64:128], ps.ap[:, 64:128], [ps], [WS])
        pfr = pwf_re.ap[:, :, 1:9].unsqueeze(3).broadcast_to(S4)
        pfi = pwf_im.ap[:, :, 1:9].unsqueeze(3).broadcast_to(S4)
        creb = cre.ap.unsqueeze(2).broadcast_to(S4)
        cimb = cim.ap.unsqueeze(2).broadcast_to(S4)
        K.tt(V_, t4a.ap, creb, pfr, ALU.mult, [cre, pwf_re], [t4a])
        K.tt(V_, t4b.ap, cimb, pfi, ALU.mult, [cim, pwf_im], [t4b])
        K.tt(V_, t4a.ap, t4a.ap, t4b.ap, ALU.subtract, [t4a, t4b], [t4a])
        for g2 in range(2):
            K.cp(V_, WY1.ap[g2 * 64:(g2 + 1) * 64, :, 0, g2, :],
                 t4a.ap[g2 * 64:(g2 + 1) * 64].rearrange("p a b c -> p a (b c)"), [t4a], [WY1])
        K.tt(V_, t4a.ap, creb, pfi, ALU.mult, [cre, pwf_im], [t4a])
        K.tt(V_, t4b.ap, cimb, pfr, ALU.mult, [cim, pwf_re], [t4b])
        K.tt(V_, t4a.ap, t4a.ap, t4b.ap, ALU.add, [t4a, t4b], [t4a])
        K.ts(V_, t4a.ap, t4a.ap, -1.0, None, ALU.mult, None, [t4a], [t4a])
        for g2 in range(2):
            K.cp(V_, WY1.ap[g2 * 64:(g2 + 1) * 64, :, 1, g2, :],
                 t4a.ap[g2 * 64:(g2 + 1) * 64].rearrange("p a b c -> p a (b c)"), [t4a], [WY1])
        dB = K.sb([128, 512], F32, "dB")
        dI = K.sb([128, 32, 8, 16], F32, "dI")
        K.dma("c0", dB.ap, ssm_d.partition_broadcast(128), w=[dB])
        K.tt(V_, dI.ap, idf.ap.rearrange("p (a b) -> p a b", a=8).unsqueeze(1).broadcast_to([128, 32, 8, 16]),
             dB.ap.rearrange("p (g c) -> p g c", g=32).unsqueeze(2).broadcast_to([128, 32, 8, 16]), ALU.mult,
             [idf, dB], [dI])
        for g in range(32):
            p_, g2 = g // 2, g % 2
            if g % 4 == 0:
                ps = nps()
            o0 = (g % 4) * 128
            sl = slice(g2 * 64, (g2 + 1) * 64)
            for j in range(8):
                oap = ps.ap[:, o0 + j * 16: o0 + (j + 1) * 16]
                K.mm(ps, oap, Fre.ap[sl, p_, 7 - j:15 - j, :].rearrange("p a b -> p (a b)"), cre.ap[sl, p_, :],
                     True, False, [Fre, cre])
                K.mm(ps, oap, Fim.ap[sl, p_, 7 - j:15 - j, :].rearrange("p a b -> p (a b)"), ncim.ap[sl, p_, :],
                     False, True, [Fim, ncim])
            if g % 4 == 3:
                K.tt(V_, WY2.ap[:, g - 3:g + 1, :], ps.ap.rearrange("p (g k) -> p g k", g=4),
                     dI.ap[:, g - 3:g + 1].rearrange("p g a b -> p g (a b)"), ALU.add, [ps, dI], [WY2])
        K.memset(V_, car_re.ap, 0.0, [car_re])
        K.memset(V_, car_im.ap, 0.0, [car_im])
        K.barrier()
        K.off = mark
        UTs = [K.sb([128, 32, 128], BF16, "UT%d" % i) for i in range(2)]
        Sres = [K.sb([128, 16, 128], F32, "Sre%d" % i) for i in range(2)]
        Sims = [K.sb([128, 16, 128], F32, "Sim%d" % i) for i in range(2)]
        gre = K.sb([128, 16, 128], F32, "gre")
        gim = K.sb([128, 16, 128], F32, "gim")
        u1 = K.sb([128, 16, 128], F32, "u1")
        u2 = K.sb([128, 16, 128], F32, "u2")
        Pre = K.sb([128, 16, 129], BF16, "Pre")
        Pim = K.sb([128, 16, 129], BF16, "Pim")
        ytm = K.sb([128, 8, 512], BF16, "ytm")
        yT = K.sb([128, 4, 1024], BF16, "yT")
        sg = K.sb([128, 512], BF16, "sg")
        sos = [K.sb([128, 4, 512], BF16, "so%d" % i) for i in range(2)]

        def S1(blk):
            UT, Sre, Sim = UTs[blk % 2], Sres[blk % 2], Sims[blk % 2]
            K.dma("ut%d" % (blk % 2), UT.ap.rearrange("p a b -> p (a b)"), UT5_s[blk], r=[ut5tok[blk]], w=[UT])
            for ri, Sx in ((0, Sre), (1, Sim)):
                for p0 in range(0, 16, 4):
                    p = nps()
                    for pi_ in range(4):
                        pp = p0 + pi_
                        K.mm(p, p.ap[:, pi_ * 128:(pi_ + 1) * 128], WS.ap[:, pp, ri, 0, :], UT.ap[:, 2 * pp, :], True, False, [WS, UT])
                        K.mm(p, p.ap[:, pi_ * 128:(pi_ + 1) * 128], WS.ap[:, pp, ri, 1, :], UT.ap[:, 2 * pp + 1, :], False, True, [WS, UT])
                    K.cp("act", Sx.ap[:, p0:p0 + 4, :], p.ap.rearrange("p (a b) -> p a b", a=4), [p], [Sx])

        def SC(blk):
            own = blk >= 4
            Sre, Sim = Sres[blk % 2], Sims[blk % 2]
            if own:
                K.cp(V_, Pre.ap[:, :, 0], car_re.ap, [car_re], [Pre])
                K.cp(V_, Pim.ap[:, :, 0], car_im.ap, [car_im], [Pim])
            K.tt(V_, ta.ap, Dre.ap, car_re.ap, ALU.mult, [Dre, car_re], [ta])
            K.tt(V_, tb_.ap, Dim.ap, car_im.ap, ALU.mult, [Dim, car_im], [tb_])
            K.tt(V_, ta.ap, ta.ap, tb_.ap, ALU.subtract, [ta, tb_], [ta])
            K.tt(V_, Sre.ap[:, :, 0], Sre.ap[:, :, 0], ta.ap, ALU.add, [Sre, ta], [Sre])
            K.tt(V_, ta.ap, Dre.ap, car_im.ap, ALU.mult, [Dre, car_im], [ta])
            K.tt(V_, tb_.ap, Dim.ap, car_re.ap, ALU.mult, [Dim, car_re], [tb_])
            K.tt(V_, ta.ap, ta.ap, tb_.ap, ALU.add, [ta, tb_], [ta])
            K.tt(V_, Sim.ap[:, :, 0], Sim.ap[:, :, 0], ta.ap, ALU.add, [Sim, ta], [Sim])
            K.tt("dve", u1.ap, Ct.ap, Sre.ap, ALU.mult, [Ct, Sre], [u1])
            K.tt("pool", u2.ap, St.ap, Sim.ap, ALU.mult, [St, Sim], [u2])
            K.tt("dve", gre.ap, u1.ap, u2.ap, ALU.add, [u1, u2], [gre])
            K.tt("dve", u1.ap, Ct.ap, Sim.ap, ALU.mult, [Ct, Sim], [u1])
            K.tt("pool", u2.ap, St.ap, Sre.ap, ALU.mult, [St, Sre], [u2])
            K.tt("dve", gim.ap, u1.ap, u2.ap, ALU.subtract, [u1, u2], [gim])
            for pp in range(16):
                rb = r8.ap[:, pp:pp + 1].to_broadcast([128, 128])
                K.op("dve", lambda e, pp=pp, rb=rb, Sre=Sre: e.tensor_tensor_scan(out=Sre.ap[:, pp, :], data0=rb, data1=gre.ap[:, pp, :],
                                                                                 initial=0.0, op0=ALU.mult, op1=ALU.add), [gre, r8], [Sre])
                K.op("dve", lambda e, pp=pp, rb=rb, Sim=Sim: e.tensor_tensor_scan(out=Sim.ap[:, pp, :], data0=rb, data1=gim.ap[:, pp, :],
                                                                                 initial=0.0, op0=ALU.mult, op1=ALU.add), [gim, r8], [Sim])
            K.tt("dve", u1.ap, Ct.ap, Sre.ap, ALU.mult, [Ct, Sre], [u1])
            K.tt("pool", u2.ap, St.ap, Sim.ap, ALU.mult, [St, Sim], [u2])
            K.tt("dve", gre.ap, u1.ap, u2.ap, ALU.subtract, [u1, u2], [gre])
            K.tt("dve", u1.ap, Ct.ap, Sim.ap, ALU.mult, [Ct, Sim], [u1])
            K.tt("pool", u2.ap, St.ap, Sre.ap, ALU.mult, [St, Sre], [u2])
            K.tt("dve", gim.ap, u1.ap, u2.ap, ALU.add, [u1, u2], [gim])
            K.cp(V_, car_re.ap, gre.ap[:, :, 127], [gre], [car_re])
            K.cp(V_, car_im.ap, gim.ap[:, :, 127], [gim], [car_im])
            if own:
                K.cp("act", Pre.ap[:, :, 1:129], gre.ap, [gre], [Pre])
                K.cp("act", Pim.ap[:, :, 1:129], gim.ap, [gim], [Pim])

        def SY(blk):
            tb = blk * 1024
            UT = UTs[blk % 2]
            for g0 in range(0, 32, 4):
                p = nps()
                for gi in range(4):
                    g = g0 + gi
                    pp, g2 = g // 2, g % 2
                    oap = p.ap[:, gi * 128:(gi + 1) * 128]
                    K.mm(p, oap, Pre.ap[:, pp, 0:128], WY1.ap[:, pp, 0, g2, :], True, False, [Pre, WY1])
                    K.mm(p, oap, Pim.ap[:, pp, 0:128], WY1.ap[:, pp, 1, g2, :], False, False, [Pim, WY1])
                    K.mm(p, oap, UT.ap[:, g, :], WY2.ap[:, g, :], False, True, [UT, WY2])
                K.act(ytm.ap.rearrange("p j (g c) -> p g j c", g=32)[:, g0:g0 + 4],
                      p.ap.rearrange("p (g j c) -> p g j c", g=4, j=8), AF.Gelu_apprx_tanh, [p], [ytm])
            for j in range(8):
                if j % 2 == 0:
                    p = nps()
                    pv = p.ap.bitcast(BF16).rearrange("p (a b) -> p a b", a=8)
                for cc in range(4):
                    K.tr(p, pv[:, (j % 2) * 4 + cc, :], ytm.ap[:, j, cc * 128:(cc + 1) * 128], idb.ap, [ytm, idb])
                K.cp("act", yT.ap[:, :, j:1024:8], pv[:, (j % 2) * 4:(j % 2) * 4 + 4, :], [p], [yT])
            for half in range(2):
                for co in range(4):
                    p = nps()
                    for kc in range(4):
                        K.mm(p, p.ap, wglu.ap[:, kc, co * 128:(co + 1) * 128], yT.ap[:, kc, half * 512:(half + 1) * 512],
                             kc == 0, kc == 3, [wglu, yT])
                    K.act(sg.ap, p.ap, AF.Sigmoid, [p], [sg])
                    so = sos[half]
                    K.tt("pool", so.ap[:, co, :], yT.ap[:, co, half * 512:(half + 1) * 512], sg.ap,
                         ALU.mult, [yT, sg], [so])
                t0_ = tb - NT_OWN + half * 512
                K.dma("oS%d" % half, ssmT_s[:, :, t0_: t0_ + 512].rearrange("c p t -> p c t"), sos[half].ap, r=[sos[half]])

        S1(0)
        for blk in range(8):
            if blk + 1 < 8:
                S1(blk + 1)
            SC(blk)
            if blk >= 4:
                SY(blk)
        K.barrier()

    def phase_B():
        K.phase_reset()
        wpsB = K.sb([128, 4, 1024], BF16, "wpsB")
        wpaB = K.sb([128, 4, 1024], BF16, "wpaB")
        woutB = K.sb([128, 8, 1024], BF16, "woutB")
        wqB = K.sb([128, 8, 2048], BF16, "wqB")
        wstg2 = K.sb([128, 2048], F32, "wstg2")
        wchunks = ([(wpsB, w_ps, kc, 1024) for kc in range(4)] + [(wpaB, w_pa, kc, 1024) for kc in range(4)]
                   + [(woutB, w_out, kc, 1024) for kc in range(8)] + [(wqB, w_q, kc, 2048) for kc in range(8)])
        KTs = [K.sb([96, NT_LOC], BF16, "KT%d" % i) for i in range(2)]
        QTs = [K.sb([96, NT_OWN], BF16, "QT%d" % i) for i in range(2)]
        Vs = [K.sb([128, 64, 65], BF16, "V%d" % i) for i in range(2)]
        QA = K.sb([64, NT_OWN], BF16, "QA")
        att = K.sb([128, 32, 512], BF16, "att")
        kmax = K.sb([64, 1], F32, "kmax")
        kmaxb = K.sb([64, 1], BF16, "kmaxb")
        ksb = K.sb([64, 32], BF16, "ksb")
        valid = K.sb([128, 512], F32, "valid")
        ownm = K.sb([128, 512], F32, "ownm")
        negb = K.sb([128, 512], F32, "negb")
        stg = K.sb([128, 2048], F32, "stgB")
        cm = K.sb([128, 4, 512], BF16, "cm")
        NG = 4
        gms = [K.sb([128, 32], F32, "gm%d" % i) for i in range(NG)]
        m8s = [K.sb([128, 8], F32, "m8%d" % i) for i in range(NG)]
        sels = [K.sb([128, 32], F32, "sel%d" % i) for i in range(NG)]
        sexs = [K.sb([128, 128], BF16, "sex%d" % i) for i in range(NG)]
        mraws = [K.sb([128, 4], F32, "mraw%d" % i) for i in range(2)]
        PTs = [K.sb([128, 512], BF16, "PT%d" % i) for i in range(3)]
        rls = [K.sb([128, 1], F32, "rl%d" % i) for i in range(4)]
        ostg = [K.sb([128, 4, 128], BF16, "ostg%d" % i) for i in range(2)]
        K.dma("c0", valid.ap, validc.partition_broadcast(128), w=[valid])
        K.dma("c0", ownm.ap, ownc.partition_broadcast(128), w=[ownm])
        K.ts("dve", negb.ap, valid.ap, -1.0, 1e30, ALU.add, ALU.mult, [valid], [negb])
        for i in range(4):
            K.dma("c0", stg.ap[:, 0:512], cm_d[i], w=[stg])
            K.cp("dve", cm.ap[:, i, :], stg.ap[:, 0:512], [stg], [cm])
        gm4 = K.sb([128, 4, 32], F32, "gm4")
        m84 = K.sb([128, 4, 8], F32, "m84")
        sel4 = K.sb([128, 4, 32], F32, "sel4")
        sex4 = K.sb([128, 4, 128], BF16, "sex4")
        K.memset("pool", sex4.ap, 0.0, [sex4])
        for kb_ in KTs:
            for c4 in range(4):
                K.dma("c0", stg.ap[64:96, :], koh_d[:, c4 * 2048:(c4 + 1) * 2048], w=[stg])
                K.cp("dve", kb_.ap[64:96, c4 * 2048:(c4 + 1) * 2048], stg.ap[64:96, :], [stg], [kb_])
        for h in range(8):
            hp, pb = h // 2, (h % 2) * 64
            KT, QT, V = KTs[h % 2], QTs[h % 2], Vs[h % 2]
            K.dma("bk%d" % (h % 2), KT.ap[0:64, :], kT_s[hp, pb:pb + 64, :], w=[KT])
            K.dma("bq%d" % (h % 2), QT.ap[0:64, :], qT_s[hp, pb:pb + 64, :], w=[QT])
            K.dma("bv%d" % (h % 2), V.ap, v_s.rearrange("(t p) c -> p t c", p=128)[:, :, h * 65:(h + 1) * 65], w=[V])
            K.act(QA.ap, QT.ap[0:64, :], AF.Abs, [QT], [QA])
            K.op("dve", lambda e, KT=KT: e.tensor_reduce(out=kmax.ap, in_=KT.ap[0:64, :], axis=AX.X, op=ALU.max,
                                                         apply_absolute_value=True), [KT], [kmax])
            K.cp("dve", kmaxb.ap, kmax.ap, [kmax], [kmaxb])
            K.cp("dve", ksb.ap, ksum.ap[pb:pb + 64, hp, :], [ksum], [ksb])
            for qg in range(8):
                q0 = qg * 512
                pg = PS[qg % 3]
                c0 = (qg // 3) * 132
                for j in range(4):
                    K.mm(pg, pg.ap[:, c0 + j * 32: c0 + (j + 1) * 32], QT.ap[0:64, q0 + j * 128: q0 + (j + 1) * 128],
                         ksb.ap, True, True, [QT, ksb])
                    K.mm(pg, pg.ap[:, c0 + 128 + j: c0 + 129 + j], QA.ap[:, q0 + j * 128: q0 + (j + 1) * 128],
                         kmaxb.ap, True, True, [QA, kmaxb])
            for qg in range(8):
                q0 = qg * 512
                pg = PS[qg % 3]
                c0 = (qg // 3) * 132
                mraw = mraws[qg % 2]
                K.cp("dve", mraw.ap, pg.ap[:, c0 + 128: c0 + 132], [pg], [mraw])
                S4 = [128, 2, 2, 32]
                qsl = slice(2 * qg * 32, (2 * qg + 2) * 32)
                bq = lambda t_: t_.ap[:, qsl].rearrange("p (a b) -> p a b", a=2).unsqueeze(2).broadcast_to(S4)
                g4 = gm4.ap.rearrange("p (a c) b -> p a c b", a=2)
                s4 = sel4.ap.rearrange("p (a c) b -> p a c b", a=2)
                K.tt("dve", g4, pg.ap[:, c0:c0 + 128].rearrange("p (a c b) -> p a c b", a=2, c=2), bq(negb), ALU.add, [pg, negb], [gm4])
                for j in range(4):
                    K.op("dve", lambda e, j=j: e.max(out=m84.ap[:, j, :], in_=gm4.ap[:, j, :]), [gm4], [m84])
                K.tt("dve", sel4.ap, gm4.ap, m84.ap[:, :, 2:3].broadcast_to([128, 4, 32]), ALU.is_ge, [gm4, m84], [sel4])
                K.tt("dve", s4, s4, bq(valid), ALU.mult, [sel4, valid], [sel4])
                K.tt("dve", s4, s4, bq(ownm), ALU.add, [sel4, ownm], [sel4])
                K.ts("dve", sel4.ap, sel4.ap, -1.0, BIG, ALU.add, ALU.mult, [sel4], [sel4])
                K.tt("dve", sex4.ap[:, :, 64:96], sel4.ap, mraw.ap.unsqueeze(2).broadcast_to([128, 4, 32]), ALU.subtract,
                     [sel4, mraw], [sex4])
                pt = PS[3]
                for j in range(4):
                    K.mm(pt, pt.ap[:, j * 128:(j + 1) * 128], sex4.ap[:, j, :], idb.ap, True, True, [sex4, idb])
                K.cp("act", QT.ap[64:96, q0:q0 + 512], pt.ap[64:96, :], [pt], [QT])
            for qg in range(8):
                q0 = qg * 512
                nkt = 36 + 4 * qg

                def qk(kt):
                    ps = PS[kt % 3]
                    last_own = kt >= nkt - 4
                    K.mm(ps, ps.ap, KT.ap[:, kt * 128:(kt + 1) * 128], QT.ap[:, q0:q0 + 512],
                         True, not last_own, [KT, QT])
                    if last_own:
                        K.mm(ps, ps.ap, idb.ap, cm.ap[:, kt - (nkt - 4), :], False, True, [idb, cm])

                qk(0)
                qk(1)
                for kt in range(nkt):
                    if kt + 2 < nkt:
                        qk(kt + 2)
                    ps = PS[kt % 3]
                    PT = PTs[kt % 3]
                    K.act(PT.ap, ps.ap, AF.Exp, [ps], [PT], scale=0.125)
                    for j in range(4):
                        po = PS[4 + j]
                        K.mm(po, po.ap[:, 0:65], PT.ap[:, j * 128:(j + 1) * 128], V.ap[:, kt, :],
                             kt == 0, kt == nkt - 1, [PT, V])
                for j in range(4):
                    po = PS[4 + j]
                    K.op("dve", lambda e, po=po, j=j: e.reciprocal(out=rls[j].ap, in_=po.ap[:, 64:65]), [po], [rls[j]])
                for j in range(4):
                    po = PS[4 + j]
                    K.ts("dve", att.ap[:, qg * 4 + j, h * 64:(h + 1) * 64], po.ap[:, 0:64], rls[j].ap[:, 0:1], None,
                         ALU.mult, None, [po, rls[j]], [att])
                wi = h * 8 + qg
                wbufs = (stg, wstg2)
                if wi < len(wchunks):
                    dst, src, kc, ncols = wchunks[wi]
                    K.dma("bw%d" % (wi % 2), wbufs[wi % 2].ap[:, 0:ncols], src[kc * 128:(kc + 1) * 128, :], w=[wbufs[wi % 2]])
                if 1 <= wi <= len(wchunks):
                    dst, src, kc, ncols = wchunks[wi - 1]
                    K.cp("dve", dst.ap[:, kc, :], wbufs[(wi - 1) % 2].ap[:, 0:ncols], [wbufs[(wi - 1) % 2]], [dst])
        for qt in range(32):
            p = PS[qt % 2]
            pv = p.ap.bitcast(BF16)[:, 0:512].rearrange("p (a b) -> p a b", a=4)
            for c in range(4):
                K.tr(p, pv[:, c, :], att.ap[:, qt, c * 128:(c + 1) * 128], idb.ap, [att, idb])
            og = ostg[qt % 2]
            K.cp("act", og.ap, pv, [p], [og])
            K.dma("ob%d" % (qt % 2), attT_s[:, :, qt * 128:(qt + 1) * 128].rearrange("c p t -> p c t"), og.ap, r=[og])
        K.barrier()

    def phase_C1():
        K.phase_reset()
        wps = K.sb([128, 4, 1024], BF16, "wps")
        wpa = K.sb([128, 4, 1024], BF16, "wpa")
        wout = K.sb([128, 8, 1024], BF16, "wout")
        wq = K.sb([128, 8, 2048], BF16, "wq")
        keysT = K.sb([128, 16, 128], F32, "keysT")
        gf = K.sb([128, 1024], F32, "gffn")
        stage = K.sb([128, 2048], F32, "stageC")
        ssmT = K.sb([128, 4, 512], BF16, "ssmT")
        attT = K.sb([128, 4, 512], BF16, "attT")
        gaT = K.sb([128, 8, 512], BF16, "gaT")
        gbT = K.sb([128, 8, 512], BF16, "gbT")
        mT = K.sb([128, 8, 512], BF16, "mT")
        t1cs = [K.sb([128, 512], F32, "t1C%d" % i) for i in range(2)]
        t2cs = [K.sb([128, 512], F32, "t2C%d" % i) for i in range(2)]
        xts = [K.sb([128, 1024], F32, "xtC%d" % i) for i in range(2)]
        x1s = [K.sb([128, 1024], F32, "x1C%d" % i) for i in range(2)]
        hbs = [K.sb([128, 1024], BF16, "hbC%d" % i) for i in range(2)]
        sq = K.sb([128, 1024], BF16, "sqC")
        ss = K.sb([128, 1], F32, "ssC")
        rs = K.sb([128, 1], F32, "rsC")
        h2T = K.sb([128, 8, 512], BF16, "h2T")
        qT = K.sb([128, 16, 512], F32, "qT")
        scs = [K.sb([128, 16, 128], F32, "sc%d" % i) for i in range(2)]
        K.dma("c0", gf.ap, g_ffn.partition_broadcast(128), w=[gf])
        for ch in range(16):
            K.dma("wst", stage.ap[:, 0:128], keys[ch], w=[stage])
            p = nps()
            K.tr(p, p.ap[:, 0:128], stage.ap[:, 0:128], idf.ap, [stage, idf])
            K.cp("dve", keysT.ap[:, ch, :], p.ap[:, 0:128], [p], [keysT])
        xi = 0

        def c1_loads(tg_):
            ta_ = tg_ * 512
            K.dma("c1a", ssmT.ap, ssmT_s[:, :, ta_:ta_ + 512].rearrange("c p t -> p c t"), w=[ssmT])
            K.dma("c1b", attT.ap, attT_s[:, :, ta_:ta_ + 512].rearrange("c p t -> p c t"), w=[attT])
            K.dma("c1c", gaT.ap, gT_s[0:8, :, ta_:ta_ + 512].rearrange("c p t -> p c t"), w=[gaT])
            K.dma("c1d", gbT.ap, gT_s[8:16, :, ta_:ta_ + 512].rearrange("c p t -> p c t"), w=[gbT])

        c1_loads(0)
        for tg in range(8):
            t0 = tg * 512
            for c in range(8):
                pa = nps()
                pb = nps()
                for kc in range(4):
                    K.mm(pa, pa.ap, wps.ap[:, kc, c * 128:(c + 1) * 128], ssmT.ap[:, kc, :], kc == 0, kc == 3, [wps, ssmT])
                for kc in range(4):
                    K.mm(pb, pb.ap, wpa.ap[:, kc, c * 128:(c + 1) * 128], attT.ap[:, kc, :], kc == 0, kc == 3, [wpa, attT])
                t1, t2 = t1cs[c % 2], t2cs[c % 2]
                K.tt("dve", t1.ap, pa.ap, gaT.ap[:, c, :], ALU.mult, [pa, gaT], [t1])
                K.tt("dve", t2.ap, pb.ap, gbT.ap[:, c, :], ALU.mult, [pb, gbT], [t2])
                K.tt("dve", mT.ap[:, c, :], t1.ap, t2.ap, ALU.add, [t1, t2], [mT])
            if tg + 1 < 8:
                c1_loads(tg + 1)
            for j in range(4):
                xt = xts[xi % 2]
                x1 = x1s[xi % 2]
                hb = hbs[xi % 2]
                xi += 1
                r0 = t0 + j * 128
                K.dma("x%d" % (xi % 2), xt.ap, xc[NT_OWN + r0: NT_OWN + r0 + 128, :], w=[xt])
                for half in range(2):
                    p = nps()
                    for kc in range(8):
                        K.mm(p, p.ap, mT.ap[:, kc, j * 128:(j + 1) * 128], wout.ap[:, kc, half * 512:(half + 1) * 512],
                             kc == 0, kc == 7, [mT, wout])
                    K.tt("dve", x1.ap[:, half * 512:(half + 1) * 512], p.ap, xt.ap[:, half * 512:(half + 1) * 512], ALU.add,
                         [p, xt], [x1])
                K.dma("x1o%d" % (xi % 2), x1_s[r0:r0 + 128, :], x1.ap, r=[x1])
                rmsnorm_tile(x1, gf, hb, sq, ss, rs)
                p = nps()
                pv = p.ap.bitcast(BF16).rearrange("p (a b) -> p a b", a=8)
                for kc in range(8):
                    K.tr(p, pv[:, kc, :], hb.ap[:, kc * 128:(kc + 1) * 128], idb.ap, [hb, idb])
                K.cp("act", h2T.ap[:, :, j * 128:(j + 1) * 128], pv, [p], [h2T])
            K.dma("h2o", h2T_s[:, :, t0:t0 + 512].rearrange("c p t -> p c t"), h2T.ap, r=[h2T])
            for ch in range(16):
                p = nps()
                for kc in range(8):
                    K.mm(p, p.ap, wq.ap[:, kc, ch * 128:(ch + 1) * 128], h2T.ap[:, kc, :], kc == 0, kc == 7, [wq, h2T])
                K.cp("act" if ch % 2 else "dve", qT.ap[:, ch, :], p.ap, [p], [qT])
            for j in range(4):
                sc = scs[j % 2]
                for c0 in range(0, 16, 4):
                    p = nps()
                    for i in range(4):
                        K.mm(p, p.ap[:, i * 128:(i + 1) * 128], qT.ap[:, c0 + i, j * 128:(j + 1) * 128], keysT.ap[:, c0 + i, :],
                             True, True, [qT, keysT])
                    K.cp("act" if (c0 // 4) % 2 else "dve", sc.ap[:, c0:c0 + 4, :], p.ap.rearrange("p (a b) -> p a b", a=4), [p], [sc])
                r0 = t0 + j * 128
                K.dma("sco%d" % (j % 2), sc_s[r0:r0 + 128, :], sc.ap.rearrange("p a b -> p (a b)"), r=[sc])
        K.barrier()

    U32 = mybir.dt.uint32

    def phase_C2():
        K.phase_reset()
        iot = K.sb([128, 128], F32, "iota")
        K.dma("c0", iot.ap, iota_d, w=[iot])
        ss_ = [K.sb([128, 16, 128], F32, "s%d" % i) for i in range(2)]
        v = K.sb([128, 16, 16], F32, "v")
        idx = K.sb([128, 8, 16], U32, "idx")
        idxf = K.sb([128, 128], F32, "idxf")
        idxT = K.sb([128, 128], F32, "idxT")
        top = K.sb([128, 8, 16], F32, "top")
        e16 = K.sb([128, 8, 16], F32, "e16")
        Z = K.sb([128, 8], F32, "Z")
        bE = K.sb([128, 8], F32, "bE")
        v1m = K.sb([128, 8, 16], F32, "v1m")
        sums = [K.sb([128, 16, 128], F32, "sum%d" % i) for i in range(3)]
        Es = [K.sb([128, 16, 128], BF16, "E%d" % i) for i in range(3)]
        Btm = K.sb([128, 8, 16, 128], BF16, "Btm")
        BT = K.sb([128, 128, 128], BF16, "BT")
        AT = K.sb([128, 128, 128], BF16, "AT")
        Gst = K.sb([128, 128, 128], BF16, "Gst")
        works = [K.sb([128, 128], F32, "wk%d" % i) for i in range(16)]
        work2s = [K.sb([128, 256], F32, "wk2%d" % i) for i in range(8)]
        K.dma("c2s0", ss_[0].ap.rearrange("p a b -> p (a b)"), sc_s[0:128, :], w=[ss_[0]])
        for tl in range(32):
            r0 = tl * 128
            s_ = ss_[tl % 2]
            if tl + 1 < 32:
                sn_ = ss_[(tl + 1) % 2]
                K.dma("c2s%d" % ((tl + 1) % 2), sn_.ap.rearrange("p a b -> p (a b)"), sc_s[r0 + 128:r0 + 256, :], w=[sn_])
            for ch in range(16):
                K.op("dve", lambda e, ch=ch, s_=s_: e.max(out=v.ap[:, ch, 0:8], in_=s_.ap[:, ch, :]), [s_], [v])
            for ch in range(0, 16, 2):
                K.op("dve", lambda e, ch=ch, s_=s_: e.max_index(out=idx.ap[:, ch // 2, 0:8], in_max=v.ap[:, ch, 0:8],
                                                                in_values=s_.ap[:, ch, :]), [v, s_], [idx])
            for ch in range(16):
                K.op("dve", lambda e, ch=ch, s_=s_: e.match_replace(out=works[ch].ap, in_to_replace=v.ap[:, ch, 0:8],
                                                                    in_values=s_.ap[:, ch, :], imm_value=-1e30), [v, s_], [works[ch]])
            for ch in range(16):
                K.op("dve", lambda e, ch=ch: e.max(out=v.ap[:, ch, 8:16], in_=works[ch].ap), [works[ch]], [v])
            for ch in range(0, 16, 2):
                K.op("dve", lambda e, ch=ch: e.max_index(out=idx.ap[:, ch // 2, 8:16], in_max=v.ap[:, ch, 8:16],
                                                         in_values=works[ch].ap), [v, works[ch]], [idx])
            vv = v.ap.rearrange("p (h s) k -> p h s k", s=2)
            v1 = vv[:, :, 0, :]
            v2 = vv[:, :, 1, :]
            cand_ap = sums[0].ap.rearrange("p a b -> p (a b)").rearrange("p (h c) -> p h c", h=8)
            K.tt("dve", cand_ap.rearrange("p h (i j) -> p h i j", i=16), v1.unsqueeze(3).broadcast_to([128, 8, 16, 16]),
                 v2.unsqueeze(2).broadcast_to([128, 8, 16, 16]), ALU.add, [v], [sums[0]])
            for h in range(8):
                K.op("dve", lambda e, h=h: e.max(out=top.ap[:, h, 0:8], in_=cand_ap[:, h, :]), [sums[0]], [top])
            for h in range(8):
                K.op("dve", lambda e, h=h: e.match_replace(out=work2s[h].ap, in_to_replace=top.ap[:, h, 0:8],
                                                           in_values=cand_ap[:, h, :], imm_value=-1e30), [top, sums[0]], [work2s[h]])
            for h in range(8):
                K.op("dve", lambda e, h=h: e.max(out=top.ap[:, h, 8:16], in_=work2s[h].ap), [work2s[h]], [top])
            mxb = top.ap[:, :, 0:1].broadcast_to([128, 8, 16])
            taub = top.ap[:, :, 15:16].broadcast_to([128, 8, 16])
            K.tt("dve", e16.ap, top.ap, mxb, ALU.subtract, [top], [e16])
            K.act(e16.ap, e16.ap, AF.Exp, [e16], [e16])
            K.op("dve", lambda e: e.tensor_reduce(out=Z.ap, in_=e16.ap, axis=AX.X, op=ALU.add), [e16], [Z])
            K.act(Z.ap, Z.ap, AF.Ln, [Z], [Z])
            K.tt("dve", bE.ap, top.ap[:, :, 15], top.ap[:, :, 0], ALU.subtract, [top], [bE])
            K.tt("dve", bE.ap, bE.ap, Z.ap, ALU.subtract, [bE, Z], [bE])
            K.tt("dve", v1m.ap, v1, taub, ALU.subtract, [v, top], [v1m])
            K.cp("dve", idxf.ap, idx.ap.rearrange("p h k -> p (h k)"), [idx], [idxf])
            p = nps()
            K.tr(p, p.ap[:, 0:128], idxf.ap, idf.ap, [idxf, idf])
            K.cp("dve", idxT.ap, p.ap[:, 0:128], [p], [idxT])
            K.tt("dve", AT.ap, iot.ap.unsqueeze(1).broadcast_to([128, 128, 128]),
                 idxT.ap.unsqueeze(2).broadcast_to([128, 128, 128]), ALU.is_equal, [iot, idxT], [AT])
            def bsum(h):
                sm_ = sums[h % 3]
                K.tt("pool" if h % 2 == 0 else "dve", sm_.ap, v1m.ap[:, h, :].unsqueeze(2).broadcast_to([128, 16, 128]),
                     s_.ap[:, 2 * h + 1, :].unsqueeze(1).broadcast_to([128, 16, 128]), ALU.add, [v1m, s_], [sm_])
                K.act(Es[h % 3].ap, sm_.ap, AF.Exp, [sm_, bE], [Es[h % 3]], bias=bE.ap[:, h:h + 1])

            bsum(0)
            bsum(1)
            for h in range(8):
                if h + 2 < 8:
                    bsum(h + 2)
                K.stt("dve", Btm.ap[:, h], sums[h % 3].ap, -1e-5, Es[h % 3].ap, ALU.is_ge, ALU.mult, [sums[h % 3], Es[h % 3]], [Btm])
            for b0 in range(0, 128, 8):
                p = nps()
                pv = p.ap.bitcast(BF16).rearrange("p (a b) -> p a b", a=8)
                for k in range(8):
                    K.tr(p, pv[:, k, :], Btm.ap[:, :, :, b0 + k].rearrange("p h i -> p (h i)"), idb.ap, [Btm, idb])
                K.cp("act", BT.ap[:, :, b0:b0 + 8], pv.rearrange("p b t -> p t b"), [p], [BT])
            for t4 in range(0, 128, 4):
                p = nps()
                for k in range(4):
                    t = t4 + k
                    K.mm(p, p.ap[:, k * 128:(k + 1) * 128], BT.ap[:, t, :], AT.ap[:, t, :], True, True, [BT, AT])
                K.cp("act", Gst.ap[:, :, t4:t4 + 4],
                     p.ap.rearrange("p (t a) -> p a t", t=4), [p], [Gst])
            K.dma("c2g", G_s[tl], Gst.ap, r=[Gst])
        K.barrier()

    def phase_D():
        K.phase_reset()
        TG = 1024
        NG_ = NT_OWN // TG
        h2T = K.sb([128, 8, TG], BF16, "h2TD")
        Ubs = [K.sb([128, 1024], F32, "Ub%d" % i) for i in range(3)]
        Ubb = [K.sb([128, 1024], BF16, "Ubb%d" % i) for i in range(2)]
        UbTs = [K.sb([128, 8, 128], BF16, "UbT%d" % i) for i in range(3)]
        Ghs = [K.sb([128, 8, 8, 128], BF16, "Gh%d" % i) for i in range(2)]
        ges = [K.sb([128, 512], BF16, "ge%d" % i) for i in range(2)]
        W = K.sb([128, 16, TG], BF16, "W")
        Vbs = [K.sb([128, 1024], F32, "Vb%d" % i) for i in range(3)]
        Vbf = K.sb([128, 16, 1024], BF16, "Vbf")
        acc = K.sb([128, 8, 1024], F32, "acc")
        x1t = [K.sb([128, 1024], F32, "x1D%d" % i) for i in range(2)]
        uttok = [Buf(None, "ut%d" % i) for i in range(128)]
        NI = NG_ * 128

        def load(n):
            grp, a = n // 128, n % 128
            t0 = grp * TG
            if grp == 0:
                K.dma("du%d" % (n % 3), Ubs[n % 3].ap, peer_u[a * 128:(a + 1) * 128, :], w=[Ubs[n % 3]])
            else:
                K.dma("du%d" % (n % 3), UbTs[n % 3].ap.rearrange("p a b -> p (a b)"), UT_s[a], r=[uttok[a]], w=[UbTs[n % 3]])
            if a % 8 == 0:
                Gh = Ghs[(n // 8) % 2]
                tl0 = t0 // 128
                K.dma("dg%d" % ((n // 8) % 2), Gh.ap, G_s[tl0:tl0 + 8, :, a:a + 8, :].rearrange("tl b a t -> b tl a t"), w=[Gh])
            K.dma("dv%d" % (n % 3), Vbs[n % 3].ap, peer_v[a * 128:(a + 1) * 128, :], w=[Vbs[n % 3]])

        def trans(n):
            if n // 128 != 0:
                return
            Ub, UbT, ub = Ubs[n % 3], UbTs[n % 3], Ubb[n % 2]
            K.cp("dve", ub.ap, Ub.ap, [Ub], [ub])
            p = nps()
            pv = p.ap.bitcast(BF16).rearrange("p (a b) -> p a b", a=8)
            for kc in range(8):
                K.tr(p, pv[:, kc, :], ub.ap[:, kc * 128:(kc + 1) * 128], idb.ap, [ub, idb])
            K.cp("act", UbT.ap, pv, [p], [UbT])

        load(0)
        load(1)
        trans(0)
        for n in range(NI):
            grp, a = n // 128, n % 128
            t0 = grp * TG
            si = a % 16
            if a == 0:
                K.dma("dh", h2T.ap, h2T_s[:, :, t0:t0 + TG].rearrange("c p t -> p c t"), w=[h2T])
                K.memset("pool", acc.ap, 0.0, [acc])
            if n + 2 < NI:
                load(n + 2)
            if n + 1 < NI:
                trans(n + 1)
            UbT, Vb = UbTs[n % 3], Vbs[n % 3]
            Gh = Ghs[(n // 8) % 2]
            if grp == 0:
                K.dma("dut%d" % (n % 3), UT_s[a], UbT.ap.rearrange("p a b -> p (a b)"), r=[UbT], w=[uttok[a]])
            K.cp("act", Vbf.ap[:, si, :], Vb.ap, [Vb], [Vbf])
            for hf in range(TG // 512):
                p = nps()
                for kc in range(8):
                    K.mm(p, p.ap, UbT.ap[:, kc, :], h2T.ap[:, kc, hf * 512:(hf + 1) * 512], kc == 0, kc == 7, [UbT, h2T])
                ge = ges[hf % 2]
                K.act(ge.ap, p.ap, AF.Gelu_apprx_tanh, [p], [ge])
                K.tt("dve", W.ap[:, si, hf * 512:(hf + 1) * 512].rearrange("p (a b) -> p a b", a=4),
                     ge.ap.rearrange("p (a b) -> p a b", a=4), Gh.ap[:, hf * 4:(hf + 1) * 4, a % 8, :], ALU.mult,
                     [ge, Gh], [W])
            if si == 15:
                for j in range(TG // 128):
                    for hf in range(2):
                        p = nps()
                        for s2 in range(16):
                            K.mm(p, p.ap, W.ap[:, s2, j * 128:(j + 1) * 128], Vbf.ap[:, s2, hf * 512:(hf + 1) * 512],
                                 s2 == 0, s2 == 15, [W, Vbf])
                        K.tt("dve", acc.ap[:, j, hf * 512:(hf + 1) * 512], p.ap, acc.ap[:, j, hf * 512:(hf + 1) * 512], ALU.add,
                             [p, acc], [acc])
            if a == 127:
                for j in range(TG // 128):
                    xt = x1t[j % 2]
                    r0 = t0 + j * 128
                    K.dma("dx%d" % (j % 2), xt.ap, x1_s[r0:r0 + 128, :], w=[xt])
                    K.tt("dve", xt.ap, xt.ap, acc.ap[:, j, :], ALU.add, [xt, acc], [xt])
                    K.dma("dx%d" % (j % 2), x2_s[r0:r0 + 128, :], xt.ap, r=[xt])
        K.barrier()

    def phase_E():
        K.phase_reset()
        wpg = K.sb([128, 8, 1024], BF16, "wpg")
        wpp = K.sb([128, 2, 1024], BF16, "wpp")
        stage = K.sb([128, 1024], F32, "stageE")
        gp = K.sb([128, 1024], F32, "gple")
        gfin = K.sb([128, 1024], F32, "gfin")
        xts = [K.sb([128, 1024], F32, "xtE%d" % i) for i in range(4)]
        pts = [K.sb([128, 256], F32, "ptE%d" % i) for i in range(2)]
        pb_s = [K.sb([128, 256], BF16, "pbE%d" % i) for i in range(2)]
        pTs = [K.sb([128, 2, 128], BF16, "pTE%d" % i) for i in range(2)]
        hb_s = [K.sb([128, 1024], BF16, "hbE%d" % i) for i in range(2)]
        hTs_ = [K.sb([128, 8, 128], BF16, "hTE%d" % i) for i in range(2)]
        sq_s = [K.sb([128, 1024], BF16, "sqE%d" % i) for i in range(2)]
        ss_s = [K.sb([128, 1], F32, "ssE%d" % i) for i in range(4)]
        rs_s = [K.sb([128, 1], F32, "rsE%d" % i) for i in range(4)]
        sgts = [K.sb([128, 1024], F32, "sgE%d" % i) for i in range(2)]
        outs = [K.sb([128, 1024], F32, "oE%d" % i) for i in range(2)]
        K.dma("c0", gp.ap, g_ple.partition_broadcast(128), w=[gp])
        K.dma("c0", gfin.ap, g_fin.partition_broadcast(128), w=[gfin])
        load_w_bf(wpg, w_pg, 8, 1024, stage, "wst")
        load_w_bf(wpp, w_pp, 2, 1024, stage, "wst")
        def e_vars(tl):
            return dict(r0=tl * 128)

        def front(tl):
            r0 = tl * 128
            xt, pt, ot = xts[tl % 4], pts[tl % 2], outs[tl % 2]
            pb_, pT, hb, hT, sq, sgt = pb_s[tl % 2], pTs[tl % 2], hb_s[tl % 2], hTs_[tl % 2], sq_s[tl % 2], sgts[tl % 2]
            ss, rs = ss_s[tl % 2], rs_s[tl % 2]
            ss2, rs2 = ss_s[2 + tl % 2], rs_s[2 + tl % 2]
            K.dma("ex%d" % (tl % 4), xt.ap, x2_s[r0:r0 + 128, :], w=[xt])
            K.dma("ep%d" % (tl % 2), pt.ap, pc[r0:r0 + 128, :], w=[pt])
            rmsnorm_tile(xt, gp, hb, sq, ss, rs)
            K.cp("dve", pb_.ap, pt.ap, [pt], [pb_])

        def front1b(tl):
            pb_, pT, hb, hT = pb_s[tl % 2], pTs[tl % 2], hb_s[tl % 2], hTs_[tl % 2]
            p = nps()
            pv = p.ap.bitcast(BF16).rearrange("p (a b) -> p a b", a=8)
            for kc in range(8):
                K.tr(p, pv[:, kc, :], hb.ap[:, kc * 128:(kc + 1) * 128], idb.ap, [hb, idb])
            K.cp("act", hT.ap, pv, [p], [hT])
            p = nps()
            pv = p.ap.bitcast(BF16).rearrange("p (a b) -> p a b", a=8)
            for kc in range(2):
                K.tr(p, pv[:, kc, :], pb_.ap[:, kc * 128:(kc + 1) * 128], idb.ap, [pb_, idb])
            K.cp("act", pT.ap, pv[:, 0:2, :], [p], [pT])

        def front2(tl):
            pT, hT, sgt = pTs[tl % 2], hTs_[tl % 2], sgts[tl % 2]
            for hf in range(2):
                pg = nps()
                pe_ = nps()
                for kc in range(8):
                    K.mm(pg, pg.ap, hT.ap[:, kc, :], wpg.ap[:, kc, hf * 512:(hf + 1) * 512], kc == 0, kc == 7, [hT, wpg])
                for kc in range(2):
                    K.mm(pe_, pe_.ap, pT.ap[:, kc, :], wpp.ap[:, kc, hf * 512:(hf + 1) * 512], kc == 0, kc == 1, [pT, wpp])
                K.act(sgt.ap[:, hf * 512:(hf + 1) * 512], pg.ap, AF.Sigmoid, [pg], [sgt])
                K.tt("dve", sgt.ap[:, hf * 512:(hf + 1) * 512], pe_.ap, sgt.ap[:, hf * 512:(hf + 1) * 512], ALU.mult, [pe_, sgt], [sgt])

        def back(tl):
            r0 = tl * 128
            xt, ot = xts[tl % 4], outs[tl % 2]
            sq, sgt = sq_s[tl % 2], sgts[tl % 2]
            ss2, rs2 = ss_s[2 + tl % 2], rs_s[2 + tl % 2]
            K.tt("pool", xt.ap, xt.ap, sgt.ap, ALU.add, [xt, sgt], [xt])
            K.act(sq.ap, xt.ap, AF.Square, [xt], [sq, ss2], accum=ss2.ap)
            K.act(rs2.ap, ss2.ap, AF.Sqrt, [ss2], [rs2], scale=1.0 / 1024, bias=eps_b.ap)
            K.op("dve", lambda e, rs2=rs2: e.reciprocal(out=rs2.ap, in_=rs2.ap), [rs2], [rs2])
            K.stt("dve", ot.ap, xt.ap, rs2.ap[:, 0:1], gfin.ap, ALU.mult, ALU.mult, [xt, rs2, gfin], [ot])
            K.dma("eo%d" % (tl % 2), out[r0:r0 + 128, :], ot.ap, r=[ot])

        for r_ in range(-3, 32):
            for stage_fn, off_ in ((back, 0), (front2, 1), (front1b, 2), (front, 3)):
                tl_ = r_ + off_
                if 0 <= tl_ < 32:
                    stage_fn(tl_)
        K.barrier()

    phases = {"A": phase_A, "B": phase_B, "S": phase_S, "C1": phase_C1, "C2": phase_C2, "D": phase_D, "E": phase_E}
    return nc, kb, st, locals()


def _consts():
    ident = np.eye(128, dtype=np.float32)
    invf = np.zeros((128, 2), np.float32)
    for p in range(128):
        hd = p % 64
        if hd < 16:
            invf[p, 0] = np.float32(500000.0) ** np.float32(-(2 * (hd % 8)) / 16.0)
            invf[p, 1] = -1.0 if hd < 8 else 1.0
    eoh = np.zeros((32, NT_LOC), np.float32)
    for n in range(32):
        eoh[n, n * 256:(n + 1) * 256] = 1.0
    cm = np.zeros((4, 128, 512), np.float32)
    for kt in range(4):
        for kp in range(128):
            kpos = kt * 128 + kp
            q = np.arange(512)
            same = (q // 256) == (kpos // 256)
            cm[kt, kp, :] = np.where(same & (kpos > q), -BIG, 0.0)
    return ident, invf, eoh, cm


def make_in_maps(inp, cores=range(8)):
    f = lambda a: np.ascontiguousarray(np.asarray(a))
    x = f(inp["x"])
    p = f(inp["p"])[0]
    pos = f(inp["positions"]).astype(np.int32)
    ident, invf, eoh, cm = _consts()
    w_in = f(inp["w_in"])[0]
    perm = np.arange(1024)
    for c in range(1024):
        hd = c % 64
        if hd < 8:
            perm[c] = c + 8
        elif hd < 16:
            perm[c] = c - 8
    w_perm = f(w_in[:, 512:1536][:, perm])

    def pair(a):
        a = f(a)[0]
        sh = a.shape
        a = a.reshape(16, 2, 64, *sh[2:])
        a = np.moveaxis(a, 0, 2)
        return f(a.reshape(128, 16, -1).reshape(128, -1))

    ldt = f(inp["ssm_log_dt"])[0]
    ldt_l = f(np.broadcast_to(ldt.reshape(16, 2, 1), (16, 2, 64)).transpose(1, 2, 0).reshape(128, 16))
    cre = f(inp["ssm_c_re"])[0].transpose(0, 2, 1)
    cim = f(inp["ssm_c_im"])[0].transpose(0, 2, 1)
    shared = {
        "ident": ident, "iota": np.ascontiguousarray(np.broadcast_to(np.arange(128, dtype=np.float32), (128, 128))), "invf": invf, "koh": eoh, "cmask": cm,
        "g_mix": f(inp["g_mix"]), "w_in": w_in, "w_perm": w_perm,
        "s5_ldt": ldt_l, "s5_are": pair(inp["ssm_a_re"]), "s5_aim": pair(inp["ssm_a_im"]),
        "s5_bre": pair(inp["ssm_b_re"]), "s5_bim": pair(inp["ssm_b_im"]),
        "s5_cre": pair(cre[None]), "s5_cim": pair(cim[None]),
        "ssm_d": f(inp["ssm_d"]), "w_glu": f(inp["ssm_w_glu"])[0],
        "w_ps": f(inp["w_proj_ssm"])[0], "w_pa": f(inp["w_proj_att"])[0], "w_out": f(inp["w_out"])[0],
        "g_ffn": f(inp["g_ffn"]), "w_q": f(inp["peer_w_q"])[0],
        "keys": f(np.stack([f(inp["peer_keys1"])[0], f(inp["peer_keys2"])[0]], axis=1).reshape(16, 128, 128)),
        "peer_u": f(inp["peer_u"])[0], "peer_v": f(inp["peer_v"])[0],
        "g_ple": f(inp["g_ple"]), "w_pg": f(inp["ple_w_gate"])[0], "w_pp": f(inp["ple_w_proj"])[0],
        "g_fin": f(inp["g_final"]).reshape(1, 1024),
    }
    maps = []
    for c in cores:
        b, half = c // 2, c % 2
        xc = np.zeros((NT_LOC, 1024), np.float32)
        posc = np.zeros((1, NT_LOC), np.int32)
        if half == 1:
            xc[:] = x[b]
            posc[0] = pos[b]
        else:
            xc[NT_OWN:] = x[b, :NT_OWN]
            posc[0, NT_OWN:] = pos[b, :NT_OWN]
        valid = np.zeros((16, 32), np.float32)
        own = np.zeros((16, 32), np.float32)
        for qb in range(16, 32):
            for n in range(32):
                if n < qb and (half == 1 or n >= 16):
                    valid[qb - 16, n] = 1.0
            own[qb - 16, qb] = 1.0
        m = dict(shared)
        m.update({"xc": xc, "pc": f(p[b, half * NT_OWN:(half + 1) * NT_OWN]), "posc": posc,
                  "validc": valid.reshape(1, 512), "ownc": own.reshape(1, 512)})
        maps.append(m)
    return maps


_CACHE = {}


def kernel(**inputs):
    if "prog" not in _CACHE:
        nc, kb, st, L = build_program()
        for ph in ("A", "S", "B", "C1", "C2", "D", "E"):
            L["phases"][ph]()
        kb.S.emit()
        st.close()
        _CACHE["prog"] = nc
    nc = _CACHE["prog"]
    maps = make_in_maps(inputs, range(8))
    res = run_bass_kernel_spmd(nc, maps, core_ids=list(range(8)))
    out = np.zeros((4, 8192, 1024), np.float32)
    for c in range(8):
        b, half = c // 2, c % 2
        out[b, half * NT_OWN:(half + 1) * NT_OWN] = np.asarray(res.results[c]["out"])
    return out
```

```python
import numpy as np
import concourse.bass as bass
import concourse.mybir as mybir
from concourse.bass_utils import run_bass_kernel_spmd

F32 = mybir.dt.float32
BF16 = mybir.dt.bfloat16
I32 = mybir.dt.int32
ALU = mybir.AluOpType
AF = mybir.ActivationFunctionType
AX = mybir.AxisListType


class Tok:
    __slots__ = ("w", "r", "name")

    def __init__(self, name=""):
        self.w = None
        self.r = {}
        self.name = name


class Sched:
    ENGS = ("pe", "act", "dve", "pool", "sp")

    def __init__(self, nc):
        self.nc = nc
        self.ops = {e: [] for e in self.ENGS}
        self.dma_cnt = {}
        self.dma_keys = []

    @staticmethod
    def _evkey(ev):
        return (ev[0], ev[1])

    def _collect(self, reads, writes):
        deps = {}

        def add(ev):
            if ev is None:
                return
            k = self._evkey(ev)
            if k not in deps or deps[k][2] < ev[2]:
                deps[k] = ev

        for t in reads:
            add(t.w)
        for t in writes:
            add(t.w)
            for ev in t.r.values():
                add(ev)
        return deps

    def _commit(self, ev, reads, writes):
        for t in reads:
            k = self._evkey(ev)
            t.r[k] = ev
        for t in writes:
            t.w = ev
            t.r = {}

    def op(self, eng, fn, reads=(), writes=()):
        deps = self._collect(reads, writes)
        idx = len(self.ops[eng])
        ev = ("e", eng, idx)
        if eng == "pe":
            deps.pop(("e", "pe"), None)
        self.ops[eng].append(dict(fn=fn, deps=list(deps.values()), dma=None, signal=False))
        self._commit(ev, reads, writes)
        return ev

    def dma(self, q, key, out, in_, reads=(), writes=()):
        deps = self._collect(reads, writes)
        if key not in self.dma_cnt:
            self.dma_cnt[key] = 0
            self.dma_keys.append(key)
        n = self.dma_cnt[key]
        if n > 0:
            k = ("d", key)
            deps[k] = ("d", key, n)
        self.dma_cnt[key] = n + 1
        ev = ("d", key, n + 1)
        self.ops[q].append(dict(fn=lambda e, o=out, i=in_: e.dma_start(out=o, in_=i),
                                deps=list(deps.values()), dma=key, signal=False))
        self._commit(ev, reads, writes)
        return ev

    def emit(self, final_keys=()):
        nc = self.nc
        ops = self.ops
        for e in self.ENGS:
            for o in ops[e]:
                for d in o["deps"]:
                    if d[0] == "e":
                        ops[d[1]][d[2]]["signal"] = True
        for e in self.ENGS:
            last = None
            for o in ops[e]:
                if "barrier" in o:
                    if last is not None and e != "sp":
                        last["signal"] = True
                else:
                    last = o
        sigval = {}
        for e in self.ENGS:
            c = 0
            vals = []
            for o in ops[e]:
                if o["signal"]:
                    c += 1
                vals.append(c)
            sigval[e] = vals
        barvals = {}
        for e in self.ENGS:
            for i, o in enumerate(ops[e]):
                if "barrier" in o:
                    barvals[(e, o["barrier"])] = sigval[e][i]
        from contextlib import ExitStack
        with ExitStack() as st:
            esem = {e: st.enter_context(nc.semaphore("s_" + e)) for e in self.ENGS if e != "sp"}
            dsem = {k: st.enter_context(nc.semaphore("d_%d" % i)) for i, k in enumerate(self.dma_keys)}
            bsem = st.enter_context(nc.semaphore("s_bar"))
            block = st.enter_context(nc.Block())

            def run(ename, eng):
                waited = {}
                for o in ops[ename]:
                    if "barrier" in o:
                        k = o["barrier"]
                        if ename == "sp":
                            for key, cnt in o["dcnt"].items():
                                if cnt > 0 and waited.get(("d", key), 0) < 16 * cnt:
                                    eng.wait_ge(dsem[key], 16 * cnt)
                            for e2 in esem:
                                v = barvals[(e2, k)]
                                if v > 0:
                                    eng.wait_ge(esem[e2], v)
                            eng.sem_inc(bsem, 1)
                        else:
                            eng.wait_ge(bsem, k)
                        for key, cnt in o["dcnt"].items():
                            waited[("d", key)] = max(waited.get(("d", key), 0), 16 * cnt)
                        for e2 in esem:
                            waited[("e", e2)] = max(waited.get(("e", e2), 0), barvals[(e2, k)])
                        continue
                    for d in sorted(o["deps"]):
                        if d[0] == "e":
                            sem = esem[d[1]]
                            val = sigval[d[1]][d[2]]
                        else:
                            sem = dsem[d[1]]
                            val = 16 * d[2]
                        wk = (d[0], d[1])
                        if waited.get(wk, 0) >= val:
                            continue
                        waited[wk] = val
                        eng.wait_ge(sem, val)
                    ins = o["fn"](eng)
                    if o["dma"] is not None:
                        ins.then_inc(dsem[o["dma"]], 16)
                    elif o["signal"]:
                        ins.then_inc(esem[ename], 1)
                if ename == "sp":
                    for k in self.dma_keys:
                        eng.wait_ge(dsem[k], 16 * self.dma_cnt[k])

            @block.sync
            def _(e):
                run("sp", e)

            @block.tensor
            def _(e):
                run("pe", e)

            @block.scalar
            def _(e):
                run("act", e)

            @block.vector
            def _(e):
                run("dve", e)

            @block.gpsimd
            def _(e):
                run("pool", e)


NT_OWN = 4096
NT_LOC = 8192
PI = float(np.pi)
BIG = 30000.0


class Buf:
    __slots__ = ("ap", "t")

    def __init__(self, ap, name=""):
        self.ap = ap
        self.t = Tok(name)

    def __getitem__(self, k):
        return self.ap[k]


class KB:
    def __init__(self, nc):
        self.nc = nc
        self.S = Sched(nc)
        self.big = nc.alloc_sbuf_tensor("bigsb", [128, 53000], F32)
        self.off = 0
        self.persist = 0
        self.nbar = 0

    def sb(self, shape, dt, name=""):
        n = int(np.prod(shape[1:]))
        esz = 4 if dt in (F32, I32, mybir.dt.uint32) else 2
        nw = (n * esz + 63) // 64 * 16
        assert self.off + nw <= 53000, ("sbuf overflow", name, self.off, nw)
        ap = self.big[:, self.off:self.off + nw]
        self.off += nw
        if dt != F32:
            ap = ap.bitcast(dt)
        ap = ap[:, 0:n]
        if len(shape) == 3:
            ap = ap.rearrange("p (a b) -> p a b", a=shape[1])
        elif len(shape) == 4:
            ap = ap.rearrange("p (a b c) -> p a b c", a=shape[1], b=shape[2])
        elif len(shape) == 5:
            ap = ap.rearrange("p (a b c d) -> p a b c d", a=shape[1], b=shape[2], c=shape[3])
        if shape[0] != 128:
            ap = ap[0:shape[0]]
        return Buf(ap, name)

    def phase_reset(self):
        self.off = self.persist

    def op(self, eng, fn, r=(), w=()):
        return self.S.op(eng, fn, [b.t for b in r], [b.t for b in w])

    def dma(self, key, out, in_, r=(), w=(), q="sp"):
        return self.S.dma(q, key, out, in_, [b.t for b in r], [b.t for b in w])

    def mm(self, pbuf, out, lhsT, rhs, start, stop, r):
        self.op("pe", lambda e: e.matmul(out, lhsT=lhsT, rhs=rhs, start=start, stop=stop), r, [pbuf])

    def tr(self, pbuf, out, in_, ident, r):
        self.op("pe", lambda e: e.transpose(out=out, in_=in_, identity=ident), r, [pbuf])

    def tt(self, eng, out, a, b, op, r, w):
        self.op(eng, lambda e: e.tensor_tensor(out=out, in0=a, in1=b, op=op), r, w)

    def ts(self, eng, out, a, s1, s2, op0, op1, r, w):
        if s2 is None:
            self.op(eng, lambda e: e.tensor_scalar(out=out, in0=a, scalar1=s1, scalar2=None, op0=op0), r, w)
        else:
            self.op(eng, lambda e: e.tensor_scalar(out=out, in0=a, scalar1=s1, scalar2=s2, op0=op0, op1=op1), r, w)

    def stt(self, eng, out, a, s, b, op0, op1, r, w):
        self.op(eng, lambda e: e.scalar_tensor_tensor(out=out, in0=a, scalar=s, in1=b, op0=op0, op1=op1), r, w)

    def cp(self, eng, out, a, r, w):
        if eng == "act":
            self.op("act", lambda e: e.activation(out=out, in_=a, func=AF.Copy), r, w)
        else:
            self.op(eng, lambda e: e.tensor_copy(out=out, in_=a), r, w)

    def act(self, out, a, func, r, w, bias=None, scale=None, accum=None):
        kw = {}
        if bias is not None:
            kw["bias"] = bias
        if scale is not None:
            kw["scale"] = scale
        if accum is not None:
            kw["accum_out"] = accum
        self.op("act", lambda e: e.activation(out=out, in_=a, func=func, **kw), r, w)

    def memset(self, eng, out, val, w):
        self.op(eng, lambda e: e.memset(out, val), (), w)

    def barrier(self):
        S = self.S
        self.nbar += 1
        k = self.nbar
        for e in S.ENGS:
            S.ops[e].append(dict(barrier=k, fn=None, deps=[], dma=None, signal=False,
                                 dcnt=dict(S.dma_cnt)))


def build_program(stop_after=None, debug=()):
    nc = bass.Bass("TRN2", target_bir_lowering=False)
    kb = KB(nc)
    K = kb

    def din(name, shape, dt=F32):
        return nc.dram_tensor(name, list(shape), dt, kind="ExternalInput").ap()

    def dscr(name, shape, dt):
        kind = "ExternalOutput" if name in debug else "Internal"
        return nc.dram_tensor(name, list(shape), dt, kind=kind).ap()

    xc = din("xc", [NT_LOC, 1024])
    pc = din("pc", [NT_OWN, 256])
    posc = din("posc", [1, NT_LOC], I32)
    validc = din("validc", [1, 512])
    ownc = din("ownc", [1, 512])
    ident_d = din("ident", [128, 128])
    iota_d = din("iota", [128, 128])
    invf_d = din("invf", [128, 2])
    koh_d = din("koh", [32, NT_LOC])
    cm_d = din("cmask", [4, 128, 512])
    g_mix = din("g_mix", [1, 1024])
    w_in = din("w_in", [1024, 4096])
    w_perm = din("w_perm", [1024, 1024])
    s5 = {n: din("s5_" + n, shp) for n, shp in [
        ("ldt", [128, 16]), ("are", [128, 16]), ("aim", [128, 16]),
        ("bre", [128, 256]), ("bim", [128, 256]), ("cre", [128, 256]), ("cim", [128, 256])]}
    ssm_d = din("ssm_d", [1, 512])
    w_glu = din("w_glu", [512, 512])
    w_ps = din("w_ps", [512, 1024])
    w_pa = din("w_pa", [512, 1024])
    w_out = din("w_out", [1024, 1024])
    g_ffn = din("g_ffn", [1, 1024])
    w_q = din("w_q", [1024, 2048])
    keys = din("keys", [16, 128, 128])
    peer_u = din("peer_u", [16384, 1024])
    peer_v = din("peer_v", [16384, 1024])
    g_ple = din("g_ple", [1, 1024])
    w_pg = din("w_pg", [1024, 1024])
    w_pp = din("w_pp", [256, 1024])
    g_fin = din("g_fin", [1, 1024])
    out = nc.dram_tensor("out", [NT_OWN, 1024], F32, kind="ExternalOutput").ap()

    qT_s = dscr("qT_s", [4, 128, NT_OWN], BF16)
    kT_s = dscr("kT_s", [4, 128, NT_LOC], BF16)
    v_s = dscr("v_s", [NT_LOC, 8 * 65], BF16)
    gT_s = dscr("gT_s", [16, 128, NT_OWN], BF16)
    ssmT_s = dscr("ssmT_s", [4, 128, NT_OWN], BF16)
    attT_s = dscr("attT_s", [4, 128, NT_OWN], BF16)
    x1_s = dscr("x1_s", [NT_OWN, 1024], F32)
    h2T_s = dscr("h2T_s", [8, 128, NT_OWN], BF16)
    sc_s = dscr("sc_s", [NT_OWN, 16 * 128], F32)
    G_s = dscr("G_s", [32, 128, 128, 128], BF16)
    x2_s = dscr("x2_s", [NT_OWN, 1024], F32)
    UT5_s = dscr("UT5_s", [8, 128, 32 * 128], BF16)
    UT_s = dscr("UT_s", [128, 128, 1024], BF16)

    from contextlib import ExitStack
    st = ExitStack()
    PS = []
    for i in range(8):
        t = st.enter_context(nc.psum_tensor("ps%d" % i, [128, 512], F32))
        PS.append(Buf(t[:], "ps%d" % i))
    psi = [0]

    def nps():
        b = PS[psi[0] % 8]
        psi[0] += 1
        return b

    idf = K.sb([128, 128], F32, "idf")
    idb = K.sb([128, 128], BF16, "idb")
    K.dma("c0", idf.ap, ident_d, w=[idf])
    K.cp("dve", idb.ap, idf.ap, [idf], [idb])
    ksum = K.sb([128, 4, 32], F32, "ksum")
    K.persist = K.off

    ut5tok = [Buf(None, "ut5_%d" % i) for i in range(8)]

    def rmsnorm_tile(xt, gt, hb, sq, ss, rs):
        K.act(sq.ap, xt.ap, AF.Square, [xt], [sq, ss], accum=ss.ap)
        K.act(rs.ap, ss.ap, AF.Sqrt, [ss], [rs], scale=1.0 / 1024, bias=eps_b.ap)
        K.op("dve", lambda e: e.reciprocal(out=rs.ap, in_=rs.ap), [rs], [rs])
        K.stt("dve", hb.ap, xt.ap, rs.ap[:, 0:1], gt.ap, ALU.mult, ALU.mult, [xt, rs, gt], [hb])

    def load_w_bf(dst, src_ap, rows_kc, ncols, stage, key):
        for kc in range(rows_kc):
            K.dma(key, stage.ap[:, 0:ncols], src_ap[kc * 128:(kc + 1) * 128, :], w=[stage])
            K.cp("dve" if kc % 2 == 0 else "act", dst.ap[:, kc, :], stage.ap[:, 0:ncols], [stage], [dst])

    eps_b = K.sb([128, 1], F32, "eps")
    K.memset("dve", eps_b.ap, 1e-6, [eps_b])
    K.persist = K.off

    def phase_A():
        K.phase_reset()
        win = K.sb([128, 8, 3584], BF16, "win")
        wu = K.sb([128, 8, 512], BF16, "wuA")
        Ustk = K.sb([128, 32, 8, 16], BF16, "UstkA")
        UTo = K.sb([128, 32, 128], BF16, "UToA")
        wpm = K.sb([128, 8, 1024], BF16, "wpm")
        stageA = K.sb([128, 1792], F32, "stageA")
        stageB = K.sb([128, 1792], F32, "stageB")
        stage = stageA
        gt = K.sb([128, 1024], F32, "gmix")
        invf = K.sb([128, 2], F32, "invf")
        xts = [K.sb([128, 1024], F32, "xt%d" % i) for i in range(2)]
        sq = K.sb([128, 1024], BF16, "sq")
        ss = K.sb([128, 1], F32, "ss")
        rs = K.sb([128, 1], F32, "rs")
        hbs = [K.sb([128, 1024], BF16, "hb%d" % i) for i in range(2)]
        hTs = [K.sb([128, 8, 1024], BF16, "hT%d" % i) for i in range(2)]
        posi = K.sb([128, 1024], I32, "posi")
        ang = K.sb([128, 1024], F32, "ang")
        tmpa = K.sb([128, 1024], F32, "tmpa")
        tmpi = K.sb([128, 1024], I32, "tmpi")
        cosTs = [K.sb([128, 1024], F32, "cosT%d" % i) for i in range(2)]
        sinTs = [K.sb([128, 1024], F32, "sinT%d" % i) for i in range(2)]
        t1s = [K.sb([128, 512], F32, "t1_%d" % i) for i in range(2)]
        t2s = [K.sb([128, 512], F32, "t2_%d" % i) for i in range(2)]
        obf = [K.sb([128, 512], BF16, "obf%d" % i) for i in range(2)]
        vts = [K.sb([128, 8, 65], BF16, "vt%d" % i) for i in range(2)]
        for v in vts:
            K.memset("pool", v.ap, 1.0, [v])
        K.dma("c0", gt.ap, g_mix.partition_broadcast(128), w=[gt])
        K.dma("c0", invf.ap, invf_d, w=[invf])
        n_st = [0]

        def stream_w(dst_ap, src_ap, ncols):
            i = n_st[0]
            n_st[0] += 1
            stg_ = (stageA, stageB)[i % 2]
            K.dma("wst%d" % (i % 2), stg_.ap[:, 0:ncols], src_ap, w=[stg_])
            K.cp("dve" if i % 2 == 0 else "act", dst_ap, stg_.ap[:, 0:ncols], [stg_], [win])

        for kc in range(8):
            stream_w(win.ap[:, kc, 0:1792], w_in[kc * 128:(kc + 1) * 128, 512:2304], 1792)
            stream_w(win.ap[:, kc, 1792:3584], w_in[kc * 128:(kc + 1) * 128, 2304:4096], 1792)
        for kc in range(8):
            i = n_st[0]
            n_st[0] += 1
            stg_ = (stageA, stageB)[i % 2]
            K.dma("wst%d" % (i % 2), stg_.ap[:, 0:1024], w_perm[kc * 128:(kc + 1) * 128, :], w=[stg_])
            K.cp("dve" if i % 2 == 0 else "act", wpm.ap[:, kc, :], stg_.ap[:, 0:1024], [stg_], [wpm])
        for kc in range(8):
            i = n_st[0]
            n_st[0] += 1
            stg_ = (stageA, stageB)[i % 2]
            K.dma("wst%d" % (i % 2), stg_.ap[:, 0:512], w_in[kc * 128:(kc + 1) * 128, 0:512], w=[stg_])
            K.cp("dve" if i % 2 == 0 else "act", wu.ap[:, kc, :], stg_.ap[:, 0:512], [stg_], [wu])

        def sincos(dst, phase):
            K.ts("dve", tmpa.ap, ang.ap, phase, 1.0 / (2 * PI), ALU.add, ALU.mult, [ang], [tmpa])
            K.cp("dve", tmpi.ap, tmpa.ap, [tmpa], [tmpi])
            K.cp("dve", tmpa.ap, tmpi.ap, [tmpi], [tmpa])
            K.stt("dve", tmpa.ap, tmpa.ap, -2 * PI, ang.ap, ALU.mult, ALU.add, [tmpa, ang], [tmpa])
            K.ts("dve", tmpa.ap, tmpa.ap, phase, None, ALU.add, None, [tmpa], [tmpa])
            K.ts("dve", dst.ap, tmpa.ap, PI, -2 * PI, ALU.is_gt, ALU.mult, [tmpa], [dst])
            K.tt("dve", tmpa.ap, tmpa.ap, dst.ap, ALU.add, [tmpa, dst], [tmpa])
            K.ts("dve", dst.ap, tmpa.ap, -PI, 2 * PI, ALU.is_lt, ALU.mult, [tmpa], [dst])
            K.tt("dve", tmpa.ap, tmpa.ap, dst.ap, ALU.add, [tmpa, dst], [tmpa])
            K.act(dst.ap, tmpa.ap, AF.Sin, [tmpa], [dst])

        xic = [0]

        def prep(blk):
            tb = blk * 1024
            hT = hTs[blk % 2]
            cosT, sinT = cosTs[blk % 2], sinTs[blk % 2]
            xi = xic[0]
            K.dma("pos", posi.ap, posc[:, tb:tb + 1024].partition_broadcast(128), w=[posi])
            K.cp("dve", ang.ap, posi.ap, [posi], [ang])
            K.ts("dve", ang.ap, ang.ap, invf.ap[:, 0:1], None, ALU.mult, None, [ang, invf], [ang])
            sincos(cosT, PI / 2)
            sincos(sinT, 0.0)
            K.ts("dve", sinT.ap, sinT.ap, invf.ap[:, 1:2], None, ALU.mult, None, [sinT, invf], [sinT])
            for ti in range(8):
                xt = xts[xi % 2]
                hb = hbs[xi % 2]
                xi += 1
                K.dma("x%d" % (xi % 2), xt.ap, xc[tb + ti * 128: tb + (ti + 1) * 128, :], w=[xt])
                rmsnorm_tile(xt, gt, hb, sq, ss, rs)

                def a_tr(ti=ti, hb=hb, hT=hT):
                    p = nps()
                    pv = p.ap.bitcast(BF16).rearrange("p (a b) -> p a b", a=8)
                    for kc in range(8):
                        K.tr(p, pv[:, kc, :], hb.ap[:, kc * 128:(kc + 1) * 128], idb.ap, [hb, idb])
                    K.cp("act", hT.ap[:, :, ti * 128:(ti + 1) * 128], pv, [p], [hT])

                if ti > 0:
                    pend_a()
                pend_a = a_tr
            pend_a()
            xic[0] = xi

        oic = [0]

        def compute(blk):
            own = blk >= 4
            tb = blk * 1024
            hT = hTs[blk % 2]
            cosT, sinT = cosTs[blk % 2], sinTs[blk % 2]
            oi = oic[0]
            for j in range(8):
                p = nps()
                for kc in range(8):
                    K.mm(p, p.ap, hT.ap[:, kc, j:1024:8], wu.ap[:, kc, :], kc == 0, kc == 7, [hT, wu])
                K.cp("act" if j % 2 else "dve", Ustk.ap[:, :, j, :], p.ap.rearrange("p (g c) -> p g c", g=32), [p], [Ustk])
            for g0 in range(0, 32, 8):
                p = nps()
                pv = p.ap.bitcast(BF16).rearrange("p (a b) -> p a b", a=8)
                for gi in range(8):
                    K.tr(p, pv[:, gi, :], Ustk.ap[:, g0 + gi, :, :].rearrange("p a b -> p (a b)"), idb.ap, [Ustk, idb])
                K.cp("act", UTo.ap[:, g0:g0 + 8, :], pv, [p], [UTo])
            K.dma("utA", UT5_s[blk], UTo.ap.rearrange("p a b -> p (a b)"), r=[UTo], w=[ut5tok[blk]])
            for which in (["q", "k"] if own else ["k"]):
                cbase = 0 if which == "q" else 512
                for c in range(4):
                    for half in range(2):
                        pa = nps()
                        pb = nps()
                        for kc in range(8):
                            K.mm(pa, pa.ap, win.ap[:, kc, cbase + c * 128: cbase + (c + 1) * 128],
                                 hT.ap[:, kc, half * 512:(half + 1) * 512], kc == 0, kc == 7, [win, hT])
                        for kc in range(8):
                            K.mm(pb, pb.ap, wpm.ap[:, kc, cbase + c * 128: cbase + (c + 1) * 128],
                                 hT.ap[:, kc, half * 512:(half + 1) * 512], kc == 0, kc == 7, [wpm, hT])
                        t1, t2 = t1s[oi % 2], t2s[oi % 2]
                        K.tt("dve", t1.ap, pa.ap, cosT.ap[:, half * 512:(half + 1) * 512], ALU.mult, [pa, cosT], [t1])
                        K.tt("dve", t2.ap, pb.ap, sinT.ap[:, half * 512:(half + 1) * 512], ALU.mult, [pb, sinT], [t2])
                        K.tt("dve", t1.ap, t1.ap, t2.ap, ALU.add, [t1, t2], [t1])
                        ob = obf[oi % 2]
                        oi += 1
                        K.cp("act", ob.ap, t1.ap, [t1], [ob])
                        t0 = tb + half * 512
                        if which == "q":
                            K.dma("oq%d" % (oi % 2), qT_s[c, :, t0 - NT_OWN: t0 - NT_OWN + 512], ob.ap, r=[ob])
                        else:
                            K.dma("oq%d" % (oi % 2), kT_s[c, :, t0: t0 + 512], ob.ap, r=[ob])
                            K.op("dve", lambda e, c=c, b0=t0 // 256, t1=t1: e.tensor_reduce(
                                out=ksum.ap[:, c, b0:b0 + 2], in_=t1.ap.rearrange("p (a b) -> p a b", a=2),
                                axis=AX.X, op=ALU.add), [t1], [ksum])
            for ti in range(8):
                p = nps()
                for kc in range(8):
                    K.mm(p, p.ap, hT.ap[:, kc, ti * 128:(ti + 1) * 128], win.ap[:, kc, 1024:1536],
                         kc == 0, kc == 7, [hT, win])
                vt = vts[ti % 2]
                K.cp("act", vt.ap[:, :, 0:64], p.ap.rearrange("p (h d) -> p h d", h=8), [p], [vt])
                K.dma("ov%d" % (ti % 2), v_s[tb + ti * 128: tb + (ti + 1) * 128, :].rearrange("p (h d) -> p h d", h=8),
                      vt.ap, r=[vt])
            if own:
                for c in range(16):
                    for half in range(2):
                        p = nps()
                        for kc in range(8):
                            K.mm(p, p.ap, win.ap[:, kc, 1536 + c * 128: 1536 + (c + 1) * 128],
                                 hT.ap[:, kc, half * 512:(half + 1) * 512], kc == 0, kc == 7, [win, hT])
                        ob = obf[oi % 2]
                        oi += 1
                        K.act(ob.ap, p.ap, AF.Sigmoid, [p], [ob])
                        t0 = tb + half * 512 - NT_OWN
                        K.dma("oq%d" % (oi % 2), gT_s[c, :, t0:t0 + 512], ob.ap, r=[ob])
            oic[0] = oi

        prep(0)
        for blk in range(8):
            if blk + 1 < 8:
                prep(blk + 1)
            compute(blk)
        K.barrier()

    def phase_S():
        K.phase_reset()
        sm = lambda name, n=16: K.sb([128, n], F32, name)
        wglu = K.sb([128, 4, 512], BF16, "wglu")
        WS = K.sb([128, 16, 2, 2, 128], BF16, "WS")
        WY1 = K.sb([128, 16, 2, 2, 128], BF16, "WY1")
        WY2 = K.sb([128, 32, 128], BF16, "WY2")
        Ct = K.sb([128, 16, 128], F32, "Ct")
        St = K.sb([128, 16, 128], F32, "St")
        r8 = sm("r8")
        Dre = sm("Dre")
        Dim = sm("Dim")
        car_re = sm("car_re")
        car_im = sm("car_im")
        ta, tb_, tc_ = sm("ta"), sm("tb"), sm("tc")
        mark = K.off
        stage = K.sb([128, 512], F32, "stageS")
        ldt, are, aim = sm("ldt"), sm("are"), sm("aim")
        bre = K.sb([128, 16, 16], F32, "bre")
        bim = K.sb([128, 16, 16], F32, "bim")
        cre = K.sb([128, 16, 16], F32, "cre")
        cim = K.sb([128, 16, 16], F32, "cim")
        ncim = K.sb([128, 16, 16], F32, "ncim")
        for t_, nm in ((ldt, "ldt"), (are, "are"), (aim, "aim")):
            K.dma("c0", t_.ap, s5[nm], w=[t_])
        for t_, nm in ((bre, "bre"), (bim, "bim"), (cre, "cre"), (cim, "cim")):
            K.dma("c0", t_.ap.rearrange("p a b -> p (a b)"), s5[nm], w=[t_])
        for kc in range(4):
            K.dma("wst", stage.ap, w_glu[kc * 128:(kc + 1) * 128, :], w=[stage])
            K.cp("dve", wglu.ap[:, kc, :], stage.ap, [stage], [wglu])
        dt_, xr, th, mag, cs, sn = sm("dt"), sm("xr"), sm("th"), sm("mag"), sm("cs"), sm("sn")
        abr, abi, den, nr, fre, fim = sm("abr"), sm("abi"), sm("den"), sm("nr"), sm("fre"), sm("fim")
        ti_ = K.sb([128, 16], I32, "ti")
        V_ = "dve"
        K.act(dt_.ap, ldt.ap, AF.Exp, [ldt], [dt_])
        K.tt(V_, xr.ap, dt_.ap, are.ap, ALU.mult, [dt_, are], [xr])
        K.tt(V_, th.ap, dt_.ap, aim.ap, ALU.mult, [dt_, aim], [th])
        K.act(mag.ap, xr.ap, AF.Exp, [xr], [mag])
        K.act(r8.ap, xr.ap, AF.Exp, [xr], [r8], scale=8.0)

        def sin_small(dst, src, phase):
            K.ts(V_, ta.ap, src.ap, phase, 1.0 / (2 * PI), ALU.add, ALU.mult, [src], [ta])
            K.cp(V_, ti_.ap, ta.ap, [ta], [ti_])
            K.cp(V_, ta.ap, ti_.ap, [ti_], [ta])
            K.stt(V_, ta.ap, ta.ap, -2 * PI, src.ap, ALU.mult, ALU.add, [ta, src], [ta])
            K.ts(V_, ta.ap, ta.ap, phase, None, ALU.add, None, [ta], [ta])
            K.ts(V_, tb_.ap, ta.ap, PI, -2 * PI, ALU.is_gt, ALU.mult, [ta], [tb_])
            K.tt(V_, ta.ap, ta.ap, tb_.ap, ALU.add, [ta, tb_], [ta])
            K.ts(V_, tb_.ap, ta.ap, -PI, 2 * PI, ALU.is_lt, ALU.mult, [ta], [tb_])
            K.tt(V_, ta.ap, ta.ap, tb_.ap, ALU.add, [ta, tb_], [ta])
            K.act(dst.ap, ta.ap, AF.Sin, [ta], [dst])

        sin_small(cs, th, PI / 2)
        sin_small(sn, th, 0.0)
        K.tt(V_, abr.ap, mag.ap, cs.ap, ALU.mult, [mag, cs], [abr])
        K.tt(V_, abi.ap, mag.ap, sn.ap, ALU.mult, [mag, sn], [abi])
        K.tt(V_, den.ap, are.ap, are.ap, ALU.mult, [are], [den])
        K.tt(V_, ta.ap, aim.ap, aim.ap, ALU.mult, [aim], [ta])
        K.tt(V_, den.ap, den.ap, ta.ap, ALU.add, [den, ta], [den])
        K.op(V_, lambda e: e.reciprocal(out=den.ap, in_=den.ap), [den], [den])
        K.ts(V_, nr.ap, abr.ap, -1.0, None, ALU.add, None, [abr], [nr])
        K.tt(V_, ta.ap, nr.ap, are.ap, ALU.mult, [nr, are], [ta])
        K.tt(V_, tb_.ap, abi.ap, aim.ap, ALU.mult, [abi, aim], [tb_])
        K.tt(V_, ta.ap, ta.ap, tb_.ap, ALU.add, [ta, tb_], [ta])
        K.tt(V_, fre.ap, ta.ap, den.ap, ALU.mult, [ta, den], [fre])
        K.tt(V_, ta.ap, abi.ap, are.ap, ALU.mult, [abi, are], [ta])
        K.tt(V_, tb_.ap, nr.ap, aim.ap, ALU.mult, [nr, aim], [tb_])
        K.tt(V_, ta.ap, ta.ap, tb_.ap, ALU.subtract, [ta, tb_], [ta])
        K.tt(V_, fim.ap, ta.ap, den.ap, ALU.mult, [ta, den], [fim])
        pwf_re = K.sb([128, 16, 9], F32, "pwf_re")
        pwf_im = K.sb([128, 16, 9], F32, "pwf_im")
        pwr_re = K.sb([128, 16, 8], F32, "pwr_re")
        pwr_im = K.sb([128, 16, 8], F32, "pwr_im")
        K.memset(V_, pwf_re.ap[:, :, 0], 1.0, [pwf_re])
        K.memset(V_, pwf_im.ap[:, :, 0], 0.0, [pwf_im])
        for d in range(8):
            K.tt(V_, ta.ap, pwf_re.ap[:, :, d], abr.ap, ALU.mult, [pwf_re, abr], [ta])
            K.tt(V_, tb_.ap, pwf_im.ap[:, :, d], abi.ap, ALU.mult, [pwf_im, abi], [tb_])
            K.tt(V_, pwf_re.ap[:, :, d + 1], ta.ap, tb_.ap, ALU.subtract, [ta, tb_], [pwf_re])
            K.tt(V_, ta.ap, pwf_re.ap[:, :, d], abi.ap, ALU.mult, [pwf_re, abi], [ta])
            K.tt(V_, tb_.ap, pwf_im.ap[:, :, d], abr.ap, ALU.mult, [pwf_im, abr], [tb_])
            K.tt(V_, pwf_im.ap[:, :, d + 1], ta.ap, tb_.ap, ALU.add, [ta, tb_], [pwf_im])
        for j in range(8):
            K.cp(V_, pwr_re.ap[:, :, j], pwf_re.ap[:, :, 7 - j], [pwf_re], [pwr_re])
            K.cp(V_, pwr_im.ap[:, :, j], pwf_im.ap[:, :, 7 - j], [pwf_im], [pwr_im])
        K.cp(V_, Dre.ap, pwf_re.ap[:, :, 8], [pwf_re], [Dre])
        K.cp(V_, Dim.ap, pwf_im.ap[:, :, 8], [pwf_im], [Dim])
        ur, ui, rr = sm("ur"), sm("ui"), sm("rr")
        K.op(V_, lambda e: e.reciprocal(out=rr.ap, in_=r8.ap), [r8], [rr])
        K.tt(V_, ur.ap, Dre.ap, rr.ap, ALU.mult, [Dre, rr], [ur])
        K.tt(V_, ui.ap, Dim.ap, rr.ap, ALU.mult, [Dim, rr], [ui])
        tm1 = K.sb([128, 16, 64], F32, "tm1")
        tm2 = K.sb([128, 16, 64], F32, "tm2")
        K.memset(V_, Ct.ap[:, :, 0], 1.0, [Ct])
        K.memset(V_, St.ap[:, :, 0], 0.0, [St])
        for k in range(7):
            n = 1 << k
            urb = ur.ap.unsqueeze(2).broadcast_to([128, 16, n])
            uib = ui.ap.unsqueeze(2).broadcast_to([128, 16, n])
            K.tt(V_, tm1.ap[:, :, 0:n], Ct.ap[:, :, 0:n], urb, ALU.mult, [Ct, ur], [tm1])
            K.tt(V_, tm2.ap[:, :, 0:n], St.ap[:, :, 0:n], uib, ALU.mult, [St, ui], [tm2])
            K.tt(V_, Ct.ap[:, :, n:2 * n], tm1.ap[:, :, 0:n], tm2.ap[:, :, 0:n], ALU.subtract, [tm1, tm2], [Ct])
            K.tt(V_, tm1.ap[:, :, 0:n], Ct.ap[:, :, 0:n], uib, ALU.mult, [Ct, ui], [tm1])
            K.tt(V_, tm2.ap[:, :, 0:n], St.ap[:, :, 0:n], urb, ALU.mult, [St, ur], [tm2])
            K.tt(V_, St.ap[:, :, n:2 * n], tm1.ap[:, :, 0:n], tm2.ap[:, :, 0:n], ALU.add, [tm1, tm2], [St])
            K.tt(V_, ta.ap, ur.ap, ur.ap, ALU.mult, [ur], [ta])
            K.tt(V_, tb_.ap, ui.ap, ui.ap, ALU.mult, [ui], [tb_])
            K.tt(V_, tc_.ap, ur.ap, ui.ap, ALU.mult, [ur, ui], [tc_])
            K.tt(V_, ur.ap, ta.ap, tb_.ap, ALU.subtract, [ta, tb_], [ur])
            K.ts(V_, ui.ap, tc_.ap, 2.0, None, ALU.mult, None, [tc_], [ui])
        bbr = K.sb([128, 16, 16], F32, "bbr")
        bbi = K.sb([128, 16, 16], F32, "bbi")
        t3a = K.sb([128, 16, 16], F32, "t3a")
        t3b = K.sb([128, 16, 16], F32, "t3b")
        freb = fre.ap.unsqueeze(2).broadcast_to([128, 16, 16])
        fimb = fim.ap.unsqueeze(2).broadcast_to([128, 16, 16])
        K.tt(V_, t3a.ap, bre.ap, freb, ALU.mult, [bre, fre], [t3a])
        K.tt(V_, t3b.ap, bim.ap, fimb, ALU.mult, [bim, fim], [t3b])
        K.tt(V_, bbr.ap, t3a.ap, t3b.ap, ALU.subtract, [t3a, t3b], [bbr])
        K.tt(V_, t3a.ap, bim.ap, freb, ALU.mult, [bim, fre], [t3a])
        K.tt(V_, t3b.ap, bre.ap, fimb, ALU.mult, [bre, fim], [t3b])
        K.tt(V_, bbi.ap, t3a.ap, t3b.ap, ALU.add, [t3a, t3b], [bbi])
        K.ts(V_, ncim.ap, cim.ap, -1.0, None, ALU.mult, None, [cim], [ncim])
        Fre = K.sb([128, 16, 15, 16], F32, "Fre")
        Fim = K.sb([128, 16, 15, 16], F32, "Fim")
        t4a = K.sb([128, 16, 8, 16], F32, "t4a")
        t4b = K.sb([128, 16, 8, 16], F32, "t4b")
        K.memset("pool", Fre.ap, 0.0, [Fre])
        K.memset("pool", Fim.ap, 0.0, [Fim])
        S4 = [128, 16, 8, 16]
        prb = pwr_re.ap.unsqueeze(3).broadcast_to(S4)
        pib = pwr_im.ap.unsqueeze(3).broadcast_to(S4)
        bbrb = bbr.ap.unsqueeze(2).broadcast_to(S4)
        bbib = bbi.ap.unsqueeze(2).broadcast_to(S4)
        K.tt(V_, t4a.ap, prb, bbrb, ALU.mult, [pwr_re, bbr], [t4a])
        K.tt(V_, t4b.ap, pib, bbib, ALU.mult, [pwr_im, bbi], [t4b])
        K.tt(V_, Fre.ap[:, :, 0:8, :], t4a.ap, t4b.ap, ALU.subtract, [t4a, t4b], [Fre])
        K.tt(V_, t4a.ap, prb, bbib, ALU.mult, [pwr_re, bbi], [t4a])
        K.tt(V_, t4b.ap, pib, bbrb, ALU.mult, [pwr_im, bbr], [t4b])
        K.tt(V_, Fim.ap[:, :, 0:8, :], t4a.ap, t4b.ap, ALU.add, [t4a, t4b], [Fim])
        K.memset("pool", WS.ap, 0.0, [WS])
        K.memset("pool", WY1.ap, 0.0, [WY1])
        for p_ in range(16):
            for ri, Ft in ((0, Fre), (1, Fim)):
                ps = nps()
                K.tr(ps, ps.ap[:, 0:128], Ft.ap[:, p_, 0:8, :].rearrange("p a b -> p (a b)"), idf.ap, [Ft, idf])
                K.cp(V_, WS.ap[:, p_, ri, 0, 0:64], ps.ap[:, 0:64], [ps], [WS])
                K.cp(V_, WS.ap[:, p_, ri, 1, 64:128], ps.ap[:, 64:128], [ps], [WS])
        pfr = pwf_re.ap[:, :, 1:9].unsqueeze(3).broadcast_to(S4)
        pfi = pwf_im.ap[:, :, 1:9].unsqueeze(3).broadcast_to(S4)
        creb = cre.ap.unsqueeze(2).broadcast_to(S4)
        cimb = cim.ap.unsqueeze(2).broadcast_to(S4)
        K.tt(V_, t4a.ap, creb, pfr, ALU.mult, [cre, pwf_re], [t4a])
        K.tt(V_, t4b.ap, cimb, pfi, ALU.mult, [cim, pwf_im], [t4b])
        K.tt(V_, t4a.ap, t4a.ap, t4b.ap, ALU.subtract, [t4a, t4b], [t4a])
        for g2 in range(2):
            K.cp(V_, WY1.ap[g2 * 64:(g2 + 1) * 64, :, 0, g2, :],
                 t4a.ap[g2 * 64:(g2 + 1) * 64].rearrange("p a b c -> p a (b c)"), [t4a], [WY1])
        K.tt(V_, t4a.ap, creb, pfi, ALU.mult, [cre, pwf_im], [t4a])
        K.tt(V_, t4b.ap, cimb, pfr, ALU.mult, [cim, pwf_re], [t4b])
        K.tt(V_, t4a.ap, t4a.ap, t4b.ap, ALU.add, [t4a, t4b], [t4a])
        K.ts(V_, t4a.ap, t4a.ap, -1.0, None, ALU.mult, None, [t4a], [t4a])
        for g2 in range(2):
            K.cp(V_, WY1.ap[g2 * 64:(g2 + 1) * 64, :, 1, g2, :],
                 t4a.ap[g2 * 64:(g2 + 1) * 64].rearrange("p a b c -> p a (b c)"), [t4a], [WY1])
        dB = K.sb([128, 512], F32, "dB")
        dI = K.sb([128, 32, 8, 16], F32, "dI")
        K.dma("c0", dB.ap, ssm_d.partition_broadcast(128), w=[dB])
        K.tt(V_, dI.ap, idf.ap.rearrange("p (a b) -> p a b", a=8).unsqueeze(1).broadcast_to([128, 32, 8, 16]),
             dB.ap.rearrange("p (g c) -> p g c", g=32).unsqueeze(2).broadcast_to([128, 32, 8, 16]), ALU.mult,
             [idf, dB], [dI])
        for g in range(32):
            p_, g2 = g // 2, g % 2
            if g % 4 == 0:
                ps = nps()
            o0 = (g % 4) * 128
            sl = slice(g2 * 64, (g2 + 1) * 64)
            for j in range(8):
                oap = ps.ap[:, o0 + j * 16: o0 + (j + 1) * 16]
                K.mm(ps, oap, Fre.ap[sl, p_, 7 - j:15 - j, :].rearrange("p a b -> p (a b)"), cre.ap[sl, p_, :],
                     True, False, [Fre, cre])
                K.mm(ps, oap, Fim.ap[sl, p_, 7 - j:15 - j, :].rearrange("p a b -> p (a b)"), ncim.ap[sl, p_, :],
                     False, True, [Fim, ncim])
            if g % 4 == 3:
                K.tt(V_, WY2.ap[:, g - 3:g + 1, :], ps.ap.rearrange("p (g k) -> p g k", g=4),
                     dI.ap[:, g - 3:g + 1].rearrange("p g a b -> p g (a b)"), ALU.add, [ps, dI], [WY2])
        K.memset(V_, car_re.ap, 0.0, [car_re])
        K.memset(V_, car_im.ap, 0.0, [car_im])
        K.barrier()
        K.off = mark
        UTs = [K.sb([128, 32, 128], BF16, "UT%d" % i) for i in range(2)]
        Sres = [K.sb([128, 16, 128], F32, "Sre%d" % i) for i in range(2)]
        Sims = [K.sb([128, 16, 128], F32, "Sim%d" % i) for i in range(2)]
        gre = K.sb([128, 16, 128], F32, "gre")
        gim = K.sb([128, 16, 128], F32, "gim")
        u1 = K.sb([128, 16, 128], F32, "u1")
        u2 = K.sb([128, 16, 128], F32, "u2")
        Pre = K.sb([128, 16, 129], BF16, "Pre")
        Pim = K.sb([128, 16, 129], BF16, "Pim")
        ytm = K.sb([128, 8, 512], BF16, "ytm")
        yT = K.sb([128, 4, 1024], BF16, "yT")
        sg = K.sb([128, 512], BF16, "sg")
        sos = [K.sb([128, 4, 512], BF16, "so%d" % i) for i in range(2)]

        def S1(blk):
            UT, Sre, Sim = UTs[blk % 2], Sres[blk % 2], Sims[blk % 2]
            K.dma("ut%d" % (blk % 2), UT.ap.rearrange("p a b -> p (a b)"), UT5_s[blk], r=[ut5tok[blk]], w=[UT])
            for ri, Sx in ((0, Sre), (1, Sim)):
                for p0 in range(0, 16, 4):
                    p = nps()
                    for pi_ in range(4):
                        pp = p0 + pi_
                        K.mm(p, p.ap[:, pi_ * 128:(pi_ + 1) * 128], WS.ap[:, pp, ri, 0, :], UT.ap[:, 2 * pp, :], True, False, [WS, UT])
                        K.mm(p, p.ap[:, pi_ * 128:(pi_ + 1) * 128], WS.ap[:, pp, ri, 1, :], UT.ap[:, 2 * pp + 1, :], False, True, [WS, UT])
                    K.cp("act", Sx.ap[:, p0:p0 + 4, :], p.ap.rearrange("p (a b) -> p a b", a=4), [p], [Sx])

        def SC(blk):
            own = blk >= 4
            Sre, Sim = Sres[blk % 2], Sims[blk % 2]
            if own:
                K.cp(V_, Pre.ap[:, :, 0], car_re.ap, [car_re], [Pre])
                K.cp(V_, Pim.ap[:, :, 0], car_im.ap, [car_im], [Pim])
            K.tt(V_, ta.ap, Dre.ap, car_re.ap, ALU.mult, [Dre, car_re], [ta])
            K.tt(V_, tb_.ap, Dim.ap, car_im.ap, ALU.mult, [Dim, car_im], [tb_])
            K.tt(V_, ta.ap, ta.ap, tb_.ap, ALU.subtract, [ta, tb_], [ta])
            K.tt(V_, Sre.ap[:, :, 0], Sre.ap[:, :, 0], ta.ap, ALU.add, [Sre, ta], [Sre])
            K.tt(V_, ta.ap, Dre.ap, car_im.ap, ALU.mult, [Dre, car_im], [ta])
            K.tt(V_, tb_.ap, Dim.ap, car_re.ap, ALU.mult, [Dim, car_re], [tb_])
            K.tt(V_, ta.ap, ta.ap, tb_.ap, ALU.add, [ta, tb_], [ta])
            K.tt(V_, Sim.ap[:, :, 0], Sim.ap[:, :, 0], ta.ap, ALU.add, [Sim, ta], [Sim])
            K.tt("dve", u1.ap, Ct.ap, Sre.ap, ALU.mult, [Ct, Sre], [u1])
            K.tt("pool", u2.ap, St.ap, Sim.ap, ALU.mult, [St, Sim], [u2])
            K.tt("dve", gre.ap, u1.ap, u2.ap, ALU.add, [u1, u2], [gre])
            K.tt("dve", u1.ap, Ct.ap, Sim.ap, ALU.mult, [Ct, Sim], [u1])
            K.tt("pool", u2.ap, St.ap, Sre.ap, ALU.mult, [St, Sre], [u2])
            K.tt("dve", gim.ap, u1.ap, u2.ap, ALU.subtract, [u1, u2], [gim])
            for pp in range(16):
                rb = r8.ap[:, pp:pp + 1].to_broadcast([128, 128])
                K.op("dve", lambda e, pp=pp, rb=rb, Sre=Sre: e.tensor_tensor_scan(out=Sre.ap[:, pp, :], data0=rb, data1=gre.ap[:, pp, :],
                                                                                 initial=0.0, op0=ALU.mult, op1=ALU.add), [gre, r8], [Sre])
                K.op("dve", lambda e, pp=pp, rb=rb, Sim=Sim: e.tensor_tensor_scan(out=Sim.ap[:, pp, :], data0=rb, data1=gim.ap[:, pp, :],
                                                                                 initial=0.0, op0=ALU.mult, op1=ALU.add), [gim, r8], [Sim])
            K.tt("dve", u1.ap, Ct.ap, Sre.ap, ALU.mult, [Ct, Sre], [u1])
            K.tt("pool", u2.ap, St.ap, Sim.ap, ALU.mult, [St, Sim], [u2])
            K.tt("dve", gre.ap, u1.ap, u2.ap, ALU.subtract, [u1, u2], [gre])
            K.tt("dve", u1.ap, Ct.ap, Sim.ap, ALU.mult, [Ct, Sim], [u1])
            K.tt("pool", u2.ap, St.ap, Sre.ap, ALU.mult, [St, Sre], [u2])
            K.tt("dve", gim.ap, u1.ap, u2.ap, ALU.add, [u1, u2], [gim])
            K.cp(V_, car_re.ap, gre.ap[:, :, 127], [gre], [car_re])
            K.cp(V_, car_im.ap, gim.ap[:, :, 127], [gim], [car_im])
            if own:
                K.cp("act", Pre.ap[:, :, 1:129], gre.ap, [gre], [Pre])
                K.cp("act", Pim.ap[:, :, 1:129], gim.ap, [gim], [Pim])

        def SY(blk):
            tb = blk * 1024
            UT = UTs[blk % 2]
            for g0 in range(0, 32, 4):
                p = nps()
                for gi in range(4):
                    g = g0 + gi
                    pp, g2 = g // 2, g % 2
                    oap = p.ap[:, gi * 128:(gi + 1) * 128]
                    K.mm(p, oap, Pre.ap[:, pp, 0:128], WY1.ap[:, pp, 0, g2, :], True, False, [Pre, WY1])
                    K.mm(p, oap, Pim.ap[:, pp, 0:128], WY1.ap[:, pp, 1, g2, :], False, False, [Pim, WY1])
                    K.mm(p, oap, UT.ap[:, g, :], WY2.ap[:, g, :], False, True, [UT, WY2])
                K.act(ytm.ap.rearrange("p j (g c) -> p g j c", g=32)[:, g0:g0 + 4],
                      p.ap.rearrange("p (g j c) -> p g j c", g=4, j=8), AF.Gelu_apprx_tanh, [p], [ytm])
            for j in range(8):
                if j % 2 == 0:
                    p = nps()
                    pv = p.ap.bitcast(BF16).rearrange("p (a b) -> p a b", a=8)
                for cc in range(4):
                    K.tr(p, pv[:, (j % 2) * 4 + cc, :], ytm.ap[:, j, cc * 128:(cc + 1) * 128], idb.ap, [ytm, idb])
                K.cp("act", yT.ap[:, :, j:1024:8], pv[:, (j % 2) * 4:(j % 2) * 4 + 4, :], [p], [yT])
            for half in range(2):
                for co in range(4):
                    p = nps()
                    for kc in range(4):
                        K.mm(p, p.ap, wglu.ap[:, kc, co * 128:(co + 1) * 128], yT.ap[:, kc, half * 512:(half + 1) * 512],
                             kc == 0, kc == 3, [wglu, yT])
                    K.act(sg.ap, p.ap, AF.Sigmoid, [p], [sg])
                    so = sos[half]
                    K.tt("pool", so.ap[:, co, :], yT.ap[:, co, half * 512:(half + 1) * 512], sg.ap,
                         ALU.mult, [yT, sg], [so])
                t0_ = tb - NT_OWN + half * 512
                K.dma("oS%d" % half, ssmT_s[:, :, t0_: t0_ + 512].rearrange("c p t -> p c t"), sos[half].ap, r=[sos[half]])

        S1(0)
        for blk in range(8):
            if blk + 1 < 8:
                S1(blk + 1)
            SC(blk)
            if blk >= 4:
                SY(blk)
        K.barrier()

    def phase_B():
        K.phase_reset()
        wpsB = K.sb([128, 4, 1024], BF16, "wpsB")
        wpaB = K.sb([128, 4, 1024], BF16, "wpaB")
        woutB = K.sb([128, 8, 1024], BF16, "woutB")
        wqB = K.sb([128, 8, 2048], BF16, "wqB")
        wstg2 = K.sb([128, 2048], F32, "wstg2")
        wchunks = ([(wpsB, w_ps, kc, 1024) for kc in range(4)] + [(wpaB, w_pa, kc, 1024) for kc in range(4)]
                   + [(woutB, w_out, kc, 1024) for kc in range(8)] + [(wqB, w_q, kc, 2048) for kc in range(8)])
        KTs = [K.sb([96, NT_LOC], BF16, "KT%d" % i) for i in range(2)]
        QTs = [K.sb([96, NT_OWN], BF16, "QT%d" % i) for i in range(2)]
        Vs = [K.sb([128, 64, 65], BF16, "V%d" % i) for i in range(2)]
        QA = K.sb([64, NT_OWN], BF16, "QA")
        att = K.sb([128, 32, 512], BF16, "att")
        kmax = K.sb([64, 1], F32, "kmax")
        kmaxb = K.sb([64, 1], BF16, "kmaxb")
        ksb = K.sb([64, 32], BF16, "ksb")
        valid = K.sb([128, 512], F32, "valid")
        ownm = K.sb([128, 512], F32, "ownm")
        negb = K.sb([128, 512], F32, "negb")
        stg = K.sb([128, 2048], F32, "stgB")
        cm = K.sb([128, 4, 512], BF16, "cm")
        NG = 4
        gms = [K.sb([128, 32], F32, "gm%d" % i) for i in range(NG)]
        m8s = [K.sb([128, 8], F32, "m8%d" % i) for i in range(NG)]
        sels = [K.sb([128, 32], F32, "sel%d" % i) for i in range(NG)]
        sexs = [K.sb([128, 128], BF16, "sex%d" % i) for i in range(NG)]
        mraws = [K.sb([128, 4], F32, "mraw%d" % i) for i in range(2)]
        PTs = [K.sb([128, 512], BF16, "PT%d" % i) for i in range(3)]
        rls = [K.sb([128, 1], F32, "rl%d" % i) for i in range(4)]
        ostg = [K.sb([128, 4, 128], BF16, "ostg%d" % i) for i in range(2)]
        K.dma("c0", valid.ap, validc.partition_broadcast(128), w=[valid])
        K.dma("c0", ownm.ap, ownc.partition_broadcast(128), w=[ownm])
        K.ts("dve", negb.ap, valid.ap, -1.0, 1e30, ALU.add, ALU.mult, [valid], [negb])
        for i in range(4):
            K.dma("c0", stg.ap[:, 0:512], cm_d[i], w=[stg])
            K.cp("dve", cm.ap[:, i, :], stg.ap[:, 0:512], [stg], [cm])
        gm4 = K.sb([128, 4, 32], F32, "gm4")
        m84 = K.sb([128, 4, 8], F32, "m84")
        sel4 = K.sb([128, 4, 32], F32, "sel4")
        sex4 = K.sb([128, 4, 128], BF16, "sex4")
        K.memset("pool", sex4.ap, 0.0, [sex4])
        for kb_ in KTs:
            for c4 in range(4):
                K.dma("c0", stg.ap[64:96, :], koh_d[:, c4 * 2048:(c4 + 1) * 2048], w=[stg])
                K.cp("dve", kb_.ap[64:96, c4 * 2048:(c4 + 1) * 2048], stg.ap[64:96, :], [stg], [kb_])
        for h in range(8):
            hp, pb = h // 2, (h % 2) * 64
            KT, QT, V = KTs[h % 2], QTs[h % 2], Vs[h % 2]
            K.dma("bk%d" % (h % 2), KT.ap[0:64, :], kT_s[hp, pb:pb + 64, :], w=[KT])
            K.dma("bq%d" % (h % 2), QT.ap[0:64, :], qT_s[hp, pb:pb + 64, :], w=[QT])
            K.dma("bv%d" % (h % 2), V.ap, v_s.rearrange("(t p) c -> p t c", p=128)[:, :, h * 65:(h + 1) * 65], w=[V])
            K.act(QA.ap, QT.ap[0:64, :], AF.Abs, [QT], [QA])
            K.op("dve", lambda e, KT=KT: e.tensor_reduce(out=kmax.ap, in_=KT.ap[0:64, :], axis=AX.X, op=ALU.max,
                                                         apply_absolute_value=True), [KT], [kmax])
            K.cp("dve", kmaxb.ap, kmax.ap, [kmax], [kmaxb])
            K.cp("dve", ksb.ap, ksum.ap[pb:pb + 64, hp, :], [ksum], [ksb])
            for qg in range(8):
                q0 = qg * 512
                pg = PS[qg % 3]
                c0 = (qg // 3) * 132
                for j in range(4):
                    K.mm(pg, pg.ap[:, c0 + j * 32: c0 + (j + 1) * 32], QT.ap[0:64, q0 + j * 128: q0 + (j + 1) * 128],
                         ksb.ap, True, True, [QT, ksb])
                    K.mm(pg, pg.ap[:, c0 + 128 + j: c0 + 129 + j], QA.ap[:, q0 + j * 128: q0 + (j + 1) * 128],
                         kmaxb.ap, True, True, [QA, kmaxb])
            for qg in range(8):
                q0 = qg * 512
                pg = PS[qg % 3]
                c0 = (qg // 3) * 132
                mraw = mraws[qg % 2]
                K.cp("dve", mraw.ap, pg.ap[:, c0 + 128: c0 + 132], [pg], [mraw])
                S4 = [128, 2, 2, 32]
                qsl = slice(2 * qg * 32, (2 * qg + 2) * 32)
                bq = lambda t_: t_.ap[:, qsl].rearrange("p (a b) -> p a b", a=2).unsqueeze(2).broadcast_to(S4)
                g4 = gm4.ap.rearrange("p (a c) b -> p a c b", a=2)
                s4 = sel4.ap.rearrange("p (a c) b -> p a c b", a=2)
                K.tt("dve", g4, pg.ap[:, c0:c0 + 128].rearrange("p (a c b) -> p a c b", a=2, c=2), bq(negb), ALU.add, [pg, negb], [gm4])
                for j in range(4):
                    K.op("dve", lambda e, j=j: e.max(out=m84.ap[:, j, :], in_=gm4.ap[:, j, :]), [gm4], [m84])
                K.tt("dve", sel4.ap, gm4.ap, m84.ap[:, :, 2:3].broadcast_to([128, 4, 32]), ALU.is_ge, [gm4, m84], [sel4])
                K.tt("dve", s4, s4, bq(valid), ALU.mult, [sel4, valid], [sel4])
                K.tt("dve", s4, s4, bq(ownm), ALU.add, [sel4, ownm], [sel4])
                K.ts("dve", sel4.ap, sel4.ap, -1.0, BIG, ALU.add, ALU.mult, [sel4], [sel4])
                K.tt("dve", sex4.ap[:, :, 64:96], sel4.ap, mraw.ap.unsqueeze(2).broadcast_to([128, 4, 32]), ALU.subtract,
                     [sel4, mraw], [sex4])
                pt = PS[3]
                for j in range(4):
                    K.mm(pt, pt.ap[:, j * 128:(j + 1) * 128], sex4.ap[:, j, :], idb.ap, True, True, [sex4, idb])
                K.cp("act", QT.ap[64:96, q0:q0 + 512], pt.ap[64:96, :], [pt], [QT])
            for qg in range(8):
                q0 = qg * 512
                nkt = 36 + 4 * qg

                def qk(kt):
                    ps = PS[kt % 3]
                    last_own = kt >= nkt - 4
                    K.mm(ps, ps.ap, KT.ap[:, kt * 128:(kt + 1) * 128], QT.ap[:, q0:q0 + 512],
                         True, not last_own, [KT, QT])
                    if last_own:
                        K.mm(ps, ps.ap, idb.ap, cm.ap[:, kt - (nkt - 4), :], False, True, [idb, cm])

                qk(0)
                qk(1)
                for kt in range(nkt):
                    if kt + 2 < nkt:
                        qk(kt + 2)
                    ps = PS[kt % 3]
                    PT = PTs[kt % 3]
                    K.act(PT.ap, ps.ap, AF.Exp, [ps], [PT], scale=0.125)
                    for j in range(4):
                        po = PS[4 + j]
                        K.mm(po, po.ap[:, 0:65], PT.ap[:, j * 128:(j + 1) * 128], V.ap[:, kt, :],
                             kt == 0, kt == nkt - 1, [PT, V])
                for j in range(4):
                    po = PS[4 + j]
                    K.op("dve", lambda e, po=po, j=j: e.reciprocal(out=rls[j].ap, in_=po.ap[:, 64:65]), [po], [rls[j]])
                for j in range(4):
                    po = PS[4 + j]
                    K.ts("dve", att.ap[:, qg * 4 + j, h * 64:(h + 1) * 64], po.ap[:, 0:64], rls[j].ap[:, 0:1], None,
                         ALU.mult, None, [po, rls[j]], [att])
                wi = h * 8 + qg
                wbufs = (stg, wstg2)
                if wi < len(wchunks):
                    dst, src, kc, ncols = wchunks[wi]
                    K.dma("bw%d" % (wi % 2), wbufs[wi % 2].ap[:, 0:ncols], src[kc * 128:(kc + 1) * 128, :], w=[wbufs[wi % 2]])
                if 1 <= wi <= len(wchunks):
                    dst, src, kc, ncols = wchunks[wi - 1]
                    K.cp("dve", dst.ap[:, kc, :], wbufs[(wi - 1) % 2].ap[:, 0:ncols], [wbufs[(wi - 1) % 2]], [dst])
        for qt in range(32):
            p = PS[qt % 2]
            pv = p.ap.bitcast(BF16)[:, 0:512].rearrange("p (a b) -> p a b", a=4)
            for c in range(4):
                K.tr(p, pv[:, c, :], att.ap[:, qt, c * 128:(c + 1) * 128], idb.ap, [att, idb])
            og = ostg[qt % 2]
            K.cp("act", og.ap, pv, [p], [og])
            K.dma("ob%d" % (qt % 2), attT_s[:, :, qt * 128:(qt + 1) * 128].rearrange("c p t -> p c t"), og.ap, r=[og])
        K.barrier()

    def phase_C1():
        K.phase_reset()
        wps = K.sb([128, 4, 1024], BF16, "wps")
        wpa = K.sb([128, 4, 1024], BF16, "wpa")
        wout = K.sb([128, 8, 1024], BF16, "wout")
        wq = K.sb([128, 8, 2048], BF16, "wq")
        keysT = K.sb([128, 16, 128], F32, "keysT")
        gf = K.sb([128, 1024], F32, "gffn")
        stage = K.sb([128, 2048], F32, "stageC")
        ssmT = K.sb([128, 4, 512], BF16, "ssmT")
        attT = K.sb([128, 4, 512], BF16, "attT")
        gaT = K.sb([128, 8, 512], BF16, "gaT")
        gbT = K.sb([128, 8, 512], BF16, "gbT")
        mT = K.sb([128, 8, 512], BF16, "mT")
        t1cs = [K.sb([128, 512], F32, "t1C%d" % i) for i in range(2)]
        t2cs = [K.sb([128, 512], F32, "t2C%d" % i) for i in range(2)]
        xts = [K.sb([128, 1024], F32, "xtC%d" % i) for i in range(2)]
        x1s = [K.sb([128, 1024], F32, "x1C%d" % i) for i in range(2)]
        hbs = [K.sb([128, 1024], BF16, "hbC%d" % i) for i in range(2)]
        sq = K.sb([128, 1024], BF16, "sqC")
        ss = K.sb([128, 1], F32, "ssC")
        rs = K.sb([128, 1], F32, "rsC")
        h2T = K.sb([128, 8, 512], BF16, "h2T")
        qT = K.sb([128, 16, 512], F32, "qT")
        scs = [K.sb([128, 16, 128], F32, "sc%d" % i) for i in range(2)]
        K.dma("c0", gf.ap, g_ffn.partition_broadcast(128), w=[gf])
        for ch in range(16):
            K.dma("wst", stage.ap[:, 0:128], keys[ch], w=[stage])
            p = nps()
            K.tr(p, p.ap[:, 0:128], stage.ap[:, 0:128], idf.ap, [stage, idf])
            K.cp("dve", keysT.ap[:, ch, :], p.ap[:, 0:128], [p], [keysT])
        xi = 0

        def c1_loads(tg_):
            ta_ = tg_ * 512
            K.dma("c1a", ssmT.ap, ssmT_s[:, :, ta_:ta_ + 512].rearrange("c p t -> p c t"), w=[ssmT])
            K.dma("c1b", attT.ap, attT_s[:, :, ta_:ta_ + 512].rearrange("c p t -> p c t"), w=[attT])
            K.dma("c1c", gaT.ap, gT_s[0:8, :, ta_:ta_ + 512].rearrange("c p t -> p c t"), w=[gaT])
            K.dma("c1d", gbT.ap, gT_s[8:16, :, ta_:ta_ + 512].rearrange("c p t -> p c t"), w=[gbT])

        c1_loads(0)
        for tg in range(8):
            t0 = tg * 512
            for c in range(8):
                pa = nps()
                pb = nps()
                for kc in range(4):
                    K.mm(pa, pa.ap, wps.ap[:, kc, c * 128:(c + 1) * 128], ssmT.ap[:, kc, :], kc == 0, kc == 3, [wps, ssmT])
                for kc in range(4):
                    K.mm(pb, pb.ap, wpa.ap[:, kc, c * 128:(c + 1) * 128], attT.ap[:, kc, :], kc == 0, kc == 3, [wpa, attT])
                t1, t2 = t1cs[c % 2], t2cs[c % 2]
                K.tt("dve", t1.ap, pa.ap, gaT.ap[:, c, :], ALU.mult, [pa, gaT], [t1])
                K.tt("dve", t2.ap, pb.ap, gbT.ap[:, c, :], ALU.mult, [pb, gbT], [t2])
                K.tt("dve", mT.ap[:, c, :], t1.ap, t2.ap, ALU.add, [t1, t2], [mT])
            if tg + 1 < 8:
                c1_loads(tg + 1)
            for j in range(4):
                xt = xts[xi % 2]
                x1 = x1s[xi % 2]
                hb = hbs[xi % 2]
                xi += 1
                r0 = t0 + j * 128
                K.dma("x%d" % (xi % 2), xt.ap, xc[NT_OWN + r0: NT_OWN + r0 + 128, :], w=[xt])
                for half in range(2):
                    p = nps()
                    for kc in range(8):
                        K.mm(p, p.ap, mT.ap[:, kc, j * 128:(j + 1) * 128], wout.ap[:, kc, half * 512:(half + 1) * 512],
                             kc == 0, kc == 7, [mT, wout])
                    K.tt("dve", x1.ap[:, half * 512:(half + 1) * 512], p.ap, xt.ap[:, half * 512:(half + 1) * 512], ALU.add,
                         [p, xt], [x1])
                K.dma("x1o%d" % (xi % 2), x1_s[r0:r0 + 128, :], x1.ap, r=[x1])
                rmsnorm_tile(x1, gf, hb, sq, ss, rs)

                def c1_tr(j=j, hb=hb):
                    p = nps()
                    pv = p.ap.bitcast(BF16).rearrange("p (a b) -> p a b", a=8)
                    for kc in range(8):
                        K.tr(p, pv[:, kc, :], hb.ap[:, kc * 128:(kc + 1) * 128], idb.ap, [hb, idb])
                    K.cp("act", h2T.ap[:, :, j * 128:(j + 1) * 128], pv, [p], [h2T])

                if j > 0:
                    pend_tr()
                pend_tr = c1_tr
            pend_tr()
            K.dma("h2o", h2T_s[:, :, t0:t0 + 512].rearrange("c p t -> p c t"), h2T.ap, r=[h2T])
            for ch in range(16):
                p = nps()
                for kc in range(8):
                    K.mm(p, p.ap, wq.ap[:, kc, ch * 128:(ch + 1) * 128], h2T.ap[:, kc, :], kc == 0, kc == 7, [wq, h2T])
                K.cp("act" if ch % 2 else "dve", qT.ap[:, ch, :], p.ap, [p], [qT])
            for j in range(4):
                sc = scs[j % 2]
                for c0 in range(0, 16, 4):
                    p = nps()
                    for i in range(4):
                        K.mm(p, p.ap[:, i * 128:(i + 1) * 128], qT.ap[:, c0 + i, j * 128:(j + 1) * 128], keysT.ap[:, c0 + i, :],
                             True, True, [qT, keysT])
                    K.cp("act" if (c0 // 4) % 2 else "dve", sc.ap[:, c0:c0 + 4, :], p.ap.rearrange("p (a b) -> p a b", a=4), [p], [sc])
                r0 = t0 + j * 128
                K.dma("sco%d" % (j % 2), sc_s[r0:r0 + 128, :], sc.ap.rearrange("p a b -> p (a b)"), r=[sc])
        K.barrier()

    U32 = mybir.dt.uint32

    def phase_C2():
        K.phase_reset()
        iot = K.sb([128, 128], F32, "iota")
        K.dma("c0", iot.ap, iota_d, w=[iot])
        ss_ = [K.sb([128, 16, 128], F32, "s%d" % i) for i in range(2)]
        v = K.sb([128, 16, 16], F32, "v")
        idx = K.sb([128, 8, 16], U32, "idx")
        idxf = K.sb([128, 128], F32, "idxf")
        idxT = K.sb([128, 128], F32, "idxT")
        top = K.sb([128, 8, 16], F32, "top")
        e16 = K.sb([128, 8, 16], F32, "e16")
        Z = K.sb([128, 8], F32, "Z")
        bE = K.sb([128, 8], F32, "bE")
        v1m = K.sb([128, 8, 16], F32, "v1m")
        sums = [K.sb([128, 16, 128], F32, "sum%d" % i) for i in range(3)]
        Es = [K.sb([128, 16, 128], BF16, "E%d" % i) for i in range(3)]
        Btm = K.sb([128, 8, 16, 128], BF16, "Btm")
        BT = K.sb([128, 128, 128], BF16, "BT")
        AT = K.sb([128, 128, 128], BF16, "AT")
        Gst = K.sb([128, 128, 128], BF16, "Gst")
        works = [K.sb([128, 128], F32, "wk%d" % i) for i in range(16)]
        work2s = [K.sb([128, 256], F32, "wk2%d" % i) for i in range(8)]
        K.dma("c2s0", ss_[0].ap.rearrange("p a b -> p (a b)"), sc_s[0:128, :], w=[ss_[0]])
        for tl in range(32):
            r0 = tl * 128
            s_ = ss_[tl % 2]
            if tl + 1 < 32:
                sn_ = ss_[(tl + 1) % 2]
                K.dma("c2s%d" % ((tl + 1) % 2), sn_.ap.rearrange("p a b -> p (a b)"), sc_s[r0 + 128:r0 + 256, :], w=[sn_])
            for ch in range(16):
                K.op("dve", lambda e, ch=ch, s_=s_: e.max(out=v.ap[:, ch, 0:8], in_=s_.ap[:, ch, :]), [s_], [v])
            for ch in range(0, 16, 2):
                K.op("dve", lambda e, ch=ch, s_=s_: e.max_index(out=idx.ap[:, ch // 2, 0:8], in_max=v.ap[:, ch, 0:8],
                                                                in_values=s_.ap[:, ch, :]), [v, s_], [idx])
            for ch in range(16):
                K.op("dve", lambda e, ch=ch, s_=s_: e.match_replace(out=works[ch].ap, in_to_replace=v.ap[:, ch, 0:8],
                                                                    in_values=s_.ap[:, ch, :], imm_value=-1e30), [v, s_], [works[ch]])
            for ch in range(16):
                K.op("dve", lambda e, ch=ch: e.max(out=v.ap[:, ch, 8:16], in_=works[ch].ap), [works[ch]], [v])
            for ch in range(0, 16, 2):
                K.op("dve", lambda e, ch=ch: e.max_index(out=idx.ap[:, ch // 2, 8:16], in_max=v.ap[:, ch, 8:16],
                                                         in_values=works[ch].ap), [v, works[ch]], [idx])
            vv = v.ap.rearrange("p (h s) k -> p h s k", s=2)
            v1 = vv[:, :, 0, :]
            v2 = vv[:, :, 1, :]
            cand_ap = sums[0].ap.rearrange("p a b -> p (a b)").rearrange("p (h c) -> p h c", h=8)
            K.tt("dve", cand_ap.rearrange("p h (i j) -> p h i j", i=16), v1.unsqueeze(3).broadcast_to([128, 8, 16, 16]),
                 v2.unsqueeze(2).broadcast_to([128, 8, 16, 16]), ALU.add, [v], [sums[0]])
            for h in range(8):
                K.op("dve", lambda e, h=h: e.max(out=top.ap[:, h, 0:8], in_=cand_ap[:, h, :]), [sums[0]], [top])
            for h in range(8):
                K.op("dve", lambda e, h=h: e.match_replace(out=work2s[h].ap, in_to_replace=top.ap[:, h, 0:8],
                                                           in_values=cand_ap[:, h, :], imm_value=-1e30), [top, sums[0]], [work2s[h]])
            for h in range(8):
                K.op("dve", lambda e, h=h: e.max(out=top.ap[:, h, 8:16], in_=work2s[h].ap), [work2s[h]], [top])
            mxb = top.ap[:, :, 0:1].broadcast_to([128, 8, 16])
            taub = top.ap[:, :, 15:16].broadcast_to([128, 8, 16])
            K.tt("dve", e16.ap, top.ap, mxb, ALU.subtract, [top], [e16])
            K.act(e16.ap, e16.ap, AF.Exp, [e16], [e16])
            K.op("dve", lambda e: e.tensor_reduce(out=Z.ap, in_=e16.ap, axis=AX.X, op=ALU.add), [e16], [Z])
            K.act(Z.ap, Z.ap, AF.Ln, [Z], [Z])
            K.tt("dve", bE.ap, top.ap[:, :, 15], top.ap[:, :, 0], ALU.subtract, [top], [bE])
            K.tt("dve", bE.ap, bE.ap, Z.ap, ALU.subtract, [bE, Z], [bE])
            K.tt("dve", v1m.ap, v1, taub, ALU.subtract, [v, top], [v1m])
            K.cp("dve", idxf.ap, idx.ap.rearrange("p h k -> p (h k)"), [idx], [idxf])
            p = nps()
            K.tr(p, p.ap[:, 0:128], idxf.ap, idf.ap, [idxf, idf])
            K.cp("dve", idxT.ap, p.ap[:, 0:128], [p], [idxT])
            K.tt("dve", AT.ap, iot.ap.unsqueeze(1).broadcast_to([128, 128, 128]),
                 idxT.ap.unsqueeze(2).broadcast_to([128, 128, 128]), ALU.is_equal, [iot, idxT], [AT])
            def bsum(h):
                sm_ = sums[h % 3]
                K.tt("pool" if h % 2 == 0 else "dve", sm_.ap, v1m.ap[:, h, :].unsqueeze(2).broadcast_to([128, 16, 128]),
                     s_.ap[:, 2 * h + 1, :].unsqueeze(1).broadcast_to([128, 16, 128]), ALU.add, [v1m, s_], [sm_])
                K.act(Es[h % 3].ap, sm_.ap, AF.Exp, [sm_, bE], [Es[h % 3]], bias=bE.ap[:, h:h + 1])

            bsum(0)
            bsum(1)
            for h in range(8):
                if h + 2 < 8:
                    bsum(h + 2)
                K.stt("dve", Btm.ap[:, h], sums[h % 3].ap, -1e-5, Es[h % 3].ap, ALU.is_ge, ALU.mult, [sums[h % 3], Es[h % 3]], [Btm])
            for b0 in range(0, 128, 8):
                p = nps()
                pv = p.ap.bitcast(BF16).rearrange("p (a b) -> p a b", a=8)
                for k in range(8):
                    K.tr(p, pv[:, k, :], Btm.ap[:, :, :, b0 + k].rearrange("p h i -> p (h i)"), idb.ap, [Btm, idb])
                K.cp("act", BT.ap[:, :, b0:b0 + 8], pv.rearrange("p b t -> p t b"), [p], [BT])
            for t4 in range(0, 128, 4):
                p = nps()
                for k in range(4):
                    t = t4 + k
                    K.mm(p, p.ap[:, k * 128:(k + 1) * 128], BT.ap[:, t, :], AT.ap[:, t, :], True, True, [BT, AT])
                K.cp("act", Gst.ap[:, :, t4:t4 + 4],
                     p.ap.rearrange("p (t a) -> p a t", t=4), [p], [Gst])
            K.dma("c2g", G_s[tl], Gst.ap, r=[Gst])
        K.barrier()

    def phase_D():
        K.phase_reset()
        TG = 1024
        NG_ = NT_OWN // TG
        h2T = K.sb([128, 8, TG], BF16, "h2TD")
        Ubs = [K.sb([128, 1024], F32, "Ub%d" % i) for i in range(3)]
        Ubb = [K.sb([128, 1024], BF16, "Ubb%d" % i) for i in range(2)]
        UbTs = [K.sb([128, 8, 128], BF16, "UbT%d" % i) for i in range(3)]
        Ghs = [K.sb([128, 8, 8, 128], BF16, "Gh%d" % i) for i in range(2)]
        ges = [K.sb([128, 512], BF16, "ge%d" % i) for i in range(2)]
        W = K.sb([128, 16, TG], BF16, "W")
        Vbs = [K.sb([128, 1024], F32, "Vb%d" % i) for i in range(3)]
        Vbf = K.sb([128, 16, 1024], BF16, "Vbf")
        acc = K.sb([128, 8, 1024], F32, "acc")
        x1t = [K.sb([128, 1024], F32, "x1D%d" % i) for i in range(2)]
        uttok = [Buf(None, "ut%d" % i) for i in range(128)]
        NI = NG_ * 128

        def load(n):
            grp, a = n // 128, n % 128
            t0 = grp * TG
            if grp == 0:
                K.dma("du%d" % (n % 3), Ubs[n % 3].ap, peer_u[a * 128:(a + 1) * 128, :], w=[Ubs[n % 3]])
            else:
                K.dma("du%d" % (n % 3), UbTs[n % 3].ap.rearrange("p a b -> p (a b)"), UT_s[a], r=[uttok[a]], w=[UbTs[n % 3]])
            if a % 8 == 0:
                Gh = Ghs[(n // 8) % 2]
                tl0 = t0 // 128
                K.dma("dg%d" % ((n // 8) % 2), Gh.ap, G_s[tl0:tl0 + 8, :, a:a + 8, :].rearrange("tl b a t -> b tl a t"), w=[Gh])
            K.dma("dv%d" % (n % 3), Vbs[n % 3].ap, peer_v[a * 128:(a + 1) * 128, :], w=[Vbs[n % 3]])

        def trans(n):
            if n // 128 != 0:
                return
            Ub, UbT, ub = Ubs[n % 3], UbTs[n % 3], Ubb[n % 2]
            K.cp("dve", ub.ap, Ub.ap, [Ub], [ub])
            p = nps()
            pv = p.ap.bitcast(BF16).rearrange("p (a b) -> p a b", a=8)
            for kc in range(8):
                K.tr(p, pv[:, kc, :], ub.ap[:, kc * 128:(kc + 1) * 128], idb.ap, [ub, idb])
            K.cp("act", UbT.ap, pv, [p], [UbT])

        load(0)
        load(1)
        trans(0)
        for n in range(NI):
            grp, a = n // 128, n % 128
            t0 = grp * TG
            si = a % 16
            if a == 0:
                K.dma("dh", h2T.ap, h2T_s[:, :, t0:t0 + TG].rearrange("c p t -> p c t"), w=[h2T])
                K.memset("pool", acc.ap, 0.0, [acc])
            if n + 2 < NI:
                load(n + 2)
            if n + 1 < NI:
                trans(n + 1)
            UbT, Vb = UbTs[n % 3], Vbs[n % 3]
            Gh = Ghs[(n // 8) % 2]
            if grp == 0:
                K.dma("dut%d" % (n % 3), UT_s[a], UbT.ap.rearrange("p a b -> p (a b)"), r=[UbT], w=[uttok[a]])
            K.cp("act", Vbf.ap[:, si, :], Vb.ap, [Vb], [Vbf])
            for hf in range(TG // 512):
                p = nps()
                for kc in range(8):
                    K.mm(p, p.ap, UbT.ap[:, kc, :], h2T.ap[:, kc, hf * 512:(hf + 1) * 512], kc == 0, kc == 7, [UbT, h2T])
                ge = ges[hf % 2]
                K.act(ge.ap, p.ap, AF.Gelu_apprx_tanh, [p], [ge])
                K.tt("dve", W.ap[:, si, hf * 512:(hf + 1) * 512].rearrange("p (a b) -> p a b", a=4),
                     ge.ap.rearrange("p (a b) -> p a b", a=4), Gh.ap[:, hf * 4:(hf + 1) * 4, a % 8, :], ALU.mult,
                     [ge, Gh], [W])
            if si == 15:
                for j in range(TG // 128):
                    for hf in range(2):
                        p = nps()
                        for s2 in range(16):
                            K.mm(p, p.ap, W.ap[:, s2, j * 128:(j + 1) * 128], Vbf.ap[:, s2, hf * 512:(hf + 1) * 512],
                                 s2 == 0, s2 == 15, [W, Vbf])
                        K.tt("dve", acc.ap[:, j, hf * 512:(hf + 1) * 512], p.ap, acc.ap[:, j, hf * 512:(hf + 1) * 512], ALU.add,
                             [p, acc], [acc])
            if a == 127:
                for j in range(TG // 128):
                    xt = x1t[j % 2]
                    r0 = t0 + j * 128
                    K.dma("dx%d" % (j % 2), xt.ap, x1_s[r0:r0 + 128, :], w=[xt])
                    K.tt("dve", xt.ap, xt.ap, acc.ap[:, j, :], ALU.add, [xt, acc], [xt])
                    K.dma("dx%d" % (j % 2), x2_s[r0:r0 + 128, :], xt.ap, r=[xt])
        K.barrier()

    def phase_E():
        K.phase_reset()
        wpg = K.sb([128, 8, 1024], BF16, "wpg")
        wpp = K.sb([128, 2, 1024], BF16, "wpp")
        stage = K.sb([128, 1024], F32, "stageE")
        gp = K.sb([128, 1024], F32, "gple")
        gfin = K.sb([128, 1024], F32, "gfin")
        xts = [K.sb([128, 1024], F32, "xtE%d" % i) for i in range(4)]
        pts = [K.sb([128, 256], F32, "ptE%d" % i) for i in range(2)]
        pb_s = [K.sb([128, 256], BF16, "pbE%d" % i) for i in range(2)]
        pTs = [K.sb([128, 2, 128], BF16, "pTE%d" % i) for i in range(2)]
        hb_s = [K.sb([128, 1024], BF16, "hbE%d" % i) for i in range(2)]
        hTs_ = [K.sb([128, 8, 128], BF16, "hTE%d" % i) for i in range(2)]
        sq_s = [K.sb([128, 1024], BF16, "sqE%d" % i) for i in range(2)]
        ss_s = [K.sb([128, 1], F32, "ssE%d" % i) for i in range(4)]
        rs_s = [K.sb([128, 1], F32, "rsE%d" % i) for i in range(4)]
        sgts = [K.sb([128, 1024], F32, "sgE%d" % i) for i in range(2)]
        outs = [K.sb([128, 1024], F32, "oE%d" % i) for i in range(2)]
        K.dma("c0", gp.ap, g_ple.partition_broadcast(128), w=[gp])
        K.dma("c0", gfin.ap, g_fin.partition_broadcast(128), w=[gfin])
        load_w_bf(wpg, w_pg, 8, 1024, stage, "wst")
        load_w_bf(wpp, w_pp, 2, 1024, stage, "wst")
        def e_vars(tl):
            return dict(r0=tl * 128)

        def front(tl):
            r0 = tl * 128
            xt, pt, ot = xts[tl % 4], pts[tl % 2], outs[tl % 2]
            pb_, pT, hb, hT, sq, sgt = pb_s[tl % 2], pTs[tl % 2], hb_s[tl % 2], hTs_[tl % 2], sq_s[tl % 2], sgts[tl % 2]
            ss, rs = ss_s[tl % 2], rs_s[tl % 2]
            ss2, rs2 = ss_s[2 + tl % 2], rs_s[2 + tl % 2]
            K.dma("ex%d" % (tl % 4), xt.ap, x2_s[r0:r0 + 128, :], w=[xt])
            K.dma("ep%d" % (tl % 2), pt.ap, pc[r0:r0 + 128, :], w=[pt])
            rmsnorm_tile(xt, gp, hb, sq, ss, rs)
            K.cp("dve", pb_.ap, pt.ap, [pt], [pb_])

        def front1b(tl):
            pb_, pT, hb, hT = pb_s[tl % 2], pTs[tl % 2], hb_s[tl % 2], hTs_[tl % 2]
            p = nps()
            pv = p.ap.bitcast(BF16).rearrange("p (a b) -> p a b", a=8)
            for kc in range(8):
                K.tr(p, pv[:, kc, :], hb.ap[:, kc * 128:(kc + 1) * 128], idb.ap, [hb, idb])
            K.cp("act", hT.ap, pv, [p], [hT])
            p = nps()
            pv = p.ap.bitcast(BF16).rearrange("p (a b) -> p a b", a=8)
            for kc in range(2):
                K.tr(p, pv[:, kc, :], pb_.ap[:, kc * 128:(kc + 1) * 128], idb.ap, [pb_, idb])
            K.cp("act", pT.ap, pv[:, 0:2, :], [p], [pT])

        def front2(tl):
            pT, hT, sgt = pTs[tl % 2], hTs_[tl % 2], sgts[tl % 2]
            for hf in range(2):
                pg = nps()
                pe_ = nps()
                for kc in range(8):
                    K.mm(pg, pg.ap, hT.ap[:, kc, :], wpg.ap[:, kc, hf * 512:(hf + 1) * 512], kc == 0, kc == 7, [hT, wpg])
                for kc in range(2):
                    K.mm(pe_, pe_.ap, pT.ap[:, kc, :], wpp.ap[:, kc, hf * 512:(hf + 1) * 512], kc == 0, kc == 1, [pT, wpp])
                K.act(sgt.ap[:, hf * 512:(hf + 1) * 512], pg.ap, AF.Sigmoid, [pg], [sgt])
                K.tt("dve", sgt.ap[:, hf * 512:(hf + 1) * 512], pe_.ap, sgt.ap[:, hf * 512:(hf + 1) * 512], ALU.mult, [pe_, sgt], [sgt])

        def back(tl):
            r0 = tl * 128
            xt, ot = xts[tl % 4], outs[tl % 2]
            sq, sgt = sq_s[tl % 2], sgts[tl % 2]
            ss2, rs2 = ss_s[2 + tl % 2], rs_s[2 + tl % 2]
            K.tt("pool", xt.ap, xt.ap, sgt.ap, ALU.add, [xt, sgt], [xt])
            K.act(sq.ap, xt.ap, AF.Square, [xt], [sq, ss2], accum=ss2.ap)
            K.act(rs2.ap, ss2.ap, AF.Sqrt, [ss2], [rs2], scale=1.0 / 1024, bias=eps_b.ap)
            K.op("dve", lambda e, rs2=rs2: e.reciprocal(out=rs2.ap, in_=rs2.ap), [rs2], [rs2])
            K.stt("dve", ot.ap, xt.ap, rs2.ap[:, 0:1], gfin.ap, ALU.mult, ALU.mult, [xt, rs2, gfin], [ot])
            K.dma("eo%d" % (tl % 2), out[r0:r0 + 128, :], ot.ap, r=[ot])

        for r_ in range(-3, 32):
            for stage_fn, off_ in ((back, 0), (front2, 1), (front1b, 2), (front, 3)):
                tl_ = r_ + off_
                if 0 <= tl_ < 32:
                    stage_fn(tl_)
        K.barrier()

    phases = {"A": phase_A, "B": phase_B, "S": phase_S, "C1": phase_C1, "C2": phase_C2, "D": phase_D, "E": phase_E}
    return nc, kb, st, locals()


def _consts():
    ident = np.eye(128, dtype=np.float32)
    invf = np.zeros((128, 2), np.float32)
    for p in range(128):
        hd = p % 64
        if hd < 16:
            invf[p, 0] = np.float32(500000.0) ** np.float32(-(2 * (hd % 8)) / 16.0)
            invf[p, 1] = -1.0 if hd < 8 else 1.0
    eoh = np.zeros((32, NT_LOC), np.float32)
    for n in range(32):
        eoh[n, n * 256:(n + 1) * 256] = 1.0
    cm = np.zeros((4, 128, 512), np.float32)
    for kt in range(4):
        for kp in range(128):
            kpos = kt * 128 + kp
            q = np.arange(512)
            same = (q // 256) == (kpos // 256)
            cm[kt, kp, :] = np.where(same & (kpos > q), -BIG, 0.0)
    return ident, invf, eoh, cm


def make_in_maps(inp, cores=range(8)):
    f = lambda a: np.ascontiguousarray(np.asarray(a))
    x = f(inp["x"])
    p = f(inp["p"])[0]
    pos = f(inp["positions"]).astype(np.int32)
    ident, invf, eoh, cm = _consts()
    w_in = f(inp["w_in"])[0]
    perm = np.arange(1024)
    for c in range(1024):
        hd = c % 64
        if hd < 8:
            perm[c] = c + 8
        elif hd < 16:
            perm[c] = c - 8
    w_perm = f(w_in[:, 512:1536][:, perm])

    def pair(a):
        a = f(a)[0]
        sh = a.shape
        a = a.reshape(16, 2, 64, *sh[2:])
        a = np.moveaxis(a, 0, 2)
        return f(a.reshape(128, 16, -1).reshape(128, -1))

    ldt = f(inp["ssm_log_dt"])[0]
    ldt_l = f(np.broadcast_to(ldt.reshape(16, 2, 1), (16, 2, 64)).transpose(1, 2, 0).reshape(128, 16))
    cre = f(inp["ssm_c_re"])[0].transpose(0, 2, 1)
    cim = f(inp["ssm_c_im"])[0].transpose(0, 2, 1)
    shared = {
        "ident": ident, "iota": np.ascontiguousarray(np.broadcast_to(np.arange(128, dtype=np.float32), (128, 128))), "invf": invf, "koh": eoh, "cmask": cm,
        "g_mix": f(inp["g_mix"]), "w_in": w_in, "w_perm": w_perm,
        "s5_ldt": ldt_l, "s5_are": pair(inp["ssm_a_re"]), "s5_aim": pair(inp["ssm_a_im"]),
        "s5_bre": pair(inp["ssm_b_re"]), "s5_bim": pair(inp["ssm_b_im"]),
        "s5_cre": pair(cre[None]), "s5_cim": pair(cim[None]),
        "ssm_d": f(inp["ssm_d"]), "w_glu": f(inp["ssm_w_glu"])[0],
        "w_ps": f(inp["w_proj_ssm"])[0], "w_pa": f(inp["w_proj_att"])[0], "w_out": f(inp["w_out"])[0],
        "g_ffn": f(inp["g_ffn"]), "w_q": f(inp["peer_w_q"])[0],
        "keys": f(np.stack([f(inp["peer_keys1"])[0], f(inp["peer_keys2"])[0]], axis=1).reshape(16, 128, 128)),
        "peer_u": f(inp["peer_u"])[0], "peer_v": f(inp["peer_v"])[0],
        "g_ple": f(inp["g_ple"]), "w_pg": f(inp["ple_w_gate"])[0], "w_pp": f(inp["ple_w_proj"])[0],
        "g_fin": f(inp["g_final"]).reshape(1, 1024),
    }
    maps = []
    for c in cores:
        b, half = c // 2, c % 2
        xc = np.zeros((NT_LOC, 1024), np.float32)
        posc = np.zeros((1, NT_LOC), np.int32)
        if half == 1:
            xc[:] = x[b]
            posc[0] = pos[b]
        else:
            xc[NT_OWN:] = x[b, :NT_OWN]
            posc[0, NT_OWN:] = pos[b, :NT_OWN]
        valid = np.zeros((16, 32), np.float32)
        own = np.zeros((16, 32), np.float32)
        for qb in range(16, 32):
            for n in range(32):
                if n < qb and (half == 1 or n >= 16):
                    valid[qb - 16, n] = 1.0
            own[qb - 16, qb] = 1.0
        m = dict(shared)
        m.update({"xc": xc, "pc": f(p[b, half * NT_OWN:(half + 1) * NT_OWN]), "posc": posc,
                  "validc": valid.reshape(1, 512), "ownc": own.reshape(1, 512)})
        maps.append(m)
    return maps


_CACHE = {}


def kernel(**inputs):
    if "prog" not in _CACHE:
        nc, kb, st, L = build_program()
        for ph in ("A", "S", "B", "C1", "C2", "D", "E"):
            L["phases"][ph]()
        kb.S.emit()
        st.close()
        _CACHE["prog"] = nc
    nc = _CACHE["prog"]
    maps = make_in_maps(inputs, range(8))
    res = run_bass_kernel_spmd(nc, maps, core_ids=list(range(8)))
    out = np.zeros((4, 8192, 1024), np.float32)
    for c in range(8):
        b, half = c // 2, c % 2
        out[b, half * NT_OWN:(half + 1) * NT_OWN] = np.asarray(res.results[c]["out"])
    return out
```

```python
import numpy as np
import concourse.bass as bass
import concourse.mybir as mybir
from concourse.bass_utils import run_bass_kernel_spmd

F32 = mybir.dt.float32
BF16 = mybir.dt.bfloat16
I32 = mybir.dt.int32
ALU = mybir.AluOpType
AF = mybir.ActivationFunctionType
AX = mybir.AxisListType


class Tok:
    __slots__ = ("w", "r", "name")

    def __init__(self, name=""):
        self.w = None
        self.r = {}
        self.name = name


class Sched:
    ENGS = ("pe", "act", "dve", "pool", "sp")

    def __init__(self, nc):
        self.nc = nc
        self.ops = {e: [] for e in self.ENGS}
        self.dma_cnt = {}
        self.dma_keys = []

    @staticmethod
    def _evkey(ev):
        return (ev[0], ev[1])

    def _collect(self, reads, writes):
        deps = {}

        def add(ev):
            if ev is None:
                return
            k = self._evkey(ev)
            if k not in deps or deps[k][2] < ev[2]:
                deps[k] = ev

        for t in reads:
            add(t.w)
        for t in writes:
            add(t.w)
            for ev in t.r.values():
                add(ev)
        return deps

    def _commit(self, ev, reads, writes):
        for t in reads:
            k = self._evkey(ev)
            t.r[k] = ev
        for t in writes:
            t.w = ev
            t.r = {}

    def op(self, eng, fn, reads=(), writes=()):
        deps = self._collect(reads, writes)
        idx = len(self.ops[eng])
        ev = ("e", eng, idx)
        if eng == "pe":
            deps.pop(("e", "pe"), None)
        self.ops[eng].append(dict(fn=fn, deps=list(deps.values()), dma=None, signal=False))
        self._commit(ev, reads, writes)
        return ev

    def dma(self, q, key, out, in_, reads=(), writes=()):
        deps = self._collect(reads, writes)
        if key not in self.dma_cnt:
            self.dma_cnt[key] = 0
            self.dma_keys.append(key)
        n = self.dma_cnt[key]
        if n > 0:
            k = ("d", key)
            deps[k] = ("d", key, n)
        self.dma_cnt[key] = n + 1
        ev = ("d", key, n + 1)
        self.ops[q].append(dict(fn=lambda e, o=out, i=in_: e.dma_start(out=o, in_=i),
                                deps=list(deps.values()), dma=key, signal=False))
        self._commit(ev, reads, writes)
        return ev

    def emit(self, final_keys=()):
        nc = self.nc
        ops = self.ops
        for e in self.ENGS:
            for o in ops[e]:
                for d in o["deps"]:
                    if d[0] == "e":
                        ops[d[1]][d[2]]["signal"] = True
        for e in self.ENGS:
            last = None
            for o in ops[e]:
                if "barrier" in o:
                    if last is not None and e != "sp":
                        last["signal"] = True
                else:
                    last = o
        sigval = {}
        for e in self.ENGS:
            c = 0
            vals = []
            for o in ops[e]:
                if o["signal"]:
                    c += 1
                vals.append(c)
            sigval[e] = vals
        barvals = {}
        for e in self.ENGS:
            for i, o in enumerate(ops[e]):
                if "barrier" in o:
                    barvals[(e, o["barrier"])] = sigval[e][i]
        from contextlib import ExitStack
        with ExitStack() as st:
            esem = {e: st.enter_context(nc.semaphore("s_" + e)) for e in self.ENGS if e != "sp"}
            dsem = {k: st.enter_context(nc.semaphore("d_%d" % i)) for i, k in enumerate(self.dma_keys)}
            bsem = st.enter_context(nc.semaphore("s_bar"))
            block = st.enter_context(nc.Block())

            def run(ename, eng):
                waited = {}
                for o in ops[ename]:
                    if "barrier" in o:
                        k = o["barrier"]
                        if ename == "sp":
                            for key, cnt in o["dcnt"].items():
                                if cnt > 0 and waited.get(("d", key), 0) < 16 * cnt:
                                    eng.wait_ge(dsem[key], 16 * cnt)
                            for e2 in esem:
                                v = barvals[(e2, k)]
                                if v > 0:
                                    eng.wait_ge(esem[e2], v)
                            eng.sem_inc(bsem, 1)
                        else:
                            eng.wait_ge(bsem, k)
                        for key, cnt in o["dcnt"].items():
                            waited[("d", key)] = max(waited.get(("d", key), 0), 16 * cnt)
                        for e2 in esem:
                            waited[("e", e2)] = max(waited.get(("e", e2), 0), barvals[(e2, k)])
                        continue
                    for d in sorted(o["deps"]):
                        if d[0] == "e":
                            sem = esem[d[1]]
                            val = sigval[d[1]][d[2]]
                        else:
                            sem = dsem[d[1]]
                            val = 16 * d[2]
                        wk = (d[0], d[1])
                        if waited.get(wk, 0) >= val:
                            continue
                        waited[wk] = val
                        eng.wait_ge(sem, val)
                    ins = o["fn"](eng)
                    if o["dma"] is not None:
                        ins.then_inc(dsem[o["dma"]], 16)
                    elif o["signal"]:
                        ins.then_inc(esem[ename], 1)
                if ename == "sp":
                    for k in self.dma_keys:
                        eng.wait_ge(dsem[k], 16 * self.dma_cnt[k])

            @block.sync
            def _(e):
                run("sp", e)

            @block.tensor
            def _(e):
                run("pe", e)

            @block.scalar
            def _(e):
                run("act", e)

            @block.vector
            def _(e):
                run("dve", e)

            @block.gpsimd
            def _(e):
                run("pool", e)


NT_OWN = 4096
NT_LOC = 8192
PI = float(np.pi)
BIG = 30000.0


class Buf:
    __slots__ = ("ap", "t")

    def __init__(self, ap, name=""):
        self.ap = ap
        self.t = Tok(name)

    def __getitem__(self, k):
        return self.ap[k]


class KB:
    def __init__(self, nc):
        self.nc = nc
        self.S = Sched(nc)
        self.big = nc.alloc_sbuf_tensor("bigsb", [128, 53000], F32)
        self.off = 0
        self.persist = 0
        self.nbar = 0

    def sb(self, shape, dt, name=""):
        n = int(np.prod(shape[1:]))
        esz = 4 if dt in (F32, I32, mybir.dt.uint32) else 2
        nw = (n * esz + 63) // 64 * 16
        assert self.off + nw <= 53000, ("sbuf overflow", name, self.off, nw)
        ap = self.big[:, self.off:self.off + nw]
        self.off += nw
        if dt != F32:
            ap = ap.bitcast(dt)
        ap = ap[:, 0:n]
        if len(shape) == 3:
            ap = ap.rearrange("p (a b) -> p a b", a=shape[1])
        elif len(shape) == 4:
            ap = ap.rearrange("p (a b c) -> p a b c", a=shape[1], b=shape[2])
        elif len(shape) == 5:
            ap = ap.rearrange("p (a b c d) -> p a b c d", a=shape[1], b=shape[2], c=shape[3])
        if shape[0] != 128:
            ap = ap[0:shape[0]]
        return Buf(ap, name)

    def phase_reset(self):
        self.off = self.persist

    def op(self, eng, fn, r=(), w=()):
        return self.S.op(eng, fn, [b.t for b in r], [b.t for b in w])

    def dma(self, key, out, in_, r=(), w=(), q="sp"):
        return self.S.dma(q, key, out, in_, [b.t for b in r], [b.t for b in w])

    def mm(self, pbuf, out, lhsT, rhs, start, stop, r):
        self.op("pe", lambda e: e.matmul(out, lhsT=lhsT, rhs=rhs, start=start, stop=stop), r, [pbuf])

    def tr(self, pbuf, out, in_, ident, r):
        self.op("pe", lambda e: e.transpose(out=out, in_=in_, identity=ident), r, [pbuf])

    def tt(self, eng, out, a, b, op, r, w):
        self.op(eng, lambda e: e.tensor_tensor(out=out, in0=a, in1=b, op=op), r, w)

    def ts(self, eng, out, a, s1, s2, op0, op1, r, w):
        if s2 is None:
            self.op(eng, lambda e: e.tensor_scalar(out=out, in0=a, scalar1=s1, scalar2=None, op0=op0), r, w)
        else:
            self.op(eng, lambda e: e.tensor_scalar(out=out, in0=a, scalar1=s1, scalar2=s2, op0=op0, op1=op1), r, w)

    def stt(self, eng, out, a, s, b, op0, op1, r, w):
        self.op(eng, lambda e: e.scalar_tensor_tensor(out=out, in0=a, scalar=s, in1=b, op0=op0, op1=op1), r, w)

    def cp(self, eng, out, a, r, w):
        if eng == "act":
            self.op("act", lambda e: e.activation(out=out, in_=a, func=AF.Copy), r, w)
        else:
            self.op(eng, lambda e: e.tensor_copy(out=out, in_=a), r, w)

    def act(self, out, a, func, r, w, bias=None, scale=None, accum=None):
        kw = {}
        if bias is not None:
            kw["bias"] = bias
        if scale is not None:
            kw["scale"] = scale
        if accum is not None:
            kw["accum_out"] = accum
        self.op("act", lambda e: e.activation(out=out, in_=a, func=func, **kw), r, w)

    def memset(self, eng, out, val, w):
        self.op(eng, lambda e: e.memset(out, val), (), w)

    def barrier(self):
        S = self.S
        self.nbar += 1
        k = self.nbar
        for e in S.ENGS:
            S.ops[e].append(dict(barrier=k, fn=None, deps=[], dma=None, signal=False,
                                 dcnt=dict(S.dma_cnt)))


def build_program(stop_after=None, debug=()):
    nc = bass.Bass("TRN2", target_bir_lowering=False)
    kb = KB(nc)
    K = kb

    def din(name, shape, dt=F32):
        return nc.dram_tensor(name, list(shape), dt, kind="ExternalInput").ap()

    def dscr(name, shape, dt):
        kind = "ExternalOutput" if name in debug else "Internal"
        return nc.dram_tensor(name, list(shape), dt, kind=kind).ap()

    xc = din("xc", [NT_LOC, 1024])
    pc = din("pc", [NT_OWN, 256])
    posc = din("posc", [1, NT_LOC], I32)
    validc = din("validc", [1, 512])
    ownc = din("ownc", [1, 512])
    ident_d = din("ident", [128, 128])
    iota_d = din("iota", [128, 128])
    invf_d = din("invf", [128, 2])
    koh_d = din("koh", [32, NT_LOC])
    cm_d = din("cmask", [4, 128, 512])
    g_mix = din("g_mix", [1, 1024])
    w_in = din("w_in", [1024, 4096])
    w_perm = din("w_perm", [1024, 1024])
    s5 = {n: din("s5_" + n, shp) for n, shp in [
        ("ldt", [128, 16]), ("are", [128, 16]), ("aim", [128, 16]),
        ("bre", [128, 256]), ("bim", [128, 256]), ("cre", [128, 256]), ("cim", [128, 256])]}
    ssm_d = din("ssm_d", [1, 512])
    w_glu = din("w_glu", [512, 512])
    w_ps = din("w_ps", [512, 1024])
    w_pa = din("w_pa", [512, 1024])
    w_out = din("w_out", [1024, 1024])
    g_ffn = din("g_ffn", [1, 1024])
    w_q = din("w_q", [1024, 2048])
    keys = din("keys", [16, 128, 128])
    peer_u = din("peer_u", [16384, 1024])
    peer_v = din("peer_v", [16384, 1024])
    g_ple = din("g_ple", [1, 1024])
    w_pg = din("w_pg", [1024, 1024])
    w_pp = din("w_pp", [256, 1024])
    g_fin = din("g_fin", [1, 1024])
    out = nc.dram_tensor("out", [NT_OWN, 1024], F32, kind="ExternalOutput").ap()

    qT_s = dscr("qT_s", [4, 128, NT_OWN], BF16)
    kT_s = dscr("kT_s", [4, 128, NT_LOC], BF16)
    v_s = dscr("v_s", [NT_LOC, 8 * 65], BF16)
    gT_s = dscr("gT_s", [16, 128, NT_OWN], BF16)
    ssmT_s = dscr("ssmT_s", [4, 128, NT_OWN], BF16)
    attT_s = dscr("attT_s", [4, 128, NT_OWN], BF16)
    x1_s = dscr("x1_s", [NT_OWN, 1024], F32)
    h2T_s = dscr("h2T_s", [8, 128, NT_OWN], BF16)
    sc_s = dscr("sc_s", [NT_OWN, 16 * 128], F32)
    G_s = dscr("G_s", [32, 128, 128, 128], BF16)
    x2_s = dscr("x2_s", [NT_OWN, 1024], F32)
    UT5_s = dscr("UT5_s", [8, 128, 32 * 128], BF16)
    UT_s = dscr("UT_s", [128, 128, 1024], BF16)

    from contextlib import ExitStack
    st = ExitStack()
    PS = []
    for i in range(8):
        t = st.enter_context(nc.psum_tensor("ps%d" % i, [128, 512], F32))
        PS.append(Buf(t[:], "ps%d" % i))
    psi = [0]

    def nps():
        b = PS[psi[0] % 8]
        psi[0] += 1
        return b

    idf = K.sb([128, 128], F32, "idf")
    idb = K.sb([128, 128], BF16, "idb")
    K.dma("c0", idf.ap, ident_d, w=[idf])
    K.cp("dve", idb.ap, idf.ap, [idf], [idb])
    ksum = K.sb([128, 4, 32], F32, "ksum")
    K.persist = K.off

    ut5tok = [Buf(None, "ut5_%d" % i) for i in range(8)]

    def rmsnorm_tile(xt, gt, hb, sq, ss, rs):
        K.act(sq.ap, xt.ap, AF.Square, [xt], [sq, ss], accum=ss.ap)
        K.act(rs.ap, ss.ap, AF.Sqrt, [ss], [rs], scale=1.0 / 1024, bias=eps_b.ap)
        K.op("dve", lambda e: e.reciprocal(out=rs.ap, in_=rs.ap), [rs], [rs])
        K.stt("dve", hb.ap, xt.ap, rs.ap[:, 0:1], gt.ap, ALU.mult, ALU.mult, [xt, rs, gt], [hb])

    def load_w_bf(dst, src_ap, rows_kc, ncols, stage, key):
        for kc in range(rows_kc):
            K.dma(key, stage.ap[:, 0:ncols], src_ap[kc * 128:(kc + 1) * 128, :], w=[stage])
            K.cp("dve" if kc % 2 == 0 else "act", dst.ap[:, kc, :], stage.ap[:, 0:ncols], [stage], [dst])

    eps_b = K.sb([128, 1], F32, "eps")
    K.memset("dve", eps_b.ap, 1e-6, [eps_b])
    K.persist = K.off

    def phase_A():
        K.phase_reset()
        win = K.sb([128, 8, 3584], BF16, "win")
        wu = K.sb([128, 8, 512], BF16, "wuA")
        Ustk = K.sb([128, 32, 8, 16], BF16, "UstkA")
        UTo = K.sb([128, 32, 128], BF16, "UToA")
        wpm = K.sb([128, 8, 1024], BF16, "wpm")
        stageA = K.sb([128, 1792], F32, "stageA")
        stageB = K.sb([128, 1792], F32, "stageB")
        stage = stageA
        gt = K.sb([128, 1024], F32, "gmix")
        invf = K.sb([128, 2], F32, "invf")
        xts = [K.sb([128, 1024], F32, "xt%d" % i) for i in range(2)]
        sq = K.sb([128, 1024], BF16, "sq")
        ss = K.sb([128, 1], F32, "ss")
        rs = K.sb([128, 1], F32, "rs")
        hbs = [K.sb([128, 1024], BF16, "hb%d" % i) for i in range(2)]
        hTs = [K.sb([128, 8, 1024], BF16, "hT%d" % i) for i in range(2)]
        posi = K.sb([128, 1024], I32, "posi")
        ang = K.sb([128, 1024], F32, "ang")
        tmpa = K.sb([128, 1024], F32, "tmpa")
        tmpi = K.sb([128, 1024], I32, "tmpi")
        cosTs = [K.sb([128, 1024], F32, "cosT%d" % i) for i in range(2)]
        sinTs = [K.sb([128, 1024], F32, "sinT%d" % i) for i in range(2)]
        t1s = [K.sb([128, 512], F32, "t1_%d" % i) for i in range(2)]
        t2s = [K.sb([128, 512], F32, "t2_%d" % i) for i in range(2)]
        obf = [K.sb([128, 512], BF16, "obf%d" % i) for i in range(2)]
        vts = [K.sb([128, 8, 65], BF16, "vt%d" % i) for i in range(2)]
        for v in vts:
            K.memset("pool", v.ap, 1.0, [v])
        K.dma("c0", gt.ap, g_mix.partition_broadcast(128), w=[gt])
        K.dma("c0", invf.ap, invf_d, w=[invf])
        n_st = [0]

        def stream_w(dst_ap, src_ap, ncols):
            i = n_st[0]
            n_st[0] += 1
            stg_ = (stageA, stageB)[i % 2]
            K.dma("wst%d" % (i % 2), stg_.ap[:, 0:ncols], src_ap, w=[stg_])
            K.cp("dve" if i % 2 == 0 else "act", dst_ap, stg_.ap[:, 0:ncols], [stg_], [win])

        for kc in range(8):
            stream_w(win.ap[:, kc, 0:1792], w_in[kc * 128:(kc + 1) * 128, 512:2304], 1792)
            stream_w(win.ap[:, kc, 1792:3584], w_in[kc * 128:(kc + 1) * 128, 2304:4096], 1792)
        for kc in range(8):
            i = n_st[0]
            n_st[0] += 1
            stg_ = (stageA, stageB)[i % 2]
            K.dma("wst%d" % (i % 2), stg_.ap[:, 0:1024], w_perm[kc * 128:(kc + 1) * 128, :], w=[stg_])
            K.cp("dve" if i % 2 == 0 else "act", wpm.ap[:, kc, :], stg_.ap[:, 0:1024], [stg_], [wpm])
        for kc in range(8):
            i = n_st[0]
            n_st[0] += 1
            stg_ = (stageA, stageB)[i % 2]
            K.dma("wst%d" % (i % 2), stg_.ap[:, 0:512], w_in[kc * 128:(kc + 1) * 128, 0:512], w=[stg_])
            K.cp("dve" if i % 2 == 0 else "act", wu.ap[:, kc, :], stg_.ap[:, 0:512], [stg_], [wu])

        def sincos(dst, phase):
            K.ts("dve", tmpa.ap, ang.ap, phase, 1.0 / (2 * PI), ALU.add, ALU.mult, [ang], [tmpa])
            K.cp("dve", tmpi.ap, tmpa.ap, [tmpa], [tmpi])
            K.cp("dve", tmpa.ap, tmpi.ap, [tmpi], [tmpa])
            K.stt("dve", tmpa.ap, tmpa.ap, -2 * PI, ang.ap, ALU.mult, ALU.add, [tmpa, ang], [tmpa])
            K.ts("dve", tmpa.ap, tmpa.ap, phase, None, ALU.add, None, [tmpa], [tmpa])
            K.ts("dve", dst.ap, tmpa.ap, PI, -2 * PI, ALU.is_gt, ALU.mult, [tmpa], [dst])
            K.tt("dve", tmpa.ap, tmpa.ap, dst.ap, ALU.add, [tmpa, dst], [tmpa])
            K.ts("dve", dst.ap, tmpa.ap, -PI, 2 * PI, ALU.is_lt, ALU.mult, [tmpa], [dst])
            K.tt("dve", tmpa.ap, tmpa.ap, dst.ap, ALU.add, [tmpa, dst], [tmpa])
            K.act(dst.ap, tmpa.ap, AF.Sin, [tmpa], [dst])

        xic = [0]

        def prep(blk):
            tb = blk * 1024
            hT = hTs[blk % 2]
            cosT, sinT = cosTs[blk % 2], sinTs[blk % 2]
            xi = xic[0]
            K.dma("pos", posi.ap, posc[:, tb:tb + 1024].partition_broadcast(128), w=[posi])
            K.cp("dve", ang.ap, posi.ap, [posi], [ang])
            K.ts("dve", ang.ap, ang.ap, invf.ap[:, 0:1], None, ALU.mult, None, [ang, invf], [ang])
            sincos(cosT, PI / 2)
            sincos(sinT, 0.0)
            K.ts("dve", sinT.ap, sinT.ap, invf.ap[:, 1:2], None, ALU.mult, None, [sinT, invf], [sinT])
            for ti in range(8):
                xt = xts[xi % 2]
                hb = hbs[xi % 2]
                xi += 1
                K.dma("x%d" % (xi % 2), xt.ap, xc[tb + ti * 128: tb + (ti + 1) * 128, :], w=[xt])
                rmsnorm_tile(xt, gt, hb, sq, ss, rs)

                def a_tr(ti=ti, hb=hb, hT=hT):
                    p = nps()
                    pv = p.ap.bitcast(BF16).rearrange("p (a b) -> p a b", a=8)
                    for kc in range(8):
                        K.tr(p, pv[:, kc, :], hb.ap[:, kc * 128:(kc + 1) * 128], idb.ap, [hb, idb])
                    K.cp("act", hT.ap[:, :, ti * 128:(ti + 1) * 128], pv, [p], [hT])

                if ti > 0:
                    pend_a()
                pend_a = a_tr
            pend_a()
            xic[0] = xi

        oic = [0]

        def compute(blk):
            own = blk >= 4
            tb = blk * 1024
            hT = hTs[blk % 2]
            cosT, sinT = cosTs[blk % 2], sinTs[blk % 2]
            oi = oic[0]
            for j in range(8):
                p = nps()
                for kc in range(8):
                    K.mm(p, p.ap, hT.ap[:, kc, j:1024:8], wu.ap[:, kc, :], kc == 0, kc == 7, [hT, wu])
                K.cp("act" if j % 2 else "dve", Ustk.ap[:, :, j, :], p.ap.rearrange("p (g c) -> p g c", g=32), [p], [Ustk])
            for g0 in range(0, 32, 8):
                p = nps()
                pv = p.ap.bitcast(BF16).rearrange("p (a b) -> p a b", a=8)
                for gi in range(8):
                    K.tr(p, pv[:, gi, :], Ustk.ap[:, g0 + gi, :, :].rearrange("p a b -> p (a b)"), idb.ap, [Ustk, idb])
                K.cp("act", UTo.ap[:, g0:g0 + 8, :], pv, [p], [UTo])
            K.dma("utA", UT5_s[blk], UTo.ap.rearrange("p a b -> p (a b)"), r=[UTo], w=[ut5tok[blk]])
            for which in (["q", "k"] if own else ["k"]):
                cbase = 0 if which == "q" else 512
                for c in range(4):
                    for half in range(2):
                        pa = nps()
                        pb = nps()
                        for kc in range(8):
                            K.mm(pa, pa.ap, win.ap[:, kc, cbase + c * 128: cbase + (c + 1) * 128],
                                 hT.ap[:, kc, half * 512:(half + 1) * 512], kc == 0, kc == 7, [win, hT])
                        for kc in range(8):
                            K.mm(pb, pb.ap, wpm.ap[:, kc, cbase + c * 128: cbase + (c + 1) * 128],
                                 hT.ap[:, kc, half * 512:(half + 1) * 512], kc == 0, kc == 7, [wpm, hT])
                        t1, t2 = t1s[oi % 2], t2s[oi % 2]
                        K.tt("dve", t1.ap, pa.ap, cosT.ap[:, half * 512:(half + 1) * 512], ALU.mult, [pa, cosT], [t1])
                        K.tt("dve", t2.ap, pb.ap, sinT.ap[:, half * 512:(half + 1) * 512], ALU.mult, [pb, sinT], [t2])
                        K.tt("dve", t1.ap, t1.ap, t2.ap, ALU.add, [t1, t2], [t1])
                        ob = obf[oi % 2]
                        oi += 1
                        K.cp("act", ob.ap, t1.ap, [t1], [ob])
                        t0 = tb + half * 512
                        if which == "q":
                            K.dma("oq%d" % (oi % 2), qT_s[c, :, t0 - NT_OWN: t0 - NT_OWN + 512], ob.ap, r=[ob])
                        else:
                            K.dma("oq%d" % (oi % 2), kT_s[c, :, t0: t0 + 512], ob.ap, r=[ob])
                            K.op("dve", lambda e, c=c, b0=t0 // 256, t1=t1: e.tensor_reduce(
                                out=ksum.ap[:, c, b0:b0 + 2], in_=t1.ap.rearrange("p (a b) -> p a b", a=2),
                                axis=AX.X, op=ALU.add), [t1], [ksum])
            for ti in range(8):
                p = nps()
                for kc in range(8):
                    K.mm(p, p.ap, hT.ap[:, kc, ti * 128:(ti + 1) * 128], win.ap[:, kc, 1024:1536],
                         kc == 0, kc == 7, [hT, win])
                vt = vts[ti % 2]
                K.cp("act", vt.ap[:, :, 0:64], p.ap.rearrange("p (h d) -> p h d", h=8), [p], [vt])
                K.dma("ov%d" % (ti % 2), v_s[tb + ti * 128: tb + (ti + 1) * 128, :].rearrange("p (h d) -> p h d", h=8),
                      vt.ap, r=[vt])
            if own:
                for c in range(16):
                    for half in range(2):
                        p = nps()
                        for kc in range(8):
                            K.mm(p, p.ap, win.ap[:, kc, 1536 + c * 128: 1536 + (c + 1) * 128],
                                 hT.ap[:, kc, half * 512:(half + 1) * 512], kc == 0, kc == 7, [win, hT])
                        ob = obf[oi % 2]
                        oi += 1
                        K.act(ob.ap, p.ap, AF.Sigmoid, [p], [ob])
                        t0 = tb + half * 512 - NT_OWN
                        K.dma("oq%d" % (oi % 2), gT_s[c, :, t0:t0 + 512], ob.ap, r=[ob])
            oic[0] = oi

        prep(0)
        for blk in range(8):
            if blk + 1 < 8:
                prep(blk + 1)
            compute(blk)
        K.barrier()

    def phase_S():
        K.phase_reset()
        sm = lambda name, n=16: K.sb([128, n], F32, name)
        wglu = K.sb([128, 4, 512], BF16, "wglu")
        WS = K.sb([128, 16, 2, 2, 128], BF16, "WS")
        WY1 = K.sb([128, 16, 2, 2, 128], BF16, "WY1")
        WY2 = K.sb([128, 32, 128], BF16, "WY2")
        Ct = K.sb([128, 16, 128], F32, "Ct")
        St = K.sb([128, 16, 128], F32, "St")
        r8 = sm("r8")
        Dre = sm("Dre")
        Dim = sm("Dim")
        car_re = sm("car_re")
        car_im = sm("car_im")
        ta, tb_, tc_ = sm("ta"), sm("tb"), sm("tc")
        mark = K.off
        stage = K.sb([128, 512], F32, "stageS")
        ldt, are, aim = sm("ldt"), sm("are"), sm("aim")
        bre = K.sb([128, 16, 16], F32, "bre")
        bim = K.sb([128, 16, 16], F32, "bim")
        cre = K.sb([128, 16, 16], F32, "cre")
        cim = K.sb([128, 16, 16], F32, "cim")
        ncim = K.sb([128, 16, 16], F32, "ncim")
        for t_, nm in ((ldt, "ldt"), (are, "are"), (aim, "aim")):
            K.dma("c0", t_.ap, s5[nm], w=[t_])
        for t_, nm in ((bre, "bre"), (bim, "bim"), (cre, "cre"), (cim, "cim")):
            K.dma("c0", t_.ap.rearrange("p a b -> p (a b)"), s5[nm], w=[t_])
        for kc in range(4):
            K.dma("wst", stage.ap, w_glu[kc * 128:(kc + 1) * 128, :], w=[stage])
            K.cp("dve", wglu.ap[:, kc, :], stage.ap, [stage], [wglu])
        dt_, xr, th, mag, cs, sn = sm("dt"), sm("xr"), sm("th"), sm("mag"), sm("cs"), sm("sn")
        abr, abi, den, nr, fre, fim = sm("abr"), sm("abi"), sm("den"), sm("nr"), sm("fre"), sm("fim")
        ti_ = K.sb([128, 16], I32, "ti")
        V_ = "dve"
        K.act(dt_.ap, ldt.ap, AF.Exp, [ldt], [dt_])
        K.tt(V_, xr.ap, dt_.ap, are.ap, ALU.mult, [dt_, are], [xr])
        K.tt(V_, th.ap, dt_.ap, aim.ap, ALU.mult, [dt_, aim], [th])
        K.act(mag.ap, xr.ap, AF.Exp, [xr], [mag])
        K.act(r8.ap, xr.ap, AF.Exp, [xr], [r8], scale=8.0)

        def sin_small(dst, src, phase):
            K.ts(V_, ta.ap, src.ap, phase, 1.0 / (2 * PI), ALU.add, ALU.mult, [src], [ta])
            K.cp(V_, ti_.ap, ta.ap, [ta], [ti_])
            K.cp(V_, ta.ap, ti_.ap, [ti_], [ta])
            K.stt(V_, ta.ap, ta.ap, -2 * PI, src.ap, ALU.mult, ALU.add, [ta, src], [ta])
            K.ts(V_, ta.ap, ta.ap, phase, None, ALU.add, None, [ta], [ta])
            K.ts(V_, tb_.ap, ta.ap, PI, -2 * PI, ALU.is_gt, ALU.mult, [ta], [tb_])
            K.tt(V_, ta.ap, ta.ap, tb_.ap, ALU.add, [ta, tb_], [ta])
            K.ts(V_, tb_.ap, ta.ap, -PI, 2 * PI, ALU.is_lt, ALU.mult, [ta], [tb_])
            K.tt(V_, ta.ap, ta.ap, tb_.ap, ALU.add, [ta, tb_], [ta])
            K.act(dst.ap, ta.ap, AF.Sin, [ta], [dst])

        sin_small(cs, th, PI / 2)
        sin_small(sn, th, 0.0)
        K.tt(V_, abr.ap, mag.ap, cs.ap, ALU.mult, [mag, cs], [abr])
        K.tt(V_, abi.ap, mag.ap, sn.ap, ALU.mult, [mag, sn], [abi])
        K.tt(V_, den.ap, are.ap, are.ap, ALU.mult, [are], [den])
        K.tt(V_, ta.ap, aim.ap, aim.ap, ALU.mult, [aim], [ta])
        K.tt(V_, den.ap, den.ap, ta.ap, ALU.add, [den, ta], [den])
        K.op(V_, lambda e: e.reciprocal(out=den.ap, in_=den.ap), [den], [den])
        K.ts(V_, nr.ap, abr.ap, -1.0, None, ALU.add, None, [abr], [nr])
        K.tt(V_, ta.ap, nr.ap, are.ap, ALU.mult, [nr, are], [ta])
        K.tt(V_, tb_.ap, abi.ap, aim.ap, ALU.mult, [abi, aim], [tb_])
        K.tt(V_, ta.ap, ta.ap, tb_.ap, ALU.add, [ta, tb_], [ta])
        K.tt(V_, fre.ap, ta.ap, den.ap, ALU.mult, [ta, den], [fre])
        K.tt(V_, ta.ap, abi.ap, are.ap, ALU.mult, [abi, are], [ta])
        K.tt(V_, tb_.ap, nr.ap, aim.ap, ALU.mult, [nr, aim], [tb_])
        K.tt(V_, ta.ap, ta.ap, tb_.ap, ALU.subtract, [ta, tb_], [ta])
        K.tt(V_, fim.ap, ta.ap, den.ap, ALU.mult, [ta, den], [fim])
        pwf_re = K.sb([128, 16, 9], F32, "pwf_re")
        pwf_im = K.sb([128, 16, 9], F32, "pwf_im")
        pwr_re = K.sb([128, 16, 8], F32, "pwr_re")
        pwr_im = K.sb([128, 16, 8], F32, "pwr_im")
        K.memset(V_, pwf_re.ap[:, :, 0], 1.0, [pwf_re])
        K.memset(V_, pwf_im.ap[:, :, 0], 0.0, [pwf_im])
        for d in range(8):
            K.tt(V_, ta.ap, pwf_re.ap[:, :, d], abr.ap, ALU.mult, [pwf_re, abr], [ta])
            K.tt(V_, tb_.ap, pwf_im.ap[:, :, d], abi.ap, ALU.mult, [pwf_im, abi], [tb_])
            K.tt(V_, pwf_re.ap[:, :, d + 1], ta.ap, tb_.ap, ALU.subtract, [ta, tb_], [pwf_re])
            K.tt(V_, ta.ap, pwf_re.ap[:, :, d], abi.ap, ALU.mult, [pwf_re, abi], [ta])
            K.tt(V_, tb_.ap, pwf_im.ap[:, :, d], abr.ap, ALU.mult, [pwf_im, abr], [tb_])
            K.tt(V_, pwf_im.ap[:, :, d + 1], ta.ap, tb_.ap, ALU.add, [ta, tb_], [pwf_im])
        for j in range(8):
            K.cp(V_, pwr_re.ap[:, :, j], pwf_re.ap[:, :, 7 - j], [pwf_re], [pwr_re])
            K.cp(V_, pwr_im.ap[:, :, j], pwf_im.ap[:, :, 7 - j], [pwf_im], [pwr_im])
        K.cp(V_, Dre.ap, pwf_re.ap[:, :, 8], [pwf_re], [Dre])
        K.cp(V_, Dim.ap, pwf_im.ap[:, :, 8], [pwf_im], [Dim])
        ur, ui, rr = sm("ur"), sm("ui"), sm("rr")
        K.op(V_, lambda e: e.reciprocal(out=rr.ap, in_=r8.ap), [r8], [rr])
        K.tt(V_, ur.ap, Dre.ap, rr.ap, ALU.mult, [Dre, rr], [ur])
        K.tt(V_, ui.ap, Dim.ap, rr.ap, ALU.mult, [Dim, rr], [ui])
        tm1 = K.sb([128, 16, 64], F32, "tm1")
        tm2 = K.sb([128, 16, 64], F32, "tm2")
        K.memset(V_, Ct.ap[:, :, 0], 1.0, [Ct])
        K.memset(V_, St.ap[:, :, 0], 0.0, [St])
        for k in range(7):
            n = 1 << k
            urb = ur.ap.unsqueeze(2).broadcast_to([128, 16, n])
            uib = ui.ap.unsqueeze(2).broadcast_to([128, 16, n])
            K.tt(V_, tm1.ap[:, :, 0:n], Ct.ap[:, :, 0:n], urb, ALU.mult, [Ct, ur], [tm1])
            K.tt(V_, tm2.ap[:, :, 0:n], St.ap[:, :, 0:n], uib, ALU.mult, [St, ui], [tm2])
            K.tt(V_, Ct.ap[:, :, n:2 * n], tm1.ap[:, :, 0:n], tm2.ap[:, :, 0:n], ALU.subtract, [tm1, tm2], [Ct])
            K.tt(V_, tm1.ap[:, :, 0:n], Ct.ap[:, :, 0:n], uib, ALU.mult, [Ct, ui], [tm1])
            K.tt(V_, tm2.ap[:, :, 0:n], St.ap[:, :, 0:n], urb, ALU.mult, [St, ur], [tm2])
            K.tt(V_, St.ap[:, :, n:2 * n], tm1.ap[:, :, 0:n], tm2.ap[:, :, 0:n], ALU.add, [tm1, tm2], [St])
            K.tt(V_, ta.ap, ur.ap, ur.ap, ALU.mult, [ur], [ta])
            K.tt(V_, tb_.ap, ui.ap, ui.ap, ALU.mult, [ui], [tb_])
            K.tt(V_, tc_.ap, ur.ap, ui.ap, ALU.mult, [ur, ui], [tc_])
            K.tt(V_, ur.ap, ta.ap, tb_.ap, ALU.subtract, [ta, tb_], [ur])
            K.ts(V_, ui.ap, tc_.ap, 2.0, None, ALU.mult, None, [tc_], [ui])
        bbr = K.sb([128, 16, 16], F32, "bbr")
        bbi = K.sb([128, 16, 16], F32, "bbi")
        t3a = K.sb([128, 16, 16], F32, "t3a")
        t3b = K.sb([128, 16, 16], F32, "t3b")
        freb = fre.ap.unsqueeze(2).broadcast_to([128, 16, 16])
        fimb = fim.ap.unsqueeze(2).broadcast_to([128, 16, 16])
        K.tt(V_, t3a.ap, bre.ap, freb, ALU.mult, [bre, fre], [t3a])
        K.tt(V_, t3b.ap, bim.ap, fimb, ALU.mult, [bim, fim], [t3b])
        K.tt(V_, bbr.ap, t3a.ap, t3b.ap, ALU.subtract, [t3a, t3b], [bbr])
        K.tt(V_, t3a.ap, bim.ap, freb, ALU.mult, [bim, fre], [t3a])
        K.tt(V_, t3b.ap, bre.ap, fimb, ALU.mult, [bre, fim], [t3b])
        K.tt(V_, bbi.ap, t3a.ap, t3b.ap, ALU.add, [t3a, t3b], [bbi])
        K.ts(V_, ncim.ap, cim.ap, -1.0, None, ALU.mult, None, [cim], [ncim])
        Fre = K.sb([128, 16, 15, 16], F32, "Fre")
        Fim = K.sb([128, 16, 15, 16], F32, "Fim")
        t4a = K.sb([128, 16, 8, 16], F32, "t4a")
        t4b = K.sb([128, 16, 8, 16], F32, "t4b")
        K.memset("pool", Fre.ap, 0.0, [Fre])
        K.memset("pool", Fim.ap, 0.0, [Fim])
        S4 = [128, 16, 8, 16]
        prb = pwr_re.ap.unsqueeze(3).broadcast_to(S4)
        pib = pwr_im.ap.unsqueeze(3).broadcast_to(S4)
        bbrb = bbr.ap.unsqueeze(2).broadcast_to(S4)
        bbib = bbi.ap.unsqueeze(2).broadcast_to(S4)
        K.tt(V_, t4a.ap, prb, bbrb, ALU.mult, [pwr_re, bbr], [t4a])
        K.tt(V_, t4b.ap, pib, bbib, ALU.mult, [pwr_im, bbi], [t4b])
        K.tt(V_, Fre.ap[:, :, 0:8, :], t4a.ap, t4b.ap, ALU.subtract, [t4a, t4b], [Fre])
        K.tt(V_, t4a.ap, prb, bbib, ALU.mult, [pwr_re, bbi], [t4a])
        K.tt(V_, t4b.ap, pib, bbrb, ALU.mult, [pwr_im, bbr], [t4b])
        K.tt(V_, Fim.ap[:, :, 0:8, :], t4a.ap, t4b.ap, ALU.add, [t4a, t4b], [Fim])
        K.memset("pool", WS.ap, 0.0, [WS])
        K.memset("pool", WY1.ap, 0.0, [WY1])
        for p_ in range(16):
            for ri, Ft in ((0, Fre), (1, Fim)):
                ps = nps()
                K.tr(ps, ps.ap[:, 0:128], Ft.ap[:, p_, 0:8, :].rearrange("p a b -> p (a b)"), idf.ap, [Ft, idf])
                K.cp(V_, WS.ap[:, p_, ri, 0, 0:64], ps.ap[:, 0:64], [ps], [WS])
                K.cp(V_, WS.ap[:, p_, ri, 1, 64:128], ps.ap[:, 64:128], [ps], [WS])
        pfr = pwf_re.ap[:, :, 1:9].unsqueeze(3).broadcast_to(S4)
        pfi = pwf_im.ap[:, :, 1:9].unsqueeze(3).broadcast_to(S4)
        creb = cre.ap.unsqueeze(2).broadcast_to(S4)
        cimb = cim.ap.unsqueeze(2).broadcast_to(S4)
        K.tt(V_, t4a.ap, creb, pfr, ALU.mult, [cre, pwf_re], [t4a])
        K.tt(V_, t4b.ap, cimb, pfi, ALU.mult, [cim, pwf_im], [t4b])
        K.tt(V_, t4a.ap, t4a.ap, t4b.ap, ALU.subtract, [t4a, t4b], [t4a])
        for g2 in range(2):
            K.cp(V_, WY1.ap[g2 * 64:(g2 + 1) * 64, :, 0, g2, :],
                 t4a.ap[g2 * 64:(g2 + 1) * 64].rearrange("p a b c -> p a (b c)"), [t4a], [WY1])
        K.tt(V_, t4a.ap, creb, pfi, ALU.mult, [cre, pwf_im], [t4a])
        K.tt(V_, t4b.ap, cimb, pfr, ALU.mult, [cim, pwf_re], [t4b])
        K.tt(V_, t4a.ap, t4a.ap, t4b.ap, ALU.add, [t4a, t4b], [t4a])
        K.ts(V_, t4a.ap, t4a.ap, -1.0, None, ALU.mult, None, [t4a], [t4a])
        for g2 in range(2):
            K.cp(V_, WY1.ap[g2 * 64:(g2 + 1) * 64, :, 1, g2, :],
                 t4a.ap[g2 * 64:(g2 + 1) * 64].rearrange("p a b c -> p a (b c)"), [t4a], [WY1])
        dB = K.sb([128, 512], F32, "dB")
        dI = K.sb([128, 32, 8, 16], F32, "dI")
        K.dma("c0", dB.ap, ssm_d.partition_broadcast(128), w=[dB])
        K.tt(V_, dI.ap, idf.ap.rearrange("p (a b) -> p a b", a=8).unsqueeze(1).broadcast_to([128, 32, 8, 16]),
             dB.ap.rearrange("p (g c) -> p g c", g=32).unsqueeze(2).broadcast_to([128, 32, 8, 16]), ALU.mult,
             [idf, dB], [dI])
        for g in range(32):
            p_, g2 = g // 2, g % 2
            if g % 4 == 0:
                ps = nps()
            o0 = (g % 4) * 128
            sl = slice(g2 * 64, (g2 + 1) * 64)
            for j in range(8):
                oap = ps.ap[:, o0 + j * 16: o0 + (j + 1) * 16]
                K.mm(ps, oap, Fre.ap[sl, p_, 7 - j:15 - j, :].rearrange("p a b -> p (a b)"), cre.ap[sl, p_, :],
                     True, False, [Fre, cre])
                K.mm(ps, oap, Fim.ap[sl, p_, 7 - j:15 - j, :].rearrange("p a b -> p (a b)"), ncim.ap[sl, p_, :],
                     False, True, [Fim, ncim])
            if g % 4 == 3:
                K.tt(V_, WY2.ap[:, g - 3:g + 1, :], ps.ap.rearrange("p (g k) -> p g k", g=4),
                     dI.ap[:, g - 3:g + 1].rearrange("p g a b -> p g (a b)"), ALU.add, [ps, dI], [WY2])
        K.memset(V_, car_re.ap, 0.0, [car_re])
        K.memset(V_, car_im.ap, 0.0, [car_im])
        K.barrier()
        K.off = mark
        UTs = [K.sb([128, 32, 128], BF16, "UT%d" % i) for i in range(2)]
        Sres = [K.sb([128, 16, 128], F32, "Sre%d" % i) for i in range(2)]
        Sims = [K.sb([128, 16, 128], F32, "Sim%d" % i) for i in range(2)]
        gre = K.sb([128, 16, 128], F32, "gre")
        gim = K.sb([128, 16, 128], F32, "gim")
        u1 = K.sb([128, 16, 128], F32, "u1")
        u2 = K.sb([128, 16, 128], F32, "u2")
        Pre = K.sb([128, 16, 129], BF16, "Pre")
        Pim = K.sb([128, 16, 129], BF16, "Pim")
        ytm = K.sb([128, 8, 512], BF16, "ytm")
        yT = K.sb([128, 4, 1024], BF16, "yT")
        sg = K.sb([128, 512], BF16, "sg")
        sos = [K.sb([128, 4, 512], BF16, "so%d" % i) for i in range(2)]

        def S1(blk):
            UT, Sre, Sim = UTs[blk % 2], Sres[blk % 2], Sims[blk % 2]
            K.dma("ut%d" % (blk % 2), UT.ap.rearrange("p a b -> p (a b)"), UT5_s[blk], r=[ut5tok[blk]], w=[UT])
            for ri, Sx in ((0, Sre), (1, Sim)):
                for p0 in range(0, 16, 4):
                    p = nps()
                    for pi_ in range(4):
                        pp = p0 + pi_
                        K.mm(p, p.ap[:, pi_ * 128:(pi_ + 1) * 128], WS.ap[:, pp, ri, 0, :], UT.ap[:, 2 * pp, :], True, False, [WS, UT])
                        K.mm(p, p.ap[:, pi_ * 128:(pi_ + 1) * 128], WS.ap[:, pp, ri, 1, :], UT.ap[:, 2 * pp + 1, :], False, True, [WS, UT])
                    K.cp("act", Sx.ap[:, p0:p0 + 4, :], p.ap.rearrange("p (a b) -> p a b", a=4), [p], [Sx])

        def SC(blk):
            own = blk >= 4
            Sre, Sim = Sres[blk % 2], Sims[blk % 2]
            if own:
                K.cp(V_, Pre.ap[:, :, 0], car_re.ap, [car_re], [Pre])
                K.cp(V_, Pim.ap[:, :, 0], car_im.ap, [car_im], [Pim])
            K.tt(V_, ta.ap, Dre.ap, car_re.ap, ALU.mult, [Dre, car_re], [ta])
            K.tt(V_, tb_.ap, Dim.ap, car_im.ap, ALU.mult, [Dim, car_im], [tb_])
            K.tt(V_, ta.ap, ta.ap, tb_.ap, ALU.subtract, [ta, tb_], [ta])
            K.tt(V_, Sre.ap[:, :, 0], Sre.ap[:, :, 0], ta.ap, ALU.add, [Sre, ta], [Sre])
            K.tt(V_, ta.ap, Dre.ap, car_im.ap, ALU.mult, [Dre, car_im], [ta])
            K.tt(V_, tb_.ap, Dim.ap, car_re.ap, ALU.mult, [Dim, car_re], [tb_])
            K.tt(V_, ta.ap, ta.ap, tb_.ap, ALU.add, [ta, tb_], [ta])
            K.tt(V_, Sim.ap[:, :, 0], Sim.ap[:, :, 0], ta.ap, ALU.add, [Sim, ta], [Sim])
            K.tt("dve", u1.ap, Ct.ap, Sre.ap, ALU.mult, [Ct, Sre], [u1])
            K.tt("pool", u2.ap, St.ap, Sim.ap, ALU.mult, [St, Sim], [u2])
            K.tt("dve", gre.ap, u1.ap, u2.ap, ALU.add, [u1, u2], [gre])
            K.tt("dve", u1.ap, Ct.ap, Sim.ap, ALU.mult, [Ct, Sim], [u1])
            K.tt("pool", u2.ap, St.ap, Sre.ap, ALU.mult, [St, Sre], [u2])
            K.tt("dve", gim.ap, u1.ap, u2.ap, ALU.subtract, [u1, u2], [gim])
            for pp in range(16):
                rb = r8.ap[:, pp:pp + 1].to_broadcast([128, 128])
                K.op("dve", lambda e, pp=pp, rb=rb, Sre=Sre: e.tensor_tensor_scan(out=Sre.ap[:, pp, :], data0=rb, data1=gre.ap[:, pp, :],
                                                                                 initial=0.0, op0=ALU.mult, op1=ALU.add), [gre, r8], [Sre])
                K.op("dve", lambda e, pp=pp, rb=rb, Sim=Sim: e.tensor_tensor_scan(out=Sim.ap[:, pp, :], data0=rb, data1=gim.ap[:, pp, :],
                                                                                 initial=0.0, op0=ALU.mult, op1=ALU.add), [gim, r8], [Sim])
            K.tt("dve", u1.ap, Ct.ap, Sre.ap, ALU.mult, [Ct, Sre], [u1])
            K.tt("pool", u2.ap, St.ap, Sim.ap, ALU.mult, [St, Sim], [u2])
            K.tt("dve", gre.ap, u1.ap, u2.ap, ALU.subtract, [u1, u2], [gre])
            K.tt("dve", u1.ap, Ct.ap, Sim.ap, ALU.mult, [Ct, Sim], [u1])
            K.tt("pool", u2.ap, St.ap, Sre.ap, ALU.mult, [St, Sre], [u2])
            K.tt("dve", gim.ap, u1.ap, u2.ap, ALU.add, [u1, u2], [gim])
            K.cp(V_, car_re.ap, gre.ap[:, :, 127], [gre], [car_re])
            K.cp(V_, car_im.ap, gim.ap[:, :, 127], [gim], [car_im])
            if own:
                K.cp("act", Pre.ap[:, :, 1:129], gre.ap, [gre], [Pre])
                K.cp("act", Pim.ap[:, :, 1:129], gim.ap, [gim], [Pim])

        def SY(blk):
            tb = blk * 1024
            UT = UTs[blk % 2]
            for g0 in range(0, 32, 4):
                p = nps()
                for gi in range(4):
                    g = g0 + gi
                    pp, g2 = g // 2, g % 2
                    oap = p.ap[:, gi * 128:(gi + 1) * 128]
                    K.mm(p, oap, Pre.ap[:, pp, 0:128], WY1.ap[:, pp, 0, g2, :], True, False, [Pre, WY1])
                    K.mm(p, oap, Pim.ap[:, pp, 0:128], WY1.ap[:, pp, 1, g2, :], False, False, [Pim, WY1])
                    K.mm(p, oap, UT.ap[:, g, :], WY2.ap[:, g, :], False, True, [UT, WY2])
                K.act(ytm.ap.rearrange("p j (g c) -> p g j c", g=32)[:, g0:g0 + 4],
                      p.ap.rearrange("p (g j c) -> p g j c", g=4, j=8), AF.Gelu_apprx_tanh, [p], [ytm])
            for j in range(8):
                if j % 2 == 0:
                    p = nps()
                    pv = p.ap.bitcast(BF16).rearrange("p (a b) -> p a b", a=8)
                for cc in range(4):
                    K.tr(p, pv[:, (j % 2) * 4 + cc, :], ytm.ap[:, j, cc * 128:(cc + 1) * 128], idb.ap, [ytm, idb])
                K.cp("act", yT.ap[:, :, j:1024:8], pv[:, (j % 2) * 4:(j % 2) * 4 + 4, :], [p], [yT])
            for half in range(2):
                for co in range(4):
                    p = nps()
                    for kc in range(4):
                        K.mm(p, p.ap, wglu.ap[:, kc, co * 128:(co + 1) * 128], yT.ap[:, kc, half * 512:(half + 1) * 512],
                             kc == 0, kc == 3, [wglu, yT])
                    K.act(sg.ap, p.ap, AF.Sigmoid, [p], [sg])
                    so = sos[half]
                    K.tt("pool", so.ap[:, co, :], yT.ap[:, co, half * 512:(half + 1) * 512], sg.ap,
                         ALU.mult, [yT, sg], [so])
                t0_ = tb - NT_OWN + half * 512
                K.dma("oS%d" % half, ssmT_s[:, :, t0_: t0_ + 512].rearrange("c p t -> p c t"), sos[half].ap, r=[sos[half]])

        S1(0)
        for blk in range(8):
            if blk + 1 < 8:
                S1(blk + 1)
            SC(blk)
            if blk >= 4:
                SY(blk)
        K.barrier()

    def phase_B():
        K.phase_reset()
        wpsB = K.sb([128, 4, 1024], BF16, "wpsB")
        wpaB = K.sb([128, 4, 1024], BF16, "wpaB")
        woutB = K.sb([128, 8, 1024], BF16, "woutB")
        wqB = K.sb([128, 8, 2048], BF16, "wqB")
        wstg2 = K.sb([128, 2048], F32, "wstg2")
        wchunks = ([(wpsB, w_ps, kc, 1024) for kc in range(4)] + [(wpaB, w_pa, kc, 1024) for kc in range(4)]
                   + [(woutB, w_out, kc, 1024) for kc in range(8)] + [(wqB, w_q, kc, 2048) for kc in range(8)])
        KTs = [K.sb([96, NT_LOC], BF16, "KT%d" % i) for i in range(2)]
        QTs = [K.sb([96, NT_OWN], BF16, "QT%d" % i) for i in range(2)]
        Vs = [K.sb([128, 64, 65], BF16, "V%d" % i) for i in range(2)]
        QA = K.sb([64, NT_OWN], BF16, "QA")
        att = K.sb([128, 32, 512], BF16, "att")
        kmax = K.sb([64, 1], F32, "kmax")
        kmaxb = K.sb([64, 1], BF16, "kmaxb")
        ksb = K.sb([64, 32], BF16, "ksb")
        valid = K.sb([128, 512], F32, "valid")
        ownm = K.sb([128, 512], F32, "ownm")
        negb = K.sb([128, 512], F32, "negb")
        stg = K.sb([128, 2048], F32, "stgB")
        cm = K.sb([128, 4, 512], BF16, "cm")
        NG = 4
        gms = [K.sb([128, 32], F32, "gm%d" % i) for i in range(NG)]
        m8s = [K.sb([128, 8], F32, "m8%d" % i) for i in range(NG)]
        sels = [K.sb([128, 32], F32, "sel%d" % i) for i in range(NG)]
        sexs = [K.sb([128, 128], BF16, "sex%d" % i) for i in range(NG)]
        mraws = [K.sb([128, 4], F32, "mraw%d" % i) for i in range(2)]
        PTs = [K.sb([128, 512], BF16, "PT%d" % i) for i in range(3)]
        rls = [K.sb([128, 1], F32, "rl%d" % i) for i in range(4)]
        ostg = [K.sb([128, 4, 128], BF16, "ostg%d" % i) for i in range(2)]
        K.dma("c0", valid.ap, validc.partition_broadcast(128), w=[valid])
        K.dma("c0", ownm.ap, ownc.partition_broadcast(128), w=[ownm])
        K.ts("dve", negb.ap, valid.ap, -1.0, 1e30, ALU.add, ALU.mult, [valid], [negb])
        for i in range(4):
            K.dma("c0", stg.ap[:, 0:512], cm_d[i], w=[stg])
            K.cp("dve", cm.ap[:, i, :], stg.ap[:, 0:512], [stg], [cm])
        gm4 = K.sb([128, 4, 32], F32, "gm4")
        m84 = K.sb([128, 4, 8], F32, "m84")
        sel4 = K.sb([128, 4, 32], F32, "sel4")
        sex4 = K.sb([128, 4, 128], BF16, "sex4")
        K.memset("pool", sex4.ap, 0.0, [sex4])
        for kb_ in KTs:
            for c4 in range(4):
                K.dma("c0", stg.ap[64:96, :], koh_d[:, c4 * 2048:(c4 + 1) * 2048], w=[stg])
                K.cp("dve", kb_.ap[64:96, c4 * 2048:(c4 + 1) * 2048], stg.ap[64:96, :], [stg], [kb_])
        for h in range(8):
            hp, pb = h // 2, (h % 2) * 64
            KT, QT, V = KTs[h % 2], QTs[h % 2], Vs[h % 2]
            K.dma("bk%d" % (h % 2), KT.ap[0:64, :], kT_s[hp, pb:pb + 64, :], w=[KT])
            K.dma("bq%d" % (h % 2), QT.ap[0:64, :], qT_s[hp, pb:pb + 64, :], w=[QT])
            K.dma("bv%d" % (h % 2), V.ap, v_s.rearrange("(t p) c -> p t c", p=128)[:, :, h * 65:(h + 1) * 65], w=[V])
            K.act(QA.ap, QT.ap[0:64, :], AF.Abs, [QT], [QA])
            K.op("dve", lambda e, KT=KT: e.tensor_reduce(out=kmax.ap, in_=KT.ap[0:64, :], axis=AX.X, op=ALU.max,
                                                         apply_absolute_value=True), [KT], [kmax])
            K.cp("dve", kmaxb.ap, kmax.ap, [kmax], [kmaxb])
            K.cp("dve", ksb.ap, ksum.ap[pb:pb + 64, hp, :], [ksum], [ksb])
            for qg in range(8):
                q0 = qg * 512
                pg = PS[qg % 3]
                c0 = (qg // 3) * 132
                for j in range(4):
                    K.mm(pg, pg.ap[:, c0 + j * 32: c0 + (j + 1) * 32], QT.ap[0:64, q0 + j * 128: q0 + (j + 1) * 128],
                         ksb.ap, True, True, [QT, ksb])
                    K.mm(pg, pg.ap[:, c0 + 128 + j: c0 + 129 + j], QA.ap[:, q0 + j * 128: q0 + (j + 1) * 128],
                         kmaxb.ap, True, True, [QA, kmaxb])
            for qg in range(8):
                q0 = qg * 512
                pg = PS[qg % 3]
                c0 = (qg // 3) * 132
                mraw = mraws[qg % 2]
                K.cp("dve", mraw.ap, pg.ap[:, c0 + 128: c0 + 132], [pg], [mraw])
                S4 = [128, 2, 2, 32]
                qsl = slice(2 * qg * 32, (2 * qg + 2) * 32)
                bq = lambda t_: t_.ap[:, qsl].rearrange("p (a b) -> p a b", a=2).unsqueeze(2).broadcast_to(S4)
                g4 = gm4.ap.rearrange("p (a c) b -> p a c b", a=2)
                s4 = sel4.ap.rearrange("p (a c) b -> p a c b", a=2)
                K.tt("dve", g4, pg.ap[:, c0:c0 + 128].rearrange("p (a c b) -> p a c b", a=2, c=2), bq(negb), ALU.add, [pg, negb], [gm4])
                for j in range(4):
                    K.op("dve", lambda e, j=j: e.max(out=m84.ap[:, j, :], in_=gm4.ap[:, j, :]), [gm4], [m84])
                K.tt("dve", sel4.ap, gm4.ap, m84.ap[:, :, 2:3].broadcast_to([128, 4, 32]), ALU.is_ge, [gm4, m84], [sel4])
                K.tt("dve", s4, s4, bq(valid), ALU.mult, [sel4, valid], [sel4])
                K.tt("dve", s4, s4, bq(ownm), ALU.add, [sel4, ownm], [sel4])
                K.ts("dve", sel4.ap, sel4.ap, -1.0, BIG, ALU.add, ALU.mult, [sel4], [sel4])
                K.tt("dve", sex4.ap[:, :, 64:96], sel4.ap, mraw.ap.unsqueeze(2).broadcast_to([128, 4, 32]), ALU.subtract,
                     [sel4, mraw], [sex4])
                pt = PS[3]
                for j in range(4):
                    K.mm(pt, pt.ap[:, j * 128:(j + 1) * 128], sex4.ap[:, j, :], idb.ap, True, True, [sex4, idb])
                K.cp("act", QT.ap[64:96, q0:q0 + 512], pt.ap[64:96, :], [pt], [QT])
            for qg in range(8):
                q0 = qg * 512
                nkt = 36 + 4 * qg

                def qk(kt):
                    ps = PS[kt % 3]
                    last_own = kt >= nkt - 4
                    K.mm(ps, ps.ap, KT.ap[:, kt * 128:(kt + 1) * 128], QT.ap[:, q0:q0 + 512],
                         True, not last_own, [KT, QT])
                    if last_own:
                        K.mm(ps, ps.ap, idb.ap, cm.ap[:, kt - (nkt - 4), :], False, True, [idb, cm])

                qk(0)
                qk(1)
                for kt in range(nkt):
                    if kt + 2 < nkt:
                        qk(kt + 2)
                    ps = PS[kt % 3]
                    PT = PTs[kt % 3]
                    K.act(PT.ap, ps.ap, AF.Exp, [ps], [PT], scale=0.125)
                    for j in range(4):
                        po = PS[4 + j]
                        K.mm(po, po.ap[:, 0:65], PT.ap[:, j * 128:(j + 1) * 128], V.ap[:, kt, :],
                             kt == 0, kt == nkt - 1, [PT, V])
                for j in range(4):
                    po = PS[4 + j]
                    K.op("dve", lambda e, po=po, j=j: e.reciprocal(out=rls[j].ap, in_=po.ap[:, 64:65]), [po], [rls[j]])
                for j in range(4):
                    po = PS[4 + j]
                    K.ts("dve", att.ap[:, qg * 4 + j, h * 64:(h + 1) * 64], po.ap[:, 0:64], rls[j].ap[:, 0:1], None,
                         ALU.mult, None, [po, rls[j]], [att])
                wi = h * 8 + qg
                wbufs = (stg, wstg2)
                if wi < len(wchunks):
                    dst, src, kc, ncols = wchunks[wi]
                    K.dma("bw%d" % (wi % 2), wbufs[wi % 2].ap[:, 0:ncols], src[kc * 128:(kc + 1) * 128, :], w=[wbufs[wi % 2]])
                if 1 <= wi <= len(wchunks):
                    dst, src, kc, ncols = wchunks[wi - 1]
                    K.cp("dve", dst.ap[:, kc, :], wbufs[(wi - 1) % 2].ap[:, 0:ncols], [wbufs[(wi - 1) % 2]], [dst])
        for qt in range(32):
            p = PS[qt % 2]
            pv = p.ap.bitcast(BF16)[:, 0:512].rearrange("p (a b) -> p a b", a=4)
            for c in range(4):
                K.tr(p, pv[:, c, :], att.ap[:, qt, c * 128:(c + 1) * 128], idb.ap, [att, idb])
            og = ostg[qt % 2]
            K.cp("act", og.ap, pv, [p], [og])
            K.dma("ob%d" % (qt % 2), attT_s[:, :, qt * 128:(qt + 1) * 128].rearrange("c p t -> p c t"), og.ap, r=[og])
        K.barrier()

    def phase_C1():
        K.phase_reset()
        wps = K.sb([128, 4, 1024], BF16, "wps")
        wpa = K.sb([128, 4, 1024], BF16, "wpa")
        wout = K.sb([128, 8, 1024], BF16, "wout")
        wq = K.sb([128, 8, 2048], BF16, "wq")
        keysT = K.sb([128, 16, 128], F32, "keysT")
        gf = K.sb([128, 1024], F32, "gffn")
        stage = K.sb([128, 2048], F32, "stageC")
        ssmT = K.sb([128, 4, 512], BF16, "ssmT")
        attT = K.sb([128, 4, 512], BF16, "attT")
        gaT = K.sb([128, 8, 512], BF16, "gaT")
        gbT = K.sb([128, 8, 512], BF16, "gbT")
        mT = K.sb([128, 8, 512], BF16, "mT")
        t1cs = [K.sb([128, 512], F32, "t1C%d" % i) for i in range(2)]
        t2cs = [K.sb([128, 512], F32, "t2C%d" % i) for i in range(2)]
        xts = [K.sb([128, 1024], F32, "xtC%d" % i) for i in range(2)]
        x1s = [K.sb([128, 1024], F32, "x1C%d" % i) for i in range(2)]
        hbs = [K.sb([128, 1024], BF16, "hbC%d" % i) for i in range(2)]
        sq = K.sb([128, 1024], BF16, "sqC")
        ss = K.sb([128, 1], F32, "ssC")
        rs = K.sb([128, 1], F32, "rsC")
        h2T = K.sb([128, 8, 512], BF16, "h2T")
        qT = K.sb([128, 16, 512], F32, "qT")
        scs = [K.sb([128, 16, 128], F32, "sc%d" % i) for i in range(2)]
        K.dma("c0", gf.ap, g_ffn.partition_broadcast(128), w=[gf])
        for ch in range(16):
            K.dma("wst", stage.ap[:, 0:128], keys[ch], w=[stage])
            p = nps()
            K.tr(p, p.ap[:, 0:128], stage.ap[:, 0:128], idf.ap, [stage, idf])
            K.cp("dve", keysT.ap[:, ch, :], p.ap[:, 0:128], [p], [keysT])
        xi = 0

        def c1_loads(tg_):
            ta_ = tg_ * 512
            K.dma("c1a", ssmT.ap, ssmT_s[:, :, ta_:ta_ + 512].rearrange("c p t -> p c t"), w=[ssmT])
            K.dma("c1b", attT.ap, attT_s[:, :, ta_:ta_ + 512].rearrange("c p t -> p c t"), w=[attT])
            K.dma("c1c", gaT.ap, gT_s[0:8, :, ta_:ta_ + 512].rearrange("c p t -> p c t"), w=[gaT])
            K.dma("c1d", gbT.ap, gT_s[8:16, :, ta_:ta_ + 512].rearrange("c p t -> p c t"), w=[gbT])

        c1_loads(0)
        for tg in range(8):
            t0 = tg * 512
            for c in range(8):
                pa = nps()
                pb = nps()
                for kc in range(4):
                    K.mm(pa, pa.ap, wps.ap[:, kc, c * 128:(c + 1) * 128], ssmT.ap[:, kc, :], kc == 0, kc == 3, [wps, ssmT])
                for kc in range(4):
                    K.mm(pb, pb.ap, wpa.ap[:, kc, c * 128:(c + 1) * 128], attT.ap[:, kc, :], kc == 0, kc == 3, [wpa, attT])
                t1, t2 = t1cs[c % 2], t2cs[c % 2]
                K.tt("dve", t1.ap, pa.ap, gaT.ap[:, c, :], ALU.mult, [pa, gaT], [t1])
                K.tt("dve", t2.ap, pb.ap, gbT.ap[:, c, :], ALU.mult, [pb, gbT], [t2])
                K.tt("dve", mT.ap[:, c, :], t1.ap, t2.ap, ALU.add, [t1, t2], [mT])
            if tg + 1 < 8:
                c1_loads(tg + 1)
            for j in range(4):
                xt = xts[xi % 2]
                x1 = x1s[xi % 2]
                hb = hbs[xi % 2]
                xi += 1
                r0 = t0 + j * 128
                K.dma("x%d" % (xi % 2), xt.ap, xc[NT_OWN + r0: NT_OWN + r0 + 128, :], w=[xt])
                for half in range(2):
                    p = nps()
                    for kc in range(8):
                        K.mm(p, p.ap, mT.ap[:, kc, j * 128:(j + 1) * 128], wout.ap[:, kc, half * 512:(half + 1) * 512],
                             kc == 0, kc == 7, [mT, wout])
                    K.tt("dve", x1.ap[:, half * 512:(half + 1) * 512], p.ap, xt.ap[:, half * 512:(half + 1) * 512], ALU.add,
                         [p, xt], [x1])
                K.dma("x1o%d" % (xi % 2), x1_s[r0:r0 + 128, :], x1.ap, r=[x1])
                rmsnorm_tile(x1, gf, hb, sq, ss, rs)

                def c1_tr(j=j, hb=hb):
                    p = nps()
                    pv = p.ap.bitcast(BF16).rearrange("p (a b) -> p a b", a=8)
                    for kc in range(8):
                        K.tr(p, pv[:, kc, :], hb.ap[:, kc * 128:(kc + 1) * 128], idb.ap, [hb, idb])
                    K.cp("act", h2T.ap[:, :, j * 128:(j + 1) * 128], pv, [p], [h2T])

                if j > 0:
                    pend_tr()
                pend_tr = c1_tr
            pend_tr()
            K.dma("h2o", h2T_s[:, :, t0:t0 + 512].rearrange("c p t -> p c t"), h2T.ap, r=[h2T])
            for ch in range(16):
                p = nps()
                for kc in range(8):
                    K.mm(p, p.ap, wq.ap[:, kc, ch * 128:(ch + 1) * 128], h2T.ap[:, kc, :], kc == 0, kc == 7, [wq, h2T])
                K.cp("act" if ch % 2 else "dve", qT.ap[:, ch, :], p.ap, [p], [qT])
            for j in range(4):
                sc = scs[j % 2]
                for c0 in range(0, 16, 4):
                    p = nps()
                    for i in range(4):
                        K.mm(p, p.ap[:, i * 128:(i + 1) * 128], qT.ap[:, c0 + i, j * 128:(j + 1) * 128], keysT.ap[:, c0 + i, :],
                             True, True, [qT, keysT])
                    K.cp("act" if (c0 // 4) % 2 else "dve", sc.ap[:, c0:c0 + 4, :], p.ap.rearrange("p (a b) -> p a b", a=4), [p], [sc])
                r0 = t0 + j * 128
                K.dma("sco%d" % (j % 2), sc_s[r0:r0 + 128, :], sc.ap.rearrange("p a b -> p (a b)"), r=[sc])
        K.barrier()

    U32 = mybir.dt.uint32

    def phase_C2():
        K.phase_reset()
        iot = K.sb([128, 128], F32, "iota")
        K.dma("c0", iot.ap, iota_d, w=[iot])
        ss_ = [K.sb([128, 16, 128], F32, "s%d" % i) for i in range(2)]
        v = K.sb([128, 16, 16], F32, "v")
        idx = K.sb([128, 8, 16], U32, "idx")
        idxf = K.sb([128, 128], F32, "idxf")
        idxT = K.sb([128, 128], F32, "idxT")
        top = K.sb([128, 8, 16], F32, "top")
        e16 = K.sb([128, 8, 16], F32, "e16")
        Z = K.sb([128, 8], F32, "Z")
        bE = K.sb([128, 8], F32, "bE")
        v1m = K.sb([128, 8, 16], F32, "v1m")
        sums = [K.sb([128, 16, 128], F32, "sum%d" % i) for i in range(3)]
        Es = [K.sb([128, 16, 128], BF16, "E%d" % i) for i in range(3)]
        Btm = K.sb([128, 8, 16, 128], BF16, "Btm")
        BT = K.sb([128, 128, 128], BF16, "BT")
        AT = K.sb([128, 128, 128], BF16, "AT")
        Gst = K.sb([128, 128, 128], BF16, "Gst")
        works = [K.sb([128, 128], F32, "wk%d" % i) for i in range(16)]
        work2s = [K.sb([128, 256], F32, "wk2%d" % i) for i in range(8)]
        K.dma("c2s0", ss_[0].ap.rearrange("p a b -> p (a b)"), sc_s[0:128, :], w=[ss_[0]])
        for tl in range(32):
            r0 = tl * 128
            s_ = ss_[tl % 2]
            if tl + 1 < 32:
                sn_ = ss_[(tl + 1) % 2]
                K.dma("c2s%d" % ((tl + 1) % 2), sn_.ap.rearrange("p a b -> p (a b)"), sc_s[r0 + 128:r0 + 256, :], w=[sn_])
            for ch in range(16):
                K.op("dve", lambda e, ch=ch, s_=s_: e.max(out=v.ap[:, ch, 0:8], in_=s_.ap[:, ch, :]), [s_], [v])
            for ch in range(0, 16, 2):
                K.op("dve", lambda e, ch=ch, s_=s_: e.max_index(out=idx.ap[:, ch // 2, 0:8], in_max=v.ap[:, ch, 0:8],
                                                                in_values=s_.ap[:, ch, :]), [v, s_], [idx])
            for ch in range(16):
                K.op("dve", lambda e, ch=ch, s_=s_: e.match_replace(out=works[ch].ap, in_to_replace=v.ap[:, ch, 0:8],
                                                                    in_values=s_.ap[:, ch, :], imm_value=-1e30), [v, s_], [works[ch]])
            for ch in range(16):
                K.op("dve", lambda e, ch=ch: e.max(out=v.ap[:, ch, 8:16], in_=works[ch].ap), [works[ch]], [v])
            for ch in range(0, 16, 2):
                K.op("dve", lambda e, ch=ch: e.max_index(out=idx.ap[:, ch // 2, 8:16], in_max=v.ap[:, ch, 8:16],
                                                         in_values=works[ch].ap), [v, works[ch]], [idx])
            vv = v.ap.rearrange("p (h s) k -> p h s k", s=2)
            v1 = vv[:, :, 0, :]
            v2 = vv[:, :, 1, :]
            cand_ap = sums[0].ap.rearrange("p a b -> p (a b)").rearrange("p (h c) -> p h c", h=8)
            K.tt("dve", cand_ap.rearrange("p h (i j) -> p h i j", i=16), v1.unsqueeze(3).broadcast_to([128, 8, 16, 16]),
                 v2.unsqueeze(2).broadcast_to([128, 8, 16, 16]), ALU.add, [v], [sums[0]])
            for h in range(8):
                K.op("dve", lambda e, h=h: e.max(out=top.ap[:, h, 0:8], in_=cand_ap[:, h, :]), [sums[0]], [top])
            for h in range(8):
                K.op("dve", lambda e, h=h: e.match_replace(out=work2s[h].ap, in_to_replace=top.ap[:, h, 0:8],
                                                           in_values=cand_ap[:, h, :], imm_value=-1e30), [top, sums[0]], [work2s[h]])
            for h in range(8):
                K.op("dve", lambda e, h=h: e.max(out=top.ap[:, h, 8:16], in_=work2s[h].ap), [work2s[h]], [top])
            mxb = top.ap[:, :, 0:1].broadcast_to([128, 8, 16])
            taub = top.ap[:, :, 15:16].broadcast_to([128, 8, 16])
            K.tt("dve", e16.ap, top.ap, mxb, ALU.subtract, [top], [e16])
            K.act(e16.ap, e16.ap, AF.Exp, [e16], [e16])
            K.op("dve", lambda e: e.tensor_reduce(out=Z.ap, in_=e16.ap, axis=AX.X, op=ALU.add), [e16], [Z])
            K.act(Z.ap, Z.ap, AF.Ln, [Z], [Z])
            K.tt("dve", bE.ap, top.ap[:, :, 15], top.ap[:, :, 0], ALU.subtract, [top], [bE])
            K.tt("dve", bE.ap, bE.ap, Z.ap, ALU.subtract, [bE, Z], [bE])
            K.tt("dve", v1m.ap, v1, taub, ALU.subtract, [v, top], [v1m])
            K.cp("dve", idxf.ap, idx.ap.rearrange("p h k -> p (h k)"), [idx], [idxf])
            p = nps()
            K.tr(p, p.ap[:, 0:128], idxf.ap, idf.ap, [idxf, idf])
            K.cp("dve", idxT.ap, p.ap[:, 0:128], [p], [idxT])
            K.tt("dve", AT.ap, iot.ap.unsqueeze(1).broadcast_to([128, 128, 128]),
                 idxT.ap.unsqueeze(2).broadcast_to([128, 128, 128]), ALU.is_equal, [iot, idxT], [AT])
            def bsum(h):
                sm_ = sums[h % 3]
                K.tt("pool" if h % 4 == 0 else "dve", sm_.ap, v1m.ap[:, h, :].unsqueeze(2).broadcast_to([128, 16, 128]),
                     s_.ap[:, 2 * h + 1, :].unsqueeze(1).broadcast_to([128, 16, 128]), ALU.add, [v1m, s_], [sm_])
                K.act(Es[h % 3].ap, sm_.ap, AF.Exp, [sm_, bE], [Es[h % 3]], bias=bE.ap[:, h:h + 1])

            bsum(0)
            bsum(1)
            for h in range(8):
                if h + 2 < 8:
                    bsum(h + 2)
                K.stt("dve", Btm.ap[:, h], sums[h % 3].ap, -1e-5, Es[h % 3].ap, ALU.is_ge, ALU.mult, [sums[h % 3], Es[h % 3]], [Btm])
            for b0 in range(0, 128, 8):
                p = nps()
                pv = p.ap.bitcast(BF16).rearrange("p (a b) -> p a b", a=8)
                for k in range(8):
                    K.tr(p, pv[:, k, :], Btm.ap[:, :, :, b0 + k].rearrange("p h i -> p (h i)"), idb.ap, [Btm, idb])
                K.cp("act", BT.ap[:, :, b0:b0 + 8], pv.rearrange("p b t -> p t b"), [p], [BT])
            for t4 in range(0, 128, 4):
                p = nps()
                for k in range(4):
                    t = t4 + k
                    K.mm(p, p.ap[:, k * 128:(k + 1) * 128], BT.ap[:, t, :], AT.ap[:, t, :], True, True, [BT, AT])
                K.cp("act", Gst.ap[:, :, t4:t4 + 4],
                     p.ap.rearrange("p (t a) -> p a t", t=4), [p], [Gst])
            K.dma("c2g", G_s[tl], Gst.ap, r=[Gst])
        K.barrier()

    def phase_D():
        K.phase_reset()
        TG = 1024
        NG_ = NT_OWN // TG
        h2T = K.sb([128, 8, TG], BF16, "h2TD")
        Ubs = [K.sb([128, 1024], F32, "Ub%d" % i) for i in range(3)]
        Ubb = [K.sb([128, 1024], BF16, "Ubb%d" % i) for i in range(2)]
        UbTs = [K.sb([128, 8, 128], BF16, "UbT%d" % i) for i in range(3)]
        Ghs = [K.sb([128, 8, 8, 128], BF16, "Gh%d" % i) for i in range(2)]
        ges = [K.sb([128, 512], BF16, "ge%d" % i) for i in range(2)]
        W = K.sb([128, 16, TG], BF16, "W")
        Vbs = [K.sb([128, 1024], F32, "Vb%d" % i) for i in range(3)]
        Vbf = K.sb([128, 16, 1024], BF16, "Vbf")
        acc = K.sb([128, 8, 1024], F32, "acc")
        x1t = [K.sb([128, 1024], F32, "x1D%d" % i) for i in range(2)]
        uttok = [Buf(None, "ut%d" % i) for i in range(128)]
        NI = NG_ * 128

        def load(n):
            grp, a = n // 128, n % 128
            t0 = grp * TG
            if grp == 0:
                K.dma("du%d" % (n % 3), Ubs[n % 3].ap, peer_u[a * 128:(a + 1) * 128, :], w=[Ubs[n % 3]])
            else:
                K.dma("du%d" % (n % 3), UbTs[n % 3].ap.rearrange("p a b -> p (a b)"), UT_s[a], r=[uttok[a]], w=[UbTs[n % 3]])
            if a % 8 == 0:
                Gh = Ghs[(n // 8) % 2]
                tl0 = t0 // 128
                K.dma("dg%d" % ((n // 8) % 2), Gh.ap, G_s[tl0:tl0 + 8, :, a:a + 8, :].rearrange("tl b a t -> b tl a t"), w=[Gh])
            K.dma("dv%d" % (n % 3), Vbs[n % 3].ap, peer_v[a * 128:(a + 1) * 128, :], w=[Vbs[n % 3]])

        def trans(n):
            if n // 128 != 0:
                return
            Ub, UbT, ub = Ubs[n % 3], UbTs[n % 3], Ubb[n % 2]
            K.cp("dve", ub.ap, Ub.ap, [Ub], [ub])
            p = nps()
            pv = p.ap.bitcast(BF16).rearrange("p (a b) -> p a b", a=8)
            for kc in range(8):
                K.tr(p, pv[:, kc, :], ub.ap[:, kc * 128:(kc + 1) * 128], idb.ap, [ub, idb])
            K.cp("act", UbT.ap, pv, [p], [UbT])

        load(0)
        load(1)
        trans(0)
        for n in range(NI):
            grp, a = n // 128, n % 128
            t0 = grp * TG
            si = a % 16
            if a == 0:
                K.dma("dh", h2T.ap, h2T_s[:, :, t0:t0 + TG].rearrange("c p t -> p c t"), w=[h2T])
                K.memset("pool", acc.ap, 0.0, [acc])
            if n + 2 < NI:
                load(n + 2)
            if n + 1 < NI:
                trans(n + 1)
            UbT, Vb = UbTs[n % 3], Vbs[n % 3]
            Gh = Ghs[(n // 8) % 2]
            if grp == 0:
                K.dma("dut%d" % (n % 3), UT_s[a], UbT.ap.rearrange("p a b -> p (a b)"), r=[UbT], w=[uttok[a]])
            K.cp("act", Vbf.ap[:, si, :], Vb.ap, [Vb], [Vbf])
            for hf in range(TG // 512):
                p = nps()
                for kc in range(8):
                    K.mm(p, p.ap, UbT.ap[:, kc, :], h2T.ap[:, kc, hf * 512:(hf + 1) * 512], kc == 0, kc == 7, [UbT, h2T])
                ge = ges[hf % 2]
                K.act(ge.ap, p.ap, AF.Gelu_apprx_tanh, [p], [ge])
                K.tt("dve", W.ap[:, si, hf * 512:(hf + 1) * 512].rearrange("p (a b) -> p a b", a=4),
                     ge.ap.rearrange("p (a b) -> p a b", a=4), Gh.ap[:, hf * 4:(hf + 1) * 4, a % 8, :], ALU.mult,
                     [ge, Gh], [W])
            if si == 15:
                for j in range(TG // 128):
                    for hf in range(2):
                        p = nps()
                        for s2 in range(16):
                            K.mm(p, p.ap, W.ap[:, s2, j * 128:(j + 1) * 128], Vbf.ap[:, s2, hf * 512:(hf + 1) * 512],
                                 s2 == 0, s2 == 15, [W, Vbf])
                        K.tt("dve", acc.ap[:, j, hf * 512:(hf + 1) * 512], p.ap, acc.ap[:, j, hf * 512:(hf + 1) * 512], ALU.add,
                             [p, acc], [acc])
            if a == 127:
                for j in range(TG // 128):
                    xt = x1t[j % 2]
                    r0 = t0 + j * 128
                    K.dma("dx%d" % (j % 2), xt.ap, x1_s[r0:r0 + 128, :], w=[xt])
                    K.tt("dve", xt.ap, xt.ap, acc.ap[:, j, :], ALU.add, [xt, acc], [xt])
                    K.dma("dx%d" % (j % 2), x2_s[r0:r0 + 128, :], xt.ap, r=[xt])
        K.barrier()

    def phase_E():
        K.phase_reset()
        wpg = K.sb([128, 8, 1024], BF16, "wpg")
        wpp = K.sb([128, 2, 1024], BF16, "wpp")
        stage = K.sb([128, 1024], F32, "stageE")
        gp = K.sb([128, 1024], F32, "gple")
        gfin = K.sb([128, 1024], F32, "gfin")
        xts = [K.sb([128, 1024], F32, "xtE%d" % i) for i in range(4)]
        pts = [K.sb([128, 256], F32, "ptE%d" % i) for i in range(2)]
        pb_s = [K.sb([128, 256], BF16, "pbE%d" % i) for i in range(2)]
        pTs = [K.sb([128, 2, 128], BF16, "pTE%d" % i) for i in range(2)]
        hb_s = [K.sb([128, 1024], BF16, "hbE%d" % i) for i in range(2)]
        hTs_ = [K.sb([128, 8, 128], BF16, "hTE%d" % i) for i in range(2)]
        sq_s = [K.sb([128, 1024], BF16, "sqE%d" % i) for i in range(2)]
        ss_s = [K.sb([128, 1], F32, "ssE%d" % i) for i in range(4)]
        rs_s = [K.sb([128, 1], F32, "rsE%d" % i) for i in range(4)]
        sgts = [K.sb([128, 1024], F32, "sgE%d" % i) for i in range(2)]
        outs = [K.sb([128, 1024], F32, "oE%d" % i) for i in range(2)]
        K.dma("c0", gp.ap, g_ple.partition_broadcast(128), w=[gp])
        K.dma("c0", gfin.ap, g_fin.partition_broadcast(128), w=[gfin])
        load_w_bf(wpg, w_pg, 8, 1024, stage, "wst")
        load_w_bf(wpp, w_pp, 2, 1024, stage, "wst")
        def e_vars(tl):
            return dict(r0=tl * 128)

        def front(tl):
            r0 = tl * 128
            xt, pt, ot = xts[tl % 4], pts[tl % 2], outs[tl % 2]
            pb_, pT, hb, hT, sq, sgt = pb_s[tl % 2], pTs[tl % 2], hb_s[tl % 2], hTs_[tl % 2], sq_s[tl % 2], sgts[tl % 2]
            ss, rs = ss_s[tl % 2], rs_s[tl % 2]
            ss2, rs2 = ss_s[2 + tl % 2], rs_s[2 + tl % 2]
            K.dma("ex%d" % (tl % 4), xt.ap, x2_s[r0:r0 + 128, :], w=[xt])
            K.dma("ep%d" % (tl % 2), pt.ap, pc[r0:r0 + 128, :], w=[pt])
            rmsnorm_tile(xt, gp, hb, sq, ss, rs)
            K.cp("dve", pb_.ap, pt.ap, [pt], [pb_])

        def front1b(tl):
            pb_, pT, hb, hT = pb_s[tl % 2], pTs[tl % 2], hb_s[tl % 2], hTs_[tl % 2]
            p = nps()
            pv = p.ap.bitcast(BF16).rearrange("p (a b) -> p a b", a=8)
            for kc in range(8):
                K.tr(p, pv[:, kc, :], hb.ap[:, kc * 128:(kc + 1) * 128], idb.ap, [hb, idb])
            K.cp("act", hT.ap, pv, [p], [hT])
            p = nps()
            pv = p.ap.bitcast(BF16).rearrange("p (a b) -> p a b", a=8)
            for kc in range(2):
                K.tr(p, pv[:, kc, :], pb_.ap[:, kc * 128:(kc + 1) * 128], idb.ap, [pb_, idb])
            K.cp("act", pT.ap, pv[:, 0:2, :], [p], [pT])

        def front2(tl):
            pT, hT, sgt = pTs[tl % 2], hTs_[tl % 2], sgts[tl % 2]
            for hf in range(2):
                pg = nps()
                pe_ = nps()
                for kc in range(8):
                    K.mm(pg, pg.ap, hT.ap[:, kc, :], wpg.ap[:, kc, hf * 512:(hf + 1) * 512], kc == 0, kc == 7, [hT, wpg])
                for kc in range(2):
                    K.mm(pe_, pe_.ap, pT.ap[:, kc, :], wpp.ap[:, kc, hf * 512:(hf + 1) * 512], kc == 0, kc == 1, [pT, wpp])
                K.act(sgt.ap[:, hf * 512:(hf + 1) * 512], pg.ap, AF.Sigmoid, [pg], [sgt])
                K.tt("dve", sgt.ap[:, hf * 512:(hf + 1) * 512], pe_.ap, sgt.ap[:, hf * 512:(hf + 1) * 512], ALU.mult, [pe_, sgt], [sgt])

        def back(tl):
            r0 = tl * 128
            xt, ot = xts[tl % 4], outs[tl % 2]
            sq, sgt = sq_s[tl % 2], sgts[tl % 2]
            ss2, rs2 = ss_s[2 + tl % 2], rs_s[2 + tl % 2]
            K.tt("pool", xt.ap, xt.ap, sgt.ap, ALU.add, [xt, sgt], [xt])
            K.act(sq.ap, xt.ap, AF.Square, [xt], [sq, ss2], accum=ss2.ap)
            K.act(rs2.ap, ss2.ap, AF.Sqrt, [ss2], [rs2], scale=1.0 / 1024, bias=eps_b.ap)
            K.op("dve", lambda e, rs2=rs2: e.reciprocal(out=rs2.ap, in_=rs2.ap), [rs2], [rs2])
            K.stt("dve", ot.ap, xt.ap, rs2.ap[:, 0:1], gfin.ap, ALU.mult, ALU.mult, [xt, rs2, gfin], [ot])
            K.dma("eo%d" % (tl % 2), out[r0:r0 + 128, :], ot.ap, r=[ot])

        for r_ in range(-3, 32):
            for stage_fn, off_ in ((back, 0), (front2, 1), (front1b, 2), (front, 3)):
                tl_ = r_ + off_
                if 0 <= tl_ < 32:
                    stage_fn(tl_)
        K.barrier()

    phases = {"A": phase_A, "B": phase_B, "S": phase_S, "C1": phase_C1, "C2": phase_C2, "D": phase_D, "E": phase_E}
    return nc, kb, st, locals()


def _consts():
    ident = np.eye(128, dtype=np.float32)
    invf = np.zeros((128, 2), np.float32)
    for p in range(128):
        hd = p % 64
        if hd < 16:
            invf[p, 0] = np.float32(500000.0) ** np.float32(-(2 * (hd % 8)) / 16.0)
            invf[p, 1] = -1.0 if hd < 8 else 1.0
    eoh = np.zeros((32, NT_LOC), np.float32)
    for n in range(32):
        eoh[n, n * 256:(n + 1) * 256] = 1.0
    cm = np.zeros((4, 128, 512), np.float32)
    for kt in range(4):
        for kp in range(128):
            kpos = kt * 128 + kp
            q = np.arange(512)
            same = (q // 256) == (kpos // 256)
            cm[kt, kp, :] = np.where(same & (kpos > q), -BIG, 0.0)
    return ident, invf, eoh, cm


def make_in_maps(inp, cores=range(8)):
    f = lambda a: np.ascontiguousarray(np.asarray(a))
    x = f(inp["x"])
    p = f(inp["p"])[0]
    pos = f(inp["positions"]).astype(np.int32)
    ident, invf, eoh, cm = _consts()
    w_in = f(inp["w_in"])[0]
    perm = np.arange(1024)
    for c in range(1024):
        hd = c % 64
        if hd < 8:
            perm[c] = c + 8
        elif hd < 16:
            perm[c] = c - 8
    w_perm = f(w_in[:, 512:1536][:, perm])

    def pair(a):
        a = f(a)[0]
        sh = a.shape
        a = a.reshape(16, 2, 64, *sh[2:])
        a = np.moveaxis(a, 0, 2)
        return f(a.reshape(128, 16, -1).reshape(128, -1))

    ldt = f(inp["ssm_log_dt"])[0]
    ldt_l = f(np.broadcast_to(ldt.reshape(16, 2, 1), (16, 2, 64)).transpose(1, 2, 0).reshape(128, 16))
    cre = f(inp["ssm_c_re"])[0].transpose(0, 2, 1)
    cim = f(inp["ssm_c_im"])[0].transpose(0, 2, 1)
    shared = {
        "ident": ident, "iota": np.ascontiguousarray(np.broadcast_to(np.arange(128, dtype=np.float32), (128, 128))), "invf": invf, "koh": eoh, "cmask": cm,
        "g_mix": f(inp["g_mix"]), "w_in": w_in, "w_perm": w_perm,
        "s5_ldt": ldt_l, "s5_are": pair(inp["ssm_a_re"]), "s5_aim": pair(inp["ssm_a_im"]),
        "s5_bre": pair(inp["ssm_b_re"]), "s5_bim": pair(inp["ssm_b_im"]),
        "s5_cre": pair(cre[None]), "s5_cim": pair(cim[None]),
        "ssm_d": f(inp["ssm_d"]), "w_glu": f(inp["ssm_w_glu"])[0],
        "w_ps": f(inp["w_proj_ssm"])[0], "w_pa": f(inp["w_proj_att"])[0], "w_out": f(inp["w_out"])[0],
        "g_ffn": f(inp["g_ffn"]), "w_q": f(inp["peer_w_q"])[0],
        "keys": f(np.stack([f(inp["peer_keys1"])[0], f(inp["peer_keys2"])[0]], axis=1).reshape(16, 128, 128)),
        "peer_u": f(inp["peer_u"])[0], "peer_v": f(inp["peer_v"])[0],
        "g_ple": f(inp["g_ple"]), "w_pg": f(inp["ple_w_gate"])[0], "w_pp": f(inp["ple_w_proj"])[0],
        "g_fin": f(inp["g_final"]).reshape(1, 1024),
    }
    maps = []
    for c in cores:
        b, half = c // 2, c % 2
        xc = np.zeros((NT_LOC, 1024), np.float32)
        posc = np.zeros((1, NT_LOC), np.int32)
        if half == 1:
            xc[:] = x[b]
            posc[0] = pos[b]
        else:
            xc[NT_OWN:] = x[b, :NT_OWN]
            posc[0, NT_OWN:] = pos[b, :NT_OWN]
        valid = np.zeros((16, 32), np.float32)
        own = np.zeros((16, 32), np.float32)
        for qb in range(16, 32):
            for n in range(32):
                if n < qb and (half == 1 or n >= 16):
                    valid[qb - 16, n] = 1.0
            own[qb - 16, qb] = 1.0
        m = dict(shared)
        m.update({"xc": xc, "pc": f(p[b, half * NT_OWN:(half + 1) * NT_OWN]), "posc": posc,
                  "validc": valid.reshape(1, 512), "ownc": own.reshape(1, 512)})
        maps.append(m)
    return maps


_CACHE = {}


def kernel(**inputs):
    if "prog" not in _CACHE:
        nc, kb, st, L = build_program()
        for ph in ("A", "S", "B", "C1", "C2", "D", "E"):
            L["phases"][ph]()
        kb.S.emit()
        st.close()
        _CACHE["prog"] = nc
    nc = _CACHE["prog"]
    maps = make_in_maps(inputs, range(8))
    res = run_bass_kernel_spmd(nc, maps, core_ids=list(range(8)))
    out = np.zeros((4, 8192, 1024), np.float32)
    for c in range(8):
        b, half = c // 2, c % 2
        out[b, half * NT_OWN:(half + 1) * NT_OWN] = np.asarray(res.results[c]["out"])
    return out
```

```python
import numpy as np
import concourse.bass as bass
import concourse.mybir as mybir
from concourse.bass_utils import run_bass_kernel_spmd

F32 = mybir.dt.float32
BF16 = mybir.dt.bfloat16
I32 = mybir.dt.int32
ALU = mybir.AluOpType
AF = mybir.ActivationFunctionType
AX = mybir.AxisListType


class Tok:
    __slots__ = ("w", "r", "name")

    def __init__(self, name=""):
        self.w = None
        self.r = {}
        self.name = name


class Sched:
    ENGS = ("pe", "act", "dve", "pool", "sp")

    def __init__(self, nc):
        self.nc = nc
        self.ops = {e: [] for e in self.ENGS}
        self.dma_cnt = {}
        self.dma_keys = []

    @staticmethod
    def _evkey(ev):
        return (ev[0], ev[1])

    def _collect(self, reads, writes):
        deps = {}

        def add(ev):
            if ev is None:
                return
            k = self._evkey(ev)
            if k not in deps or deps[k][2] < ev[2]:
                deps[k] = ev

        for t in reads:
            add(t.w)
        for t in writes:
            add(t.w)
            for ev in t.r.values():
                add(ev)
        return deps

    def _commit(self, ev, reads, writes):
        for t in reads:
            k = self._evkey(ev)
            t.r[k] = ev
        for t in writes:
            t.w = ev
            t.r = {}

    def op(self, eng, fn, reads=(), writes=()):
        deps = self._collect(reads, writes)
        idx = len(self.ops[eng])
        ev = ("e", eng, idx)
        if eng == "pe":
            deps.pop(("e", "pe"), None)
        self.ops[eng].append(dict(fn=fn, deps=list(deps.values()), dma=None, signal=False))
        self._commit(ev, reads, writes)
        return ev

    def dma(self, q, key, out, in_, reads=(), writes=()):
        deps = self._collect(reads, writes)
        if key not in self.dma_cnt:
            self.dma_cnt[key] = 0
            self.dma_keys.append(key)
        n = self.dma_cnt[key]
        if n > 0:
            k = ("d", key)
            deps[k] = ("d", key, n)
        self.dma_cnt[key] = n + 1
        ev = ("d", key, n + 1)
        self.ops[q].append(dict(fn=lambda e, o=out, i=in_: e.dma_start(out=o, in_=i),
                                deps=list(deps.values()), dma=key, signal=False))
        self._commit(ev, reads, writes)
        return ev

    def emit(self, final_keys=()):
        nc = self.nc
        ops = self.ops
        for e in self.ENGS:
            for o in ops[e]:
                for d in o["deps"]:
                    if d[0] == "e":
                        ops[d[1]][d[2]]["signal"] = True
        for e in self.ENGS:
            last = None
            for o in ops[e]:
                if "barrier" in o:
                    if last is not None and e != "sp":
                        last["signal"] = True
                else:
                    last = o
        sigval = {}
        for e in self.ENGS:
            c = 0
            vals = []
            for o in ops[e]:
                if o["signal"]:
                    c += 1
                vals.append(c)
            sigval[e] = vals
        barvals = {}
        for e in self.ENGS:
            for i, o in enumerate(ops[e]):
                if "barrier" in o:
                    barvals[(e, o["barrier"])] = sigval[e][i]
        from contextlib import ExitStack
        with ExitStack() as st:
            esem = {e: st.enter_context(nc.semaphore("s_" + e)) for e in self.ENGS if e != "sp"}
            dsem = {k: st.enter_context(nc.semaphore("d_%d" % i)) for i, k in enumerate(self.dma_keys)}
            bsem = st.enter_context(nc.semaphore("s_bar"))
            block = st.enter_context(nc.Block())

            def run(ename, eng):
                waited = {}
                for o in ops[ename]:
                    if "barrier" in o:
                        k = o["barrier"]
                        if ename == "sp":
                            for key, cnt in o["dcnt"].items():
                                if cnt > 0 and waited.get(("d", key), 0) < 16 * cnt:
                                    eng.wait_ge(dsem[key], 16 * cnt)
                            for e2 in esem:
                                v = barvals[(e2, k)]
                                if v > 0:
                                    eng.wait_ge(esem[e2], v)
                            eng.sem_inc(bsem, 1)
                        else:
                            eng.wait_ge(bsem, k)
                        for key, cnt in o["dcnt"].items():
                            waited[("d", key)] = max(waited.get(("d", key), 0), 16 * cnt)
                        for e2 in esem:
                            waited[("e", e2)] = max(waited.get(("e", e2), 0), barvals[(e2, k)])
                        continue
                    for d in sorted(o["deps"]):
                        if d[0] == "e":
                            sem = esem[d[1]]
                            val = sigval[d[1]][d[2]]
                        else:
                            sem = dsem[d[1]]
                            val = 16 * d[2]
                        wk = (d[0], d[1])
                        if waited.get(wk, 0) >= val:
                            continue
                        waited[wk] = val
                        eng.wait_ge(sem, val)
                    ins = o["fn"](eng)
                    if o["dma"] is not None:
                        ins.then_inc(dsem[o["dma"]], 16)
                    elif o["signal"]:
                        ins.then_inc(esem[ename], 1)
                if ename == "sp":
                    for k in self.dma_keys:
                        eng.wait_ge(dsem[k], 16 * self.dma_cnt[k])

            @block.sync
            def _(e):
                run("sp", e)

            @block.tensor
            def _(e):
                run("pe", e)

            @block.scalar
            def _(e):
                run("act", e)

            @block.vector
            def _(e):
                run("dve", e)

            @block.gpsimd
            def _(e):
                run("pool", e)


NT_OWN = 4096
NT_LOC = 8192
PI = float(np.pi)
BIG = 30000.0


class Buf:
    __slots__ = ("ap", "t")

    def __init__(self, ap, name=""):
        self.ap = ap
        self.t = Tok(name)

    def __getitem__(self, k):
        return self.ap[k]


class KB:
    def __init__(self, nc):
        self.nc = nc
        self.S = Sched(nc)
        self.big = nc.alloc_sbuf_tensor("bigsb", [128, 53000], F32)
        self.off = 0
        self.persist = 0
        self.nbar = 0

    def sb(self, shape, dt, name=""):
        n = int(np.prod(shape[1:]))
        esz = 4 if dt in (F32, I32, mybir.dt.uint32) else 2
        nw = (n * esz + 63) // 64 * 16
        assert self.off + nw <= 53000, ("sbuf overflow", name, self.off, nw)
        ap = self.big[:, self.off:self.off + nw]
        self.off += nw
        if dt != F32:
            ap = ap.bitcast(dt)
        ap = ap[:, 0:n]
        if len(shape) == 3:
            ap = ap.rearrange("p (a b) -> p a b", a=shape[1])
        elif len(shape) == 4:
            ap = ap.rearrange("p (a b c) -> p a b c", a=shape[1], b=shape[2])
        elif len(shape) == 5:
            ap = ap.rearrange("p (a b c d) -> p a b c d", a=shape[1], b=shape[2], c=shape[3])
        if shape[0] != 128:
            ap = ap[0:shape[0]]
        return Buf(ap, name)

    def phase_reset(self):
        self.off = self.persist

    def op(self, eng, fn, r=(), w=()):
        return self.S.op(eng, fn, [b.t for b in r], [b.t for b in w])

    def dma(self, key, out, in_, r=(), w=(), q="sp"):
        return self.S.dma(q, key, out, in_, [b.t for b in r], [b.t for b in w])

    def mm(self, pbuf, out, lhsT, rhs, start, stop, r):
        self.op("pe", lambda e: e.matmul(out, lhsT=lhsT, rhs=rhs, start=start, stop=stop), r, [pbuf])

    def tr(self, pbuf, out, in_, ident, r):
        self.op("pe", lambda e: e.transpose(out=out, in_=in_, identity=ident), r, [pbuf])

    def tt(self, eng, out, a, b, op, r, w):
        self.op(eng, lambda e: e.tensor_tensor(out=out, in0=a, in1=b, op=op), r, w)

    def ts(self, eng, out, a, s1, s2, op0, op1, r, w):
        if s2 is None:
            self.op(eng, lambda e: e.tensor_scalar(out=out, in0=a, scalar1=s1, scalar2=None, op0=op0), r, w)
        else:
            self.op(eng, lambda e: e.tensor_scalar(out=out, in0=a, scalar1=s1, scalar2=s2, op0=op0, op1=op1), r, w)

    def stt(self, eng, out, a, s, b, op0, op1, r, w):
        self.op(eng, lambda e: e.scalar_tensor_tensor(out=out, in0=a, scalar=s, in1=b, op0=op0, op1=op1), r, w)

    def cp(self, eng, out, a, r, w):
        if eng == "act":
            self.op("act", lambda e: e.activation(out=out, in_=a, func=AF.Copy), r, w)
        else:
            self.op(eng, lambda e: e.tensor_copy(out=out, in_=a), r, w)

    def act(self, out, a, func, r, w, bias=None, scale=None, accum=None):
        kw = {}
        if bias is not None:
            kw["bias"] = bias
        if scale is not None:
            kw["scale"] = scale
        if accum is not None:
            kw["accum_out"] = accum
        self.op("act", lambda e: e.activation(out=out, in_=a, func=func, **kw), r, w)

    def memset(self, eng, out, val, w):
        self.op(eng, lambda e: e.memset(out, val), (), w)

    def barrier(self):
        S = self.S
        self.nbar += 1
        k = self.nbar
        for e in S.ENGS:
            S.ops[e].append(dict(barrier=k, fn=None, deps=[], dma=None, signal=False,
                                 dcnt=dict(S.dma_cnt)))


def build_program(stop_after=None, debug=()):
    nc = bass.Bass("TRN2", target_bir_lowering=False)
    kb = KB(nc)
    K = kb

    def din(name, shape, dt=F32):
        return nc.dram_tensor(name, list(shape), dt, kind="ExternalInput").ap()

    def dscr(name, shape, dt):
        kind = "ExternalOutput" if name in debug else "Internal"
        return nc.dram_tensor(name, list(shape), dt, kind=kind).ap()

    xc = din("xc", [NT_LOC, 1024])
    pc = din("pc", [NT_OWN, 256])
    posc = din("posc", [1, NT_LOC], I32)
    validc = din("validc", [1, 512])
    ownc = din("ownc", [1, 512])
    ident_d = din("ident", [128, 128])
    iota_d = din("iota", [128, 128])
    invf_d = din("invf", [128, 2])
    koh_d = din("koh", [32, NT_LOC])
    cm_d = din("cmask", [4, 128, 512])
    g_mix = din("g_mix", [1, 1024])
    w_in = din("w_in", [1024, 4096])
    w_perm = din("w_perm", [1024, 1024])
    s5 = {n: din("s5_" + n, shp) for n, shp in [
        ("ldt", [128, 16]), ("are", [128, 16]), ("aim", [128, 16]),
        ("bre", [128, 256]), ("bim", [128, 256]), ("cre", [128, 256]), ("cim", [128, 256])]}
    ssm_d = din("ssm_d", [1, 512])
    w_glu = din("w_glu", [512, 512])
    w_ps = din("w_ps", [512, 1024])
    w_pa = din("w_pa", [512, 1024])
    w_out = din("w_out", [1024, 1024])
    g_ffn = din("g_ffn", [1, 1024])
    w_q = din("w_q", [1024, 2048])
    keys = din("keys", [16, 128, 128])
    peer_u = din("peer_u", [16384, 1024])
    peer_v = din("peer_v", [16384, 1024])
    g_ple = din("g_ple", [1, 1024])
    w_pg = din("w_pg", [1024, 1024])
    w_pp = din("w_pp", [256, 1024])
    g_fin = din("g_fin", [1, 1024])
    out = nc.dram_tensor("out", [NT_OWN, 1024], F32, kind="ExternalOutput").ap()

    qT_s = dscr("qT_s", [4, 128, NT_OWN], BF16)
    kT_s = dscr("kT_s", [4, 128, NT_LOC], BF16)
    v_s = dscr("v_s", [NT_LOC, 8 * 65], BF16)
    gT_s = dscr("gT_s", [16, 128, NT_OWN], BF16)
    ssmT_s = dscr("ssmT_s", [4, 128, NT_OWN], BF16)
    attT_s = dscr("attT_s", [4, 128, NT_OWN], BF16)
    x1_s = dscr("x1_s", [NT_OWN, 1024], F32)
    h2T_s = dscr("h2T_s", [8, 128, NT_OWN], BF16)
    sc_s = dscr("sc_s", [NT_OWN, 16 * 128], F32)
    G_s = dscr("G_s", [32, 128, 128, 128], BF16)
    x2_s = dscr("x2_s", [NT_OWN, 1024], F32)
    UT5_s = dscr("UT5_s", [8, 128, 32 * 128], BF16)
    UT_s = dscr("UT_s", [128, 128, 1024], BF16)

    from contextlib import ExitStack
    st = ExitStack()
    PS = []
    for i in range(8):
        t = st.enter_context(nc.psum_tensor("ps%d" % i, [128, 512], F32))
        PS.append(Buf(t[:], "ps%d" % i))
    psi = [0]

    def nps():
        b = PS[psi[0] % 8]
        psi[0] += 1
        return b

    idf = K.sb([128, 128], F32, "idf")
    idb = K.sb([128, 128], BF16, "idb")
    K.dma("c0", idf.ap, ident_d, w=[idf])
    K.cp("dve", idb.ap, idf.ap, [idf], [idb])
    ksum = K.sb([128, 4, 32], F32, "ksum")
    K.persist = K.off

    ut5tok = [Buf(None, "ut5_%d" % i) for i in range(8)]

    def rmsnorm_tile(xt, gt, hb, sq, ss, rs):
        K.act(sq.ap, xt.ap, AF.Square, [xt], [sq, ss], accum=ss.ap)
        K.act(rs.ap, ss.ap, AF.Sqrt, [ss], [rs], scale=1.0 / 1024, bias=eps_b.ap)
        K.op("dve", lambda e: e.reciprocal(out=rs.ap, in_=rs.ap), [rs], [rs])
        K.stt("dve", hb.ap, xt.ap, rs.ap[:, 0:1], gt.ap, ALU.mult, ALU.mult, [xt, rs, gt], [hb])

    def load_w_bf(dst, src_ap, rows_kc, ncols, stage, key):
        for kc in range(rows_kc):
            K.dma(key, stage.ap[:, 0:ncols], src_ap[kc * 128:(kc + 1) * 128, :], w=[stage])
            K.cp("dve" if kc % 2 == 0 else "act", dst.ap[:, kc, :], stage.ap[:, 0:ncols], [stage], [dst])

    eps_b = K.sb([128, 1], F32, "eps")
    K.memset("dve", eps_b.ap, 1e-6, [eps_b])
    K.persist = K.off

    def phase_A():
        K.phase_reset()
        win = K.sb([128, 8, 3584], BF16, "win")
        wu = K.sb([128, 8, 512], BF16, "wuA")
        Ustk = K.sb([128, 32, 8, 16], BF16, "UstkA")
        UTo = K.sb([128, 32, 128], BF16, "UToA")
        wpm = K.sb([128, 8, 1024], BF16, "wpm")
        stageA = K.sb([128, 1792], F32, "stageA")
        stageB = K.sb([128, 1792], F32, "stageB")
        stage = stageA
        gt = K.sb([128, 1024], F32, "gmix")
        invf = K.sb([128, 2], F32, "invf")
        xts = [K.sb([128, 1024], F32, "xt%d" % i) for i in range(2)]
        sq = K.sb([128, 1024], BF16, "sq")
        ss = K.sb([128, 1], F32, "ss")
        rs = K.sb([128, 1], F32, "rs")
        hbs = [K.sb([128, 1024], BF16, "hb%d" % i) for i in range(2)]
        hTs = [K.sb([128, 8, 1024], BF16, "hT%d" % i) for i in range(2)]
        posi = K.sb([128, 1024], I32, "posi")
        ang = K.sb([128, 1024], F32, "ang")
        tmpa = K.sb([128, 1024], F32, "tmpa")
        tmpi = K.sb([128, 1024], I32, "tmpi")
        cosTs = [K.sb([128, 1024], F32, "cosT%d" % i) for i in range(2)]
        sinTs = [K.sb([128, 1024], F32, "sinT%d" % i) for i in range(2)]
        t1s = [K.sb([128, 512], F32, "t1_%d" % i) for i in range(2)]
        t2s = [K.sb([128, 512], F32, "t2_%d" % i) for i in range(2)]
        obf = [K.sb([128, 512], BF16, "obf%d" % i) for i in range(2)]
        vts = [K.sb([128, 8, 65], BF16, "vt%d" % i) for i in range(2)]
        for v in vts:
            K.memset("pool", v.ap, 1.0, [v])
        K.dma("c0", gt.ap, g_mix.partition_broadcast(128), w=[gt])
        K.dma("c0", invf.ap, invf_d, w=[invf])
        n_st = [0]

        def stream_w(dst_ap, src_ap, ncols):
            i = n_st[0]
            n_st[0] += 1
            stg_ = (stageA, stageB)[i % 2]
            K.dma("wst%d" % (i % 2), stg_.ap[:, 0:ncols], src_ap, w=[stg_])
            K.cp("dve" if i % 2 == 0 else "act", dst_ap, stg_.ap[:, 0:ncols], [stg_], [win])

        for kc in range(8):
            stream_w(win.ap[:, kc, 0:1792], w_in[kc * 128:(kc + 1) * 128, 512:2304], 1792)
            stream_w(win.ap[:, kc, 1792:3584], w_in[kc * 128:(kc + 1) * 128, 2304:4096], 1792)
        for kc in range(8):
            i = n_st[0]
            n_st[0] += 1
            stg_ = (stageA, stageB)[i % 2]
            K.dma("wst%d" % (i % 2), stg_.ap[:, 0:1024], w_perm[kc * 128:(kc + 1) * 128, :], w=[stg_])
            K.cp("dve" if i % 2 == 0 else "act", wpm.ap[:, kc, :], stg_.ap[:, 0:1024], [stg_], [wpm])
        for kc in range(8):
            i = n_st[0]
            n_st[0] += 1
            stg_ = (stageA, stageB)[i % 2]
            K.dma("wst%d" % (i % 2), stg_.ap[:, 0:512], w_in[kc * 128:(kc + 1) * 128, 0:512], w=[stg_])
            K.cp("dve" if i % 2 == 0 else "act", wu.ap[:, kc, :], stg_.ap[:, 0:512], [stg_], [wu])

        def sincos(dst, phase):
            K.ts("dve", tmpa.ap, ang.ap, phase, 1.0 / (2 * PI), ALU.add, ALU.mult, [ang], [tmpa])
            K.cp("dve", tmpi.ap, tmpa.ap, [tmpa], [tmpi])
            K.cp("dve", tmpa.ap, tmpi.ap, [tmpi], [tmpa])
            K.stt("dve", tmpa.ap, tmpa.ap, -2 * PI, ang.ap, ALU.mult, ALU.add, [tmpa, ang], [tmpa])
            K.ts("dve", tmpa.ap, tmpa.ap, phase, None, ALU.add, None, [tmpa], [tmpa])
            K.ts("dve", dst.ap, tmpa.ap, PI, -2 * PI, ALU.is_gt, ALU.mult, [tmpa], [dst])
            K.tt("dve", tmpa.ap, tmpa.ap, dst.ap, ALU.add, [tmpa, dst], [tmpa])
            K.ts("dve", dst.ap, tmpa.ap, -PI, 2 * PI, ALU.is_lt, ALU.mult, [tmpa], [dst])
            K.tt("dve", tmpa.ap, tmpa.ap, dst.ap, ALU.add, [tmpa, dst], [tmpa])
            K.act(dst.ap, tmpa.ap, AF.Sin, [tmpa], [dst])

        xic = [0]

        def prep(blk):
            tb = blk * 1024
            hT = hTs[blk % 2]
            cosT, sinT = cosTs[blk % 2], sinTs[blk % 2]
            xi = xic[0]
            K.dma("pos", posi.ap, posc[:, tb:tb + 1024].partition_broadcast(128), w=[posi])
            K.cp("dve", ang.ap, posi.ap, [posi], [ang])
            K.ts("dve", ang.ap, ang.ap, invf.ap[:, 0:1], None, ALU.mult, None, [ang, invf], [ang])
            sincos(cosT, PI / 2)
            sincos(sinT, 0.0)
            K.ts("dve", sinT.ap, sinT.ap, invf.ap[:, 1:2], None, ALU.mult, None, [sinT, invf], [sinT])
            for ti in range(8):
                xt = xts[xi % 2]
                hb = hbs[xi % 2]
                xi += 1
                K.dma("x%d" % (xi % 2), xt.ap, xc[tb + ti * 128: tb + (ti + 1) * 128, :], w=[xt])
                rmsnorm_tile(xt, gt, hb, sq, ss, rs)

                def a_tr(ti=ti, hb=hb, hT=hT):
                    p = nps()
                    pv = p.ap.bitcast(BF16).rearrange("p (a b) -> p a b", a=8)
                    for kc in range(8):
                        K.tr(p, pv[:, kc, :], hb.ap[:, kc * 128:(kc + 1) * 128], idb.ap, [hb, idb])
                    K.cp("act", hT.ap[:, :, ti * 128:(ti + 1) * 128], pv, [p], [hT])

                if ti > 0:
                    pend_a()
                pend_a = a_tr
            pend_a()
            xic[0] = xi

        oic = [0]

        def compute(blk):
            own = blk >= 4
            tb = blk * 1024
            hT = hTs[blk % 2]
            cosT, sinT = cosTs[blk % 2], sinTs[blk % 2]
            oi = oic[0]
            for j in range(8):
                p = nps()
                for kc in range(8):
                    K.mm(p, p.ap, hT.ap[:, kc, j:1024:8], wu.ap[:, kc, :], kc == 0, kc == 7, [hT, wu])
                K.cp("act" if j % 2 else "dve", Ustk.ap[:, :, j, :], p.ap.rearrange("p (g c) -> p g c", g=32), [p], [Ustk])
            for g0 in range(0, 32, 8):
                p = nps()
                pv = p.ap.bitcast(BF16).rearrange("p (a b) -> p a b", a=8)
                for gi in range(8):
                    K.tr(p, pv[:, gi, :], Ustk.ap[:, g0 + gi, :, :].rearrange("p a b -> p (a b)"), idb.ap, [Ustk, idb])
                K.cp("act", UTo.ap[:, g0:g0 + 8, :], pv, [p], [UTo])
            K.dma("utA", UT5_s[blk], UTo.ap.rearrange("p a b -> p (a b)"), r=[UTo], w=[ut5tok[blk]])
            for which in (["q", "k"] if own else ["k"]):
                cbase = 0 if which == "q" else 512
                for c in range(4):
                    for half in range(2):
                        pa = nps()
                        pb = nps()
                        for kc in range(8):
                            K.mm(pa, pa.ap, win.ap[:, kc, cbase + c * 128: cbase + (c + 1) * 128],
                                 hT.ap[:, kc, half * 512:(half + 1) * 512], kc == 0, kc == 7, [win, hT])
                        for kc in range(8):
                            K.mm(pb, pb.ap, wpm.ap[:, kc, cbase + c * 128: cbase + (c + 1) * 128],
                                 hT.ap[:, kc, half * 512:(half + 1) * 512], kc == 0, kc == 7, [wpm, hT])
                        t1, t2 = t1s[oi % 2], t2s[oi % 2]
                        K.tt("dve", t1.ap, pa.ap, cosT.ap[:, half * 512:(half + 1) * 512], ALU.mult, [pa, cosT], [t1])
                        K.tt("dve", t2.ap, pb.ap, sinT.ap[:, half * 512:(half + 1) * 512], ALU.mult, [pb, sinT], [t2])
                        K.tt("dve", t1.ap, t1.ap, t2.ap, ALU.add, [t1, t2], [t1])
                        ob = obf[oi % 2]
                        oi += 1
                        K.cp("act", ob.ap, t1.ap, [t1], [ob])
                        t0 = tb + half * 512
                        if which == "q":
                            K.dma("oq%d" % (oi % 2), qT_s[c, :, t0 - NT_OWN: t0 - NT_OWN + 512], ob.ap, r=[ob])
                        else:
                            K.dma("oq%d" % (oi % 2), kT_s[c, :, t0: t0 + 512], ob.ap, r=[ob])
                            K.op("dve", lambda e, c=c, b0=t0 // 256, t1=t1: e.tensor_reduce(
                                out=ksum.ap[:, c, b0:b0 + 2], in_=t1.ap.rearrange("p (a b) -> p a b", a=2),
                                axis=AX.X, op=ALU.add), [t1], [ksum])
            for ti in range(8):
                p = nps()
                for kc in range(8):
                    K.mm(p, p.ap, hT.ap[:, kc, ti * 128:(ti + 1) * 128], win.ap[:, kc, 1024:1536],
                         kc == 0, kc == 7, [hT, win])
                vt = vts[ti % 2]
                K.cp("act", vt.ap[:, :, 0:64], p.ap.rearrange("p (h d) -> p h d", h=8), [p], [vt])
                K.dma("ov%d" % (ti % 2), v_s[tb + ti * 128: tb + (ti + 1) * 128, :].rearrange("p (h d) -> p h d", h=8),
                      vt.ap, r=[vt])
            if own:
                for c in range(16):
                    for half in range(2):
                        p = nps()
                        for kc in range(8):
                            K.mm(p, p.ap, win.ap[:, kc, 1536 + c * 128: 1536 + (c + 1) * 128],
                                 hT.ap[:, kc, half * 512:(half + 1) * 512], kc == 0, kc == 7, [win, hT])
                        ob = obf[oi % 2]
                        oi += 1
                        K.act(ob.ap, p.ap, AF.Sigmoid, [p], [ob])
                        t0 = tb + half * 512 - NT_OWN
                        K.dma("oq%d" % (oi % 2), gT_s[c, :, t0:t0 + 512], ob.ap, r=[ob])
            oic[0] = oi

        prep(0)
        for blk in range(8):
            if blk + 1 < 8:
                prep(blk + 1)
            compute(blk)
        K.barrier()

    def phase_S():
        K.phase_reset()
        sm = lambda name, n=16: K.sb([128, n], F32, name)
        wglu = K.sb([128, 4, 512], BF16, "wglu")
        WS = K.sb([128, 16, 2, 2, 128], BF16, "WS")
        WY1 = K.sb([128, 16, 2, 2, 128], BF16, "WY1")
        WY2 = K.sb([128, 32, 128], BF16, "WY2")
        Ct = K.sb([128, 16, 128], F32, "Ct")
        St = K.sb([128, 16, 128], F32, "St")
        r8 = sm("r8")
        Dre = sm("Dre")
        Dim = sm("Dim")
        car_re = sm("car_re")
        car_im = sm("car_im")
        ta, tb_, tc_ = sm("ta"), sm("tb"), sm("tc")
        mark = K.off
        stage = K.sb([128, 512], F32, "stageS")
        ldt, are, aim = sm("ldt"), sm("are"), sm("aim")
        bre = K.sb([128, 16, 16], F32, "bre")
        bim = K.sb([128, 16, 16], F32, "bim")
        cre = K.sb([128, 16, 16], F32, "cre")
        cim = K.sb([128, 16, 16], F32, "cim")
        ncim = K.sb([128, 16, 16], F32, "ncim")
        for t_, nm in ((ldt, "ldt"), (are, "are"), (aim, "aim")):
            K.dma("c0", t_.ap, s5[nm], w=[t_])
        for t_, nm in ((bre, "bre"), (bim, "bim"), (cre, "cre"), (cim, "cim")):
            K.dma("c0", t_.ap.rearrange("p a b -> p (a b)"), s5[nm], w=[t_])
        for kc in range(4):
            K.dma("wst", stage.ap, w_glu[kc * 128:(kc + 1) * 128, :], w=[stage])
            K.cp("dve", wglu.ap[:, kc, :], stage.ap, [stage], [wglu])
        dt_, xr, th, mag, cs, sn = sm("dt"), sm("xr"), sm("th"), sm("mag"), sm("cs"), sm("sn")
        abr, abi, den, nr, fre, fim = sm("abr"), sm("abi"), sm("den"), sm("nr"), sm("fre"), sm("fim")
        ti_ = K.sb([128, 16], I32, "ti")
        V_ = "dve"
        K.act(dt_.ap, ldt.ap, AF.Exp, [ldt], [dt_])
        K.tt(V_, xr.ap, dt_.ap, are.ap, ALU.mult, [dt_, are], [xr])
        K.tt(V_, th.ap, dt_.ap, aim.ap, ALU.mult, [dt_, aim], [th])
        K.act(mag.ap, xr.ap, AF.Exp, [xr], [mag])
        K.act(r8.ap, xr.ap, AF.Exp, [xr], [r8], scale=8.0)

        def sin_small(dst, src, phase):
            K.ts(V_, ta.ap, src.ap, phase, 1.0 / (2 * PI), ALU.add, ALU.mult, [src], [ta])
            K.cp(V_, ti_.ap, ta.ap, [ta], [ti_])
            K.cp(V_, ta.ap, ti_.ap, [ti_], [ta])
            K.stt(V_, ta.ap, ta.ap, -2 * PI, src.ap, ALU.mult, ALU.add, [ta, src], [ta])
            K.ts(V_, ta.ap, ta.ap, phase, None, ALU.add, None, [ta], [ta])
            K.ts(V_, tb_.ap, ta.ap, PI, -2 * PI, ALU.is_gt, ALU.mult, [ta], [tb_])
            K.tt(V_, ta.ap, ta.ap, tb_.ap, ALU.add, [ta, tb_], [ta])
            K.ts(V_, tb_.ap, ta.ap, -PI, 2 * PI, ALU.is_lt, ALU.mult, [ta], [tb_])
            K.tt(V_, ta.ap, ta.ap, tb_.ap, ALU.add, [ta, tb_], [ta])
            K.act(dst.ap, ta.ap, AF.Sin, [ta], [dst])

        sin_small(cs, th, PI / 2)
        sin_small(sn, th, 0.0)
        K.tt(V_, abr.ap, mag.ap, cs.ap, ALU.mult, [mag, cs], [abr])
        K.tt(V_, abi.ap, mag.ap, sn.ap, ALU.mult, [mag, sn], [abi])
        K.tt(V_, den.ap, are.ap, are.ap, ALU.mult, [are], [den])
        K.tt(V_, ta.ap, aim.ap, aim.ap, ALU.mult, [aim], [ta])
        K.tt(V_, den.ap, den.ap, ta.ap, ALU.add, [den, ta], [den])
        K.op(V_, lambda e: e.reciprocal(out=den.ap, in_=den.ap), [den], [den])
        K.ts(V_, nr.ap, abr.ap, -1.0, None, ALU.add, None, [abr], [nr])
        K.tt(V_, ta.ap, nr.ap, are.ap, ALU.mult, [nr, are], [ta])
        K.tt(V_, tb_.ap, abi.ap, aim.ap, ALU.mult, [abi, aim], [tb_])
        K.tt(V_, ta.ap, ta.ap, tb_.ap, ALU.add, [ta, tb_], [ta])
        K.tt(V_, fre.ap, ta.ap, den.ap, ALU.mult, [ta, den], [fre])
        K.tt(V_, ta.ap, abi.ap, are.ap, ALU.mult, [abi, are], [ta])
        K.tt(V_, tb_.ap, nr.ap, aim.ap, ALU.mult, [nr, aim], [tb_])
        K.tt(V_, ta.ap, ta.ap, tb_.ap, ALU.subtract, [ta, tb_], [ta])
        K.tt(V_, fim.ap, ta.ap, den.ap, ALU.mult, [ta, den], [fim])
        pwf_re = K.sb([128, 16, 9], F32, "pwf_re")
        pwf_im = K.sb([128, 16, 9], F32, "pwf_im")
        pwr_re = K.sb([128, 16, 8], F32, "pwr_re")
        pwr_im = K.sb([128, 16, 8], F32, "pwr_im")
        K.memset(V_, pwf_re.ap[:, :, 0], 1.0, [pwf_re])
        K.memset(V_, pwf_im.ap[:, :, 0], 0.0, [pwf_im])
        for d in range(8):
            K.tt(V_, ta.ap, pwf_re.ap[:, :, d], abr.ap, ALU.mult, [pwf_re, abr], [ta])
            K.tt(V_, tb_.ap, pwf_im.ap[:, :, d], abi.ap, ALU.mult, [pwf_im, abi], [tb_])
            K.tt(V_, pwf_re.ap[:, :, d + 1], ta.ap, tb_.ap, ALU.subtract, [ta, tb_], [pwf_re])
            K.tt(V_, ta.ap, pwf_re.ap[:, :, d], abi.ap, ALU.mult, [pwf_re, abi], [ta])
            K.tt(V_, tb_.ap, pwf_im.ap[:, :, d], abr.ap, ALU.mult, [pwf_im, abr], [tb_])
            K.tt(V_, pwf_im.ap[:, :, d + 1], ta.ap, tb_.ap, ALU.add, [ta, tb_], [pwf_im])
        for j in range(8):
            K.cp(V_, pwr_re.ap[:, :, j], pwf_re.ap[:, :, 7 - j], [pwf_re], [pwr_re])
            K.cp(V_, pwr_im.ap[:, :, j], pwf_im.ap[:, :, 7 - j], [pwf_im], [pwr_im])
        K.cp(V_, Dre.ap, pwf_re.ap[:, :, 8], [pwf_re], [Dre])
        K.cp(V_, Dim.ap, pwf_im.ap[:, :, 8], [pwf_im], [Dim])
        ur, ui, rr = sm("ur"), sm("ui"), sm("rr")
        K.op(V_, lambda e: e.reciprocal(out=rr.ap, in_=r8.ap), [r8], [rr])
        K.tt(V_, ur.ap, Dre.ap, rr.ap, ALU.mult, [Dre, rr], [ur])
        K.tt(V_, ui.ap, Dim.ap, rr.ap, ALU.mult, [Dim, rr], [ui])
        tm1 = K.sb([128, 16, 64], F32, "tm1")
        tm2 = K.sb([128, 16, 64], F32, "tm2")
        K.memset(V_, Ct.ap[:, :, 0], 1.0, [Ct])
        K.memset(V_, St.ap[:, :, 0], 0.0, [St])
        for k in range(7):
            n = 1 << k
            urb = ur.ap.unsqueeze(2).broadcast_to([128, 16, n])
            uib = ui.ap.unsqueeze(2).broadcast_to([128, 16, n])
            K.tt(V_, tm1.ap[:, :, 0:n], Ct.ap[:, :, 0:n], urb, ALU.mult, [Ct, ur], [tm1])
            K.tt(V_, tm2.ap[:, :, 0:n], St.ap[:, :, 0:n], uib, ALU.mult, [St, ui], [tm2])
            K.tt(V_, Ct.ap[:, :, n:2 * n], tm1.ap[:, :, 0:n], tm2.ap[:, :, 0:n], ALU.subtract, [tm1, tm2], [Ct])
            K.tt(V_, tm1.ap[:, :, 0:n], Ct.ap[:, :, 0:n], uib, ALU.mult, [Ct, ui], [tm1])
            K.tt(V_, tm2.ap[:, :, 0:n], St.ap[:, :, 0:n], urb, ALU.mult, [St, ur], [tm2])
            K.tt(V_, St.ap[:, :, n:2 * n], tm1.ap[:, :, 0:n], tm2.ap[:, :, 0:n], ALU.add, [tm1, tm2], [St])
            K.tt(V_, ta.ap, ur.ap, ur.ap, ALU.mult, [ur], [ta])
            K.tt(V_, tb_.ap, ui.ap, ui.ap, ALU.mult, [ui], [tb_])
            K.tt(V_, tc_.ap, ur.ap, ui.ap, ALU.mult, [ur, ui], [tc_])
            K.tt(V_, ur.ap, ta.ap, tb_.ap, ALU.subtract, [ta, tb_], [ur])
            K.ts(V_, ui.ap, tc_.ap, 2.0, None, ALU.mult, None, [tc_], [ui])
        bbr = K.sb([128, 16, 16], F32, "bbr")
        bbi = K.sb([128, 16, 16], F32, "bbi")
        t3a = K.sb([128, 16, 16], F32, "t3a")
        t3b = K.sb([128, 16, 16], F32, "t3b")
        freb = fre.ap.unsqueeze(2).broadcast_to([128, 16, 16])
        fimb = fim.ap.unsqueeze(2).broadcast_to([128, 16, 16])
        K.tt(V_, t3a.ap, bre.ap, freb, ALU.mult, [bre, fre], [t3a])
        K.tt(V_, t3b.ap, bim.ap, fimb, ALU.mult, [bim, fim], [t3b])
        K.tt(V_, bbr.ap, t3a.ap, t3b.ap, ALU.subtract, [t3a, t3b], [bbr])
        K.tt(V_, t3a.ap, bim.ap, freb, ALU.mult, [bim, fre], [t3a])
        K.tt(V_, t3b.ap, bre.ap, fimb, ALU.mult, [bre, fim], [t3b])
        K.tt(V_, bbi.ap, t3a.ap, t3b.ap, ALU.add, [t3a, t3b], [bbi])
        K.ts(V_, ncim.ap, cim.ap, -1.0, None, ALU.mult, None, [cim], [ncim])
        Fre = K.sb([128, 16, 15, 16], F32, "Fre")
        Fim = K.sb([128, 16, 15, 16], F32, "Fim")
        t4a = K.sb([128, 16, 8, 16], F32, "t4a")
        t4b = K.sb([128, 16, 8, 16], F32, "t4b")
        K.memset("pool", Fre.ap, 0.0, [Fre])
        K.memset("pool", Fim.ap, 0.0, [Fim])
        S4 = [128, 16, 8, 16]
        prb = pwr_re.ap.unsqueeze(3).broadcast_to(S4)
        pib = pwr_im.ap.unsqueeze(3).broadcast_to(S4)
        bbrb = bbr.ap.unsqueeze(2).broadcast_to(S4)
        bbib = bbi.ap.unsqueeze(2).broadcast_to(S4)
        K.tt(V_, t4a.ap, prb, bbrb, ALU.mult, [pwr_re, bbr], [t4a])
        K.tt(V_, t4b.ap, pib, bbib, ALU.mult, [pwr_im, bbi], [t4b])
        K.tt(V_, Fre.ap[:, :, 0:8, :], t4a.ap, t4b.ap, ALU.subtract, [t4a, t4b], [Fre])
        K.tt(V_, t4a.ap, prb, bbib, ALU.mult, [pwr_re, bbi], [t4a])
        K.tt(V_, t4b.ap, pib, bbrb, ALU.mult, [pwr_im, bbr], [t4b])
        K.tt(V_, Fim.ap[:, :, 0:8, :], t4a.ap, t4b.ap, ALU.add, [t4a, t4b], [Fim])
        K.memset("pool", WS.ap, 0.0, [WS])
        K.memset("pool", WY1.ap, 0.0, [WY1])
        for p_ in range(16):
            for ri, Ft in ((0, Fre), (1, Fim)):
                ps = nps()
                K.tr(ps, ps.ap[:, 0:128], Ft.ap[:, p_, 0:8, :].rearrange("p a b -> p (a b)"), idf.ap, [Ft, idf])
                K.cp(V_, WS.ap[:, p_, ri, 0, 0:64], ps.ap[:, 0:64], [ps], [WS])
                K.cp(V_, WS.ap[:, p_, ri, 1, 64:128], ps.ap[:, 64:128], [ps], [WS])
        pfr = pwf_re.ap[:, :, 1:9].unsqueeze(3).broadcast_to(S4)
        pfi = pwf_im.ap[:, :, 1:9].unsqueeze(3).broadcast_to(S4)
        creb = cre.ap.unsqueeze(2).broadcast_to(S4)
        cimb = cim.ap.unsqueeze(2).broadcast_to(S4)
        K.tt(V_, t4a.ap, creb, pfr, ALU.mult, [cre, pwf_re], [t4a])
        K.tt(V_, t4b.ap, cimb, pfi, ALU.mult, [cim, pwf_im], [t4b])
        K.tt(V_, t4a.ap, t4a.ap, t4b.ap, ALU.subtract, [t4a, t4b], [t4a])
        for g2 in range(2):
            K.cp(V_, WY1.ap[g2 * 64:(g2 + 1) * 64, :, 0, g2, :],
                 t4a.ap[g2 * 64:(g2 + 1) * 64].rearrange("p a b c -> p a (b c)"), [t4a], [WY1])
        K.tt(V_, t4a.ap, creb, pfi, ALU.mult, [cre, pwf_im], [t4a])
        K.tt(V_, t4b.ap, cimb, pfr, ALU.mult, [cim, pwf_re], [t4b])
        K.tt(V_, t4a.ap, t4a.ap, t4b.ap, ALU.add, [t4a, t4b], [t4a])
        K.ts(V_, t4a.ap, t4a.ap, -1.0, None, ALU.mult, None, [t4a], [t4a])
        for g2 in range(2):
            K.cp(V_, WY1.ap[g2 * 64:(g2 + 1) * 64, :, 1, g2, :],
                 t4a.ap[g2 * 64:(g2 + 1) * 64].rearrange("p a b c -> p a (b c)"), [t4a], [WY1])
        dB = K.sb([128, 512], F32, "dB")
        dI = K.sb([128, 32, 8, 16], F32, "dI")
        K.dma("c0", dB.ap, ssm_d.partition_broadcast(128), w=[dB])
        K.tt(V_, dI.ap, idf.ap.rearrange("p (a b) -> p a b", a=8).unsqueeze(1).broadcast_to([128, 32, 8, 16]),
             dB.ap.rearrange("p (g c) -> p g c", g=32).unsqueeze(2).broadcast_to([128, 32, 8, 16]), ALU.mult,
             [idf, dB], [dI])
        for g in range(32):
            p_, g2 = g // 2, g % 2
            if g % 4 == 0:
                ps = nps()
            o0 = (g % 4) * 128
            sl = slice(g2 * 64, (g2 + 1) * 64)
            for j in range(8):
                oap = ps.ap[:, o0 + j * 16: o0 + (j + 1) * 16]
                K.mm(ps, oap, Fre.ap[sl, p_, 7 - j:15 - j, :].rearrange("p a b -> p (a b)"), cre.ap[sl, p_, :],
                     True, False, [Fre, cre])
                K.mm(ps, oap, Fim.ap[sl, p_, 7 - j:15 - j, :].rearrange("p a b -> p (a b)"), ncim.ap[sl, p_, :],
                     False, True, [Fim, ncim])
            if g % 4 == 3:
                K.tt(V_, WY2.ap[:, g - 3:g + 1, :], ps.ap.rearrange("p (g k) -> p g k", g=4),
                     dI.ap[:, g - 3:g + 1].rearrange("p g a b -> p g (a b)"), ALU.add, [ps, dI], [WY2])
        K.memset(V_, car_re.ap, 0.0, [car_re])
        K.memset(V_, car_im.ap, 0.0, [car_im])
        K.barrier()
        K.off = mark
        UTs = [K.sb([128, 32, 128], BF16, "UT%d" % i) for i in range(2)]
        Sres = [K.sb([128, 16, 128], F32, "Sre%d" % i) for i in range(2)]
        Sims = [K.sb([128, 16, 128], F32, "Sim%d" % i) for i in range(2)]
        gre = K.sb([128, 16, 128], F32, "gre")
        gim = K.sb([128, 16, 128], F32, "gim")
        u1 = K.sb([128, 16, 128], F32, "u1")
        u2 = K.sb([128, 16, 128], F32, "u2")
        Pre = K.sb([128, 16, 129], BF16, "Pre")
        Pim = K.sb([128, 16, 129], BF16, "Pim")
        ytm = K.sb([128, 8, 512], BF16, "ytm")
        yT = K.sb([128, 4, 1024], BF16, "yT")
        sg = K.sb([128, 512], BF16, "sg")
        sos = [K.sb([128, 4, 512], BF16, "so%d" % i) for i in range(2)]

        def S1(blk):
            UT, Sre, Sim = UTs[blk % 2], Sres[blk % 2], Sims[blk % 2]
            K.dma("ut%d" % (blk % 2), UT.ap.rearrange("p a b -> p (a b)"), UT5_s[blk], r=[ut5tok[blk]], w=[UT])
            for ri, Sx in ((0, Sre), (1, Sim)):
                for p0 in range(0, 16, 4):
                    p = nps()
                    for pi_ in range(4):
                        pp = p0 + pi_
                        K.mm(p, p.ap[:, pi_ * 128:(pi_ + 1) * 128], WS.ap[:, pp, ri, 0, :], UT.ap[:, 2 * pp, :], True, False, [WS, UT])
                        K.mm(p, p.ap[:, pi_ * 128:(pi_ + 1) * 128], WS.ap[:, pp, ri, 1, :], UT.ap[:, 2 * pp + 1, :], False, True, [WS, UT])
                    K.cp("act", Sx.ap[:, p0:p0 + 4, :], p.ap.rearrange("p (a b) -> p a b", a=4), [p], [Sx])

        def SC(blk):
            own = blk >= 4
            Sre, Sim = Sres[blk % 2], Sims[blk % 2]
            if own:
                K.cp(V_, Pre.ap[:, :, 0], car_re.ap, [car_re], [Pre])
                K.cp(V_, Pim.ap[:, :, 0], car_im.ap, [car_im], [Pim])
            K.tt(V_, ta.ap, Dre.ap, car_re.ap, ALU.mult, [Dre, car_re], [ta])
            K.tt(V_, tb_.ap, Dim.ap, car_im.ap, ALU.mult, [Dim, car_im], [tb_])
            K.tt(V_, ta.ap, ta.ap, tb_.ap, ALU.subtract, [ta, tb_], [ta])
            K.tt(V_, Sre.ap[:, :, 0], Sre.ap[:, :, 0], ta.ap, ALU.add, [Sre, ta], [Sre])
            K.tt(V_, ta.ap, Dre.ap, car_im.ap, ALU.mult, [Dre, car_im], [ta])
            K.tt(V_, tb_.ap, Dim.ap, car_re.ap, ALU.mult, [Dim, car_re], [tb_])
            K.tt(V_, ta.ap, ta.ap, tb_.ap, ALU.add, [ta, tb_], [ta])
            K.tt(V_, Sim.ap[:, :, 0], Sim.ap[:, :, 0], ta.ap, ALU.add, [Sim, ta], [Sim])
            K.tt("dve", u1.ap, Ct.ap, Sre.ap, ALU.mult, [Ct, Sre], [u1])
            K.tt("pool", u2.ap, St.ap, Sim.ap, ALU.mult, [St, Sim], [u2])
            K.tt("dve", gre.ap, u1.ap, u2.ap, ALU.add, [u1, u2], [gre])
            K.tt("dve", u1.ap, Ct.ap, Sim.ap, ALU.mult, [Ct, Sim], [u1])
            K.tt("pool", u2.ap, St.ap, Sre.ap, ALU.mult, [St, Sre], [u2])
            K.tt("dve", gim.ap, u1.ap, u2.ap, ALU.subtract, [u1, u2], [gim])
            for pp in range(16):
                rb = r8.ap[:, pp:pp + 1].to_broadcast([128, 128])
                K.op("dve", lambda e, pp=pp, rb=rb, Sre=Sre: e.tensor_tensor_scan(out=Sre.ap[:, pp, :], data0=rb, data1=gre.ap[:, pp, :],
                                                                                 initial=0.0, op0=ALU.mult, op1=ALU.add), [gre, r8], [Sre])
                K.op("dve", lambda e, pp=pp, rb=rb, Sim=Sim: e.tensor_tensor_scan(out=Sim.ap[:, pp, :], data0=rb, data1=gim.ap[:, pp, :],
                                                                                 initial=0.0, op0=ALU.mult, op1=ALU.add), [gim, r8], [Sim])
            K.tt("dve", u1.ap, Ct.ap, Sre.ap, ALU.mult, [Ct, Sre], [u1])
            K.tt("pool", u2.ap, St.ap, Sim.ap, ALU.mult, [St, Sim], [u2])
            K.tt("dve", gre.ap, u1.ap, u2.ap, ALU.subtract, [u1, u2], [gre])
            K.tt("dve", u1.ap, Ct.ap, Sim.ap, ALU.mult, [Ct, Sim], [u1])
            K.tt("pool", u2.ap, St.ap, Sre.ap, ALU.mult, [St, Sre], [u2])
            K.tt("dve", gim.ap, u1.ap, u2.ap, ALU.add, [u1, u2], [gim])
            K.cp(V_, car_re.ap, gre.ap[:, :, 127], [gre], [car_re])
            K.cp(V_, car_im.ap, gim.ap[:, :, 127], [gim], [car_im])
            if own:
                K.cp("act", Pre.ap[:, :, 1:129], gre.ap, [gre], [Pre])
                K.cp("act", Pim.ap[:, :, 1:129], gim.ap, [gim], [Pim])

        def SY(blk):
            tb = blk * 1024
            UT = UTs[blk % 2]
            for g0 in range(0, 32, 4):
                p = nps()
                for gi in range(4):
                    g = g0 + gi
                    pp, g2 = g // 2, g % 2
                    oap = p.ap[:, gi * 128:(gi + 1) * 128]
                    K.mm(p, oap, Pre.ap[:, pp, 0:128], WY1.ap[:, pp, 0, g2, :], True, False, [Pre, WY1])
                    K.mm(p, oap, Pim.ap[:, pp, 0:128], WY1.ap[:, pp, 1, g2, :], False, False, [Pim, WY1])
                    K.mm(p, oap, UT.ap[:, g, :], WY2.ap[:, g, :], False, True, [UT, WY2])
                K.act(ytm.ap.rearrange("p j (g c) -> p g j c", g=32)[:, g0:g0 + 4],
                      p.ap.rearrange("p (g j c) -> p g j c", g=4, j=8), AF.Gelu_apprx_tanh, [p], [ytm])
            for j in range(8):
                if j % 2 == 0:
                    p = nps()
                    pv = p.ap.bitcast(BF16).rearrange("p (a b) -> p a b", a=8)
                for cc in range(4):
                    K.tr(p, pv[:, (j % 2) * 4 + cc, :], ytm.ap[:, j, cc * 128:(cc + 1) * 128], idb.ap, [ytm, idb])
                K.cp("act", yT.ap[:, :, j:1024:8], pv[:, (j % 2) * 4:(j % 2) * 4 + 4, :], [p], [yT])
            for half in range(2):
                for co in range(4):
                    p = nps()
                    for kc in range(4):
                        K.mm(p, p.ap, wglu.ap[:, kc, co * 128:(co + 1) * 128], yT.ap[:, kc, half * 512:(half + 1) * 512],
                             kc == 0, kc == 3, [wglu, yT])
                    K.act(sg.ap, p.ap, AF.Sigmoid, [p], [sg])
                    so = sos[half]
                    K.tt("pool", so.ap[:, co, :], yT.ap[:, co, half * 512:(half + 1) * 512], sg.ap,
                         ALU.mult, [yT, sg], [so])
                t0_ = tb - NT_OWN + half * 512
                K.dma("oS%d" % half, ssmT_s[:, :, t0_: t0_ + 512].rearrange("c p t -> p c t"), sos[half].ap, r=[sos[half]])

        S1(0)
        for blk in range(8):
            if blk + 1 < 8:
                S1(blk + 1)
            SC(blk)
            if blk >= 4:
                SY(blk)
        K.barrier()

    def phase_B():
        K.phase_reset()
        wpsB = K.sb([128, 4, 1024], BF16, "wpsB")
        wpaB = K.sb([128, 4, 1024], BF16, "wpaB")
        woutB = K.sb([128, 8, 1024], BF16, "woutB")
        wqB = K.sb([128, 8, 2048], BF16, "wqB")
        wstg2 = K.sb([128, 2048], F32, "wstg2")
        wchunks = ([(wpsB, w_ps, kc, 1024) for kc in range(4)] + [(wpaB, w_pa, kc, 1024) for kc in range(4)]
                   + [(woutB, w_out, kc, 1024) for kc in range(8)] + [(wqB, w_q, kc, 2048) for kc in range(8)])
        KTs = [K.sb([96, NT_LOC], BF16, "KT%d" % i) for i in range(2)]
        QTs = [K.sb([96, NT_OWN], BF16, "QT%d" % i) for i in range(2)]
        Vs = [K.sb([128, 64, 65], BF16, "V%d" % i) for i in range(2)]
        QA = K.sb([64, NT_OWN], BF16, "QA")
        att = K.sb([128, 32, 512], BF16, "att")
        kmax = K.sb([64, 1], F32, "kmax")
        kmaxb = K.sb([64, 1], BF16, "kmaxb")
        ksb = K.sb([64, 32], BF16, "ksb")
        valid = K.sb([128, 512], F32, "valid")
        ownm = K.sb([128, 512], F32, "ownm")
        negb = K.sb([128, 512], F32, "negb")
        stg = K.sb([128, 2048], F32, "stgB")
        cm = K.sb([128, 4, 512], BF16, "cm")
        NG = 4
        gms = [K.sb([128, 32], F32, "gm%d" % i) for i in range(NG)]
        m8s = [K.sb([128, 8], F32, "m8%d" % i) for i in range(NG)]
        sels = [K.sb([128, 32], F32, "sel%d" % i) for i in range(NG)]
        sexs = [K.sb([128, 128], BF16, "sex%d" % i) for i in range(NG)]
        mraws = [K.sb([128, 4], F32, "mraw%d" % i) for i in range(2)]
        PTs = [K.sb([128, 512], BF16, "PT%d" % i) for i in range(3)]
        rls = [K.sb([128, 1], F32, "rl%d" % i) for i in range(4)]
        ostg = [K.sb([128, 4, 128], BF16, "ostg%d" % i) for i in range(2)]
        K.dma("c0", valid.ap, validc.partition_broadcast(128), w=[valid])
        K.dma("c0", ownm.ap, ownc.partition_broadcast(128), w=[ownm])
        K.ts("dve", negb.ap, valid.ap, -1.0, 1e30, ALU.add, ALU.mult, [valid], [negb])
        for i in range(4):
            K.dma("c0", stg.ap[:, 0:512], cm_d[i], w=[stg])
            K.cp("dve", cm.ap[:, i, :], stg.ap[:, 0:512], [stg], [cm])
        gm4 = K.sb([128, 4, 32], F32, "gm4")
        m84 = K.sb([128, 4, 8], F32, "m84")
        sel4 = K.sb([128, 4, 32], F32, "sel4")
        sex4 = K.sb([128, 4, 128], BF16, "sex4")
        K.memset("pool", sex4.ap, 0.0, [sex4])
        for kb_ in KTs:
            for c4 in range(4):
                K.dma("c0", stg.ap[64:96, :], koh_d[:, c4 * 2048:(c4 + 1) * 2048], w=[stg])
                K.cp("dve", kb_.ap[64:96, c4 * 2048:(c4 + 1) * 2048], stg.ap[64:96, :], [stg], [kb_])
        for h in range(8):
            hp, pb = h // 2, (h % 2) * 64
            KT, QT, V = KTs[h % 2], QTs[h % 2], Vs[h % 2]
            K.dma("bk%d" % (h % 2), KT.ap[0:64, :], kT_s[hp, pb:pb + 64, :], w=[KT])
            K.dma("bq%d" % (h % 2), QT.ap[0:64, :], qT_s[hp, pb:pb + 64, :], w=[QT])
            K.dma("bv%d" % (h % 2), V.ap, v_s.rearrange("(t p) c -> p t c", p=128)[:, :, h * 65:(h + 1) * 65], w=[V])
            K.act(QA.ap, QT.ap[0:64, :], AF.Abs, [QT], [QA])
            K.op("dve", lambda e, KT=KT: e.tensor_reduce(out=kmax.ap, in_=KT.ap[0:64, :], axis=AX.X, op=ALU.max,
                                                         apply_absolute_value=True), [KT], [kmax])
            K.cp("dve", kmaxb.ap, kmax.ap, [kmax], [kmaxb])
            K.cp("dve", ksb.ap, ksum.ap[pb:pb + 64, hp, :], [ksum], [ksb])
            for qg in range(8):
                q0 = qg * 512
                pg = PS[qg % 3]
                c0 = (qg // 3) * 132
                for j in range(4):
                    K.mm(pg, pg.ap[:, c0 + j * 32: c0 + (j + 1) * 32], QT.ap[0:64, q0 + j * 128: q0 + (j + 1) * 128],
                         ksb.ap, True, True, [QT, ksb])
                    K.mm(pg, pg.ap[:, c0 + 128 + j: c0 + 129 + j], QA.ap[:, q0 + j * 128: q0 + (j + 1) * 128],
                         kmaxb.ap, True, True, [QA, kmaxb])
            for qg in range(8):
                q0 = qg * 512
                pg = PS[qg % 3]
                c0 = (qg // 3) * 132
                mraw = mraws[qg % 2]
                K.cp("dve", mraw.ap, pg.ap[:, c0 + 128: c0 + 132], [pg], [mraw])
                S4 = [128, 2, 2, 32]
                qsl = slice(2 * qg * 32, (2 * qg + 2) * 32)
                bq = lambda t_: t_.ap[:, qsl].rearrange("p (a b) -> p a b", a=2).unsqueeze(2).broadcast_to(S4)
                g4 = gm4.ap.rearrange("p (a c) b -> p a c b", a=2)
                s4 = sel4.ap.rearrange("p (a c) b -> p a c b", a=2)
                K.tt("dve", g4, pg.ap[:, c0:c0 + 128].rearrange("p (a c b) -> p a c b", a=2, c=2), bq(negb), ALU.add, [pg, negb], [gm4])
                for j in range(4):
                    K.op("dve", lambda e, j=j: e.max(out=m84.ap[:, j, :], in_=gm4.ap[:, j, :]), [gm4], [m84])
                K.tt("dve", sel4.ap, gm4.ap, m84.ap[:, :, 2:3].broadcast_to([128, 4, 32]), ALU.is_ge, [gm4, m84], [sel4])
                K.tt("dve", s4, s4, bq(valid), ALU.mult, [sel4, valid], [sel4])
                K.tt("dve", s4, s4, bq(ownm), ALU.add, [sel4, ownm], [sel4])
                K.ts("dve", sel4.ap, sel4.ap, -1.0, BIG, ALU.add, ALU.mult, [sel4], [sel4])
                K.tt("dve", sex4.ap[:, :, 64:96], sel4.ap, mraw.ap.unsqueeze(2).broadcast_to([128, 4, 32]), ALU.subtract,
                     [sel4, mraw], [sex4])
                pt = PS[3]
                for j in range(4):
                    K.mm(pt, pt.ap[:, j * 128:(j + 1) * 128], sex4.ap[:, j, :], idb.ap, True, True, [sex4, idb])
                K.cp("act", QT.ap[64:96, q0:q0 + 512], pt.ap[64:96, :], [pt], [QT])
            for qg in range(8):
                q0 = qg * 512
                nkt = 36 + 4 * qg

                def qk(kt):
                    ps = PS[kt % 3]
                    last_own = kt >= nkt - 4
                    K.mm(ps, ps.ap, KT.ap[:, kt * 128:(kt + 1) * 128], QT.ap[:, q0:q0 + 512],
                         True, not last_own, [KT, QT])
                    if last_own:
                        K.mm(ps, ps.ap, idb.ap, cm.ap[:, kt - (nkt - 4), :], False, True, [idb, cm])

                qk(0)
                qk(1)
                for kt in range(nkt):
                    if kt + 2 < nkt:
                        qk(kt + 2)
                    ps = PS[kt % 3]
                    PT = PTs[kt % 3]
                    K.act(PT.ap, ps.ap, AF.Exp, [ps], [PT], scale=0.125)
                    for j in range(4):
                        po = PS[4 + j]
                        K.mm(po, po.ap[:, 0:65], PT.ap[:, j * 128:(j + 1) * 128], V.ap[:, kt, :],
                             kt == 0, kt == nkt - 1, [PT, V])
                for j in range(4):
                    po = PS[4 + j]
                    K.op("dve", lambda e, po=po, j=j: e.reciprocal(out=rls[j].ap, in_=po.ap[:, 64:65]), [po], [rls[j]])
                for j in range(4):
                    po = PS[4 + j]
                    K.ts("dve", att.ap[:, qg * 4 + j, h * 64:(h + 1) * 64], po.ap[:, 0:64], rls[j].ap[:, 0:1], None,
                         ALU.mult, None, [po, rls[j]], [att])
                wi = h * 8 + qg
                wbufs = (stg, wstg2)
                if wi < len(wchunks):
                    dst, src, kc, ncols = wchunks[wi]
                    K.dma("bw%d" % (wi % 2), wbufs[wi % 2].ap[:, 0:ncols], src[kc * 128:(kc + 1) * 128, :], w=[wbufs[wi % 2]])
                if 1 <= wi <= len(wchunks):
                    dst, src, kc, ncols = wchunks[wi - 1]
                    K.cp("dve", dst.ap[:, kc, :], wbufs[(wi - 1) % 2].ap[:, 0:ncols], [wbufs[(wi - 1) % 2]], [dst])
        for qt in range(32):
            p = PS[qt % 2]
            pv = p.ap.bitcast(BF16)[:, 0:512].rearrange("p (a b) -> p a b", a=4)
            for c in range(4):
                K.tr(p, pv[:, c, :], att.ap[:, qt, c * 128:(c + 1) * 128], idb.ap, [att, idb])
            og = ostg[qt % 2]
            K.cp("act", og.ap, pv, [p], [og])
            K.dma("ob%d" % (qt % 2), attT_s[:, :, qt * 128:(qt + 1) * 128].rearrange("c p t -> p c t"), og.ap, r=[og])
        K.barrier()

    def phase_C1():
        K.phase_reset()
        wps = K.sb([128, 4, 1024], BF16, "wps")
        wpa = K.sb([128, 4, 1024], BF16, "wpa")
        wout = K.sb([128, 8, 1024], BF16, "wout")
        wq = K.sb([128, 8, 2048], BF16, "wq")
        keysT = K.sb([128, 16, 128], F32, "keysT")
        gf = K.sb([128, 1024], F32, "gffn")
        stage = K.sb([128, 2048], F32, "stageC")
        ssmT = K.sb([128, 4, 512], BF16, "ssmT")
        attT = K.sb([128, 4, 512], BF16, "attT")
        gaT = K.sb([128, 8, 512], BF16, "gaT")
        gbT = K.sb([128, 8, 512], BF16, "gbT")
        mT = K.sb([128, 8, 512], BF16, "mT")
        t1cs = [K.sb([128, 512], F32, "t1C%d" % i) for i in range(2)]
        t2cs = [K.sb([128, 512], F32, "t2C%d" % i) for i in range(2)]
        xts = [K.sb([128, 1024], F32, "xtC%d" % i) for i in range(2)]
        x1s = [K.sb([128, 1024], F32, "x1C%d" % i) for i in range(2)]
        hbs = [K.sb([128, 1024], BF16, "hbC%d" % i) for i in range(2)]
        sq = K.sb([128, 1024], BF16, "sqC")
        ss = K.sb([128, 1], F32, "ssC")
        rs = K.sb([128, 1], F32, "rsC")
        h2T = K.sb([128, 8, 512], BF16, "h2T")
        qT = K.sb([128, 16, 512], F32, "qT")
        scs = [K.sb([128, 16, 128], F32, "sc%d" % i) for i in range(2)]
        K.dma("c0", gf.ap, g_ffn.partition_broadcast(128), w=[gf])
        for ch in range(16):
            K.dma("wst", stage.ap[:, 0:128], keys[ch], w=[stage])
            p = nps()
            K.tr(p, p.ap[:, 0:128], stage.ap[:, 0:128], idf.ap, [stage, idf])
            K.cp("dve", keysT.ap[:, ch, :], p.ap[:, 0:128], [p], [keysT])
        xi = 0

        def c1_loads(tg_):
            ta_ = tg_ * 512
            K.dma("c1a", ssmT.ap, ssmT_s[:, :, ta_:ta_ + 512].rearrange("c p t -> p c t"), w=[ssmT])
            K.dma("c1b", attT.ap, attT_s[:, :, ta_:ta_ + 512].rearrange("c p t -> p c t"), w=[attT])
            K.dma("c1c", gaT.ap, gT_s[0:8, :, ta_:ta_ + 512].rearrange("c p t -> p c t"), w=[gaT])
            K.dma("c1d", gbT.ap, gT_s[8:16, :, ta_:ta_ + 512].rearrange("c p t -> p c t"), w=[gbT])

        c1_loads(0)
        for tg in range(8):
            t0 = tg * 512
            for c in range(8):
                pa = nps()
                pb = nps()
                for kc in range(4):
                    K.mm(pa, pa.ap, wps.ap[:, kc, c * 128:(c + 1) * 128], ssmT.ap[:, kc, :], kc == 0, kc == 3, [wps, ssmT])
                for kc in range(4):
                    K.mm(pb, pb.ap, wpa.ap[:, kc, c * 128:(c + 1) * 128], attT.ap[:, kc, :], kc == 0, kc == 3, [wpa, attT])
                t1, t2 = t1cs[c % 2], t2cs[c % 2]
                K.tt("dve", t1.ap, pa.ap, gaT.ap[:, c, :], ALU.mult, [pa, gaT], [t1])
                K.tt("dve", t2.ap, pb.ap, gbT.ap[:, c, :], ALU.mult, [pb, gbT], [t2])
                K.tt("dve", mT.ap[:, c, :], t1.ap, t2.ap, ALU.add, [t1, t2], [mT])
            if tg + 1 < 8:
                c1_loads(tg + 1)
            for j in range(4):
                xt = xts[xi % 2]
                x1 = x1s[xi % 2]
                hb = hbs[xi % 2]
                xi += 1
                r0 = t0 + j * 128
                K.dma("x%d" % (xi % 2), xt.ap, xc[NT_OWN + r0: NT_OWN + r0 + 128, :], w=[xt])
                for half in range(2):
                    p = nps()
                    for kc in range(8):
                        K.mm(p, p.ap, mT.ap[:, kc, j * 128:(j + 1) * 128], wout.ap[:, kc, half * 512:(half + 1) * 512],
                             kc == 0, kc == 7, [mT, wout])
                    K.tt("dve", x1.ap[:, half * 512:(half + 1) * 512], p.ap, xt.ap[:, half * 512:(half + 1) * 512], ALU.add,
                         [p, xt], [x1])
                K.dma("x1o%d" % (xi % 2), x1_s[r0:r0 + 128, :], x1.ap, r=[x1])
                rmsnorm_tile(x1, gf, hb, sq, ss, rs)

                def c1_tr(j=j, hb=hb):
                    p = nps()
                    pv = p.ap.bitcast(BF16).rearrange("p (a b) -> p a b", a=8)
                    for kc in range(8):
                        K.tr(p, pv[:, kc, :], hb.ap[:, kc * 128:(kc + 1) * 128], idb.ap, [hb, idb])
                    K.cp("act", h2T.ap[:, :, j * 128:(j + 1) * 128], pv, [p], [h2T])

                if j > 0:
                    pend_tr()
                pend_tr = c1_tr
            pend_tr()
            K.dma("h2o", h2T_s[:, :, t0:t0 + 512].rearrange("c p t -> p c t"), h2T.ap, r=[h2T])
            for ch in range(16):
                p = nps()
                for kc in range(8):
                    K.mm(p, p.ap, wq.ap[:, kc, ch * 128:(ch + 1) * 128], h2T.ap[:, kc, :], kc == 0, kc == 7, [wq, h2T])
                K.cp("act" if ch % 2 else "dve", qT.ap[:, ch, :], p.ap, [p], [qT])
            for j in range(4):
                sc = scs[j % 2]
                for c0 in range(0, 16, 4):
                    p = nps()
                    for i in range(4):
                        K.mm(p, p.ap[:, i * 128:(i + 1) * 128], qT.ap[:, c0 + i, j * 128:(j + 1) * 128], keysT.ap[:, c0 + i, :],
                             True, True, [qT, keysT])
                    K.cp("act" if (c0 // 4) % 2 else "dve", sc.ap[:, c0:c0 + 4, :], p.ap.rearrange("p (a b) -> p a b", a=4), [p], [sc])
                r0 = t0 + j * 128
                K.dma("sco%d" % (j % 2), sc_s[r0:r0 + 128, :], sc.ap.rearrange("p a b -> p (a b)"), r=[sc])
        K.barrier()

    U32 = mybir.dt.uint32

    def phase_C2():
        K.phase_reset()
        iot = K.sb([128, 128], F32, "iota")
        K.dma("c0", iot.ap, iota_d, w=[iot])
        ss_ = [K.sb([128, 16, 128], F32, "s%d" % i) for i in range(2)]
        v = K.sb([128, 16, 16], F32, "v")
        idx = K.sb([128, 8, 16], U32, "idx")
        idxf = K.sb([128, 128], F32, "idxf")
        idxT = K.sb([128, 128], F32, "idxT")
        top = K.sb([128, 8, 16], F32, "top")
        e16 = K.sb([128, 8, 16], F32, "e16")
        Z = K.sb([128, 8], F32, "Z")
        bE = K.sb([128, 8], F32, "bE")
        v1m = K.sb([128, 8, 16], F32, "v1m")
        sums = [K.sb([128, 16, 128], F32, "sum%d" % i) for i in range(3)]
        Es = [K.sb([128, 16, 128], BF16, "E%d" % i) for i in range(3)]
        Btm = K.sb([128, 8, 16, 128], BF16, "Btm")
        BT = K.sb([128, 128, 128], BF16, "BT")
        AT = K.sb([128, 128, 128], BF16, "AT")
        Gst = K.sb([128, 128, 128], BF16, "Gst")
        works = [K.sb([128, 128], F32, "wk%d" % i) for i in range(16)]
        work2s = [K.sb([128, 256], F32, "wk2%d" % i) for i in range(8)]
        K.dma("c2s0", ss_[0].ap.rearrange("p a b -> p (a b)"), sc_s[0:128, :], w=[ss_[0]])
        for tl in range(32):
            r0 = tl * 128
            s_ = ss_[tl % 2]
            if tl + 1 < 32:
                sn_ = ss_[(tl + 1) % 2]
                K.dma("c2s%d" % ((tl + 1) % 2), sn_.ap.rearrange("p a b -> p (a b)"), sc_s[r0 + 128:r0 + 256, :], w=[sn_])
            for ch in range(16):
                K.op("dve", lambda e, ch=ch, s_=s_: e.max(out=v.ap[:, ch, 0:8], in_=s_.ap[:, ch, :]), [s_], [v])
            for ch in range(0, 16, 2):
                K.op("dve", lambda e, ch=ch, s_=s_: e.max_index(out=idx.ap[:, ch // 2, 0:8], in_max=v.ap[:, ch, 0:8],
                                                                in_values=s_.ap[:, ch, :]), [v, s_], [idx])
            for ch in range(16):
                K.op("dve", lambda e, ch=ch, s_=s_: e.match_replace(out=works[ch].ap, in_to_replace=v.ap[:, ch, 0:8],
                                                                    in_values=s_.ap[:, ch, :], imm_value=-1e30), [v, s_], [works[ch]])
            for ch in range(16):
                K.op("dve", lambda e, ch=ch: e.max(out=v.ap[:, ch, 8:16], in_=works[ch].ap), [works[ch]], [v])
            for ch in range(0, 16, 2):
                K.op("dve", lambda e, ch=ch: e.max_index(out=idx.ap[:, ch // 2, 8:16], in_max=v.ap[:, ch, 8:16],
                                                         in_values=works[ch].ap), [v, works[ch]], [idx])
            vv = v.ap.rearrange("p (h s) k -> p h s k", s=2)
            v1 = vv[:, :, 0, :]
            v2 = vv[:, :, 1, :]
            cand_ap = sums[0].ap.rearrange("p a b -> p (a b)").rearrange("p (h c) -> p h c", h=8)
            K.tt("dve", cand_ap.rearrange("p h (i j) -> p h i j", i=16), v1.unsqueeze(3).broadcast_to([128, 8, 16, 16]),
                 v2.unsqueeze(2).broadcast_to([128, 8, 16, 16]), ALU.add, [v], [sums[0]])
            for h in range(8):
                K.op("dve", lambda e, h=h: e.max(out=top.ap[:, h, 0:8], in_=cand_ap[:, h, :]), [sums[0]], [top])
            for h in range(8):
                K.op("dve", lambda e, h=h: e.match_replace(out=work2s[h].ap, in_to_replace=top.ap[:, h, 0:8],
                                                           in_values=cand_ap[:, h, :], imm_value=-1e30), [top, sums[0]], [work2s[h]])
            for h in range(8):
                K.op("dve", lambda e, h=h: e.max(out=top.ap[:, h, 8:16], in_=work2s[h].ap), [work2s[h]], [top])
            mxb = top.ap[:, :, 0:1].broadcast_to([128, 8, 16])
            taub = top.ap[:, :, 15:16].broadcast_to([128, 8, 16])
            K.tt("dve", e16.ap, top.ap, mxb, ALU.subtract, [top], [e16])
            K.act(e16.ap, e16.ap, AF.Exp, [e16], [e16])
            K.op("dve", lambda e: e.tensor_reduce(out=Z.ap, in_=e16.ap, axis=AX.X, op=ALU.add), [e16], [Z])
            K.act(Z.ap, Z.ap, AF.Ln, [Z], [Z])
            K.tt("dve", bE.ap, top.ap[:, :, 15], top.ap[:, :, 0], ALU.subtract, [top], [bE])
            K.tt("dve", bE.ap, bE.ap, Z.ap, ALU.subtract, [bE, Z], [bE])
            K.tt("dve", v1m.ap, v1, taub, ALU.subtract, [v, top], [v1m])
            K.cp("dve", idxf.ap, idx.ap.rearrange("p h k -> p (h k)"), [idx], [idxf])
            p = nps()
            K.tr(p, p.ap[:, 0:128], idxf.ap, idf.ap, [idxf, idf])
            K.cp("dve", idxT.ap, p.ap[:, 0:128], [p], [idxT])
            K.tt("dve", AT.ap, iot.ap.unsqueeze(1).broadcast_to([128, 128, 128]),
                 idxT.ap.unsqueeze(2).broadcast_to([128, 128, 128]), ALU.is_equal, [iot, idxT], [AT])
            def bsum(h):
                sm_ = sums[h % 3]
                K.tt("dve", sm_.ap, v1m.ap[:, h, :].unsqueeze(2).broadcast_to([128, 16, 128]),
                     s_.ap[:, 2 * h + 1, :].unsqueeze(1).broadcast_to([128, 16, 128]), ALU.add, [v1m, s_], [sm_])
                K.act(Es[h % 3].ap, sm_.ap, AF.Exp, [sm_, bE], [Es[h % 3]], bias=bE.ap[:, h:h + 1])

            bsum(0)
            bsum(1)
            for h in range(8):
                if h + 2 < 8:
                    bsum(h + 2)
                K.stt("dve", Btm.ap[:, h], sums[h % 3].ap, -1e-5, Es[h % 3].ap, ALU.is_ge, ALU.mult, [sums[h % 3], Es[h % 3]], [Btm])
            for b0 in range(0, 128, 8):
                p = nps()
                pv = p.ap.bitcast(BF16).rearrange("p (a b) -> p a b", a=8)
                for k in range(8):
                    K.tr(p, pv[:, k, :], Btm.ap[:, :, :, b0 + k].rearrange("p h i -> p (h i)"), idb.ap, [Btm, idb])
                K.cp("act", BT.ap[:, :, b0:b0 + 8], pv.rearrange("p b t -> p t b"), [p], [BT])
            for t4 in range(0, 128, 4):
                p = nps()
                for k in range(4):
                    t = t4 + k
                    K.mm(p, p.ap[:, k * 128:(k + 1) * 128], BT.ap[:, t, :], AT.ap[:, t, :], True, True, [BT, AT])
                K.cp("act", Gst.ap[:, :, t4:t4 + 4],
                     p.ap.rearrange("p (t a) -> p a t", t=4), [p], [Gst])
            K.dma("c2g", G_s[tl], Gst.ap, r=[Gst])
        K.barrier()

    def phase_D():
        K.phase_reset()
        TG = 1024
        NG_ = NT_OWN // TG
        h2T = K.sb([128, 8, TG], BF16, "h2TD")
        Ubs = [K.sb([128, 1024], F32, "Ub%d" % i) for i in range(3)]
        Ubb = [K.sb([128, 1024], BF16, "Ubb%d" % i) for i in range(2)]
        UbTs = [K.sb([128, 8, 128], BF16, "UbT%d" % i) for i in range(3)]
        Ghs = [K.sb([128, 8, 8, 128], BF16, "Gh%d" % i) for i in range(2)]
        ges = [K.sb([128, 512], BF16, "ge%d" % i) for i in range(2)]
        W = K.sb([128, 16, TG], BF16, "W")
        Vbs = [K.sb([128, 1024], F32, "Vb%d" % i) for i in range(3)]
        Vbf = K.sb([128, 16, 1024], BF16, "Vbf")
        acc = K.sb([128, 8, 1024], F32, "acc")
        x1t = [K.sb([128, 1024], F32, "x1D%d" % i) for i in range(2)]
        uttok = [Buf(None, "ut%d" % i) for i in range(128)]
        NI = NG_ * 128

        def load(n):
            grp, a = n // 128, n % 128
            t0 = grp * TG
            if grp == 0:
                K.dma("du%d" % (n % 3), Ubs[n % 3].ap, peer_u[a * 128:(a + 1) * 128, :], w=[Ubs[n % 3]])
            else:
                K.dma("du%d" % (n % 3), UbTs[n % 3].ap.rearrange("p a b -> p (a b)"), UT_s[a], r=[uttok[a]], w=[UbTs[n % 3]])
            if a % 8 == 0:
                Gh = Ghs[(n // 8) % 2]
                tl0 = t0 // 128
                K.dma("dg%d" % ((n // 8) % 2), Gh.ap, G_s[tl0:tl0 + 8, :, a:a + 8, :].rearrange("tl b a t -> b tl a t"), w=[Gh])
            K.dma("dv%d" % (n % 3), Vbs[n % 3].ap, peer_v[a * 128:(a + 1) * 128, :], w=[Vbs[n % 3]])

        def trans(n):
            if n // 128 != 0:
                return
            Ub, UbT, ub = Ubs[n % 3], UbTs[n % 3], Ubb[n % 2]
            K.cp("dve", ub.ap, Ub.ap, [Ub], [ub])
            p = nps()
            pv = p.ap.bitcast(BF16).rearrange("p (a b) -> p a b", a=8)
            for kc in range(8):
                K.tr(p, pv[:, kc, :], ub.ap[:, kc * 128:(kc + 1) * 128], idb.ap, [ub, idb])
            K.cp("act", UbT.ap, pv, [p], [UbT])

        load(0)
        load(1)
        trans(0)
        for n in range(NI):
            grp, a = n // 128, n % 128
            t0 = grp * TG
            si = a % 16
            if a == 0:
                K.dma("dh", h2T.ap, h2T_s[:, :, t0:t0 + TG].rearrange("c p t -> p c t"), w=[h2T])
                K.memset("pool", acc.ap, 0.0, [acc])
            if n + 2 < NI:
                load(n + 2)
            if n + 1 < NI:
                trans(n + 1)
            UbT, Vb = UbTs[n % 3], Vbs[n % 3]
            Gh = Ghs[(n // 8) % 2]
            if grp == 0:
                K.dma("dut%d" % (n % 3), UT_s[a], UbT.ap.rearrange("p a b -> p (a b)"), r=[UbT], w=[uttok[a]])
            K.cp("act", Vbf.ap[:, si, :], Vb.ap, [Vb], [Vbf])
            for hf in range(TG // 512):
                p = nps()
                for kc in range(8):
                    K.mm(p, p.ap, UbT.ap[:, kc, :], h2T.ap[:, kc, hf * 512:(hf + 1) * 512], kc == 0, kc == 7, [UbT, h2T])
                ge = ges[hf % 2]
                K.act(ge.ap, p.ap, AF.Gelu_apprx_tanh, [p], [ge])
                K.tt("dve", W.ap[:, si, hf * 512:(hf + 1) * 512].rearrange("p (a b) -> p a b", a=4),
                     ge.ap.rearrange("p (a b) -> p a b", a=4), Gh.ap[:, hf * 4:(hf + 1) * 4, a % 8, :], ALU.mult,
                     [ge, Gh], [W])
            if si == 15:
                for j in range(TG // 128):
                    for hf in range(2):
                        p = nps()
                        for s2 in range(16):
                            K.mm(p, p.ap, W.ap[:, s2, j * 128:(j + 1) * 128], Vbf.ap[:, s2, hf * 512:(hf + 1) * 512],
                                 s2 == 0, s2 == 15, [W, Vbf])
                        K.tt("dve", acc.ap[:, j, hf * 512:(hf + 1) * 512], p.ap, acc.ap[:, j, hf * 512:(hf + 1) * 512], ALU.add,
                             [p, acc], [acc])
            if a == 127:
                for j in range(TG // 128):
                    xt = x1t[j % 2]
                    r0 = t0 + j * 128
                    K.dma("dx%d" % (j % 2), xt.ap, x1_s[r0:r0 + 128, :], w=[xt])
                    K.tt("dve", xt.ap, xt.ap, acc.ap[:, j, :], ALU.add, [xt, acc], [xt])
                    K.dma("dx%d" % (j % 2), x2_s[r0:r0 + 128, :], xt.ap, r=[xt])
        K.barrier()

    def phase_E():
        K.phase_reset()
        wpg = K.sb([128, 8, 1024], BF16, "wpg")
        wpp = K.sb([128, 2, 1024], BF16, "wpp")
        stage = K.sb([128, 1024], F32, "stageE")
        gp = K.sb([128, 1024], F32, "gple")
        gfin = K.sb([128, 1024], F32, "gfin")
        xts = [K.sb([128, 1024], F32, "xtE%d" % i) for i in range(4)]
        pts = [K.sb([128, 256], F32, "ptE%d" % i) for i in range(2)]
        pb_s = [K.sb([128, 256], BF16, "pbE%d" % i) for i in range(2)]
        pTs = [K.sb([128, 2, 128], BF16, "pTE%d" % i) for i in range(2)]
        hb_s = [K.sb([128, 1024], BF16, "hbE%d" % i) for i in range(2)]
        hTs_ = [K.sb([128, 8, 128], BF16, "hTE%d" % i) for i in range(2)]
        sq_s = [K.sb([128, 1024], BF16, "sqE%d" % i) for i in range(2)]
        ss_s = [K.sb([128, 1], F32, "ssE%d" % i) for i in range(4)]
        rs_s = [K.sb([128, 1], F32, "rsE%d" % i) for i in range(4)]
        sgts = [K.sb([128, 1024], F32, "sgE%d" % i) for i in range(2)]
        outs = [K.sb([128, 1024], F32, "oE%d" % i) for i in range(2)]
        K.dma("c0", gp.ap, g_ple.partition_broadcast(128), w=[gp])
        K.dma("c0", gfin.ap, g_fin.partition_broadcast(128), w=[gfin])
        load_w_bf(wpg, w_pg, 8, 1024, stage, "wst")
        load_w_bf(wpp, w_pp, 2, 1024, stage, "wst")
        def e_vars(tl):
            return dict(r0=tl * 128)

        def front(tl):
            r0 = tl * 128
            xt, pt, ot = xts[tl % 4], pts[tl % 2], outs[tl % 2]
            pb_, pT, hb, hT, sq, sgt = pb_s[tl % 2], pTs[tl % 2], hb_s[tl % 2], hTs_[tl % 2], sq_s[tl % 2], sgts[tl % 2]
            ss, rs = ss_s[tl % 2], rs_s[tl % 2]
            ss2, rs2 = ss_s[2 + tl % 2], rs_s[2 + tl % 2]
            K.dma("ex%d" % (tl % 4), xt.ap, x2_s[r0:r0 + 128, :], w=[xt])
            K.dma("ep%d" % (tl % 2), pt.ap, pc[r0:r0 + 128, :], w=[pt])
            rmsnorm_tile(xt, gp, hb, sq, ss, rs)
            K.cp("dve", pb_.ap, pt.ap, [pt], [pb_])

        def front1b(tl):
            pb_, pT, hb, hT = pb_s[tl % 2], pTs[tl % 2], hb_s[tl % 2], hTs_[tl % 2]
            p = nps()
            pv = p.ap.bitcast(BF16).rearrange("p (a b) -> p a b", a=8)
            for kc in range(8):
                K.tr(p, pv[:, kc, :], hb.ap[:, kc * 128:(kc + 1) * 128], idb.ap, [hb, idb])
            K.cp("act", hT.ap, pv, [p], [hT])
            p = nps()
            pv = p.ap.bitcast(BF16).rearrange("p (a b) -> p a b", a=8)
            for kc in range(2):
                K.tr(p, pv[:, kc, :], pb_.ap[:, kc * 128:(kc + 1) * 128], idb.ap, [pb_, idb])
            K.cp("act", pT.ap, pv[:, 0:2, :], [p], [pT])

        def front2(tl):
            pT, hT, sgt = pTs[tl % 2], hTs_[tl % 2], sgts[tl % 2]
            for hf in range(2):
                pg = nps()
                pe_ = nps()
                for kc in range(8):
                    K.mm(pg, pg.ap, hT.ap[:, kc, :], wpg.ap[:, kc, hf * 512:(hf + 1) * 512], kc == 0, kc == 7, [hT, wpg])
                for kc in range(2):
                    K.mm(pe_, pe_.ap, pT.ap[:, kc, :], wpp.ap[:, kc, hf * 512:(hf + 1) * 512], kc == 0, kc == 1, [pT, wpp])
                K.act(sgt.ap[:, hf * 512:(hf + 1) * 512], pg.ap, AF.Sigmoid, [pg], [sgt])
                K.tt("dve", sgt.ap[:, hf * 512:(hf + 1) * 512], pe_.ap, sgt.ap[:, hf * 512:(hf + 1) * 512], ALU.mult, [pe_, sgt], [sgt])

        def back(tl):
            r0 = tl * 128
            xt, ot = xts[tl % 4], outs[tl % 2]
            sq, sgt = sq_s[tl % 2], sgts[tl % 2]
            ss2, rs2 = ss_s[2 + tl % 2], rs_s[2 + tl % 2]
            K.tt("pool", xt.ap, xt.ap, sgt.ap, ALU.add, [xt, sgt], [xt])
            K.act(sq.ap, xt.ap, AF.Square, [xt], [sq, ss2], accum=ss2.ap)
            K.act(rs2.ap, ss2.ap, AF.Sqrt, [ss2], [rs2], scale=1.0 / 1024, bias=eps_b.ap)
            K.op("dve", lambda e, rs2=rs2: e.reciprocal(out=rs2.ap, in_=rs2.ap), [rs2], [rs2])
            K.stt("dve", ot.ap, xt.ap, rs2.ap[:, 0:1], gfin.ap, ALU.mult, ALU.mult, [xt, rs2, gfin], [ot])
            K.dma("eo%d" % (tl % 2), out[r0:r0 + 128, :], ot.ap, r=[ot])

        for r_ in range(-3, 32):
            for stage_fn, off_ in ((back, 0), (front2, 1), (front1b, 2), (front, 3)):
                tl_ = r_ + off_
                if 0 <= tl_ < 32:
                    stage_fn(tl_)
        K.barrier()

    phases = {"A": phase_A, "B": phase_B, "S": phase_S, "C1": phase_C1, "C2": phase_C2, "D": phase_D, "E": phase_E}
    return nc, kb, st, locals()


def _consts():
    ident = np.eye(128, dtype=np.float32)
    invf = np.zeros((128, 2), np.float32)
    for p in range(128):
        hd = p % 64
        if hd < 16:
            invf[p, 0] = np.float32(500000.0) ** np.float32(-(2 * (hd % 8)) / 16.0)
            invf[p, 1] = -1.0 if hd < 8 else 1.0
    eoh = np.zeros((32, NT_LOC), np.float32)
    for n in range(32):
        eoh[n, n * 256:(n + 1) * 256] = 1.0
    cm = np.zeros((4, 128, 512), np.float32)
    for kt in range(4):
        for kp in range(128):
            kpos = kt * 128 + kp
            q = np.arange(512)
            same = (q // 256) == (kpos // 256)
            cm[kt, kp, :] = np.where(same & (kpos > q), -BIG, 0.0)
    return ident, invf, eoh, cm


def make_in_maps(inp, cores=range(8)):
    f = lambda a: np.ascontiguousarray(np.asarray(a))
    x = f(inp["x"])
    p = f(inp["p"])[0]
    pos = f(inp["positions"]).astype(np.int32)
    ident, invf, eoh, cm = _consts()
    w_in = f(inp["w_in"])[0]
    perm = np.arange(1024)
    for c in range(1024):
        hd = c % 64
        if hd < 8:
            perm[c] = c + 8
        elif hd < 16:
            perm[c] = c - 8
    w_perm = f(w_in[:, 512:1536][:, perm])

    def pair(a):
        a = f(a)[0]
        sh = a.shape
        a = a.reshape(16, 2, 64, *sh[2:])
        a = np.moveaxis(a, 0, 2)
        return f(a.reshape(128, 16, -1).reshape(128, -1))

    ldt = f(inp["ssm_log_dt"])[0]
    ldt_l = f(np.broadcast_to(ldt.reshape(16, 2, 1), (16, 2, 64)).transpose(1, 2, 0).reshape(128, 16))
    cre = f(inp["ssm_c_re"])[0].transpose(0, 2, 1)
    cim = f(inp["ssm_c_im"])[0].transpose(0, 2, 1)
    shared = {
        "ident": ident, "iota": np.ascontiguousarray(np.broadcast_to(np.arange(128, dtype=np.float32), (128, 128))), "invf": invf, "koh": eoh, "cmask": cm,
        "g_mix": f(inp["g_mix"]), "w_in": w_in, "w_perm": w_perm,
        "s5_ldt": ldt_l, "s5_are": pair(inp["ssm_a_re"]), "s5_aim": pair(inp["ssm_a_im"]),
        "s5_bre": pair(inp["ssm_b_re"]), "s5_bim": pair(inp["ssm_b_im"]),
        "s5_cre": pair(cre[None]), "s5_cim": pair(cim[None]),
        "ssm_d": f(inp["ssm_d"]), "w_glu": f(inp["ssm_w_glu"])[0],
        "w_ps": f(inp["w_proj_ssm"])[0], "w_pa": f(inp["w_proj_att"])[0], "w_out": f(inp["w_out"])[0],
        "g_ffn": f(inp["g_ffn"]), "w_q": f(inp["peer_w_q"])[0],
        "keys": f(np.stack([f(inp["peer_keys1"])[0], f(inp["peer_keys2"])[0]], axis=1).reshape(16, 128, 128)),
        "peer_u": f(inp["peer_u"])[0], "peer_v": f(inp["peer_v"])[0],
        "g_ple": f(inp["g_ple"]), "w_pg": f(inp["ple_w_gate"])[0], "w_pp": f(inp["ple_w_proj"])[0],
        "g_fin": f(inp["g_final"]).reshape(1, 1024),
    }
    maps = []
    for c in cores:
        b, half = c // 2, c % 2
        xc = np.zeros((NT_LOC, 1024), np.float32)
        posc = np.zeros((1, NT_LOC), np.int32)
        if half == 1:
            xc[:] = x[b]
            posc[0] = pos[b]
        else:
            xc[NT_OWN:] = x[b, :NT_OWN]
            posc[0, NT_OWN:] = pos[b, :NT_OWN]
        valid = np.zeros((16, 32), np.float32)
        own = np.zeros((16, 32), np.float32)
        for qb in range(16, 32):
            for n in range(32):
                if n < qb and (half == 1 or n >= 16):
                    valid[qb - 16, n] = 1.0
            own[qb - 16, qb] = 1.0
        m = dict(shared)
        m.update({"xc": xc, "pc": f(p[b, half * NT_OWN:(half + 1) * NT_OWN]), "posc": posc,
                  "validc": valid.reshape(1, 512), "ownc": own.reshape(1, 512)})
        maps.append(m)
    return maps


_CACHE = {}


def kernel(**inputs):
    if "prog" not in _CACHE:
        nc, kb, st, L = build_program()
        for ph in ("A", "S", "B", "C1", "C2", "D", "E"):
            L["phases"][ph]()
        kb.S.emit()
        st.close()
        _CACHE["prog"] = nc
    nc = _CACHE["prog"]
    maps = make_in_maps(inputs, range(8))
    res = run_bass_kernel_spmd(nc, maps, core_ids=list(range(8)))
    out = np.zeros((4, 8192, 1024), np.float32)
    for c in range(8):
        b, half = c // 2, c % 2
        out[b, half * NT_OWN:(half + 1) * NT_OWN] = np.asarray(res.results[c]["out"])
    return out
```
